# Optimizing a Trainium2 kernel written in Bass

```python
import math
import jax, jax.numpy as jnp
from jax import lax
import numpy as np

D_MODEL = 1024
BATCH = 1
SEQ = 16384
DEPTH = 1

SSD_EXPAND = 2
D_SSD = SSD_EXPAND * D_MODEL
SSD_HEADDIM = 64
SSD_HEADS = D_SSD // SSD_HEADDIM
SSD_GROUPS = 8
SSD_STATE = 128
SSD_CONV = 4
SSD_CHUNK = 128
D_XBC = D_SSD + 2 * SSD_GROUPS * SSD_STATE
POOL_WINDOWS = (2, 4, 8, 16)
POOL_GROUPS = len(POOL_WINDOWS)
D_POOL = D_MODEL
POOL_GDIM = D_POOL // POOL_GROUPS
N_BRANCHES = 2
SPLIT_OFFSETS = (D_SSD, D_SSD + D_XBC, D_SSD + D_XBC + SSD_HEADS, D_SSD + D_XBC + SSD_HEADS + D_POOL)
D_IN_PROJ = D_SSD + D_XBC + SSD_HEADS + D_POOL + N_BRANCHES * D_MODEL
N_EXPERTS = 64
TOP_K = 8
N_EXPERT_GROUPS = 8
EXPERTS_PER_GROUP = N_EXPERTS // N_EXPERT_GROUPS
TOPK_GROUPS = 4
D_EXPERT = 256
D_SHARED = 256
ROUTED_SCALE = 2.5
MOE_BLOCK = 128
N_ADA = 6
EPS = 1e-6

kernel_name = 'hybrid_ssd_pool_moe_block'


def rmsnorm(x, g):
    xf = x.astype(jnp.float32)
    inv = lax.rsqrt(jnp.mean(xf * xf, axis=-1, keepdims=True) + EPS)
    return (xf * inv * g.astype(jnp.float32)).astype(x.dtype)


def modulate(x, shift, scale):
    return x * (1 + scale[:, None, :]) + shift[:, None, :]


def causal_dwconv(x, w, b):
    K, C = w.shape
    y = lax.conv_general_dilated(x, w[:, None, :].astype(x.dtype), window_strides=(1,),
                                 padding=[(K - 1, 0)], dimension_numbers=('NWC', 'WIO', 'NWC'),
                                 feature_group_count=C)
    return y + b


def segsum_exp(a):
    Q = a.shape[-1]
    cs = jnp.cumsum(a, axis=-1)
    diff = cs[..., :, None] - cs[..., None, :]
    mask = jnp.tril(jnp.ones((Q, Q), dtype=bool))
    return jnp.where(mask, jnp.exp(jnp.where(mask, diff, 0.0)), 0.0)


def ssd_chunked(xh, dt, A, Bm, Cm):
    Bsz, S, H, P = xh.shape
    G, N = Bm.shape[-2:]
    R = H // G
    Q = SSD_CHUNK
    nc = S // Q
    x = (xh * dt[..., None]).reshape(Bsz, nc, Q, G, R, P)
    a = (dt * A).reshape(Bsz, nc, Q, G, R).transpose(0, 1, 3, 4, 2)
    Bc = Bm.reshape(Bsz, nc, Q, G, N)
    Cc = Cm.reshape(Bsz, nc, Q, G, N)
    a_cs = jnp.cumsum(a, axis=-1)
    L = segsum_exp(a)
    CB = jnp.einsum('bclgn,bcsgn->bcgls', Cc, Bc)
    y_diag = jnp.einsum('bcgls,bcgrls,bcsgrp->bclgrp', CB, L, x)
    decay_states = jnp.exp(a_cs[..., -1:] - a_cs)
    states = jnp.einsum('bclgn,bcgrl,bclgrp->bcgrpn', Bc, decay_states, x)
    chunk_decay = jnp.exp(a_cs[..., -1])

    def step(carry, inp):
        st, dec = inp
        return carry * dec[..., None, None] + st, carry

    init = jnp.zeros(states.shape[:1] + states.shape[2:], states.dtype)
    _, prev = lax.scan(step, init, (jnp.moveaxis(states, 1, 0), jnp.moveaxis(chunk_decay, 1, 0)))
    prev = jnp.moveaxis(prev, 0, 1)
    y_off = jnp.einsum('bclgn,bcgrpn,bcgrl->bclgrp', Cc, prev, jnp.exp(a_cs))
    return (y_diag + y_off).reshape(Bsz, S, H, P)


def ssd_branch(z, xbc, dt_raw, conv_w, conv_b, dt_bias, A_log, D_skip, norm_g):
    xbc = jax.nn.silu(causal_dwconv(xbc, conv_w, conv_b))
    Bsz, S, _ = xbc.shape
    GN = SSD_GROUPS * SSD_STATE
    xs = xbc[..., :D_SSD].reshape(Bsz, S, SSD_HEADS, SSD_HEADDIM)
    Bm = xbc[..., D_SSD:D_SSD + GN].reshape(Bsz, S, SSD_GROUPS, SSD_STATE)
    Cm = xbc[..., D_SSD + GN:].reshape(Bsz, S, SSD_GROUPS, SSD_STATE)
    dt = jax.nn.softplus((dt_raw + dt_bias).astype(jnp.float32))
    A = -jnp.exp(A_log.astype(jnp.float32))
    y = ssd_chunked(xs, dt, A, Bm, Cm) + D_skip[:, None] * xs
    y = y.reshape(Bsz, S, D_SSD) * jax.nn.silu(z)
    y = rmsnorm(y.reshape(Bsz, S, SSD_GROUPS, D_SSD // SSD_GROUPS), norm_g.reshape(SSD_GROUPS, -1))
    return y.reshape(Bsz, S, D_SSD)


def pool_branch(xp, pool_w, pool_scale):
    Bsz, S, _ = xp.shape
    xg = xp.reshape(Bsz, S, POOL_GROUPS, POOL_GDIM).astype(jnp.float32)
    csp = jnp.pad(jnp.cumsum(xg, axis=1), ((0, 0), (1, 0), (0, 0), (0, 0)))
    pos = jnp.arange(1, S + 1, dtype=jnp.float32)
    outs = []
    for gi, w in enumerate(POOL_WINDOWS):
        c_g = csp[:, :, gi]
        lower = jnp.concatenate([jnp.zeros((Bsz, w - 1, POOL_GDIM), c_g.dtype), c_g[:, :S + 1 - w]], axis=1)
        mean = (c_g[:, 1:] - lower) / jnp.minimum(pos, w)[None, :, None]
        outs.append(mean - xg[:, :, gi])
    pooled = jnp.stack(outs, axis=2).astype(xp.dtype)
    mixed = jnp.einsum('bsgc,gcd->bsgd', pooled, pool_w)
    return mixed.reshape(Bsz, S, D_POOL) * pool_scale


def moe_ffn(u, w_router, router_bias, we_gate, we_up, we_down, ws_gate, ws_up, ws_down):
    Bsz, S, D = u.shape
    N = Bsz * S
    uf = u.reshape(N, D)
    scores = jax.nn.sigmoid((uf @ w_router).astype(jnp.float32))
    choice = scores + router_bias.astype(jnp.float32)
    grp = choice.reshape(N, N_EXPERT_GROUPS, EXPERTS_PER_GROUP)
    grp_score = lax.top_k(grp, 2)[0].sum(-1)
    _, top_grp = lax.top_k(grp_score, TOPK_GROUPS)
    grp_mask = jnp.any(top_grp[:, :, None] == jnp.arange(N_EXPERT_GROUPS)[None, None, :], axis=1)
    masked = jnp.where(jnp.repeat(grp_mask, EXPERTS_PER_GROUP, axis=1), choice, -jnp.inf)
    _, top_e = lax.top_k(masked, TOP_K)
    wts = jnp.take_along_axis(scores, top_e, axis=1)
    wts = wts / jnp.sum(wts, axis=-1, keepdims=True) * ROUTED_SCALE
    NK = N * TOP_K
    e_flat = top_e.reshape(NK).astype(jnp.int32)
    tok_flat = jnp.arange(NK, dtype=jnp.int32) // TOP_K
    order = jnp.argsort(e_flat)
    e_sorted = e_flat[order]
    counts = jax.ops.segment_sum(jnp.ones((NK,), jnp.int32), e_flat, num_segments=N_EXPERTS)
    padded = (counts + MOE_BLOCK - 1) // MOE_BLOCK * MOE_BLOCK
    starts = jnp.cumsum(counts) - counts
    pends = jnp.cumsum(padded)
    pstarts = pends - padded
    dest = pstarts[e_sorted] + jnp.arange(NK, dtype=jnp.int32) - starts[e_sorted]
    n_blocks = -(-NK // MOE_BLOCK) + N_EXPERTS
    n_slots = n_blocks * MOE_BLOCK
    slot_tok = jnp.full((n_slots,), N, jnp.int32).at[dest].set(tok_flat[order])
    slot_w = jnp.zeros((n_slots,), u.dtype).at[dest].set(wts.reshape(NK)[order].astype(u.dtype))
    block_e = jnp.minimum(jnp.searchsorted(pends, jnp.arange(n_blocks, dtype=jnp.int32) * MOE_BLOCK, side='right'),
                          N_EXPERTS - 1).astype(jnp.int32)
    u_pad = jnp.concatenate([uf, jnp.zeros((1, D), uf.dtype)], axis=0)

    def expert_block(args):
        toks, ws, e = args
        xb = u_pad[toks]
        hid = jax.nn.silu(xb @ we_gate[e]) * (xb @ we_up[e])
        return (hid @ we_down[e]) * ws[:, None]

    y_slots = lax.map(expert_block, (slot_tok.reshape(n_blocks, MOE_BLOCK),
                                     slot_w.reshape(n_blocks, MOE_BLOCK), block_e))
    routed = jax.ops.segment_sum(y_slots.reshape(n_slots, D), slot_tok, num_segments=N + 1)[:N]
    shared = (jax.nn.silu(uf @ ws_gate) * (uf @ ws_up)) @ ws_down
    return (routed + shared).reshape(Bsz, S, D)


def setup_inputs(seed: int = 0) -> dict:
    key = jax.random.key(seed)
    ks = jax.random.split(key, 32)
    f32 = jnp.float32

    def nrm(k, shape, scale):
        return jax.random.normal(k, shape, f32) * scale

    dt0 = jnp.exp(jax.random.uniform(ks[8], (DEPTH, SSD_HEADS), f32, math.log(1e-3), math.log(1e-1)))
    return {
        'x': nrm(ks[0], (BATCH, SEQ, D_MODEL), 1.0),
        'c': nrm(ks[1], (BATCH, D_MODEL), 1.0),
        'w_ada': nrm(ks[2], (DEPTH, D_MODEL, N_ADA * D_MODEL), 0.5 * D_MODEL ** -0.5),
        'b_ada': nrm(ks[3], (DEPTH, N_ADA * D_MODEL), 0.02),
        'norm_mix_g': 1.0 + nrm(ks[4], (DEPTH, D_MODEL), 0.02),
        'w_in': nrm(ks[5], (DEPTH, D_MODEL, D_IN_PROJ), D_MODEL ** -0.5),
        'conv_w': nrm(ks[6], (DEPTH, SSD_CONV, D_XBC), SSD_CONV ** -0.5),
        'conv_b': nrm(ks[7], (DEPTH, D_XBC), 0.02),
        'dt_bias': dt0 + jnp.log(-jnp.expm1(-dt0)),
        'A_log': jnp.log(jax.random.uniform(ks[9], (DEPTH, SSD_HEADS), f32, 1.0, 16.0)),
        'D_skip': 1.0 + nrm(ks[10], (DEPTH, SSD_HEADS), 0.02),
        'ssd_norm_g': 1.0 + nrm(ks[11], (DEPTH, D_SSD), 0.02),
        'pool_w': nrm(ks[12], (DEPTH, POOL_GROUPS, POOL_GDIM, POOL_GDIM), POOL_GDIM ** -0.5),
        'pool_scale': 1.0 + nrm(ks[13], (DEPTH, D_POOL), 0.02),
        'w_br_ssd': nrm(ks[14], (DEPTH, D_SSD, D_MODEL), D_SSD ** -0.5),
        'w_br_pool': nrm(ks[15], (DEPTH, D_POOL, D_MODEL), D_POOL ** -0.5),
        'w_out': nrm(ks[16], (DEPTH, D_MODEL, D_MODEL), D_MODEL ** -0.5),
        'norm_ffn_g': 1.0 + nrm(ks[17], (DEPTH, D_MODEL), 0.02),
        'w_router': nrm(ks[18], (DEPTH, D_MODEL, N_EXPERTS), D_MODEL ** -0.5),
        'router_bias': nrm(ks[19], (DEPTH, N_EXPERTS), 0.01),
        'we_gate': nrm(ks[20], (DEPTH, N_EXPERTS, D_MODEL, D_EXPERT), D_MODEL ** -0.5),
        'we_up': nrm(ks[21], (DEPTH, N_EXPERTS, D_MODEL, D_EXPERT), D_MODEL ** -0.5),
        'we_down': nrm(ks[22], (DEPTH, N_EXPERTS, D_EXPERT, D_MODEL), D_EXPERT ** -0.5),
        'ws_gate': nrm(ks[23], (DEPTH, D_MODEL, D_SHARED), D_MODEL ** -0.5),
        'ws_up': nrm(ks[24], (DEPTH, D_MODEL, D_SHARED), D_MODEL ** -0.5),
        'ws_down': nrm(ks[25], (DEPTH, D_SHARED, D_MODEL), D_SHARED ** -0.5),
        'final_norm_g': 1.0 + nrm(ks[26], (D_MODEL,), 0.02),
    }


def reference(x, c, w_ada, b_ada, norm_mix_g, w_in, conv_w, conv_b, dt_bias, A_log, D_skip, ssd_norm_g,
              pool_w, pool_scale, w_br_ssd, w_br_pool, w_out, norm_ffn_g, w_router, router_bias,
              we_gate, we_up, we_down, ws_gate, ws_up, ws_down, final_norm_g):
    h = x
    c_act = jax.nn.silu(c)
    for l in range(DEPTH):
        mod = c_act @ w_ada[l] + b_ada[l]
        sh1, sc1, g1, sh2, sc2, g2 = jnp.split(mod, N_ADA, axis=-1)
        u = modulate(rmsnorm(h, norm_mix_g[l]), sh1, sc1)
        proj = u @ w_in[l]
        z, xbc, dt_raw, xp, gts = jnp.split(proj, SPLIT_OFFSETS, axis=-1)
        y_ssd = ssd_branch(z, xbc, dt_raw, conv_w[l], conv_b[l], dt_bias[l], A_log[l], D_skip[l],
                           ssd_norm_g[l]) @ w_br_ssd[l]
        y_pool = pool_branch(xp, pool_w[l], pool_scale[l]) @ w_br_pool[l]
        g_ssd, g_pool = jnp.split(jax.nn.sigmoid(gts), N_BRANCHES, axis=-1)
        mixed = (g_ssd * y_ssd + g_pool * y_pool) @ w_out[l]
        h = h + g1[:, None, :] * mixed
        u2 = modulate(rmsnorm(h, norm_ffn_g[l]), sh2, sc2)
        h = h + g2[:, None, :] * moe_ffn(u2, w_router[l], router_bias[l], we_gate[l], we_up[l], we_down[l],
                                         ws_gate[l], ws_up[l], ws_down[l])
    return rmsnorm(h, final_norm_g)
```

```python
import numpy as np
import concourse.bass as bass
import concourse.mybir as mybir

F32 = mybir.dt.float32
BF16 = mybir.dt.bfloat16
AF = mybir.ActivationFunctionType
ALU = mybir.AluOpType
AX = mybir.AxisListType


class Reg:
    __slots__ = ("name", "w", "r")

    ALL = []

    def __init__(self, name):
        self.name = name
        self.w = None
        self.r = []
        Reg.ALL.append(self)


class Prog:
    ENGS = ("pe", "act", "dve", "pool", "sp")

    def __init__(self, nc, ndma_sems=8, same_engine_sync=True):
        self.nc = nc
        self.q = {e: [] for e in self.ENGS}
        self.cnt = {e: 0 for e in self.ENGS}
        self.known = {e: {} for e in self.ENGS}
        self.sems = {}
        self.same = same_engine_sync
        self.ndma = ndma_sems
        self.dma_rr = {e: 0 for e in self.ENGS}
        self.dma_val = {}
        self.stack = None

    def _semkeys(self):
        keys = list(self.ENGS) + ["cc"]
        for e in ("sp", "pool", "act"):
            for i in range(self.ndma):
                keys.append(f"d_{e}_{i}")
        return keys

    def _need(self, eng, events):
        mx = {}
        for ev in events:
            if ev is None:
                continue
            k, v = ev
            if k == eng and not self.same:
                continue
            if k == "cc" and eng != "pool":
                continue
            if v > mx.get(k, 0):
                mx[k] = v
        out = []
        kn = self.known[eng]
        for k, v in mx.items():
            if kn.get(k, 0) >= v:
                continue
            kn[k] = v
            out.append((k, v))
        return out

    def _deps(self, reads, writes):
        evs = []
        for r in reads:
            evs.append(r.w)
        for w in writes:
            evs.append(w.w)
            evs.extend(w.r)
        return evs

    def _commit(self, ev, reads, writes):
        for r in reads:
            r.r.append(ev)
        for w in writes:
            w.w = ev
            w.r = []

    def op(self, eng, fn, reads=(), writes=(), extra=()):
        evs = self._deps(reads, writes) + list(extra)
        waits = self._need(eng, evs)
        self.cnt[eng] += 1
        ev = (eng, self.cnt[eng])
        self.q[eng].append((waits, fn, (eng, 1)))
        self._commit(ev, reads, writes)
        return ev

    def dma(self, eng, fn, reads=(), writes=(), extra=()):
        i = self.dma_rr[eng]
        self.dma_rr[eng] = (i + 1) % self.ndma
        key = f"d_{eng}_{i}"
        prev = self.dma_val.get(key, 0)
        evs = self._deps(reads, writes) + list(extra)
        if prev:
            evs.append((key, prev))
        waits = self._need(eng, evs)
        val = prev + 16
        self.dma_val[key] = val
        ev = (key, val)
        self.q[eng].append((waits, fn, (key, 16)))
        self._commit(ev, reads, writes)
        return ev

    def cc(self, fn, reads=(), writes=(), extra=(), inc=1):
        eng = "pool"
        evs = self._deps(reads, writes) + list(extra)
        waits = self._need(eng, evs)
        val = self.dma_val.get("cc", 0) + inc
        self.dma_val["cc"] = val
        ev = ("cc", val)
        self.q[eng].append((waits, fn, ("cc", inc)))
        self._commit(ev, reads, writes)
        return ev

    def wait_all(self, eng, events):
        waits = self._need(eng, list(events))
        self.q[eng].append((waits, None, None))

    def all_events(self):
        evs = [(e, c) for e, c in self.cnt.items() if c]
        evs += [(k, v) for k, v in self.dma_val.items()]
        return evs

    def begin(self, stack):
        self.gen = 0
        self.semh = {k: stack.enter_context(self.nc.semaphore(k)) for k in self._semkeys()}

    def rebase(self, stack):
        self.gen += 1
        self.semh = {k: stack.enter_context(self.nc.semaphore(f"{k}_g{self.gen}")) for k in self._semkeys()}
        self.cnt = {e: 0 for e in self.ENGS}
        self.known = {e: {} for e in self.ENGS}
        self.dma_rr = {e: 0 for e in self.ENGS}
        self.dma_val = {}
        for r in Reg.ALL:
            r.w = None
            r.r = []

    def flush(self):
        nc = self.nc
        sems = self.semh
        q = self.q

        def run(engh, lst):
            for waits, fn, inc in lst:
                for k, v in waits:
                    engh.wait_ge(sems[k], v)
                if fn is None:
                    continue
                ins = fn(engh)
                ins.then_inc(sems[inc[0]], inc[1])

        with nc.Block() as block:
            @block.tensor
            def _(e):
                run(e, q["pe"])

            @block.scalar
            def _(e):
                run(e, q["act"])

            @block.vector
            def _(e):
                run(e, q["dve"])

            @block.gpsimd
            def _(e):
                run(e, q["pool"])

            @block.sync
            def _(e):
                run(e, q["sp"])
        self.q = {e: [] for e in self.ENGS}


from contextlib import ExitStack
from concourse.bass_utils import run_bass_kernel_spmd

NCORES = 8
T = 2048
NT = 16
TS = 256
NSL = T // TS
D = 1024
EPS = 1e-6
OFF_Z, OFF_X, OFF_B, OFF_C, OFF_DT, OFF_P, OFF_GS, OFF_GP = 0, 2048, 4096, 5120, 6144, 6176, 7200, 8224
DIN = 9248
PB = "dve"
NCOL = 208
C_NMG, C_NFG, C_PSC, C_SNG, C_C, C_CB, C_CW = 0, 8, 16, 24, 40, 48, 80
R_BADA, R_DTB, R_ALOG, R_DSK, R_RB, R_FNG = 0, 6144, 6176, 6208, 6240, 6304
NROW = 7328
BIG = 1.0e4
DEBUG = False
import os
DBGV = int(os.environ.get("DBGV", "0"))


class _Stop(Exception):
    pass


def build_nc(n_experts=65, do_moe=True, stop_after=None, n_slabs=NSL, n_pre_slots=NCORES - 1):
    nc = bass.Bass("TRN2", target_bir_lowering=False)
    di = lambda n, s, dt=F32: nc.dram_tensor(n, s, dt, kind="ExternalInput").ap()
    x_own = di("x_own", [T, D]); x_halo = di("x_halo", [128, D]); x_all = di("x_all", [(NCORES - 1) * T, D])
    rk_d = di("rk", [128, 8]); nf_d = di("nf", [128, 1]); pos_d = di("posrow", [128, 16])
    cols_d = di("cols", [128, NCOL]); rows_d = di("rows", [128, NROW])
    w_ada = di("w_ada", [D, 6 * D]); w_in = di("w_in", [D, DIN])
    pool_w = di("pool_w", [1024, 256]); w_br_ssd = di("w_br_ssd", [2048, D])
    w_br_pool = di("w_br_pool", [D, D]); w_out = di("w_out", [D, D])
    w_router = di("w_router", [D, 64])
    NEW = 65 if stop_after is None else 1
    wgu = di("wgu", [NEW, 128, 8, 512]); wdn = di("wdn", [NEW, 128, 2, 1024])
    out_d = nc.dram_tensor("out", [T, D], F32, kind="ExternalOutput").ap()
    w_in_b = nc.dram_tensor("w_in_b", [D, DIN], BF16).ap()
    wbp_b = nc.dram_tensor("wbp_b", [D, D], BF16).ap()
    wbs_b = nc.dram_tensor("wbs_b", [2048, D], BF16).ap()
    wout_b = nc.dram_tensor("wout_b", [D, D], BF16).ap()
    bounce = nc.dram_tensor("bounce", [128, 2080], F32).ap()
    gathered = nc.dram_tensor("gathered", [1024, 2080], F32).ap()
    dbg = {}

    Reg.ALL.clear()
    P = Prog(nc)
    regs = {}
    _top0 = ExitStack()
    P.begin(_top0)

    ALIAS = {"yn": ["bigA", "bigB"], "big": ["bigA", "bigB"], "junk": ["gsig"], "tmpD": ["xn"]}

    def R(name):
        if name not in regs:
            regs[name] = Reg(name)
        return regs[name]

    top = ExitStack()

    def sb(st, name, shape, dt):
        return st.enter_context(nc.sbuf_tensor("s_" + name, shape, dt))

    def rl(x):
        out = []
        for n in x:
            if isinstance(n, str):
                for m in ALIAS.get(n, [n]):
                    out.append(R(m))
            else:
                out.append(n)
        return out

    def tt(eng, out, a, b, op, rd, wr):
        return P.op(eng, lambda e: e.tensor_tensor(out=out, in0=a, in1=b, op=op), rl(rd), rl(wr))

    def ts(eng, out, a, s1, s2, op0, op1, rd, wr):
        if op1 is None:
            return P.op(eng, lambda e: e.tensor_single_scalar(out=out, in_=a, scalar=s1, op=op0), rl(rd), rl(wr))
        return P.op(eng, lambda e: e.tensor_scalar(out=out, in0=a, scalar1=s1, scalar2=s2, op0=op0, op1=op1), rl(rd), rl(wr))

    def stt(eng, out, a, sc, b, op0, op1, rd, wr):
        return P.op(eng, lambda e: e.scalar_tensor_tensor(out=out, in0=a, scalar=sc, in1=b, op0=op0, op1=op1), rl(rd), rl(wr))

    def act(out, in_, func, rd, wr, bias=0.0, scale=1.0, accum_out=None):
        if accum_out is None:
            return P.op("act", lambda e: e.activation(out=out, in_=in_, func=func, bias=bias, scale=scale), rl(rd), rl(wr))
        return P.op("act", lambda e: e.activation(out=out, in_=in_, func=func, bias=bias, scale=scale, accum_out=accum_out), rl(rd), rl(wr))

    def cp(eng, out, in_, rd, wr):
        if eng == "act":
            return P.op("act", lambda e: e.copy(out=out, in_=in_), rl(rd), rl(wr))
        return P.op(eng, lambda e: e.tensor_copy(out=out, in_=in_), rl(rd), rl(wr))

    def mms(lst, rd, wr):
        def f(e):
            for (o, l, r, s0, s1) in lst:
                ins = e.matmul(o, lhsT=l, rhs=r, start=s0, stop=s1)
            return ins
        return P.op("pe", f, rl(rd), rl(wr))

    def trs(lst, rd, wr):
        def f(e):
            for (o, i, idn) in lst:
                ins = e.transpose(out=o, in_=i, identity=idn)
            return ins
        return P.op("pe", f, rl(rd), rl(wr))

    def dma(q, out, in_, rd, wr):
        if q == "pool":
            return P.dma(q, lambda e: e.dma_start(out=out, in_=in_, max_dma_last_dim=4096), rl(rd), rl(wr))
        return P.dma(q, lambda e: e.dma_start(out=out, in_=in_), rl(rd), rl(wr))

    def barrier():
        evs = P.all_events()
        for e in ("pe", "act", "dve", "pool", "sp"):
            P.wait_all(e, evs)

    def checkpoint(name, tensors):
        if stop_after != name:
            return
        for tn, t in tensors.items():
            a = t if hasattr(t, "ap") and not hasattr(t, "__enter__") and type(t).__name__ == "AP" else t[:]
            shp = list(a.shape)
            d = nc.dram_tensor("dbg_" + tn, shp, a.dtype, kind="ExternalOutput").ap()
            dma("sp", d, a, [tn], [])
        P.wait_all("sp", P.all_events())
        P.flush()
        raise _Stop()

    banks = [top.enter_context(nc.psum_tensor(f"bank{i}", [128, 512], F32)) for i in range(8)]
    bstate = {"i": 0, "y": 0}

    def bank():
        i = bstate["i"]
        bstate["i"] = (i + 1) % 4
        return banks[i], R(f"bank{i}")

    def ybank(i=None):
        if i is None:
            i = bstate["y"]
            bstate["y"] = (i + 1) % 4
        return banks[4 + i], R(f"bank{4 + i}")

    H = sb(top, "H", [128, NT, D], F32)
    state = sb(top, "state", [128, 2048], F32)
    state_bf = sb(top, "state_bf", [128, 2048], BF16)
    identF = sb(top, "identF", [128, 128], F32)
    SLf = sb(top, "SLf", [128, 128], F32)
    ONf = sb(top, "ONf", [128, 128], F32)
    SLb = sb(top, "SLb", [128, 128], BF16)
    ONb = sb(top, "ONb", [128, 128], BF16)
    UTb = sb(top, "UTb", [128, 128], BF16)
    MKf = sb(top, "MKf", [128, 128], F32)
    cols = sb(top, "cols", [128, NCOL], F32)
    rowS = sb(top, "rowS", [128, 160], F32)
    rk = sb(top, "rk_s", [128, 8], F32)
    nf = sb(top, "nf_s", [128, 1], F32)
    mcol = sb(top, "mcol", [128, 32], F32)
    gsc = sb(top, "gsc", [128, 16], F32)
    g2_d = nc.dram_tensor("g2_d", [128, D], F32).ap()

    def mask(tn, tensor, cmp, sgn=1):
        P.op("pool", lambda e: e.memset(tensor[:], 1.0), [], [R(tn)])
        P.op("pool", lambda e: e.affine_select(out=tensor[:], in_=tensor[:], pattern=[[-sgn, 128]], compare_op=cmp,
                                               fill=0.0, base=0, channel_multiplier=sgn), [R(tn)], [R(tn)])
    mask("identF", identF, ALU.is_equal)
    mask("SLf", SLf, ALU.is_gt)
    mask("MKf", MKf, ALU.is_ge, -1)
    P.op("pool", lambda e: e.memset(ONf[:], 1.0), [], [R("ONf")])
    cp("pool", SLb[:], SLf[:], ["SLf"], ["SLb"])
    cp("pool", ONb[:], ONf[:], ["ONf"], ["ONb"])
    cp("pool", UTb[:], MKf[:], ["MKf"], ["UTb"])

    dma("sp", cols[:], cols_d, [], ["cols"])
    dma("sp", rowS[:], rows_d[:, R_DTB:R_DTB + 160], [], ["rowS"])
    dma("sp", rk[:], rk_d, [], ["rk"])
    dma("sp", nf[:], nf_d, [], ["nf"])
    dma("sp", H[:], x_own.rearrange("(t p) d -> p t d", p=128), [], ["H"])
    _wv_o = w_in_b.rearrange("a b -> (a b)").rearrange("(r c) -> r c", c=2048)
    _wv_i = w_in.rearrange("a b -> (a b)").rearrange("(r c) -> r c", c=2048)
    for _q in range(8):
        dma("pool", _wv_o[_q * 578:(_q + 1) * 578, :], _wv_i[_q * 578:(_q + 1) * 578, :], [], ["w_in_b"])
    dma("pool", wbp_b, w_br_pool, [], ["wbp_b"])

    try:
        with ExitStack() as s0:
            bcm = sb(s0, "bcm", [128, 6 * D], F32)
            cact = sb(s0, "cact", [128, 8], F32)
            CBC = sb(s0, "CBC", [128, 8, 128], F32)
            wa = [sb(s0, f"wa{i}", [128, 8, 512], F32) for i in range(2)]
            ba = [sb(s0, f"ba{i}", [128, 512], F32) for i in range(2)]
            stg = [sb(s0, f"stg{i}", [128, 4, D], F32) for i in range(2)]
            stb = [sb(s0, f"stb{i}", [128, 4, D], BF16) for i in range(2)]
            act(cact[:], cols[:, C_C:C_C + 8], AF.Silu, ["cols"], ["cact"])
            cp("dve", CBC[:], cact[:].unsqueeze(2).to_broadcast([128, 8, 128]), ["cact"], ["CBC"])
            for ns in range(12):
                b = ns % 2
                dma("sp", wa[b][:], w_ada[:, ns * 512:(ns + 1) * 512].rearrange("(c p) n -> p c n", p=128), [], [f"wa{b}"])
                dma("sp", ba[b][:], rows_d[:, R_BADA + ns * 512:R_BADA + (ns + 1) * 512], [], [f"ba{b}"])
                bk, br = bank()
                mms([(bk[:], CBC[:, dc, :], wa[b][:, dc, :], dc == 0, dc == 7) for dc in range(8)], ["CBC", f"wa{b}"], [br])
                tt("dve", bcm[:, ns * 512:(ns + 1) * 512], bk[:], ba[b][:], ALU.add, [br, f"ba{b}"], ["bcm"])
            for vi, v in enumerate((0, 1, 3, 4)):
                for q in range(2):
                    bk, br = bank()
                    trs([(bk[:, j * 128:(j + 1) * 128], bcm[:, v * D + (q * 4 + j) * 128: v * D + (q * 4 + j + 1) * 128], identF[:])
                         for j in range(4)], ["bcm", "identF"], [br])
                    cp("dve", mcol[:, vi * 8 + q * 4: vi * 8 + q * 4 + 4],
                       bk[:].rearrange("p (j n) -> p j n", n=128)[:, :, 0], [br], ["mcol"])
            stt("dve", gsc[:, 0:8], mcol[:, 8:16], 1.0, cols[:, C_NMG:C_NMG + 8], ALU.add, ALU.mult, ["mcol", "cols"], ["gsc"])
            stt("dve", gsc[:, 8:16], mcol[:, 24:32], 1.0, cols[:, C_NFG:C_NFG + 8], ALU.add, ALU.mult, ["mcol", "cols"], ["gsc"])
            dma("sp", g2_d, bcm[:, 5 * D:6 * D], ["bcm"], ["g2_d"])
            act(rowS[:, 32:64], rowS[:, 32:64], AF.Exp, ["rowS"], ["rowS"])
            ts("dve", rowS[:, 32:64], rowS[:, 32:64], -1.0, None, ALU.mult, None, ["rowS"], ["rowS"])
            k = 0
            for half in range(2):
                b = k % 2; k += 1
                dma("sp", stg[b][:], w_out[half * 512:(half + 1) * 512, :].rearrange("(c p) n -> p c n", p=128), [], [f"stg{b}"])
                tt("dve", stb[b][:], stg[b][:], bcm[:, 2 * D:3 * D].unsqueeze(1).to_broadcast([128, 4, D]), ALU.mult,
                   [f"stg{b}", "bcm"], [f"stb{b}"])
                dma("sp", wout_b[half * 512:(half + 1) * 512, :].rearrange("(c p) n -> p c n", p=128), stb[b][:], [f"stb{b}"], ["wout_b"])
            for q in range(4):
                b = k % 2; k += 1
                dma("sp", stg[b][:], w_br_ssd[q * 512:(q + 1) * 512, :].rearrange("(c p) n -> p c n", p=128), [], [f"stg{b}"])
                for j in range(4):
                    ts("pool", stb[b][:, j, :], stg[b][:, j, :], cols[:, C_SNG + q * 4 + j:C_SNG + q * 4 + j + 1], None, ALU.mult, None,
                       [f"stg{b}", "cols"], [f"stb{b}"])
                dma("sp", wbs_b[q * 512:(q + 1) * 512, :].rearrange("(c p) n -> p c n", p=128), stb[b][:], [f"stb{b}"], ["wbs_b"])
            checkpoint("p0", {"mcol": mcol, "gsc": gsc, "rowS": rowS, "bcm": bcm})
            barrier()
            P.flush()

        with ExitStack() as sA:
            WS = [sb(sA, f"WS{i}", [128, 8, 512], BF16) for i in range(2)]
            wsi = {"i": 0}
            wdt = sb(sA, "wdt", [128, 8, 32], BF16)
            poolw = sb(sA, "poolw", [128, 8, 256], BF16)
            pcorr = sb(sA, "pcorr", [128, 4, 16], F32)
            posr = sb(sA, "posr", [128, 16], F32)
            tailx = sb(sA, "tailx", [128, 32, 3], F32)
            tailx0 = sb(sA, "tailx0", [128, 32, 3], F32)
            ptail = sb(sA, "ptail", [128, 8, 15], F32)
            xpw = [sb(sA, f"xpw{i}", [128, 15 + TS], F32) for i in range(2)]
            logD = sb(sA, "logD", [128, 32], F32)
            logDp = [logD, sb(sA, "logD1", [128, 32], F32)]
            ldi = {"i": 0}
            uT = sb(sA, "uT", [128, 8, TS], BF16)
            xn = sb(sA, "xn", [128, D], F32)
            st1 = sb(sA, "st1", [128, 4], F32)
            pooledT = sb(sA, "pooledT", [128, 8, TS], BF16)
            mixedT = sb(sA, "mixedT", [128, 8, TS], BF16)
            gp = sb(sA, "gp", [128, 8, TS], BF16)
            pS = [sb(sA, f"pS{i}", [128, 15 + TS], F32) for i in range(2)]
            gsig = sb(sA, "gsig", [128, TS], F32)
            NSET = 4
            xcs = [sb(sA, f"xc{i}", [128, 3 + TS], F32) for i in range(NSET)]
            caccs = [sb(sA, f"cacc{i}", [128, TS], F32) for i in range(NSET)]
            big = sb(sA, "big", [128, 2080], F32)
            Bfs = [sb(sA, f"Bf{i}", [128, TS], F32) for i in range(2)]
            xs_tok = sb(sA, "xs_tok", [128, 2, 2048], BF16)
            B_tok = sb(sA, "B_tok", [128, 2, 1024], BF16)
            BT = sb(sA, "BT", [128, 8, TS], BF16)
            CT = sb(sA, "CT", [128, 8, TS], BF16)
            dtb = sb(sA, "dtb", [128, 2, 32], F32)
            ab = sb(sA, "ab", [128, 2, 32], F32)
            ab16 = sb(sA, "ab16", [128, 2, 32], BF16)
            sm = sb(sA, "sm", [128, 4, 32], F32)
            sz_tok = sb(sA, "sz_tok", [128, 2, 2048], BF16)
            xdt = sb(sA, "xdt", [128, 2048], BF16)
            xdtd = sb(sA, "xdtd", [128, 2048], BF16)
            AL = sb(sA, "AL", [128, 4, 128], BF16)
            AO = sb(sA, "AO", [128, 4, 128], BF16)
            LT = sb(sA, "LT", [128, 4, 128], BF16)
            Eb = sb(sA, "Eb", [128, 4, 128], BF16)
            CBm = sb(sA, "CBm", [128, 8, 128], BF16)
            MT = sb(sA, "MT", [128, 4, 128], BF16)
            CE = sb(sA, "CE", [128, 4, 128], BF16)
            tmpD = xn
            yn = big
            junk = gsig
            ss8 = sb(sA, "ss8", [128, 8], F32)
            ynT = sb(sA, "ynT", [128, 16, TS], BF16)
            combT = sb(sA, "combT", [128, 8, TS], BF16)
            stS = big

            def wblock(src, rd):
                i = wsi["i"]; wsi["i"] = (i + 1) % 2
                dma("sp", WS[i][:], src.rearrange("(c p) n -> p c n", p=128), rd, [f"WS{i}"])
                return WS[i], f"WS{i}"

            dma("pool", wdt[:], w_in[:, OFF_DT:OFF_DT + 32].rearrange("(c p) n -> p c n", p=128), [], ["wdt"])
            dma("pool", poolw[:], pool_w.rearrange("(c p) n -> p c n", p=128), [], ["poolw"])
            dma("sp", posr[:], pos_d, [], ["posr"])
            for g, w in enumerate((2, 4, 8, 16)):
                ts("dve", pcorr[:, g, :], posr[:], float(w), None, ALU.min, None, ["posr"], ["pcorr"])
                P.op("dve", lambda e, g=g: e.reciprocal(out=pcorr[:, g, :], in_=pcorr[:, g, :]), rl(["pcorr"]), rl(["pcorr"]))
                ts("dve", pcorr[:, g, :], pcorr[:, g, :], float(w), None, ALU.mult, None, ["pcorr"], ["pcorr"])

            def make_uT(src_ap, rdname, n_tiles, col0=0):
                for tti in range(n_tiles):
                    xin = src_ap(tti)
                    act(junk_big[:], xin, AF.Square, [rdname], ["xn", "st1"], accum_out=st1[:, 0:1])
                    act(st1[:, 1:2], st1[:, 0:1], AF.Sqrt, ["st1"], ["st1"], bias=EPS, scale=1.0 / D)
                    P.op("dve", lambda e: e.reciprocal(out=st1[:, 2:3], in_=st1[:, 1:2]), rl(["st1"]), rl(["st1"]))
                    ts("dve", xn[:], xin, st1[:, 2:3], None, ALU.mult, None, [rdname, "st1"], ["xn"])
                    for half in range(2):
                        bk, br = bank()
                        trs([(bk[:, j * 128:(j + 1) * 128], xn[:, (half * 4 + j) * 128:(half * 4 + j + 1) * 128], identF[:])
                             for j in range(4)], ["xn", "identF"], [br])
                        for j in range(4):
                            dc = half * 4 + j
                            ts("dve" if half else "pool_no", uT[:, dc, col0 + tti * 128: col0 + (tti + 1) * 128], bk[:, j * 128:(j + 1) * 128],
                               gsc[:, dc:dc + 1], mcol[:, dc:dc + 1], ALU.mult, ALU.add, [br, "gsc", "mcol"], ["uT"])

            junk_big = xn

            _ts_orig = ts

            def ts(eng, out, a, s1, s2, op0, op1, rd, wr):
                if eng == "pool_no":
                    return P.op("act", lambda e: e.activation(out=out, in_=a, func=AF.Identity, bias=s2, scale=s1), rl(rd), rl(wr))
                return _ts_orig(eng, out, a, s1, s2, op0, op1, rd, wr)

            xh = xsA = big
            dma("sp", big[:, 0:D], x_halo, [], ["bigA"])
            make_uT(lambda tti: big[:, 0:D], "bigA", 1)
            for blk in range(10):
                c0 = OFF_X + blk * 512 if blk < 8 else OFF_P + (blk - 8) * 512
                wsb, wr_ = wblock(w_in_b[:, c0:c0 + 512], ["w_in_b"])
                for j in range(4):
                    bk, br = bank()
                    mms([(bk[:, 0:128], wsb[:, dc, j * 128:(j + 1) * 128], uT[:, dc, 0:128], dc == 0, dc == 7) for dc in range(8)],
                        [wr_, "uT"], [br])
                    if blk < 8:
                        c = blk * 4 + j
                        ts("dve", tailx0[:, c, :], bk[:, 125:128], nf[:, 0:1], None, ALU.mult, None, [br, "nf"], ["tailx0"])
                    else:
                        c = (blk - 8) * 4 + j
                        ts("dve", ptail[:, c, :], bk[:, 113:128], nf[:, 0:1], None, ALU.mult, None, [br, "nf"], ["ptail"])

            checkpoint("halo", {"tailx0": tailx0, "xpb": ptail, "uT": uT[:, :, 0:128]})
            def slab(si, pre, src=None):
                t0 = 2 * si
                if src is None:
                    make_uT(lambda tti: H[:, t0 + tti, :], "H", 2)
                else:
                    make_uT(src, "sz_tok", 2)
                if not pre:
                    for blk in range(2):
                        wsb, wr_ = wblock(w_in_b[:, OFF_P + blk * 512: OFF_P + (blk + 1) * 512], ["w_in_b"])
                        for j in range(4):
                            c = blk * 4 + j
                            g = c // 2
                            w = 2 << g
                            bk, br = bank()
                            mms([(bk[:, 0:TS], wsb[:, dc, j * 128:(j + 1) * 128], uT[:, dc, :], dc == 0, dc == 7) for dc in range(8)],
                                [wr_, "uT"], [br])
                            xw = xpw[c % 2]; xwn = f"xpw{c % 2}"
                            cp("pool", xw[:, 0:15], ptail[:, c, :], ["ptail"], [xwn])
                            cp("act", xw[:, 15:15 + TS], bk[:, 0:TS], [br], [xwn])
                            cp("pool", ptail[:, c, :], xw[:, TS:TS + 15], [xwn], ["ptail"])
                            src = xw[:]
                            lo = 0
                            step = 1
                            k = 0
                            while step < w:
                                dst = pS[k % 2]
                                nlo = lo + step
                                tt("dve", dst[:, nlo:15 + TS], src[:, nlo:15 + TS], src[:, nlo - step:15 + TS - step], ALU.add,
                                   [xwn, f"pS{(k + 1) % 2}"], [f"pS{k % 2}"])
                                src = dst[:]
                                lo = nlo
                                step *= 2
                                k += 1
                            sname = f"pS{(k - 1) % 2}"
                            if si == 0:
                                tt("dve", src[:, 15:31], src[:, 15:31], pcorr[:, g, :], ALU.mult, [sname, "pcorr"], [sname])
                            stt("dve", pooledT[:, c, :], src[:, 15:15 + TS], 1.0 / w, xw[:, 15:15 + TS], ALU.mult, ALU.subtract,
                                [sname, xwn], ["pooledT"])
                            pass
                    for g in range(4):
                        for dch in range(2):
                            bk, br = bank()
                            mms([(bk[:, 0:TS], poolw[:, 2 * g + kc, dch * 128:(dch + 1) * 128], pooledT[:, 2 * g + kc, :], kc == 0, kc == 1)
                                 for kc in range(2)], ["poolw", "pooledT"], [br])
                            ts("dve", mixedT[:, 2 * g + dch, :], bk[:, 0:TS], cols[:, C_PSC + 2 * g + dch:C_PSC + 2 * g + dch + 1], None,
                               ALU.mult, None, [br, "cols"], ["mixedT"])
                    for blk in range(2):
                        wsb, wr_ = wblock(wbp_b[:, blk * 512:(blk + 1) * 512], ["wbp_b"])
                        wsg, wg_ = wblock(w_in_b[:, OFF_GP + blk * 512: OFF_GP + (blk + 1) * 512], ["w_in_b"])
                        for j in range(4):
                            ec = blk * 4 + j
                            bk, br = bank()
                            mms([(bk[:, 0:TS], wsg[:, dc, j * 128:(j + 1) * 128], uT[:, dc, :], dc == 0, dc == 7) for dc in range(8)],
                                [wg_, "uT"], [br])
                            act(gsig[:], bk[:, 0:TS], AF.Sigmoid, [br], ["gsig"])
                            bk2, br2 = bank()
                            mms([(bk2[:, 0:TS], wsb[:, dc, j * 128:(j + 1) * 128], mixedT[:, dc, :], dc == 0, dc == 7) for dc in range(8)],
                                [wr_, "mixedT"], [br2])
                            tt("dve", gp[:, ec, :], bk2[:, 0:TS], gsig[:], ALU.mult, [br2, "gsig"], ["gp"])
                checkpoint(f"ut{int(pre)}", {"uT": uT})
                checkpoint(f"pool{int(pre)}", {"gp": gp})
                nblk = 6 if pre else 8
                for blk in range(nblk):
                    wsb, wr_ = wblock(w_in_b[:, OFF_X + blk * 512: OFF_X + (blk + 1) * 512], ["w_in_b"])
                    for j in range(4):
                        c = blk * 4 + j
                        bk, br = bank()
                        mms([(bk[:, 0:TS], wsb[:, dc, j * 128:(j + 1) * 128], uT[:, dc, :], dc == 0, dc == 7) for dc in range(8)],
                            [wr_, "uT"], [br])
                        xc = xcs[c % NSET]; cacc = caccs[c % NSET]; xcn = f"xc{c % NSET}"; can = f"cacc{c % NSET}"
                        Bf = Bfs[c % 2]; Bfn = f"Bf{c % 2}"
                        cp("pool", xc[:, 0:3], tailx[:, c, :], [f"tailx{c}"], [xcn])
                        cp("act", xc[:, 3:3 + TS], bk[:, 0:TS], [br], [xcn])
                        cp("pool", tailx[:, c, :], xc[:, TS:TS + 3], [xcn], [f"tailx{c}"])
                        cw = lambda k_: cols[:, C_CW + c * 4 + k_: C_CW + c * 4 + k_ + 1]
                        P.op("act", lambda e, cacc=cacc, bk=bk, s_=cw(3): e.activation(out=cacc[:], in_=bk[:, 0:TS], func=AF.Copy, scale=s_),
                             rl([br, "cols"]), rl([can]))
                        for k_ in (0, 1, 2):
                            stt("dve", cacc[:], xc[:, k_:k_ + TS], cw(k_), cacc[:], ALU.mult, ALU.add, [xcn, "cols", can], [can])
                        bcol = cols[:, C_CB + c:C_CB + c + 1]
                        if c < 16:
                            hb = (c // 4) % 2; hbn = "bigA" if hb == 0 else "bigB"
                            act(big[:, hb * 1024 + (c % 4) * TS: hb * 1024 + (c % 4 + 1) * TS], cacc[:], AF.Silu, [can, "cols"], [hbn], bias=bcol)
                            if c % 4 == 3:
                                cg = c // 4
                                for tti in range(2):
                                    bk2, br2 = bank()
                                    trs([(bk2[:, q * 128:(q + 1) * 128], big[:, hb * 1024 + q * TS + tti * 128: hb * 1024 + q * TS + (tti + 1) * 128], identF[:])
                                         for q in range(4)], [hbn, "identF"], [br2])
                                    cp("dve", xs_tok[:, tti, cg * 512:(cg + 1) * 512], bk2[:], [br2], ["xs_tok"])
                        elif c < 24:
                            g = c - 16
                            act(Bf[:], cacc[:], AF.Silu, [can, "cols"], [Bfn], bias=bcol)
                            cp("dve", BT[:, g, :], Bf[:], [Bfn], ["BT"])
                            bk2, br2 = bank()
                            trs([(bk2[:, tti * 128:(tti + 1) * 128], Bf[:, tti * 128:(tti + 1) * 128], identF[:]) for tti in range(2)],
                                [Bfn, "identF"], [br2])
                            cp("dve", B_tok[:, :, g * 128:(g + 1) * 128], bk2[:, 0:256].rearrange("p (t n) -> p t n", n=128), [br2], ["B_tok"])
                        else:
                            g = c - 24
                            act(CT[:, g, :], cacc[:], AF.Silu, [can, "cols"], ["CT"], bias=bcol)
                checkpoint(f"conv{int(pre)}", {"xs_tok": xs_tok, "B_tok": B_tok})
                for tti in range(2):
                    bk, br = bank()
                    mms([(bk[:, 0:32], uT[:, dc, tti * 128:(tti + 1) * 128], wdt[:, dc, :], dc == 0, dc == 7) for dc in range(8)],
                        ["uT", "wdt"], [br])
                    tt("dve", dtb[:, tti, :], bk[:, 0:32], rowS[:, 0:32], ALU.add, [br, "rowS"], ["dtb"])
                    act(dtb[:, tti, :], dtb[:, tti, :], AF.Exp, ["dtb"], ["dtb"])
                    act(dtb[:, tti, :], dtb[:, tti, :], AF.Ln, ["dtb"], ["dtb"], bias=1.0)
                    tt("dve", ab[:, tti, :], dtb[:, tti, :], rowS[:, 32:64], ALU.mult, ["dtb", "rowS"], ["ab"])
                    cp("dve", ab16[:, tti, :], ab[:, tti, :], ["ab"], ["ab16"])
                checkpoint(f"dt{int(pre)}", {"dtb": dtb, "ab": ab})
                if not pre:
                    for zc in range(4):
                        wsb, wr_ = wblock(w_in_b[:, OFF_Z + zc * 512: OFF_Z + (zc + 1) * 512], ["w_in_b"])
                        for tti in range(2):
                            bk, br = bank()
                            mms([(bk[:], uT[:, dc, tti * 128:(tti + 1) * 128], wsb[:, dc, :], dc == 0, dc == 7) for dc in range(8)],
                                ["uT", wr_], [br])
                            act(sz_tok[:, tti, zc * 512:(zc + 1) * 512], bk[:], AF.Silu, [br], ["sz_tok"])
                checkpoint(f"z{int(pre)}", {"sz_tok": sz_tok})
                for tti in range(2):
                    tsl = slice(tti * 128, (tti + 1) * 128)
                    bk, br = bank()
                    mms([(bk[:, 0:32], SLf[:], ab[:, tti, :], True, True)], ["SLf", "ab"], [br])
                    bkB, brB = bank()
                    mms([(bkB[:, 0:32], ONf[:], ab[:, tti, :], True, True)], ["ONf", "ab"], [brB])
                    act(sm[:, 0, :], bk[:, 0:32], AF.Exp, [br], ["sm"])
                    act(sm[:, 1, :], bkB[:, 0:32], AF.Exp, [brB], ["sm"])
                    checkpoint(f"ssdA{int(pre)}", {"sm": sm[:, 0:2, :]})
                    checkpoint(f"ssdA2{int(pre)}", {"logD": logD})
                    tt("dve", xdt[:].rearrange("p (h d) -> p h d", h=32), xs_tok[:, tti, :].rearrange("p (h d) -> p h d", h=32),
                       dtb[:, tti, :].unsqueeze(2).to_broadcast([128, 32, 64]), ALU.mult, ["xs_tok", "dtb"], ["xdt"])
                    checkpoint(f"ssdA3{int(pre)}", {"xdt": xdt})
                    tt(PB, xdtd[:].rearrange("p (h d) -> p h d", h=32), xdt[:].rearrange("p (h d) -> p h d", h=32),
                       sm[:, 0, :].unsqueeze(2).to_broadcast([128, 32, 64]), ALU.mult, ["xdt", "sm"], ["xdtd"])
                    checkpoint(f"ssdB{int(pre)}", {"xdt": xdt, "xdtd": xdtd})
                    if not pre:
                        for gh in range(2):
                            bk, br = bank()
                            mms([(bk[:, q * 128:(q + 1) * 128], BT[:, gh * 4 + q, tsl], CT[:, gh * 4 + q, tsl], True, True) for q in range(4)],
                                ["BT", "CT"], [br])
                            tt("dve", CBm[:, gh * 4:(gh + 1) * 4, :], bk[:].rearrange("p (q n) -> p q n", n=128),
                               MKf[:].unsqueeze(1).to_broadcast([128, 4, 128]), ALU.mult, [br, "MKf"], ["CBm"])
                        checkpoint(f"cb{int(pre)}", {"CBm": CBm})
                        ybanks = []
                        for g in range(8):
                            a4 = ab16[:, tti, 4 * g:4 * g + 4].unsqueeze(2).to_broadcast([128, 4, 128])
                            tt("dve", AL[:], SLb[:].unsqueeze(1).to_broadcast([128, 4, 128]), a4, ALU.mult, ["SLb", "ab16"], ["AL"])
                            tt("dve", AO[:], ONb[:].unsqueeze(1).to_broadcast([128, 4, 128]), a4, ALU.mult, ["ONb", "ab16"], ["AO"])
                            bk, br = bank()
                            mms([(bk[:, q * 128:(q + 1) * 128], AL[:, q, :], UTb[:], True, True) for q in range(4)], ["AL", "UTb"], [br])
                            act(LT[:], bk[:].rearrange("p (q n) -> p q n", n=128), AF.Exp, [br], ["LT"])
                            bk2, br2 = bank()
                            mms([(bk2[:, q * 128:(q + 1) * 128], AO[:, q, :], UTb[:], True, True) for q in range(4)], ["AO", "UTb"], [br2])
                            act(Eb[:], bk2[:].rearrange("p (q n) -> p q n", n=128), AF.Exp, [br2], ["Eb"])
                            tt("dve", MT[:], LT[:], CBm[:, g, :].unsqueeze(1).to_broadcast([128, 4, 128]), ALU.mult, ["LT", "CBm"], ["MT"])
                            tt("dve", CE[:], Eb[:], CT[:, g, tsl].unsqueeze(1).to_broadcast([128, 4, 128]), ALU.mult, ["Eb", "CT"], ["CE"])
                            if g % 2 == 0:
                                yb, ybr = ybank(g // 2)
                                ybanks.append((yb, ybr))
                            lst = []
                            for q in range(4):
                                h = 4 * g + q
                                o = yb[:, (g % 2) * 256 + q * 64:(g % 2) * 256 + (q + 1) * 64]
                                lst.append((o, MT[:, q, :], xdt[:, h * 64:(h + 1) * 64], True, False))
                                lst.append((o, CE[:, q, :], state_bf[:, h * 64:(h + 1) * 64], False, True))
                            mms(lst, ["MT", "CE", "xdt", "state_bf"], [ybr])
                        checkpoint(f"grp{int(pre)}", {"MT": MT, "CE": CE})
                        for b4 in range(4):
                            yb, ybr = ybanks[b4]
                            csl = slice(b4 * 512, (b4 + 1) * 512)
                            tt("dve", tmpD[:, 0:512].rearrange("p (h d) -> p h d", h=8), xs_tok[:, tti, csl].rearrange("p (h d) -> p h d", h=8),
                               rowS[:, 64 + b4 * 8:64 + b4 * 8 + 8].unsqueeze(2).to_broadcast([128, 8, 64]), ALU.mult,
                               ["xs_tok", "rowS"], ["tmpD"])
                            tt("dve", yn[:, csl], yb[:], tmpD[:, 0:512], ALU.add, [ybr, "tmpD"], ["yn"])
                            tt("dve", yn[:, csl], yn[:, csl], sz_tok[:, tti, csl], ALU.mult, ["yn", "sz_tok"], ["yn"])
                        for g in range(8):
                            act(junk[:], yn[:, g * 256:(g + 1) * 256], AF.Square, ["yn"], ["junk", "ss8"], accum_out=ss8[:, g:g + 1])
                        act(ss8[:], ss8[:], AF.Sqrt, ["ss8"], ["ss8"], bias=EPS, scale=1.0 / 256)
                        P.op("dve", lambda e: e.reciprocal(out=ss8[:], in_=ss8[:]), rl(["ss8"]), rl(["ss8"]))
                        tt("dve", yn[:, 0:2048].rearrange("p (g d) -> p g d", g=8), yn[:, 0:2048].rearrange("p (g d) -> p g d", g=8),
                           ss8[:].unsqueeze(2).to_broadcast([128, 8, 256]), ALU.mult, ["yn", "ss8"], ["yn"])
                        checkpoint(f"yn{int(pre)}", {"yn": yn[:, 0:2048]})
                        for q4 in range(4):
                            bk, br = bank()
                            trs([(bk[:, q * 128:(q + 1) * 128], yn[:, (q4 * 4 + q) * 128:(q4 * 4 + q + 1) * 128], identF[:]) for q in range(4)],
                                ["yn", "identF"], [br])
                            cp("act", ynT[:, q4 * 4:(q4 + 1) * 4, tsl], bk[:].rearrange("p (q n) -> p q n", n=128), [br], ["ynT"])
                    checkpoint(f"ynT{int(pre)}", {"ynT": ynT[:, :, 0:128]})
                    sbanks = []
                    for b4 in range(4):
                        bk, br = bank()
                        mms([(bk[:, q * 256:(q + 1) * 256], B_tok[:, tti, (2 * b4 + q) * 128:(2 * b4 + q + 1) * 128],
                              xdtd[:, (2 * b4 + q) * 256:(2 * b4 + q + 1) * 256], True, True) for q in range(2)], ["B_tok", "xdtd"], [br])
                        sbanks.append((bk, br))
                    checkpoint(f"ssdC{int(pre)}", {"xdt": xdt})
                    tt(PB, state[:].rearrange("p (h d) -> p h d", h=32), state[:].rearrange("p (h d) -> p h d", h=32),
                       sm[:, 1, :].unsqueeze(2).to_broadcast([128, 32, 64]), ALU.mult, ["state", "sm", "state_bf"], ["state"])
                    for b4 in range(4):
                        bk, br = sbanks[b4]
                        tt("dve", state[:, b4 * 512:(b4 + 1) * 512], bk[:], state[:, b4 * 512:(b4 + 1) * 512], ALU.add, ["state", br], ["state"])
                    if not pre:
                        cp("act", state_bf[:], state[:], ["state"], ["state_bf"])
                if pre:
                    return
                for eh in range(2):
                    pb = [ybank(q_) for q_ in range(4)]
                    for kh in range(2):
                        wsb, wr_ = wblock(wbs_b[kh * 1024:(kh + 1) * 1024, eh * 512:(eh + 1) * 512], ["wbs_b"])
                        for j in range(4):
                            mms([(pb[j][0][:, 0:TS], wsb[:, kc, j * 128:(j + 1) * 128], ynT[:, kh * 8 + kc, :], (kh == 0 and kc == 0),
                                  (kh == 1 and kc == 7)) for kc in range(8)], [wr_, "ynT"], [pb[j][1]])
                    wsg, wg_ = wblock(w_in_b[:, OFF_GS + eh * 512: OFF_GS + (eh + 1) * 512], ["w_in_b"])
                    for j in range(4):
                        ec = eh * 4 + j
                        bk, br = bank()
                        mms([(bk[:, 0:TS], wsg[:, dc, j * 128:(j + 1) * 128], uT[:, dc, :], dc == 0, dc == 7) for dc in range(8)],
                            [wg_, "uT"], [br])
                        act(gsig[:], bk[:, 0:TS], AF.Sigmoid, [br], ["gsig"])
                        tt("dve", gsig[:], pb[j][0][:, 0:TS], gsig[:], ALU.mult, [pb[j][1], "gsig"], ["gsig"])
                        tt("dve", combT[:, ec, :], gsig[:], gp[:, ec, :], ALU.add, ["gsig", "gp"], ["combT"])
                checkpoint(f"comb{int(pre)}", {"combT": combT})
                wo = [wblock(wout_b[:, hh * 512:(hh + 1) * 512], ["wout_b"]) for hh in range(2)]
                for tti in range(2):
                    for hh in range(2):
                        bk, br = bank()
                        mms([(bk[:], combT[:, ec, tti * 128:(tti + 1) * 128], wo[hh][0][:, ec, :], ec == 0, ec == 7) for ec in range(8)],
                            ["combT", wo[hh][1]], [br])
                        tt("dve", H[:, t0 + tti, hh * 512:(hh + 1) * 512], bk[:], H[:, t0 + tti, hh * 512:(hh + 1) * 512], ALU.add,
                           ["H", br], ["H"])

            xst = lambda tti: sz_tok[:, tti, :].bitcast(F32)
            Iacc = ynT[:].rearrange("p a b -> p (a b)").bitcast(F32)
            P.op("pool", lambda e: e.memset(state[:], 0.0), [], rl(["state"]))
            P.op("pool", lambda e: e.memset(Iacc, 0.0), [], rl(["ynT"]))
            P.op("pool", lambda e: e.memset(tailx[:], 0.0), [], rl([f"tailx{c_}" for c_ in range(32)]))
            for j in range(n_pre_slots):
                for si in range(n_slabs):
                    r0 = j * T + si * TS
                    for tti in range(2):
                        dma("sp", xst(tti), x_all[r0 + tti * 128: r0 + (tti + 1) * 128, :], [], ["sz_tok"])
                    slab(si, True, src=xst)
                stt("dve", Iacc, state[:], rk[:, j:j + 1], Iacc, ALU.mult, ALU.add, ["state", "rk", "ynT"], ["ynT"])
            checkpoint("pre", {"state": state, "xs_tok": xs_tok, "B_tok": B_tok, "dtb": dtb})
            cp("dve", state[:], Iacc, ["ynT"], ["state"])
            cp("act", state_bf[:], state[:], ["state"], ["state_bf"])
            cp("pool", tailx[:], tailx0[:], ["tailx0"], [f"tailx{c_}" for c_ in range(32)])
            checkpoint("xchg", {"state": state})
            for si in range(n_slabs):
                slab(si, False)
            checkpoint("main", {"H": H, "ynT": ynT, "gp": gp, "combT": combT, "sz_tok": sz_tok, "CT": CT, "BT": BT})
            barrier()
            P.flush()

        if DEBUG:
            dbg_out = nc.dram_tensor("dbg_h", [T, D], F32, kind="ExternalOutput").ap()
            dma("sp", dbg_out.rearrange("(t p) d -> p t d", p=128), H[:], ["H"], [])

        with ExitStack() as sB:
            U2 = sb(sB, "U2", [128, 8, T], BF16)
            u2f = sb(sB, "u2f", [128, 8, 128], F32)
            xn2 = sb(sB, "xn2", [128, D], F32)
            st2 = sb(sB, "st2", [128, 4], F32)
            wr = sb(sB, "wr", [128, 8, 64], F32)
            wtab = sb(sB, "wtab", [128, NT, 65], F32)
            r64 = [sb(sB, f"r64_{i}", [128, 64], F32) for i in range(4)]
            r8 = [sb(sB, f"r8_{i}", [128, 8], F32) for i in range(5)]
            wguS = [sb(sB, f"wguS{i}", [128, 8, 512], BF16) for i in range(2)]
            wdF = [sb(sB, f"wdF{i}", [128, 2, D], F32) for i in range(2)]
            wdS = [sb(sB, f"wdS{i}", [128, 2, D], BF16) for i in range(2)]
            hidT = [sb(sB, f"hidT{i}", [128, 2, T], BF16) for i in range(2)]
            sg = [sb(sB, f"sg{i}", [128, 512], BF16) for i in range(2)]
            fng = sb(sB, "fng", [128, D], F32)
            g2bc = sb(sB, "g2bc", [128, D], F32)
            dma("sp", g2bc[:], g2_d, ["g2_d"], ["g2bc"])
            ob = [sb(sB, f"ob{i}", [128, D], F32) for i in range(2)]

            dma("sp", wr[:], w_router.rearrange("(c p) n -> p c n", p=128), [], ["wr"])
            dma("sp", fng[:], rows_d[:, R_FNG:R_FNG + D], [], ["fng"])
            P.op("pool", lambda e: e.memset(wtab[:], 1.0), [], rl(["wtab"]))
            for ti in range(NT):
                xin = H[:, ti, :]
                act(xn2[:], xin, AF.Square, ["H"], ["xn2", "st2"], accum_out=st2[:, 0:1])
                act(st2[:, 1:2], st2[:, 0:1], AF.Sqrt, ["st2"], ["st2"], bias=EPS, scale=1.0 / D)
                P.op("dve", lambda e: e.reciprocal(out=st2[:, 2:3], in_=st2[:, 1:2]), rl(["st2"]), rl(["st2"]))
                ts("dve", xn2[:], xin, st2[:, 2:3], None, ALU.mult, None, ["H", "st2"], ["xn2"])
                for half in range(2):
                    bk, br = bank()
                    trs([(bk[:, j * 128:(j + 1) * 128], xn2[:, (half * 4 + j) * 128:(half * 4 + j + 1) * 128], identF[:]) for j in range(4)],
                        ["xn2", "identF"], [br])
                    for j in range(4):
                        dc = half * 4 + j
                        P.op("act", lambda e, dc=dc, j=j, bk=bk: e.activation(out=u2f[:, dc, :], in_=bk[:, j * 128:(j + 1) * 128], func=AF.Identity,
                                                                         bias=mcol[:, 16 + dc:17 + dc], scale=gsc[:, 8 + dc:9 + dc]),
                             rl([br, "gsc", "mcol"]), rl(["u2f"]))
                cp("pool", U2[:, :, ti * 128:(ti + 1) * 128], u2f[:], ["u2f"], ["U2"])
                bk, br = bank()
                mms([(bk[:, 0:64], u2f[:, dc, :], wr[:, dc, :], dc == 0, dc == 7) for dc in range(8)], ["u2f", "wr"], [br])
                s_, ch, t1, t2 = r64
                m1, m2, gs, g8, gm = r8
                act(s_[:], bk[:, 0:64], AF.Sigmoid, [br], ["r0"])
                tt("dve", ch[:], s_[:], rowS[:, 96:160], ALU.add, ["r0", "rowS"], ["r1"])
                v3 = lambda a: a[:].rearrange("p (g e) -> p g e", g=8)
                P.op("dve", lambda e: e.tensor_reduce(out=m1[:], in_=v3(ch), axis=AX.X, op=ALU.max), rl(["r1"]), rl(["q0"]))
                tt("dve", v3(t1), v3(ch), m1[:].unsqueeze(2).to_broadcast([128, 8, 8]), ALU.is_equal, ["r1", "q0"], ["r2"])
                stt("dve", t1[:], t1[:], -BIG, ch[:], ALU.mult, ALU.add, ["r2", "r1"], ["r2"])
                P.op("dve", lambda e: e.tensor_reduce(out=m2[:], in_=v3(t1), axis=AX.X, op=ALU.max), rl(["r2"]), rl(["q1"]))
                tt("dve", gs[:], m1[:], m2[:], ALU.add, ["q0", "q1"], ["q2"])
                P.op("dve", lambda e: e.max(out=g8[:], in_=gs[:]), rl(["q2"]), rl(["q3"]))
                ts("dve", gm[:], gs[:], g8[:, 3:4], None, ALU.is_ge, None, ["q2", "q3"], ["q4"])
                tt("dve", v3(t1), v3(ch), gm[:].unsqueeze(2).to_broadcast([128, 8, 8]), ALU.mult, ["r1", "q4"], ["r2"])
                ts("dve", gm[:], gm[:], -1.0, BIG, ALU.add, ALU.mult, ["q4"], ["q4"])
                tt("dve", v3(t1), v3(t1), gm[:].unsqueeze(2).to_broadcast([128, 8, 8]), ALU.add, ["r2", "q4"], ["r2"])
                P.op("dve", lambda e: e.max(out=g8[:], in_=t1[:]), rl(["r2"]), rl(["q3"]))
                ts("dve", t2[:], t1[:], g8[:, 7:8], None, ALU.is_ge, None, ["r2", "q3"], ["r3"])
                tt("dve", t2[:], t2[:], s_[:], ALU.mult, ["r3", "r0"], ["r3"])
                P.op("dve", lambda e: e.tensor_reduce(out=m1[:, 0:1], in_=t2[:], axis=AX.X, op=ALU.add), rl(["r3"]), rl(["q0"]))
                P.op("dve", lambda e: e.reciprocal(out=m1[:, 0:1], in_=m1[:, 0:1]), rl(["q0"]), rl(["q0"]))
                ts("dve", wtab[:, ti, 0:64], t2[:], m1[:, 0:1], 2.5, ALU.mult, ALU.mult, ["r3", "q0"], ["wtab"])

            ne = n_experts if do_moe else 0
            for e_ in range(ne):
                b = e_ % 2
                P.dma("pool", lambda e, e_=e_, b=b: e.dma_start(out=wguS[b][:], in_=wgu[e_], max_dma_last_dim=4096), rl([]), rl([f"wguS{b}"]))
                dma("sp", wdF[b][:], wdn[e_], [], [f"wdF{b}"])
                tt("pool", wdS[b][:], wdF[b][:], g2bc[:].unsqueeze(1).to_broadcast([128, 2, D]), ALU.mult, [f"wdF{b}", "g2bc"], [f"wdS{b}"])
                k = 0
                for jc in range(2):
                    for sl in range(4):
                        bg, bgr = bank()
                        bu, bur = bank()
                        rhs = lambda dc: U2[:, dc, sl * 512:(sl + 1) * 512]
                        mms([(bg[:], wguS[b][:, dc, jc * 128:(jc + 1) * 128], rhs(dc), dc == 0, dc == 7) for dc in range(8)],
                            [f"wguS{b}", "U2"], [bgr])
                        mms([(bu[:], wguS[b][:, dc, 256 + jc * 128:256 + (jc + 1) * 128], rhs(dc), dc == 0, dc == 7) for dc in range(8)],
                            [f"wguS{b}", "U2"], [bur])
                        act(sg[k % 2][:], bg[:], AF.Silu, [bgr], [f"sg{k % 2}"])
                        tt("dve", hidT[b][:, jc, sl * 512:(sl + 1) * 512], bu[:], sg[k % 2][:], ALU.mult, [bur, f"sg{k % 2}"], [f"hidT{b}"])
                        k += 1
                for ti in range(NT):
                    for hh in range(2):
                        bk, br = ybank()
                        mms([(bk[:], hidT[b][:, jc, ti * 128:(ti + 1) * 128], wdS[b][:, jc, hh * 512:(hh + 1) * 512], jc == 0, jc == 1)
                             for jc in range(2)], [f"hidT{b}", f"wdS{b}"], [br])
                        stt("dve", H[:, ti, hh * 512:(hh + 1) * 512], bk[:], wtab[:, ti, e_:e_ + 1], H[:, ti, hh * 512:(hh + 1) * 512],
                            ALU.mult, ALU.add, [br, "wtab", f"Ht{ti}"], [f"Ht{ti}"])
            for ti in range(NT):
                b = ti % 2
                act(xn2[:], H[:, ti, :], AF.Square, ["H", f"Ht{ti}"], ["xn2", "st2"], accum_out=st2[:, 0:1])
                act(st2[:, 1:2], st2[:, 0:1], AF.Sqrt, ["st2"], ["st2"], bias=EPS, scale=1.0 / D)
                P.op("dve", lambda e: e.reciprocal(out=st2[:, 2:3], in_=st2[:, 1:2]), rl(["st2"]), rl(["st2"]))
                stt("dve", ob[b][:], H[:, ti, :], st2[:, 2:3], fng[:], ALU.mult, ALU.mult, ["H", f"Ht{ti}", "st2", "fng"], [f"ob{b}"])
                dma("sp", out_d[ti * 128:(ti + 1) * 128, :], ob[b][:], [f"ob{b}"], [])
            P.wait_all("sp", P.all_events())
            P.flush()
    except _Stop:
        return nc
    top.close()
    _top0.close()
    return nc


def _colform(v):
    v = np.asarray(v, np.float32).reshape(-1)
    return np.ascontiguousarray(v.reshape(-1, 128).T)


def _prep_inputs(x, c, w_ada, b_ada, norm_mix_g, w_in, conv_w, conv_b, dt_bias, A_log, D_skip, ssd_norm_g,
                 pool_w, pool_scale, w_br_ssd, w_br_pool, w_out, norm_ffn_g, w_router, router_bias,
                 we_gate, we_up, we_down, ws_gate, ws_up, ws_down, final_norm_g):
    f = lambda a: np.ascontiguousarray(np.asarray(a, np.float32))
    x2 = f(x).reshape(NCORES * T, D)
    cols = np.zeros((128, NCOL), np.float32)
    cols[:, C_NMG:C_NMG + 8] = _colform(norm_mix_g)
    cols[:, C_NFG:C_NFG + 8] = _colform(norm_ffn_g)
    cols[:, C_PSC:C_PSC + 8] = _colform(pool_scale)
    cols[:, C_SNG:C_SNG + 16] = _colform(ssd_norm_g)
    cols[:, C_C:C_C + 8] = _colform(c)
    cols[:, C_CB:C_CB + 32] = _colform(conv_b)
    cw = f(conv_w).reshape(4, 32, 128)
    cols[:, C_CW:C_CW + 128] = cw.transpose(2, 1, 0).reshape(128, 128)
    rows = np.zeros((1, NROW), np.float32)
    rows[0, R_BADA:R_BADA + 6144] = f(b_ada).reshape(-1)
    rows[0, R_DTB:R_DTB + 32] = f(dt_bias).reshape(-1)
    rows[0, R_ALOG:R_ALOG + 32] = f(A_log).reshape(-1)
    rows[0, R_DSK:R_DSK + 32] = f(D_skip).reshape(-1)
    rows[0, R_RB:R_RB + 64] = f(router_bias).reshape(-1)
    rows[0, R_FNG:R_FNG + 1024] = f(final_norm_g).reshape(-1)
    rows = np.ascontiguousarray(np.broadcast_to(rows, (128, NROW)))
    wg = f(we_gate)[0].reshape(64, 8, 128, 256)
    wu = f(we_up)[0].reshape(64, 8, 128, 256)
    wgu = np.empty((65, 128, 8, 512), np.float32)
    wgu[:64, :, :, :256] = wg.transpose(0, 2, 1, 3)
    wgu[:64, :, :, 256:] = wu.transpose(0, 2, 1, 3)
    wgu[64, :, :, :256] = f(ws_gate)[0].reshape(8, 128, 256).transpose(1, 0, 2)
    wgu[64, :, :, 256:] = f(ws_up)[0].reshape(8, 128, 256).transpose(1, 0, 2)
    wdn = np.empty((65, 128, 2, 1024), np.float32)
    wdn[:64] = f(we_down)[0].reshape(64, 2, 128, 1024).transpose(0, 2, 1, 3)
    wdn[64] = f(ws_down)[0].reshape(2, 128, 1024).transpose(1, 0, 2)
    shared = {
        "cols": cols, "rows": rows, "w_ada": f(w_ada)[0], "w_in": f(w_in)[0],
        "pool_w": f(pool_w)[0].reshape(1024, 256), "w_br_ssd": f(w_br_ssd)[0], "w_br_pool": f(w_br_pool)[0],
        "w_out": f(w_out)[0], "w_router": f(w_router)[0], "wgu": wgu, "wdn": wdn,
    }
    in_maps = []
    for k in range(NCORES):
        m = dict(shared)
        m["x_own"] = x2[k * T:(k + 1) * T]
        m["x_halo"] = x2[k * T - 128:k * T] if k > 0 else np.zeros((128, D), np.float32)
        m["rk"] = np.ascontiguousarray(np.broadcast_to((np.arange(8) == k - 1).astype(np.float32)[None, :], (128, 8)))
        m["x_all"] = x2[:(NCORES - 1) * T]
        m["nf"] = np.full((128, 1), 1.0 if k > 0 else 0.0, np.float32)
        m["posrow"] = np.ascontiguousarray(np.broadcast_to((k * T + 1 + np.arange(16)).astype(np.float32)[None, :], (128, 16)))
        in_maps.append(m)
    return in_maps


_NC_CACHE = {}


def kernel(**inputs):
    in_maps = _prep_inputs(**inputs)
    if "nc" not in _NC_CACHE:
        _NC_CACHE["nc"] = build_nc()
    res = run_bass_kernel_spmd(_NC_CACHE["nc"], in_maps, core_ids=list(range(NCORES)))
    out = np.concatenate([np.asarray(r["out"], np.float32) for r in res.results], axis=0)
    return out.reshape(1, NCORES * T, D)
```

```python
import numpy as np
import concourse.bass as bass
import concourse.mybir as mybir

F32 = mybir.dt.float32
BF16 = mybir.dt.bfloat16
AF = mybir.ActivationFunctionType
ALU = mybir.AluOpType
AX = mybir.AxisListType


class Reg:
    __slots__ = ("name", "w", "r")

    ALL = []

    def __init__(self, name):
        self.name = name
        self.w = None
        self.r = []
        Reg.ALL.append(self)


class Prog:
    ENGS = ("pe", "act", "dve", "pool", "sp")

    def __init__(self, nc, ndma_sems=8, same_engine_sync=True):
        self.nc = nc
        self.q = {e: [] for e in self.ENGS}
        self.cnt = {e: 0 for e in self.ENGS}
        self.known = {e: {} for e in self.ENGS}
        self.sems = {}
        self.same = same_engine_sync
        self.ndma = ndma_sems
        self.dma_rr = {e: 0 for e in self.ENGS}
        self.dma_val = {}
        self.stack = None

    def _semkeys(self):
        keys = list(self.ENGS) + ["cc"]
        for e in ("sp", "pool", "act"):
            for i in range(self.ndma):
                keys.append(f"d_{e}_{i}")
        return keys

    def _need(self, eng, events):
        mx = {}
        for ev in events:
            if ev is None:
                continue
            k, v = ev
            if k == eng and not self.same:
                continue
            if k == "cc" and eng != "pool":
                continue
            if v > mx.get(k, 0):
                mx[k] = v
        out = []
        kn = self.known[eng]
        for k, v in mx.items():
            if kn.get(k, 0) >= v:
                continue
            kn[k] = v
            out.append((k, v))
        return out

    def _deps(self, reads, writes):
        evs = []
        for r in reads:
            evs.append(r.w)
        for w in writes:
            evs.append(w.w)
            evs.extend(w.r)
        return evs

    def _commit(self, ev, reads, writes):
        for r in reads:
            r.r.append(ev)
        for w in writes:
            w.w = ev
            w.r = []

    def op(self, eng, fn, reads=(), writes=(), extra=()):
        evs = self._deps(reads, writes) + list(extra)
        waits = self._need(eng, evs)
        self.cnt[eng] += 1
        ev = (eng, self.cnt[eng])
        self.q[eng].append((waits, fn, (eng, 1)))
        self._commit(ev, reads, writes)
        return ev

    def dma(self, eng, fn, reads=(), writes=(), extra=()):
        i = self.dma_rr[eng]
        self.dma_rr[eng] = (i + 1) % self.ndma
        key = f"d_{eng}_{i}"
        prev = self.dma_val.get(key, 0)
        evs = self._deps(reads, writes) + list(extra)
        if prev:
            evs.append((key, prev))
        waits = self._need(eng, evs)
        val = prev + 16
        self.dma_val[key] = val
        ev = (key, val)
        self.q[eng].append((waits, fn, (key, 16)))
        self._commit(ev, reads, writes)
        return ev

    def cc(self, fn, reads=(), writes=(), extra=(), inc=1):
        eng = "pool"
        evs = self._deps(reads, writes) + list(extra)
        waits = self._need(eng, evs)
        val = self.dma_val.get("cc", 0) + inc
        self.dma_val["cc"] = val
        ev = ("cc", val)
        self.q[eng].append((waits, fn, ("cc", inc)))
        self._commit(ev, reads, writes)
        return ev

    def wait_all(self, eng, events):
        waits = self._need(eng, list(events))
        self.q[eng].append((waits, None, None))

    def all_events(self):
        evs = [(e, c) for e, c in self.cnt.items() if c]
        evs += [(k, v) for k, v in self.dma_val.items()]
        return evs

    def begin(self, stack):
        self.gen = 0
        self.semh = {k: stack.enter_context(self.nc.semaphore(k)) for k in self._semkeys()}

    def rebase(self, stack):
        self.gen += 1
        self.semh = {k: stack.enter_context(self.nc.semaphore(f"{k}_g{self.gen}")) for k in self._semkeys()}
        self.cnt = {e: 0 for e in self.ENGS}
        self.known = {e: {} for e in self.ENGS}
        self.dma_rr = {e: 0 for e in self.ENGS}
        self.dma_val = {}
        for r in Reg.ALL:
            r.w = None
            r.r = []

    def flush(self):
        nc = self.nc
        sems = self.semh
        q = self.q

        def run(engh, lst):
            for waits, fn, inc in lst:
                for k, v in waits:
                    engh.wait_ge(sems[k], v)
                if fn is None:
                    continue
                ins = fn(engh)
                ins.then_inc(sems[inc[0]], inc[1])

        with nc.Block() as block:
            @block.tensor
            def _(e):
                run(e, q["pe"])

            @block.scalar
            def _(e):
                run(e, q["act"])

            @block.vector
            def _(e):
                run(e, q["dve"])

            @block.gpsimd
            def _(e):
                run(e, q["pool"])

            @block.sync
            def _(e):
                run(e, q["sp"])
        self.q = {e: [] for e in self.ENGS}


from contextlib import ExitStack
from concourse.bass_utils import run_bass_kernel_spmd

NCORES = 8
T = 2048
NT = 16
TS = 256
NSL = T // TS
D = 1024
EPS = 1e-6
OFF_Z, OFF_X, OFF_B, OFF_C, OFF_DT, OFF_P, OFF_GS, OFF_GP = 0, 2048, 4096, 5120, 6144, 6176, 7200, 8224
DIN = 9248
PB = "dve"
NCOL = 208
C_NMG, C_NFG, C_PSC, C_SNG, C_C, C_CB, C_CW = 0, 8, 16, 24, 40, 48, 80
R_BADA, R_DTB, R_ALOG, R_DSK, R_RB, R_FNG = 0, 6144, 6176, 6208, 6240, 6304
NROW = 7328
BIG = 1.0e4
DEBUG = False
import os
DBGV = int(os.environ.get("DBGV", "0"))


class _Stop(Exception):
    pass


def build_nc(n_experts=65, do_moe=True, stop_after=None, n_slabs=NSL, n_pre_slots=NCORES - 1):
    nc = bass.Bass("TRN2", target_bir_lowering=False)
    di = lambda n, s, dt=F32: nc.dram_tensor(n, s, dt, kind="ExternalInput").ap()
    x_own = di("x_own", [T, D]); x_halo = di("x_halo", [128, D]); x_all = di("x_all", [(NCORES - 1) * T, D])
    rk_d = di("rk", [128, 8]); nf_d = di("nf", [128, 1]); pos_d = di("posrow", [128, 16])
    cols_d = di("cols", [128, NCOL]); rows_d = di("rows", [128, NROW])
    w_ada = di("w_ada", [D, 6 * D]); w_in = di("w_in", [D, DIN])
    pool_w = di("pool_w", [1024, 256]); w_br_ssd = di("w_br_ssd", [2048, D])
    w_br_pool = di("w_br_pool", [D, D]); w_out = di("w_out", [D, D])
    w_router = di("w_router", [D, 64])
    NEW = 65 if stop_after is None else 1
    wgu = di("wgu", [NEW, 128, 8, 512]); wdn = di("wdn", [NEW, 128, 2, 1024])
    out_d = nc.dram_tensor("out", [T, D], F32, kind="ExternalOutput").ap()
    w_in_b = nc.dram_tensor("w_in_b", [D, DIN], BF16).ap()
    wbp_b = nc.dram_tensor("wbp_b", [D, D], BF16).ap()
    wbs_b = nc.dram_tensor("wbs_b", [2048, D], BF16).ap()
    wout_b = nc.dram_tensor("wout_b", [D, D], BF16).ap()
    bounce = nc.dram_tensor("bounce", [128, 2080], F32).ap()
    gathered = nc.dram_tensor("gathered", [1024, 2080], F32).ap()
    dbg = {}

    Reg.ALL.clear()
    P = Prog(nc)
    regs = {}
    _top0 = ExitStack()
    P.begin(_top0)

    ALIAS = {"yn": ["bigA", "bigB"], "big": ["bigA", "bigB"], "junk": ["gsig"], "xn": ["bigB"]}

    def R(name):
        if name not in regs:
            regs[name] = Reg(name)
        return regs[name]

    top = ExitStack()

    def sb(st, name, shape, dt):
        return st.enter_context(nc.sbuf_tensor("s_" + name, shape, dt))

    def rl(x):
        out = []
        for n in x:
            if isinstance(n, str):
                for m in ALIAS.get(n, [n]):
                    out.append(R(m))
            else:
                out.append(n)
        return out

    def tt(eng, out, a, b, op, rd, wr):
        return P.op(eng, lambda e: e.tensor_tensor(out=out, in0=a, in1=b, op=op), rl(rd), rl(wr))

    def ts(eng, out, a, s1, s2, op0, op1, rd, wr):
        if op1 is None:
            return P.op(eng, lambda e: e.tensor_single_scalar(out=out, in_=a, scalar=s1, op=op0), rl(rd), rl(wr))
        return P.op(eng, lambda e: e.tensor_scalar(out=out, in0=a, scalar1=s1, scalar2=s2, op0=op0, op1=op1), rl(rd), rl(wr))

    def stt(eng, out, a, sc, b, op0, op1, rd, wr):
        return P.op(eng, lambda e: e.scalar_tensor_tensor(out=out, in0=a, scalar=sc, in1=b, op0=op0, op1=op1), rl(rd), rl(wr))

    def act(out, in_, func, rd, wr, bias=0.0, scale=1.0, accum_out=None):
        if accum_out is None:
            return P.op("act", lambda e: e.activation(out=out, in_=in_, func=func, bias=bias, scale=scale), rl(rd), rl(wr))
        return P.op("act", lambda e: e.activation(out=out, in_=in_, func=func, bias=bias, scale=scale, accum_out=accum_out), rl(rd), rl(wr))

    def cp(eng, out, in_, rd, wr):
        if eng == "act":
            return P.op("act", lambda e: e.copy(out=out, in_=in_), rl(rd), rl(wr))
        return P.op(eng, lambda e: e.tensor_copy(out=out, in_=in_), rl(rd), rl(wr))

    def mms(lst, rd, wr):
        def f(e):
            for (o, l, r, s0, s1) in lst:
                ins = e.matmul(o, lhsT=l, rhs=r, start=s0, stop=s1)
            return ins
        return P.op("pe", f, rl(rd), rl(wr))

    def trs(lst, rd, wr):
        def f(e):
            for (o, i, idn) in lst:
                ins = e.transpose(out=o, in_=i, identity=idn)
            return ins
        return P.op("pe", f, rl(rd), rl(wr))

    def dma(q, out, in_, rd, wr):
        if q == "pool":
            return P.dma(q, lambda e: e.dma_start(out=out, in_=in_, max_dma_last_dim=4096), rl(rd), rl(wr))
        return P.dma(q, lambda e: e.dma_start(out=out, in_=in_), rl(rd), rl(wr))

    def barrier():
        evs = P.all_events()
        for e in ("pe", "act", "dve", "pool", "sp"):
            P.wait_all(e, evs)

    def checkpoint(name, tensors):
        if stop_after != name:
            return
        for tn, t in tensors.items():
            a = t if hasattr(t, "ap") and not hasattr(t, "__enter__") and type(t).__name__ == "AP" else t[:]
            shp = list(a.shape)
            d = nc.dram_tensor("dbg_" + tn, shp, a.dtype, kind="ExternalOutput").ap()
            dma("sp", d, a, [tn], [])
        P.wait_all("sp", P.all_events())
        P.flush()
        raise _Stop()

    banks = [top.enter_context(nc.psum_tensor(f"bank{i}", [128, 512], F32)) for i in range(8)]
    bstate = {"i": 0, "y": 0}

    def bank():
        n = bstate.get("n", 4)
        i = bstate["i"] % n
        bstate["i"] = (i + 1) % n
        return banks[i], R(f"bank{i}")

    def ybank(i=None):
        if i is None:
            i = bstate["y"]
            bstate["y"] = (i + 1) % 4
        return banks[4 + i], R(f"bank{4 + i}")

    H = sb(top, "H", [128, NT, D], F32)
    state = sb(top, "state", [128, 2048], F32)
    state_bf = sb(top, "state_bf", [128, 2048], BF16)
    identF = sb(top, "identF", [128, 128], F32)
    SLf = sb(top, "SLf", [128, 128], F32)
    ONf = sb(top, "ONf", [128, 128], F32)
    SLb = sb(top, "SLb", [128, 128], BF16)
    ONb = sb(top, "ONb", [128, 128], BF16)
    UTb = sb(top, "UTb", [128, 128], BF16)
    MKf = sb(top, "MKf", [128, 128], F32)
    cols = sb(top, "cols", [128, NCOL], F32)
    rowS = sb(top, "rowS", [128, 160], F32)
    rk = sb(top, "rk_s", [128, 8], F32)
    nf = sb(top, "nf_s", [128, 1], F32)
    mcol = sb(top, "mcol", [128, 32], F32)
    gsc = sb(top, "gsc", [128, 16], F32)
    g2_d = nc.dram_tensor("g2_d", [128, D], F32).ap()

    def mask(tn, tensor, cmp, sgn=1):
        P.op("pool", lambda e: e.memset(tensor[:], 1.0), [], [R(tn)])
        P.op("pool", lambda e: e.affine_select(out=tensor[:], in_=tensor[:], pattern=[[-sgn, 128]], compare_op=cmp,
                                               fill=0.0, base=0, channel_multiplier=sgn), [R(tn)], [R(tn)])
    mask("identF", identF, ALU.is_equal)
    mask("SLf", SLf, ALU.is_gt)
    mask("MKf", MKf, ALU.is_ge, -1)
    P.op("pool", lambda e: e.memset(ONf[:], 1.0), [], [R("ONf")])
    cp("pool", SLb[:], SLf[:], ["SLf"], ["SLb"])
    cp("pool", ONb[:], ONf[:], ["ONf"], ["ONb"])
    cp("pool", UTb[:], MKf[:], ["MKf"], ["UTb"])

    dma("sp", cols[:], cols_d, [], ["cols"])
    dma("sp", rowS[:], rows_d[:, R_DTB:R_DTB + 160], [], ["rowS"])
    dma("sp", rk[:], rk_d, [], ["rk"])
    dma("sp", nf[:], nf_d, [], ["nf"])
    dma("sp", H[:], x_own.rearrange("(t p) d -> p t d", p=128), [], ["H"])
    _wv_o = w_in_b.rearrange("a b -> (a b)").rearrange("(r c) -> r c", c=2048)
    _wv_i = w_in.rearrange("a b -> (a b)").rearrange("(r c) -> r c", c=2048)
    for _q in range(8):
        dma("pool", _wv_o[_q * 578:(_q + 1) * 578, :], _wv_i[_q * 578:(_q + 1) * 578, :], [], ["w_in_b"])
    dma("pool", wbp_b, w_br_pool, [], ["wbp_b"])

    try:
        with ExitStack() as s0:
            bcm = sb(s0, "bcm", [128, 6 * D], F32)
            cact = sb(s0, "cact", [128, 8], F32)
            CBC = sb(s0, "CBC", [128, 8, 128], F32)
            wa = [sb(s0, f"wa{i}", [128, 8, 512], F32) for i in range(2)]
            ba = [sb(s0, f"ba{i}", [128, 512], F32) for i in range(2)]
            stg = [sb(s0, f"stg{i}", [128, 4, D], F32) for i in range(2)]
            stb = [sb(s0, f"stb{i}", [128, 4, D], BF16) for i in range(2)]
            act(cact[:], cols[:, C_C:C_C + 8], AF.Silu, ["cols"], ["cact"])
            cp("dve", CBC[:], cact[:].unsqueeze(2).to_broadcast([128, 8, 128]), ["cact"], ["CBC"])
            for ns in range(12):
                b = ns % 2
                dma("sp", wa[b][:], w_ada[:, ns * 512:(ns + 1) * 512].rearrange("(c p) n -> p c n", p=128), [], [f"wa{b}"])
                dma("sp", ba[b][:], rows_d[:, R_BADA + ns * 512:R_BADA + (ns + 1) * 512], [], [f"ba{b}"])
                bk, br = bank()
                mms([(bk[:], CBC[:, dc, :], wa[b][:, dc, :], dc == 0, dc == 7) for dc in range(8)], ["CBC", f"wa{b}"], [br])
                tt("dve", bcm[:, ns * 512:(ns + 1) * 512], bk[:], ba[b][:], ALU.add, [br, f"ba{b}"], ["bcm"])
            for vi, v in enumerate((0, 1, 3, 4)):
                for q in range(2):
                    bk, br = bank()
                    trs([(bk[:, j * 128:(j + 1) * 128], bcm[:, v * D + (q * 4 + j) * 128: v * D + (q * 4 + j + 1) * 128], identF[:])
                         for j in range(4)], ["bcm", "identF"], [br])
                    cp("dve", mcol[:, vi * 8 + q * 4: vi * 8 + q * 4 + 4],
                       bk[:].rearrange("p (j n) -> p j n", n=128)[:, :, 0], [br], ["mcol"])
            stt("dve", gsc[:, 0:8], mcol[:, 8:16], 1.0, cols[:, C_NMG:C_NMG + 8], ALU.add, ALU.mult, ["mcol", "cols"], ["gsc"])
            stt("dve", gsc[:, 8:16], mcol[:, 24:32], 1.0, cols[:, C_NFG:C_NFG + 8], ALU.add, ALU.mult, ["mcol", "cols"], ["gsc"])
            dma("sp", g2_d, bcm[:, 5 * D:6 * D], ["bcm"], ["g2_d"])
            act(rowS[:, 32:64], rowS[:, 32:64], AF.Exp, ["rowS"], ["rowS"])
            ts("dve", rowS[:, 32:64], rowS[:, 32:64], -1.0, None, ALU.mult, None, ["rowS"], ["rowS"])
            k = 0
            for half in range(2):
                b = k % 2; k += 1
                dma("sp", stg[b][:], w_out[half * 512:(half + 1) * 512, :].rearrange("(c p) n -> p c n", p=128), [], [f"stg{b}"])
                tt("dve", stb[b][:], stg[b][:], bcm[:, 2 * D:3 * D].unsqueeze(1).to_broadcast([128, 4, D]), ALU.mult,
                   [f"stg{b}", "bcm"], [f"stb{b}"])
                dma("sp", wout_b[half * 512:(half + 1) * 512, :].rearrange("(c p) n -> p c n", p=128), stb[b][:], [f"stb{b}"], ["wout_b"])
            for q in range(4):
                b = k % 2; k += 1
                dma("sp", stg[b][:], w_br_ssd[q * 512:(q + 1) * 512, :].rearrange("(c p) n -> p c n", p=128), [], [f"stg{b}"])
                for j in range(4):
                    ts("pool", stb[b][:, j, :], stg[b][:, j, :], cols[:, C_SNG + q * 4 + j:C_SNG + q * 4 + j + 1], None, ALU.mult, None,
                       [f"stg{b}", "cols"], [f"stb{b}"])
                dma("sp", wbs_b[q * 512:(q + 1) * 512, :].rearrange("(c p) n -> p c n", p=128), stb[b][:], [f"stb{b}"], ["wbs_b"])
            checkpoint("p0", {"mcol": mcol, "gsc": gsc, "rowS": rowS, "bcm": bcm})
            barrier()
            P.flush()

        with ExitStack() as sA:
            WS = [sb(sA, f"WS{i}", [128, 8, 512], BF16) for i in range(3)]
            wsi = {"i": 0}
            wdt = sb(sA, "wdt", [128, 8, 32], BF16)
            poolw = sb(sA, "poolw", [128, 8, 256], BF16)
            pcorr = sb(sA, "pcorr", [128, 4, 16], F32)
            posr = sb(sA, "posr", [128, 16], F32)
            tailx = sb(sA, "tailx", [128, 32, 3], F32)
            tailx0 = sb(sA, "tailx0", [128, 32, 3], F32)
            ptail = sb(sA, "ptail", [128, 8, 15], F32)
            xpw = [sb(sA, f"xpw{i}", [128, 15 + TS], F32) for i in range(2)]
            logD = sb(sA, "logD", [128, 32], F32)
            logDp = [logD, sb(sA, "logD1", [128, 32], F32)]
            ldi = {"i": 0}
            uT = sb(sA, "uT", [128, 8, TS], BF16)
            xn_late = True
            st1 = sb(sA, "st1", [128, 4], F32)
            pooledT = sb(sA, "pooledT", [128, 8, TS], BF16)
            mixedT = sb(sA, "mixedT", [128, 8, TS], BF16)
            gp = sb(sA, "gp", [128, 8, TS], BF16)
            pS = [sb(sA, f"pS{i}", [128, 15 + TS], F32) for i in range(2)]
            gsig = sb(sA, "gsig", [128, TS], F32)
            NSET = 3
            xcs = [sb(sA, f"xc{i}", [128, 3 + TS], F32) for i in range(NSET)]
            caccs = [sb(sA, f"cacc{i}", [128, TS], F32) for i in range(NSET)]
            big = sb(sA, "big", [128, 2080], F32)
            xn = big[:, 1024:2048]
            tmpD = sb(sA, "tmpD", [128, 512], F32)
            Bfs = [sb(sA, f"Bf{i}", [128, TS], F32) for i in range(2)]
            xs_tok = sb(sA, "xs_tok", [128, 2, 2048], BF16)
            B_tok = sb(sA, "B_tok", [128, 2, 1024], BF16)
            BT = sb(sA, "BT", [128, 8, TS], BF16)
            CT = sb(sA, "CT", [128, 8, TS], BF16)
            dtb = sb(sA, "dtb", [128, 2, 32], F32)
            ab = sb(sA, "ab", [128, 2, 32], F32)
            ab16 = sb(sA, "ab16", [128, 2, 32], BF16)
            sm = sb(sA, "sm", [128, 4, 32], F32)
            sz_tok = sb(sA, "sz_tok", [128, 2, 2048], BF16)
            xdt = sb(sA, "xdt", [128, 2048], BF16)
            xdtd = sb(sA, "xdtd", [128, 2048], BF16)
            AL = sb(sA, "AL", [128, 4, 128], BF16)
            AO = sb(sA, "AO", [128, 4, 128], BF16)
            LT = sb(sA, "LT", [128, 4, 128], BF16)
            Eb = sb(sA, "Eb", [128, 4, 128], BF16)
            CBm = sb(sA, "CBm", [128, 8, 128], BF16)
            MT = sb(sA, "MT", [128, 4, 128], BF16)
            CE = sb(sA, "CE", [128, 4, 128], BF16)
            yn = big
            junk = gsig
            ss8 = sb(sA, "ss8", [128, 8], F32)
            ynT = sb(sA, "ynT", [128, 16, TS], BF16)
            combT = sb(sA, "combT", [128, 8, TS], BF16)
            stS = big

            def wblock(src, rd):
                i = wsi["i"]; wsi["i"] = (i + 1) % 3
                dma("sp", WS[i][:], src.rearrange("(c p) n -> p c n", p=128), rd, [f"WS{i}"])
                return WS[i], f"WS{i}"

            dma("pool", wdt[:], w_in[:, OFF_DT:OFF_DT + 32].rearrange("(c p) n -> p c n", p=128), [], ["wdt"])
            dma("pool", poolw[:], pool_w.rearrange("(c p) n -> p c n", p=128), [], ["poolw"])
            dma("sp", posr[:], pos_d, [], ["posr"])
            for g, w in enumerate((2, 4, 8, 16)):
                ts("dve", pcorr[:, g, :], posr[:], float(w), None, ALU.min, None, ["posr"], ["pcorr"])
                P.op("dve", lambda e, g=g: e.reciprocal(out=pcorr[:, g, :], in_=pcorr[:, g, :]), rl(["pcorr"]), rl(["pcorr"]))
                ts("dve", pcorr[:, g, :], pcorr[:, g, :], float(w), None, ALU.mult, None, ["pcorr"], ["pcorr"])

            def make_uT(src_ap, rdname, n_tiles, col0=0):
                for tti in range(n_tiles):
                    xin = src_ap(tti)
                    act(junk_big[:], xin, AF.Square, [rdname], ["xn", "st1"], accum_out=st1[:, 0:1])
                    act(st1[:, 1:2], st1[:, 0:1], AF.Sqrt, ["st1"], ["st1"], bias=EPS, scale=1.0 / D)
                    P.op("dve", lambda e: e.reciprocal(out=st1[:, 2:3], in_=st1[:, 1:2]), rl(["st1"]), rl(["st1"]))
                    ts("dve", xn[:], xin, st1[:, 2:3], None, ALU.mult, None, [rdname, "st1"], ["xn"])
                    for half in range(2):
                        bk, br = bank()
                        trs([(bk[:, j * 128:(j + 1) * 128], xn[:, (half * 4 + j) * 128:(half * 4 + j + 1) * 128], identF[:])
                             for j in range(4)], ["xn", "identF"], [br])
                        for j in range(4):
                            dc = half * 4 + j
                            ts("dve" if half else "pool_no", uT[:, dc, col0 + tti * 128: col0 + (tti + 1) * 128], bk[:, j * 128:(j + 1) * 128],
                               gsc[:, dc:dc + 1], mcol[:, dc:dc + 1], ALU.mult, ALU.add, [br, "gsc", "mcol"], ["uT"])

            junk_big = xn

            _ts_orig = ts

            def ts(eng, out, a, s1, s2, op0, op1, rd, wr):
                if eng == "pool_no":
                    return P.op("act", lambda e: e.activation(out=out, in_=a, func=AF.Identity, bias=s2, scale=s1), rl(rd), rl(wr))
                return _ts_orig(eng, out, a, s1, s2, op0, op1, rd, wr)

            xh = xsA = big
            dma("sp", big[:, 0:D], x_halo, [], ["bigA"])
            make_uT(lambda tti: big[:, 0:D], "bigA", 1)
            for blk in range(10):
                c0 = OFF_X + blk * 512 if blk < 8 else OFF_P + (blk - 8) * 512
                wsb, wr_ = wblock(w_in_b[:, c0:c0 + 512], ["w_in_b"])
                for j in range(4):
                    bk, br = bank()
                    mms([(bk[:, 0:128], wsb[:, dc, j * 128:(j + 1) * 128], uT[:, dc, 0:128], dc == 0, dc == 7) for dc in range(8)],
                        [wr_, "uT"], [br])
                    if blk < 8:
                        c = blk * 4 + j
                        ts("dve", tailx0[:, c, :], bk[:, 125:128], nf[:, 0:1], None, ALU.mult, None, [br, "nf"], ["tailx0"])
                    else:
                        c = (blk - 8) * 4 + j
                        ts("dve", ptail[:, c, :], bk[:, 113:128], nf[:, 0:1], None, ALU.mult, None, [br, "nf"], ["ptail"])

            checkpoint("halo", {"tailx0": tailx0, "xpb": ptail, "uT": uT[:, :, 0:128]})
            def slab(si, pre, src=None):
                t0 = 2 * si
                bstate["n"] = 8
                if src is None:
                    make_uT(lambda tti: H[:, t0 + tti, :], "H", 2)
                else:
                    make_uT(src, "sz_tok", 2)
                if not pre:
                    for blk in range(2):
                        wsb, wr_ = wblock(w_in_b[:, OFF_P + blk * 512: OFF_P + (blk + 1) * 512], ["w_in_b"])
                        for j in range(4):
                            c = blk * 4 + j
                            g = c // 2
                            w = 2 << g
                            bk, br = bank()
                            mms([(bk[:, 0:TS], wsb[:, dc, j * 128:(j + 1) * 128], uT[:, dc, :], dc == 0, dc == 7) for dc in range(8)],
                                [wr_, "uT"], [br])
                            xw = xpw[c % 2]; xwn = f"xpw{c % 2}"
                            cp("pool", xw[:, 0:15], ptail[:, c, :], ["ptail"], [xwn])
                            cp("act", xw[:, 15:15 + TS], bk[:, 0:TS], [br], [xwn])
                            cp("pool", ptail[:, c, :], xw[:, TS:TS + 15], [xwn], ["ptail"])
                            src = xw[:]
                            lo = 0
                            step = 1
                            k = 0
                            while step < w:
                                dst = pS[k % 2]
                                nlo = lo + step
                                tt("dve", dst[:, nlo:15 + TS], src[:, nlo:15 + TS], src[:, nlo - step:15 + TS - step], ALU.add,
                                   [xwn, f"pS{(k + 1) % 2}"], [f"pS{k % 2}"])
                                src = dst[:]
                                lo = nlo
                                step *= 2
                                k += 1
                            sname = f"pS{(k - 1) % 2}"
                            if si == 0:
                                tt("dve", src[:, 15:31], src[:, 15:31], pcorr[:, g, :], ALU.mult, [sname, "pcorr"], [sname])
                            stt("dve", pooledT[:, c, :], src[:, 15:15 + TS], 1.0 / w, xw[:, 15:15 + TS], ALU.mult, ALU.subtract,
                                [sname, xwn], ["pooledT"])
                            pass
                    for g in range(4):
                        for dch in range(2):
                            bk, br = bank()
                            mms([(bk[:, 0:TS], poolw[:, 2 * g + kc, dch * 128:(dch + 1) * 128], pooledT[:, 2 * g + kc, :], kc == 0, kc == 1)
                                 for kc in range(2)], ["poolw", "pooledT"], [br])
                            ts("dve", mixedT[:, 2 * g + dch, :], bk[:, 0:TS], cols[:, C_PSC + 2 * g + dch:C_PSC + 2 * g + dch + 1], None,
                               ALU.mult, None, [br, "cols"], ["mixedT"])
                    for blk in range(2):
                        wsb, wr_ = wblock(wbp_b[:, blk * 512:(blk + 1) * 512], ["wbp_b"])
                        wsg, wg_ = wblock(w_in_b[:, OFF_GP + blk * 512: OFF_GP + (blk + 1) * 512], ["w_in_b"])
                        for j in range(4):
                            ec = blk * 4 + j
                            bk, br = bank()
                            mms([(bk[:, 0:TS], wsg[:, dc, j * 128:(j + 1) * 128], uT[:, dc, :], dc == 0, dc == 7) for dc in range(8)],
                                [wg_, "uT"], [br])
                            act(gsig[:], bk[:, 0:TS], AF.Sigmoid, [br], ["gsig"])
                            bk2, br2 = bank()
                            mms([(bk2[:, 0:TS], wsb[:, dc, j * 128:(j + 1) * 128], mixedT[:, dc, :], dc == 0, dc == 7) for dc in range(8)],
                                [wr_, "mixedT"], [br2])
                            tt("dve", gp[:, ec, :], bk2[:, 0:TS], gsig[:], ALU.mult, [br2, "gsig"], ["gp"])
                checkpoint(f"ut{int(pre)}", {"uT": uT})
                checkpoint(f"pool{int(pre)}", {"gp": gp})
                nblk = 6 if pre else 8
                for blk in range(nblk):
                    wsb, wr_ = wblock(w_in_b[:, OFF_X + blk * 512: OFF_X + (blk + 1) * 512], ["w_in_b"])
                    for j in range(4):
                        c = blk * 4 + j
                        bk, br = bank()
                        mms([(bk[:, 0:TS], wsb[:, dc, j * 128:(j + 1) * 128], uT[:, dc, :], dc == 0, dc == 7) for dc in range(8)],
                            [wr_, "uT"], [br])
                        xc = xcs[c % NSET]; cacc = caccs[c % NSET]; xcn = f"xc{c % NSET}"; can = f"cacc{c % NSET}"
                        Bf = Bfs[c % 2]; Bfn = f"Bf{c % 2}"
                        cp("pool", xc[:, 0:3], tailx[:, c, :], [f"tailx{c}"], [xcn])
                        cp("act", xc[:, 3:3 + TS], bk[:, 0:TS], [br], [xcn])
                        cp("pool", tailx[:, c, :], xc[:, TS:TS + 3], [xcn], [f"tailx{c}"])
                        cw = lambda k_: cols[:, C_CW + c * 4 + k_: C_CW + c * 4 + k_ + 1]
                        P.op("act", lambda e, cacc=cacc, bk=bk, s_=cw(3): e.activation(out=cacc[:], in_=bk[:, 0:TS], func=AF.Copy, scale=s_),
                             rl([br, "cols"]), rl([can]))
                        for k_ in (0, 1, 2):
                            stt("dve", cacc[:], xc[:, k_:k_ + TS], cw(k_), cacc[:], ALU.mult, ALU.add, [xcn, "cols", can], [can])
                        bcol = cols[:, C_CB + c:C_CB + c + 1]
                        if c < 16:
                            hb = (c // 4) % 2; hbn = "bigA" if hb == 0 else "bigB"
                            act(big[:, hb * 1024 + (c % 4) * TS: hb * 1024 + (c % 4 + 1) * TS], cacc[:], AF.Silu, [can, "cols"], [hbn], bias=bcol)
                            if c % 4 == 3:
                                cg = c // 4
                                for tti in range(2):
                                    bk2, br2 = bank()
                                    trs([(bk2[:, q * 128:(q + 1) * 128], big[:, hb * 1024 + q * TS + tti * 128: hb * 1024 + q * TS + (tti + 1) * 128], identF[:])
                                         for q in range(4)], [hbn, "identF"], [br2])
                                    cp("dve", xs_tok[:, tti, cg * 512:(cg + 1) * 512], bk2[:], [br2], ["xs_tok"])
                        elif c < 24:
                            g = c - 16
                            act(Bf[:], cacc[:], AF.Silu, [can, "cols"], [Bfn], bias=bcol)
                            cp("dve", BT[:, g, :], Bf[:], [Bfn], ["BT"])
                            bk2, br2 = bank()
                            trs([(bk2[:, tti * 128:(tti + 1) * 128], Bf[:, tti * 128:(tti + 1) * 128], identF[:]) for tti in range(2)],
                                [Bfn, "identF"], [br2])
                            cp("dve", B_tok[:, :, g * 128:(g + 1) * 128], bk2[:, 0:256].rearrange("p (t n) -> p t n", n=128), [br2], ["B_tok"])
                        else:
                            g = c - 24
                            act(CT[:, g, :], cacc[:], AF.Silu, [can, "cols"], ["CT"], bias=bcol)
                checkpoint(f"conv{int(pre)}", {"xs_tok": xs_tok, "B_tok": B_tok})
                for tti in range(2):
                    bk, br = bank()
                    mms([(bk[:, 0:32], uT[:, dc, tti * 128:(tti + 1) * 128], wdt[:, dc, :], dc == 0, dc == 7) for dc in range(8)],
                        ["uT", "wdt"], [br])
                    tt("dve", dtb[:, tti, :], bk[:, 0:32], rowS[:, 0:32], ALU.add, [br, "rowS"], ["dtb"])
                    act(dtb[:, tti, :], dtb[:, tti, :], AF.Exp, ["dtb"], ["dtb"])
                    act(dtb[:, tti, :], dtb[:, tti, :], AF.Ln, ["dtb"], ["dtb"], bias=1.0)
                    tt("dve", ab[:, tti, :], dtb[:, tti, :], rowS[:, 32:64], ALU.mult, ["dtb", "rowS"], ["ab"])
                    cp("dve", ab16[:, tti, :], ab[:, tti, :], ["ab"], ["ab16"])
                checkpoint(f"dt{int(pre)}", {"dtb": dtb, "ab": ab})
                if not pre:
                    for zc in range(4):
                        wsb, wr_ = wblock(w_in_b[:, OFF_Z + zc * 512: OFF_Z + (zc + 1) * 512], ["w_in_b"])
                        for tti in range(2):
                            bk, br = bank()
                            mms([(bk[:], uT[:, dc, tti * 128:(tti + 1) * 128], wsb[:, dc, :], dc == 0, dc == 7) for dc in range(8)],
                                ["uT", wr_], [br])
                            act(sz_tok[:, tti, zc * 512:(zc + 1) * 512], bk[:], AF.Silu, [br], ["sz_tok"])
                checkpoint(f"z{int(pre)}", {"sz_tok": sz_tok})
                if not pre:
                    bstate["n"] = 4
                    bstate["i"] = 0
                for tti in range(2):
                    tsl = slice(tti * 128, (tti + 1) * 128)
                    bk, br = bank()
                    mms([(bk[:, 0:32], SLf[:], ab[:, tti, :], True, True)], ["SLf", "ab"], [br])
                    bkB, brB = bank()
                    mms([(bkB[:, 0:32], ONf[:], ab[:, tti, :], True, True)], ["ONf", "ab"], [brB])
                    act(sm[:, 0, :], bk[:, 0:32], AF.Exp, [br], ["sm"])
                    act(sm[:, 1, :], bkB[:, 0:32], AF.Exp, [brB], ["sm"])
                    checkpoint(f"ssdA{int(pre)}", {"sm": sm[:, 0:2, :]})
                    checkpoint(f"ssdA2{int(pre)}", {"logD": logD})
                    tt("dve", xdt[:].rearrange("p (h d) -> p h d", h=32), xs_tok[:, tti, :].rearrange("p (h d) -> p h d", h=32),
                       dtb[:, tti, :].unsqueeze(2).to_broadcast([128, 32, 64]), ALU.mult, ["xs_tok", "dtb"], ["xdt"])
                    checkpoint(f"ssdA3{int(pre)}", {"xdt": xdt})
                    tt(PB, xdtd[:].rearrange("p (h d) -> p h d", h=32), xdt[:].rearrange("p (h d) -> p h d", h=32),
                       sm[:, 0, :].unsqueeze(2).to_broadcast([128, 32, 64]), ALU.mult, ["xdt", "sm"], ["xdtd"])
                    checkpoint(f"ssdB{int(pre)}", {"xdt": xdt, "xdtd": xdtd})
                    if not pre:
                        for gh in range(2):
                            bk, br = bank()
                            mms([(bk[:, q * 128:(q + 1) * 128], BT[:, gh * 4 + q, tsl], CT[:, gh * 4 + q, tsl], True, True) for q in range(4)],
                                ["BT", "CT"], [br])
                            tt("dve", CBm[:, gh * 4:(gh + 1) * 4, :], bk[:].rearrange("p (q n) -> p q n", n=128),
                               MKf[:].unsqueeze(1).to_broadcast([128, 4, 128]), ALU.mult, [br, "MKf"], ["CBm"])
                        checkpoint(f"cb{int(pre)}", {"CBm": CBm})
                        ybanks = []
                        for g in range(8):
                            a4 = ab16[:, tti, 4 * g:4 * g + 4].unsqueeze(2).to_broadcast([128, 4, 128])
                            tt("dve", AL[:], SLb[:].unsqueeze(1).to_broadcast([128, 4, 128]), a4, ALU.mult, ["SLb", "ab16"], ["AL"])
                            tt("dve", AO[:], ONb[:].unsqueeze(1).to_broadcast([128, 4, 128]), a4, ALU.mult, ["ONb", "ab16"], ["AO"])
                            bk, br = bank()
                            mms([(bk[:, q * 128:(q + 1) * 128], AL[:, q, :], UTb[:], True, True) for q in range(4)], ["AL", "UTb"], [br])
                            act(LT[:], bk[:].rearrange("p (q n) -> p q n", n=128), AF.Exp, [br], ["LT"])
                            bk2, br2 = bank()
                            mms([(bk2[:, q * 128:(q + 1) * 128], AO[:, q, :], UTb[:], True, True) for q in range(4)], ["AO", "UTb"], [br2])
                            act(Eb[:], bk2[:].rearrange("p (q n) -> p q n", n=128), AF.Exp, [br2], ["Eb"])
                            tt("dve", MT[:], LT[:], CBm[:, g, :].unsqueeze(1).to_broadcast([128, 4, 128]), ALU.mult, ["LT", "CBm"], ["MT"])
                            tt("dve", CE[:], Eb[:], CT[:, g, tsl].unsqueeze(1).to_broadcast([128, 4, 128]), ALU.mult, ["Eb", "CT"], ["CE"])
                            if g % 2 == 0:
                                yb, ybr = ybank(g // 2)
                                ybanks.append((yb, ybr))
                            lst = []
                            for q in range(4):
                                h = 4 * g + q
                                o = yb[:, (g % 2) * 256 + q * 64:(g % 2) * 256 + (q + 1) * 64]
                                lst.append((o, MT[:, q, :], xdt[:, h * 64:(h + 1) * 64], True, False))
                                lst.append((o, CE[:, q, :], state_bf[:, h * 64:(h + 1) * 64], False, True))
                            mms(lst, ["MT", "CE", "xdt", "state_bf"], [ybr])
                        checkpoint(f"grp{int(pre)}", {"MT": MT, "CE": CE})
                        for b4 in range(4):
                            yb, ybr = ybanks[b4]
                            csl = slice(b4 * 512, (b4 + 1) * 512)
                            tt("dve", tmpD[:, 0:512].rearrange("p (h d) -> p h d", h=8), xs_tok[:, tti, csl].rearrange("p (h d) -> p h d", h=8),
                               rowS[:, 64 + b4 * 8:64 + b4 * 8 + 8].unsqueeze(2).to_broadcast([128, 8, 64]), ALU.mult,
                               ["xs_tok", "rowS"], ["tmpD"])
                            tt("dve", yn[:, csl], yb[:], tmpD[:, 0:512], ALU.add, [ybr, "tmpD"], ["yn"])
                            tt("dve", yn[:, csl], yn[:, csl], sz_tok[:, tti, csl], ALU.mult, ["yn", "sz_tok"], ["yn"])
                        for g in range(8):
                            act(junk[:], yn[:, g * 256:(g + 1) * 256], AF.Square, ["yn"], ["junk", "ss8"], accum_out=ss8[:, g:g + 1])
                        act(ss8[:], ss8[:], AF.Sqrt, ["ss8"], ["ss8"], bias=EPS, scale=1.0 / 256)
                        P.op("dve", lambda e: e.reciprocal(out=ss8[:], in_=ss8[:]), rl(["ss8"]), rl(["ss8"]))
                        tt("dve", yn[:, 0:2048].rearrange("p (g d) -> p g d", g=8), yn[:, 0:2048].rearrange("p (g d) -> p g d", g=8),
                           ss8[:].unsqueeze(2).to_broadcast([128, 8, 256]), ALU.mult, ["yn", "ss8"], ["yn"])
                        checkpoint(f"yn{int(pre)}", {"yn": yn[:, 0:2048]})
                        for q4 in range(4):
                            bk, br = bank()
                            trs([(bk[:, q * 128:(q + 1) * 128], yn[:, (q4 * 4 + q) * 128:(q4 * 4 + q + 1) * 128], identF[:]) for q in range(4)],
                                ["yn", "identF"], [br])
                            cp("act", ynT[:, q4 * 4:(q4 + 1) * 4, tsl], bk[:].rearrange("p (q n) -> p q n", n=128), [br], ["ynT"])
                    checkpoint(f"ynT{int(pre)}", {"ynT": ynT[:, :, 0:128]})
                    sbanks = []
                    for b4 in range(4):
                        bk, br = bank()
                        mms([(bk[:, q * 256:(q + 1) * 256], B_tok[:, tti, (2 * b4 + q) * 128:(2 * b4 + q + 1) * 128],
                              xdtd[:, (2 * b4 + q) * 256:(2 * b4 + q + 1) * 256], True, True) for q in range(2)], ["B_tok", "xdtd"], [br])
                        sbanks.append((bk, br))
                    checkpoint(f"ssdC{int(pre)}", {"xdt": xdt})
                    tt(PB, state[:].rearrange("p (h d) -> p h d", h=32), state[:].rearrange("p (h d) -> p h d", h=32),
                       sm[:, 1, :].unsqueeze(2).to_broadcast([128, 32, 64]), ALU.mult, ["state", "sm", "state_bf"], ["state"])
                    for b4 in range(4):
                        bk, br = sbanks[b4]
                        tt("dve", state[:, b4 * 512:(b4 + 1) * 512], bk[:], state[:, b4 * 512:(b4 + 1) * 512], ALU.add, ["state", br], ["state"])
                    if not pre:
                        cp("act", state_bf[:], state[:], ["state"], ["state_bf"])
                if pre:
                    return
                for eh in range(2):
                    pb = [ybank(q_) for q_ in range(4)]
                    for kh in range(2):
                        wsb, wr_ = wblock(wbs_b[kh * 1024:(kh + 1) * 1024, eh * 512:(eh + 1) * 512], ["wbs_b"])
                        for j in range(4):
                            mms([(pb[j][0][:, 0:TS], wsb[:, kc, j * 128:(j + 1) * 128], ynT[:, kh * 8 + kc, :], (kh == 0 and kc == 0),
                                  (kh == 1 and kc == 7)) for kc in range(8)], [wr_, "ynT"], [pb[j][1]])
                    wsg, wg_ = wblock(w_in_b[:, OFF_GS + eh * 512: OFF_GS + (eh + 1) * 512], ["w_in_b"])
                    for j in range(4):
                        ec = eh * 4 + j
                        bk, br = bank()
                        mms([(bk[:, 0:TS], wsg[:, dc, j * 128:(j + 1) * 128], uT[:, dc, :], dc == 0, dc == 7) for dc in range(8)],
                            [wg_, "uT"], [br])
                        act(gsig[:], bk[:, 0:TS], AF.Sigmoid, [br], ["gsig"])
                        tt("dve", gsig[:], pb[j][0][:, 0:TS], gsig[:], ALU.mult, [pb[j][1], "gsig"], ["gsig"])
                        tt("dve", combT[:, ec, :], gsig[:], gp[:, ec, :], ALU.add, ["gsig", "gp"], ["combT"])
                checkpoint(f"comb{int(pre)}", {"combT": combT})
                wo = [wblock(wout_b[:, hh * 512:(hh + 1) * 512], ["wout_b"]) for hh in range(2)]
                for tti in range(2):
                    for hh in range(2):
                        bk, br = bank()
                        mms([(bk[:], combT[:, ec, tti * 128:(tti + 1) * 128], wo[hh][0][:, ec, :], ec == 0, ec == 7) for ec in range(8)],
                            ["combT", wo[hh][1]], [br])
                        tt("dve", H[:, t0 + tti, hh * 512:(hh + 1) * 512], bk[:], H[:, t0 + tti, hh * 512:(hh + 1) * 512], ALU.add,
                           ["H", br], ["H"])

            xst = lambda tti: sz_tok[:, tti, :].bitcast(F32)
            Iacc = ynT[:].rearrange("p a b -> p (a b)").bitcast(F32)
            P.op("pool", lambda e: e.memset(state[:], 0.0), [], rl(["state"]))
            P.op("pool", lambda e: e.memset(Iacc, 0.0), [], rl(["ynT"]))
            P.op("pool", lambda e: e.memset(tailx[:], 0.0), [], rl([f"tailx{c_}" for c_ in range(32)]))
            for j in range(n_pre_slots):
                for si in range(n_slabs):
                    r0 = j * T + si * TS
                    for tti in range(2):
                        dma("sp", xst(tti), x_all[r0 + tti * 128: r0 + (tti + 1) * 128, :], [], ["sz_tok"])
                    slab(si, True, src=xst)
                stt("dve", Iacc, state[:], rk[:, j:j + 1], Iacc, ALU.mult, ALU.add, ["state", "rk", "ynT"], ["ynT"])
            checkpoint("pre", {"state": state, "xs_tok": xs_tok, "B_tok": B_tok, "dtb": dtb})
            cp("dve", state[:], Iacc, ["ynT"], ["state"])
            cp("act", state_bf[:], state[:], ["state"], ["state_bf"])
            cp("pool", tailx[:], tailx0[:], ["tailx0"], [f"tailx{c_}" for c_ in range(32)])
            checkpoint("xchg", {"state": state})
            for si in range(n_slabs):
                slab(si, False)
            checkpoint("main", {"H": H, "ynT": ynT, "gp": gp, "combT": combT, "sz_tok": sz_tok, "CT": CT, "BT": BT})
            barrier()
            P.flush()

        if DEBUG:
            dbg_out = nc.dram_tensor("dbg_h", [T, D], F32, kind="ExternalOutput").ap()
            dma("sp", dbg_out.rearrange("(t p) d -> p t d", p=128), H[:], ["H"], [])

        with ExitStack() as sB:
            U2 = sb(sB, "U2", [128, 8, T], BF16)
            u2f = sb(sB, "u2f", [128, 8, 128], F32)
            xn2 = sb(sB, "xn2", [128, D], F32)
            st2 = sb(sB, "st2", [128, 4], F32)
            wr = sb(sB, "wr", [128, 8, 64], F32)
            wtab = sb(sB, "wtab", [128, NT, 65], F32)
            r64 = [sb(sB, f"r64_{i}", [128, 64], F32) for i in range(4)]
            r8 = [sb(sB, f"r8_{i}", [128, 8], F32) for i in range(5)]
            wguS = [sb(sB, f"wguS{i}", [128, 8, 512], BF16) for i in range(2)]
            wdF = [sb(sB, f"wdF{i}", [128, 2, D], F32) for i in range(2)]
            wdS = [sb(sB, f"wdS{i}", [128, 2, D], BF16) for i in range(2)]
            hidT = [sb(sB, f"hidT{i}", [128, 2, T], BF16) for i in range(2)]
            sg = [sb(sB, f"sg{i}", [128, 512], BF16) for i in range(2)]
            fng = sb(sB, "fng", [128, D], F32)
            g2bc = sb(sB, "g2bc", [128, D], F32)
            dma("sp", g2bc[:], g2_d, ["g2_d"], ["g2bc"])
            ob = [sb(sB, f"ob{i}", [128, D], F32) for i in range(2)]

            dma("sp", wr[:], w_router.rearrange("(c p) n -> p c n", p=128), [], ["wr"])
            dma("sp", fng[:], rows_d[:, R_FNG:R_FNG + D], [], ["fng"])
            P.op("pool", lambda e: e.memset(wtab[:], 1.0), [], rl(["wtab"]))
            for ti in range(NT):
                xin = H[:, ti, :]
                act(xn2[:], xin, AF.Square, ["H"], ["xn2", "st2"], accum_out=st2[:, 0:1])
                act(st2[:, 1:2], st2[:, 0:1], AF.Sqrt, ["st2"], ["st2"], bias=EPS, scale=1.0 / D)
                P.op("dve", lambda e: e.reciprocal(out=st2[:, 2:3], in_=st2[:, 1:2]), rl(["st2"]), rl(["st2"]))
                ts("dve", xn2[:], xin, st2[:, 2:3], None, ALU.mult, None, ["H", "st2"], ["xn2"])
                for half in range(2):
                    bk, br = bank()
                    trs([(bk[:, j * 128:(j + 1) * 128], xn2[:, (half * 4 + j) * 128:(half * 4 + j + 1) * 128], identF[:]) for j in range(4)],
                        ["xn2", "identF"], [br])
                    for j in range(4):
                        dc = half * 4 + j
                        P.op("act", lambda e, dc=dc, j=j, bk=bk: e.activation(out=u2f[:, dc, :], in_=bk[:, j * 128:(j + 1) * 128], func=AF.Identity,
                                                                         bias=mcol[:, 16 + dc:17 + dc], scale=gsc[:, 8 + dc:9 + dc]),
                             rl([br, "gsc", "mcol"]), rl(["u2f"]))
                cp("pool", U2[:, :, ti * 128:(ti + 1) * 128], u2f[:], ["u2f"], ["U2"])
                bk, br = bank()
                mms([(bk[:, 0:64], u2f[:, dc, :], wr[:, dc, :], dc == 0, dc == 7) for dc in range(8)], ["u2f", "wr"], [br])
                s_, ch, t1, t2 = r64
                m1, m2, gs, g8, gm = r8
                act(s_[:], bk[:, 0:64], AF.Sigmoid, [br], ["r0"])
                tt("dve", ch[:], s_[:], rowS[:, 96:160], ALU.add, ["r0", "rowS"], ["r1"])
                v3 = lambda a: a[:].rearrange("p (g e) -> p g e", g=8)
                P.op("dve", lambda e: e.tensor_reduce(out=m1[:], in_=v3(ch), axis=AX.X, op=ALU.max), rl(["r1"]), rl(["q0"]))
                tt("dve", v3(t1), v3(ch), m1[:].unsqueeze(2).to_broadcast([128, 8, 8]), ALU.is_equal, ["r1", "q0"], ["r2"])
                stt("dve", t1[:], t1[:], -BIG, ch[:], ALU.mult, ALU.add, ["r2", "r1"], ["r2"])
                P.op("dve", lambda e: e.tensor_reduce(out=m2[:], in_=v3(t1), axis=AX.X, op=ALU.max), rl(["r2"]), rl(["q1"]))
                tt("dve", gs[:], m1[:], m2[:], ALU.add, ["q0", "q1"], ["q2"])
                P.op("dve", lambda e: e.max(out=g8[:], in_=gs[:]), rl(["q2"]), rl(["q3"]))
                ts("dve", gm[:], gs[:], g8[:, 3:4], None, ALU.is_ge, None, ["q2", "q3"], ["q4"])
                tt("dve", v3(t1), v3(ch), gm[:].unsqueeze(2).to_broadcast([128, 8, 8]), ALU.mult, ["r1", "q4"], ["r2"])
                ts("dve", gm[:], gm[:], -1.0, BIG, ALU.add, ALU.mult, ["q4"], ["q4"])
                tt("dve", v3(t1), v3(t1), gm[:].unsqueeze(2).to_broadcast([128, 8, 8]), ALU.add, ["r2", "q4"], ["r2"])
                P.op("dve", lambda e: e.max(out=g8[:], in_=t1[:]), rl(["r2"]), rl(["q3"]))
                ts("dve", t2[:], t1[:], g8[:, 7:8], None, ALU.is_ge, None, ["r2", "q3"], ["r3"])
                tt("dve", t2[:], t2[:], s_[:], ALU.mult, ["r3", "r0"], ["r3"])
                P.op("dve", lambda e: e.tensor_reduce(out=m1[:, 0:1], in_=t2[:], axis=AX.X, op=ALU.add), rl(["r3"]), rl(["q0"]))
                P.op("dve", lambda e: e.reciprocal(out=m1[:, 0:1], in_=m1[:, 0:1]), rl(["q0"]), rl(["q0"]))
                ts("dve", wtab[:, ti, 0:64], t2[:], m1[:, 0:1], 2.5, ALU.mult, ALU.mult, ["r3", "q0"], ["wtab"])

            ne = n_experts if do_moe else 0
            for e_ in range(ne):
                b = e_ % 2
                P.dma("pool", lambda e, e_=e_, b=b: e.dma_start(out=wguS[b][:], in_=wgu[e_], max_dma_last_dim=4096), rl([]), rl([f"wguS{b}"]))
                dma("sp", wdF[b][:], wdn[e_], [], [f"wdF{b}"])
                tt("pool", wdS[b][:], wdF[b][:], g2bc[:].unsqueeze(1).to_broadcast([128, 2, D]), ALU.mult, [f"wdF{b}", "g2bc"], [f"wdS{b}"])
                k = 0
                for jc in range(2):
                    for sl in range(4):
                        bg, bgr = bank()
                        bu, bur = bank()
                        rhs = lambda dc: U2[:, dc, sl * 512:(sl + 1) * 512]
                        mms([(bg[:], wguS[b][:, dc, jc * 128:(jc + 1) * 128], rhs(dc), dc == 0, dc == 7) for dc in range(8)],
                            [f"wguS{b}", "U2"], [bgr])
                        mms([(bu[:], wguS[b][:, dc, 256 + jc * 128:256 + (jc + 1) * 128], rhs(dc), dc == 0, dc == 7) for dc in range(8)],
                            [f"wguS{b}", "U2"], [bur])
                        act(sg[k % 2][:], bg[:], AF.Silu, [bgr], [f"sg{k % 2}"])
                        tt("dve", hidT[b][:, jc, sl * 512:(sl + 1) * 512], bu[:], sg[k % 2][:], ALU.mult, [bur, f"sg{k % 2}"], [f"hidT{b}"])
                        k += 1
                for ti in range(NT):
                    for hh in range(2):
                        bk, br = ybank()
                        mms([(bk[:], hidT[b][:, jc, ti * 128:(ti + 1) * 128], wdS[b][:, jc, hh * 512:(hh + 1) * 512], jc == 0, jc == 1)
                             for jc in range(2)], [f"hidT{b}", f"wdS{b}"], [br])
                        stt("dve", H[:, ti, hh * 512:(hh + 1) * 512], bk[:], wtab[:, ti, e_:e_ + 1], H[:, ti, hh * 512:(hh + 1) * 512],
                            ALU.mult, ALU.add, [br, "wtab", f"Ht{ti}"], [f"Ht{ti}"])
            for ti in range(NT):
                b = ti % 2
                act(xn2[:], H[:, ti, :], AF.Square, ["H", f"Ht{ti}"], ["xn2", "st2"], accum_out=st2[:, 0:1])
                act(st2[:, 1:2], st2[:, 0:1], AF.Sqrt, ["st2"], ["st2"], bias=EPS, scale=1.0 / D)
                P.op("dve", lambda e: e.reciprocal(out=st2[:, 2:3], in_=st2[:, 1:2]), rl(["st2"]), rl(["st2"]))
                stt("dve", ob[b][:], H[:, ti, :], st2[:, 2:3], fng[:], ALU.mult, ALU.mult, ["H", f"Ht{ti}", "st2", "fng"], [f"ob{b}"])
                dma("sp", out_d[ti * 128:(ti + 1) * 128, :], ob[b][:], [f"ob{b}"], [])
            P.wait_all("sp", P.all_events())
            P.flush()
    except _Stop:
        return nc
    top.close()
    _top0.close()
    return nc


def _colform(v):
    v = np.asarray(v, np.float32).reshape(-1)
    return np.ascontiguousarray(v.reshape(-1, 128).T)


def _prep_inputs(x, c, w_ada, b_ada, norm_mix_g, w_in, conv_w, conv_b, dt_bias, A_log, D_skip, ssd_norm_g,
                 pool_w, pool_scale, w_br_ssd, w_br_pool, w_out, norm_ffn_g, w_router, router_bias,
                 we_gate, we_up, we_down, ws_gate, ws_up, ws_down, final_norm_g):
    f = lambda a: np.ascontiguousarray(np.asarray(a, np.float32))
    x2 = f(x).reshape(NCORES * T, D)
    cols = np.zeros((128, NCOL), np.float32)
    cols[:, C_NMG:C_NMG + 8] = _colform(norm_mix_g)
    cols[:, C_NFG:C_NFG + 8] = _colform(norm_ffn_g)
    cols[:, C_PSC:C_PSC + 8] = _colform(pool_scale)
    cols[:, C_SNG:C_SNG + 16] = _colform(ssd_norm_g)
    cols[:, C_C:C_C + 8] = _colform(c)
    cols[:, C_CB:C_CB + 32] = _colform(conv_b)
    cw = f(conv_w).reshape(4, 32, 128)
    cols[:, C_CW:C_CW + 128] = cw.transpose(2, 1, 0).reshape(128, 128)
    rows = np.zeros((1, NROW), np.float32)
    rows[0, R_BADA:R_BADA + 6144] = f(b_ada).reshape(-1)
    rows[0, R_DTB:R_DTB + 32] = f(dt_bias).reshape(-1)
    rows[0, R_ALOG:R_ALOG + 32] = f(A_log).reshape(-1)
    rows[0, R_DSK:R_DSK + 32] = f(D_skip).reshape(-1)
    rows[0, R_RB:R_RB + 64] = f(router_bias).reshape(-1)
    rows[0, R_FNG:R_FNG + 1024] = f(final_norm_g).reshape(-1)
    rows = np.ascontiguousarray(np.broadcast_to(rows, (128, NROW)))
    wg = f(we_gate)[0].reshape(64, 8, 128, 256)
    wu = f(we_up)[0].reshape(64, 8, 128, 256)
    wgu = np.empty((65, 128, 8, 512), np.float32)
    wgu[:64, :, :, :256] = wg.transpose(0, 2, 1, 3)
    wgu[:64, :, :, 256:] = wu.transpose(0, 2, 1, 3)
    wgu[64, :, :, :256] = f(ws_gate)[0].reshape(8, 128, 256).transpose(1, 0, 2)
    wgu[64, :, :, 256:] = f(ws_up)[0].reshape(8, 128, 256).transpose(1, 0, 2)
    wdn = np.empty((65, 128, 2, 1024), np.float32)
    wdn[:64] = f(we_down)[0].reshape(64, 2, 128, 1024).transpose(0, 2, 1, 3)
    wdn[64] = f(ws_down)[0].reshape(2, 128, 1024).transpose(1, 0, 2)
    shared = {
        "cols": cols, "rows": rows, "w_ada": f(w_ada)[0], "w_in": f(w_in)[0],
        "pool_w": f(pool_w)[0].reshape(1024, 256), "w_br_ssd": f(w_br_ssd)[0], "w_br_pool": f(w_br_pool)[0],
        "w_out": f(w_out)[0], "w_router": f(w_router)[0], "wgu": wgu, "wdn": wdn,
    }
    in_maps = []
    for k in range(NCORES):
        m = dict(shared)
        m["x_own"] = x2[k * T:(k + 1) * T]
        m["x_halo"] = x2[k * T - 128:k * T] if k > 0 else np.zeros((128, D), np.float32)
        m["rk"] = np.ascontiguousarray(np.broadcast_to((np.arange(8) == k - 1).astype(np.float32)[None, :], (128, 8)))
        m["x_all"] = x2[:(NCORES - 1) * T]
        m["nf"] = np.full((128, 1), 1.0 if k > 0 else 0.0, np.float32)
        m["posrow"] = np.ascontiguousarray(np.broadcast_to((k * T + 1 + np.arange(16)).astype(np.float32)[None, :], (128, 16)))
        in_maps.append(m)
    return in_maps


_NC_CACHE = {}


def kernel(**inputs):
    in_maps = _prep_inputs(**inputs)
    if "nc" not in _NC_CACHE:
        _NC_CACHE["nc"] = build_nc()
    res = run_bass_kernel_spmd(_NC_CACHE["nc"], in_maps, core_ids=list(range(NCORES)))
    out = np.concatenate([np.asarray(r["out"], np.float32) for r in res.results], axis=0)
    return out.reshape(1, NCORES * T, D)
```

```python
import numpy as np
import concourse.bass as bass
import concourse.mybir as mybir

F32 = mybir.dt.float32
BF16 = mybir.dt.bfloat16
AF = mybir.ActivationFunctionType
ALU = mybir.AluOpType
AX = mybir.AxisListType


class Reg:
    __slots__ = ("name", "w", "r")

    ALL = []

    def __init__(self, name):
        self.name = name
        self.w = None
        self.r = []
        Reg.ALL.append(self)


class Prog:
    ENGS = ("pe", "act", "dve", "pool", "sp")

    def __init__(self, nc, ndma_sems=8, same_engine_sync=True):
        self.nc = nc
        self.q = {e: [] for e in self.ENGS}
        self.cnt = {e: 0 for e in self.ENGS}
        self.known = {e: {} for e in self.ENGS}
        self.sems = {}
        self.same = same_engine_sync
        self.ndma = ndma_sems
        self.dma_rr = {e: 0 for e in self.ENGS}
        self.dma_val = {}
        self.stack = None

    def _semkeys(self):
        keys = list(self.ENGS) + ["cc"]
        for e in ("sp", "pool", "act"):
            for i in range(self.ndma):
                keys.append(f"d_{e}_{i}")
        return keys

    def _need(self, eng, events):
        mx = {}
        for ev in events:
            if ev is None:
                continue
            k, v = ev
            if k == eng and not self.same:
                continue
            if k == "cc" and eng != "pool":
                continue
            if v > mx.get(k, 0):
                mx[k] = v
        out = []
        kn = self.known[eng]
        for k, v in mx.items():
            if kn.get(k, 0) >= v:
                continue
            kn[k] = v
            out.append((k, v))
        return out

    def _deps(self, reads, writes):
        evs = []
        for r in reads:
            evs.append(r.w)
        for w in writes:
            evs.append(w.w)
            evs.extend(w.r)
        return evs

    def _commit(self, ev, reads, writes):
        for r in reads:
            r.r.append(ev)
        for w in writes:
            w.w = ev
            w.r = []

    def op(self, eng, fn, reads=(), writes=(), extra=()):
        evs = self._deps(reads, writes) + list(extra)
        waits = self._need(eng, evs)
        self.cnt[eng] += 1
        ev = (eng, self.cnt[eng])
        self.q[eng].append((waits, fn, (eng, 1)))
        self._commit(ev, reads, writes)
        return ev

    def dma(self, eng, fn, reads=(), writes=(), extra=()):
        i = self.dma_rr[eng]
        self.dma_rr[eng] = (i + 1) % self.ndma
        key = f"d_{eng}_{i}"
        prev = self.dma_val.get(key, 0)
        evs = self._deps(reads, writes) + list(extra)
        if prev:
            evs.append((key, prev))
        waits = self._need(eng, evs)
        val = prev + 16
        self.dma_val[key] = val
        ev = (key, val)
        self.q[eng].append((waits, fn, (key, 16)))
        self._commit(ev, reads, writes)
        return ev

    def cc(self, fn, reads=(), writes=(), extra=(), inc=1):
        eng = "pool"
        evs = self._deps(reads, writes) + list(extra)
        waits = self._need(eng, evs)
        val = self.dma_val.get("cc", 0) + inc
        self.dma_val["cc"] = val
        ev = ("cc", val)
        self.q[eng].append((waits, fn, ("cc", inc)))
        self._commit(ev, reads, writes)
        return ev

    def wait_all(self, eng, events):
        waits = self._need(eng, list(events))
        self.q[eng].append((waits, None, None))

    def all_events(self):
        evs = [(e, c) for e, c in self.cnt.items() if c]
        evs += [(k, v) for k, v in self.dma_val.items()]
        return evs

    def begin(self, stack):
        self.gen = 0
        self.semh = {k: stack.enter_context(self.nc.semaphore(k)) for k in self._semkeys()}

    def rebase(self, stack):
        self.gen += 1
        self.semh = {k: stack.enter_context(self.nc.semaphore(f"{k}_g{self.gen}")) for k in self._semkeys()}
        self.cnt = {e: 0 for e in self.ENGS}
        self.known = {e: {} for e in self.ENGS}
        self.dma_rr = {e: 0 for e in self.ENGS}
        self.dma_val = {}
        for r in Reg.ALL:
            r.w = None
            r.r = []

    def flush(self):
        nc = self.nc
        sems = self.semh
        q = self.q

        def run(engh, lst):
            for waits, fn, inc in lst:
                for k, v in waits:
                    engh.wait_ge(sems[k], v)
                if fn is None:
                    continue
                ins = fn(engh)
                ins.then_inc(sems[inc[0]], inc[1])

        with nc.Block() as block:
            @block.tensor
            def _(e):
                run(e, q["pe"])

            @block.scalar
            def _(e):
                run(e, q["act"])

            @block.vector
            def _(e):
                run(e, q["dve"])

            @block.gpsimd
            def _(e):
                run(e, q["pool"])

            @block.sync
            def _(e):
                run(e, q["sp"])
        self.q = {e: [] for e in self.ENGS}


from contextlib import ExitStack
from concourse.bass_utils import run_bass_kernel_spmd

NCORES = 8
T = 2048
NT = 16
TS = 256
NSL = T // TS
D = 1024
EPS = 1e-6
OFF_Z, OFF_X, OFF_B, OFF_C, OFF_DT, OFF_P, OFF_GS, OFF_GP = 0, 2048, 4096, 5120, 6144, 6176, 7200, 8224
DIN = 9248
PB = "dve"
NCOL = 208
C_NMG, C_NFG, C_PSC, C_SNG, C_C, C_CB, C_CW = 0, 8, 16, 24, 40, 48, 80
R_BADA, R_DTB, R_ALOG, R_DSK, R_RB, R_FNG = 0, 6144, 6176, 6208, 6240, 6304
NROW = 7328
BIG = 1.0e4
DEBUG = False
import os
DBGV = int(os.environ.get("DBGV", "0"))


class _Stop(Exception):
    pass


def build_nc(n_experts=65, do_moe=True, stop_after=None, n_slabs=NSL, n_pre_slots=NCORES - 1):
    nc = bass.Bass("TRN2", target_bir_lowering=False)
    di = lambda n, s, dt=F32: nc.dram_tensor(n, s, dt, kind="ExternalInput").ap()
    x_own = di("x_own", [T, D]); x_halo = di("x_halo", [128, D]); x_all = di("x_all", [(NCORES - 1) * T, D])
    rk_d = di("rk", [128, 8]); nf_d = di("nf", [128, 1]); pos_d = di("posrow", [128, 16])
    cols_d = di("cols", [128, NCOL]); rows_d = di("rows", [128, NROW])
    w_ada = di("w_ada", [D, 6 * D]); w_in = di("w_in", [D, DIN])
    pool_w = di("pool_w", [1024, 256]); w_br_ssd = di("w_br_ssd", [2048, D])
    w_br_pool = di("w_br_pool", [D, D]); w_out = di("w_out", [D, D])
    w_router = di("w_router", [D, 64])
    NEW = 65 if stop_after is None else 1
    wgu = di("wgu", [NEW, 128, 8, 512]); wdn = di("wdn", [NEW, 128, 2, 1024])
    out_d = nc.dram_tensor("out", [T, D], F32, kind="ExternalOutput").ap()
    w_in_b = nc.dram_tensor("w_in_b", [D, DIN], BF16).ap()
    wbp_b = nc.dram_tensor("wbp_b", [D, D], BF16).ap()
    wbs_b = nc.dram_tensor("wbs_b", [2048, D], BF16).ap()
    wout_b = nc.dram_tensor("wout_b", [D, D], BF16).ap()
    bounce = nc.dram_tensor("bounce", [128, 2080], F32).ap()
    gathered = nc.dram_tensor("gathered", [1024, 2080], F32).ap()
    dbg = {}

    Reg.ALL.clear()
    P = Prog(nc)
    regs = {}
    _top0 = ExitStack()
    P.begin(_top0)

    ALIAS = {"yn": ["bigA", "bigB"], "big": ["bigA", "bigB"], "junk": ["gsig"], "xn": ["bigB"]}

    def R(name):
        if name not in regs:
            regs[name] = Reg(name)
        return regs[name]

    top = ExitStack()

    def sb(st, name, shape, dt):
        return st.enter_context(nc.sbuf_tensor("s_" + name, shape, dt))

    def rl(x):
        out = []
        for n in x:
            if isinstance(n, str):
                for m in ALIAS.get(n, [n]):
                    out.append(R(m))
            else:
                out.append(n)
        return out

    def tt(eng, out, a, b, op, rd, wr):
        return P.op(eng, lambda e: e.tensor_tensor(out=out, in0=a, in1=b, op=op), rl(rd), rl(wr))

    def ts(eng, out, a, s1, s2, op0, op1, rd, wr):
        if op1 is None:
            return P.op(eng, lambda e: e.tensor_single_scalar(out=out, in_=a, scalar=s1, op=op0), rl(rd), rl(wr))
        return P.op(eng, lambda e: e.tensor_scalar(out=out, in0=a, scalar1=s1, scalar2=s2, op0=op0, op1=op1), rl(rd), rl(wr))

    def stt(eng, out, a, sc, b, op0, op1, rd, wr):
        return P.op(eng, lambda e: e.scalar_tensor_tensor(out=out, in0=a, scalar=sc, in1=b, op0=op0, op1=op1), rl(rd), rl(wr))

    def act(out, in_, func, rd, wr, bias=0.0, scale=1.0, accum_out=None):
        if accum_out is None:
            return P.op("act", lambda e: e.activation(out=out, in_=in_, func=func, bias=bias, scale=scale), rl(rd), rl(wr))
        return P.op("act", lambda e: e.activation(out=out, in_=in_, func=func, bias=bias, scale=scale, accum_out=accum_out), rl(rd), rl(wr))

    def cp(eng, out, in_, rd, wr):
        if eng == "act":
            return P.op("act", lambda e: e.copy(out=out, in_=in_), rl(rd), rl(wr))
        return P.op(eng, lambda e: e.tensor_copy(out=out, in_=in_), rl(rd), rl(wr))

    def mms(lst, rd, wr):
        def f(e):
            for (o, l, r, s0, s1) in lst:
                ins = e.matmul(o, lhsT=l, rhs=r, start=s0, stop=s1)
            return ins
        return P.op("pe", f, rl(rd), rl(wr))

    def trs(lst, rd, wr):
        def f(e):
            for (o, i, idn) in lst:
                ins = e.transpose(out=o, in_=i, identity=idn)
            return ins
        return P.op("pe", f, rl(rd), rl(wr))

    def dma(q, out, in_, rd, wr):
        if q == "pool":
            return P.dma(q, lambda e: e.dma_start(out=out, in_=in_, max_dma_last_dim=4096), rl(rd), rl(wr))
        return P.dma(q, lambda e: e.dma_start(out=out, in_=in_), rl(rd), rl(wr))

    def barrier():
        evs = P.all_events()
        for e in ("pe", "act", "dve", "pool", "sp"):
            P.wait_all(e, evs)

    def checkpoint(name, tensors):
        if stop_after != name:
            return
        for tn, t in tensors.items():
            a = t if hasattr(t, "ap") and not hasattr(t, "__enter__") and type(t).__name__ == "AP" else t[:]
            shp = list(a.shape)
            d = nc.dram_tensor("dbg_" + tn, shp, a.dtype, kind="ExternalOutput").ap()
            dma("sp", d, a, [tn], [])
        P.wait_all("sp", P.all_events())
        P.flush()
        raise _Stop()

    banks = [top.enter_context(nc.psum_tensor(f"bank{i}", [128, 512], F32)) for i in range(8)]
    bstate = {"i": 0, "y": 0}

    def bank():
        n = bstate.get("n", 4)
        i = bstate["i"] % n
        bstate["i"] = (i + 1) % n
        return banks[i], R(f"bank{i}")

    def ybank(i=None):
        if i is None:
            i = bstate["y"]
            bstate["y"] = (i + 1) % 4
        return banks[4 + i], R(f"bank{4 + i}")

    H = sb(top, "H", [128, NT, D], F32)
    state = sb(top, "state", [128, 2048], F32)
    state_bf = sb(top, "state_bf", [128, 2048], BF16)
    identF = sb(top, "identF", [128, 128], F32)
    SLf = sb(top, "SLf", [128, 128], F32)
    ONf = sb(top, "ONf", [128, 128], F32)
    SLb = sb(top, "SLb", [128, 128], BF16)
    ONb = sb(top, "ONb", [128, 128], BF16)
    UTb = sb(top, "UTb", [128, 128], BF16)
    MKf = sb(top, "MKf", [128, 128], F32)
    cols = sb(top, "cols", [128, NCOL], F32)
    rowS = sb(top, "rowS", [128, 160], F32)
    rk = sb(top, "rk_s", [128, 8], F32)
    nf = sb(top, "nf_s", [128, 1], F32)
    mcol = sb(top, "mcol", [128, 32], F32)
    gsc = sb(top, "gsc", [128, 16], F32)
    g2_d = nc.dram_tensor("g2_d", [128, D], F32).ap()

    def mask(tn, tensor, cmp, sgn=1):
        P.op("pool", lambda e: e.memset(tensor[:], 1.0), [], [R(tn)])
        P.op("pool", lambda e: e.affine_select(out=tensor[:], in_=tensor[:], pattern=[[-sgn, 128]], compare_op=cmp,
                                               fill=0.0, base=0, channel_multiplier=sgn), [R(tn)], [R(tn)])
    mask("identF", identF, ALU.is_equal)
    mask("SLf", SLf, ALU.is_gt)
    mask("MKf", MKf, ALU.is_ge, -1)
    P.op("pool", lambda e: e.memset(ONf[:], 1.0), [], [R("ONf")])
    cp("pool", SLb[:], SLf[:], ["SLf"], ["SLb"])
    cp("pool", ONb[:], ONf[:], ["ONf"], ["ONb"])
    cp("pool", UTb[:], MKf[:], ["MKf"], ["UTb"])

    dma("sp", cols[:], cols_d, [], ["cols"])
    dma("sp", rowS[:], rows_d[:, R_DTB:R_DTB + 160], [], ["rowS"])
    dma("sp", rk[:], rk_d, [], ["rk"])
    dma("sp", nf[:], nf_d, [], ["nf"])
    dma("sp", H[:], x_own.rearrange("(t p) d -> p t d", p=128), [], ["H"])
    _wv_o = w_in_b.rearrange("a b -> (a b)").rearrange("(r c) -> r c", c=2048)
    _wv_i = w_in.rearrange("a b -> (a b)").rearrange("(r c) -> r c", c=2048)
    for _q in range(8):
        dma("pool", _wv_o[_q * 578:(_q + 1) * 578, :], _wv_i[_q * 578:(_q + 1) * 578, :], [], ["w_in_b"])
    dma("pool", wbp_b, w_br_pool, [], ["wbp_b"])

    try:
        with ExitStack() as s0:
            bcm = sb(s0, "bcm", [128, 6 * D], F32)
            cact = sb(s0, "cact", [128, 8], F32)
            CBC = sb(s0, "CBC", [128, 8, 128], F32)
            wa = [sb(s0, f"wa{i}", [128, 8, 512], F32) for i in range(2)]
            ba = [sb(s0, f"ba{i}", [128, 512], F32) for i in range(2)]
            stg = [sb(s0, f"stg{i}", [128, 4, D], F32) for i in range(2)]
            stb = [sb(s0, f"stb{i}", [128, 4, D], BF16) for i in range(2)]
            act(cact[:], cols[:, C_C:C_C + 8], AF.Silu, ["cols"], ["cact"])
            cp("dve", CBC[:], cact[:].unsqueeze(2).to_broadcast([128, 8, 128]), ["cact"], ["CBC"])
            for ns in range(12):
                b = ns % 2
                dma("sp", wa[b][:], w_ada[:, ns * 512:(ns + 1) * 512].rearrange("(c p) n -> p c n", p=128), [], [f"wa{b}"])
                dma("sp", ba[b][:], rows_d[:, R_BADA + ns * 512:R_BADA + (ns + 1) * 512], [], [f"ba{b}"])
                bk, br = bank()
                mms([(bk[:], CBC[:, dc, :], wa[b][:, dc, :], dc == 0, dc == 7) for dc in range(8)], ["CBC", f"wa{b}"], [br])
                tt("dve", bcm[:, ns * 512:(ns + 1) * 512], bk[:], ba[b][:], ALU.add, [br, f"ba{b}"], ["bcm"])
            for vi, v in enumerate((0, 1, 3, 4)):
                for q in range(2):
                    bk, br = bank()
                    trs([(bk[:, j * 128:(j + 1) * 128], bcm[:, v * D + (q * 4 + j) * 128: v * D + (q * 4 + j + 1) * 128], identF[:])
                         for j in range(4)], ["bcm", "identF"], [br])
                    cp("dve", mcol[:, vi * 8 + q * 4: vi * 8 + q * 4 + 4],
                       bk[:].rearrange("p (j n) -> p j n", n=128)[:, :, 0], [br], ["mcol"])
            stt("dve", gsc[:, 0:8], mcol[:, 8:16], 1.0, cols[:, C_NMG:C_NMG + 8], ALU.add, ALU.mult, ["mcol", "cols"], ["gsc"])
            stt("dve", gsc[:, 8:16], mcol[:, 24:32], 1.0, cols[:, C_NFG:C_NFG + 8], ALU.add, ALU.mult, ["mcol", "cols"], ["gsc"])
            dma("sp", g2_d, bcm[:, 5 * D:6 * D], ["bcm"], ["g2_d"])
            act(rowS[:, 32:64], rowS[:, 32:64], AF.Exp, ["rowS"], ["rowS"])
            ts("dve", rowS[:, 32:64], rowS[:, 32:64], -1.0, None, ALU.mult, None, ["rowS"], ["rowS"])
            k = 0
            for half in range(2):
                b = k % 2; k += 1
                dma("sp", stg[b][:], w_out[half * 512:(half + 1) * 512, :].rearrange("(c p) n -> p c n", p=128), [], [f"stg{b}"])
                tt("dve", stb[b][:], stg[b][:], bcm[:, 2 * D:3 * D].unsqueeze(1).to_broadcast([128, 4, D]), ALU.mult,
                   [f"stg{b}", "bcm"], [f"stb{b}"])
                dma("sp", wout_b[half * 512:(half + 1) * 512, :].rearrange("(c p) n -> p c n", p=128), stb[b][:], [f"stb{b}"], ["wout_b"])
            for q in range(4):
                b = k % 2; k += 1
                dma("sp", stg[b][:], w_br_ssd[q * 512:(q + 1) * 512, :].rearrange("(c p) n -> p c n", p=128), [], [f"stg{b}"])
                for j in range(4):
                    ts("pool", stb[b][:, j, :], stg[b][:, j, :], cols[:, C_SNG + q * 4 + j:C_SNG + q * 4 + j + 1], None, ALU.mult, None,
                       [f"stg{b}", "cols"], [f"stb{b}"])
                dma("sp", wbs_b[q * 512:(q + 1) * 512, :].rearrange("(c p) n -> p c n", p=128), stb[b][:], [f"stb{b}"], ["wbs_b"])
            checkpoint("p0", {"mcol": mcol, "gsc": gsc, "rowS": rowS, "bcm": bcm})
            barrier()
            P.flush()

        with ExitStack() as sA:
            WS = [sb(sA, f"WS{i}", [128, 8, 512], BF16) for i in range(3)]
            wsi = {"i": 0}
            wdt = sb(sA, "wdt", [128, 8, 32], BF16)
            poolw = sb(sA, "poolw", [128, 8, 256], BF16)
            pcorr = sb(sA, "pcorr", [128, 4, 16], F32)
            posr = sb(sA, "posr", [128, 16], F32)
            tailx = sb(sA, "tailx", [128, 32, 3], F32)
            tailx0 = sb(sA, "tailx0", [128, 32, 3], F32)
            ptail = sb(sA, "ptail", [128, 8, 15], F32)
            xpw = [sb(sA, f"xpw{i}", [128, 15 + TS], F32) for i in range(2)]
            logD = sb(sA, "logD", [128, 32], F32)
            logDp = [logD, sb(sA, "logD1", [128, 32], F32)]
            ldi = {"i": 0}
            uT = sb(sA, "uT", [128, 8, TS], BF16)
            xn_late = True
            st1 = sb(sA, "st1", [128, 4], F32)
            pooledT = sb(sA, "pooledT", [128, 8, TS], BF16)
            mixedT = sb(sA, "mixedT", [128, 8, TS], BF16)
            gp = sb(sA, "gp", [128, 8, TS], BF16)
            pS = [sb(sA, f"pS{i}", [128, 15 + TS], F32) for i in range(2)]
            gsig = sb(sA, "gsig", [128, TS], F32)
            NSET = 3
            xcs = [sb(sA, f"xc{i}", [128, 3 + TS], F32) for i in range(NSET)]
            caccs = [sb(sA, f"cacc{i}", [128, TS], F32) for i in range(NSET)]
            big = sb(sA, "big", [128, 2080], F32)
            xn = big[:, 1024:2048]
            tmpD = sb(sA, "tmpD", [128, 512], F32)
            Bfs = [sb(sA, f"Bf{i}", [128, TS], F32) for i in range(2)]
            xs_tok = sb(sA, "xs_tok", [128, 2, 2048], BF16)
            B_tok = sb(sA, "B_tok", [128, 2, 1024], BF16)
            BT = sb(sA, "BT", [128, 8, TS], BF16)
            CT = sb(sA, "CT", [128, 8, TS], BF16)
            dtb = sb(sA, "dtb", [128, 2, 32], F32)
            ab = sb(sA, "ab", [128, 2, 32], F32)
            ab16 = sb(sA, "ab16", [128, 2, 32], BF16)
            sm = sb(sA, "sm", [128, 4, 32], F32)
            sz_tok = sb(sA, "sz_tok", [128, 2, 2048], BF16)
            xdt = sb(sA, "xdt", [128, 2048], BF16)
            xdtd = sb(sA, "xdtd", [128, 2048], BF16)
            AL = sb(sA, "AL", [128, 4, 128], BF16)
            AO = sb(sA, "AO", [128, 4, 128], BF16)
            LT = sb(sA, "LT", [128, 4, 128], BF16)
            Eb = sb(sA, "Eb", [128, 4, 128], BF16)
            CBm = sb(sA, "CBm", [128, 8, 128], BF16)
            MT = sb(sA, "MT", [128, 4, 128], BF16)
            CE = sb(sA, "CE", [128, 4, 128], BF16)
            yn = big
            junk = gsig
            ss8 = sb(sA, "ss8", [128, 8], F32)
            ynT = sb(sA, "ynT", [128, 16, TS], BF16)
            combT = sb(sA, "combT", [128, 8, TS], BF16)
            stS = big

            def wblock(src, rd):
                i = wsi["i"]; wsi["i"] = (i + 1) % 3
                dma("sp", WS[i][:], src.rearrange("(c p) n -> p c n", p=128), rd, [f"WS{i}"])
                return WS[i], f"WS{i}"

            dma("pool", wdt[:], w_in[:, OFF_DT:OFF_DT + 32].rearrange("(c p) n -> p c n", p=128), [], ["wdt"])
            dma("pool", poolw[:], pool_w.rearrange("(c p) n -> p c n", p=128), [], ["poolw"])
            dma("sp", posr[:], pos_d, [], ["posr"])
            for g, w in enumerate((2, 4, 8, 16)):
                ts("dve", pcorr[:, g, :], posr[:], float(w), None, ALU.min, None, ["posr"], ["pcorr"])
                P.op("dve", lambda e, g=g: e.reciprocal(out=pcorr[:, g, :], in_=pcorr[:, g, :]), rl(["pcorr"]), rl(["pcorr"]))
                ts("dve", pcorr[:, g, :], pcorr[:, g, :], float(w), None, ALU.mult, None, ["pcorr"], ["pcorr"])

            def make_uT(src_ap, rdname, n_tiles, col0=0):
                for tti in range(n_tiles):
                    xin = src_ap(tti)
                    act(junk_big[:], xin, AF.Square, [rdname], ["xn", "st1"], accum_out=st1[:, 0:1])
                    act(st1[:, 1:2], st1[:, 0:1], AF.Sqrt, ["st1"], ["st1"], bias=EPS, scale=1.0 / D)
                    P.op("dve", lambda e: e.reciprocal(out=st1[:, 2:3], in_=st1[:, 1:2]), rl(["st1"]), rl(["st1"]))
                    ts("dve", xn[:], xin, st1[:, 2:3], None, ALU.mult, None, [rdname, "st1"], ["xn"])
                    for half in range(2):
                        bk, br = bank()
                        trs([(bk[:, j * 128:(j + 1) * 128], xn[:, (half * 4 + j) * 128:(half * 4 + j + 1) * 128], identF[:])
                             for j in range(4)], ["xn", "identF"], [br])
                        for j in range(4):
                            dc = half * 4 + j
                            ts("dve" if half else "pool_no", uT[:, dc, col0 + tti * 128: col0 + (tti + 1) * 128], bk[:, j * 128:(j + 1) * 128],
                               gsc[:, dc:dc + 1], mcol[:, dc:dc + 1], ALU.mult, ALU.add, [br, "gsc", "mcol"], ["uT"])

            junk_big = xn

            _ts_orig = ts

            def ts(eng, out, a, s1, s2, op0, op1, rd, wr):
                if eng == "pool_no":
                    return P.op("act", lambda e: e.activation(out=out, in_=a, func=AF.Identity, bias=s2, scale=s1), rl(rd), rl(wr))
                return _ts_orig(eng, out, a, s1, s2, op0, op1, rd, wr)

            xh = xsA = big
            dma("sp", big[:, 0:D], x_halo, [], ["bigA"])
            make_uT(lambda tti: big[:, 0:D], "bigA", 1)
            for blk in range(10):
                c0 = OFF_X + blk * 512 if blk < 8 else OFF_P + (blk - 8) * 512
                wsb, wr_ = wblock(w_in_b[:, c0:c0 + 512], ["w_in_b"])
                for j in range(4):
                    bk, br = bank()
                    mms([(bk[:, 0:128], wsb[:, dc, j * 128:(j + 1) * 128], uT[:, dc, 0:128], dc == 0, dc == 7) for dc in range(8)],
                        [wr_, "uT"], [br])
                    if blk < 8:
                        c = blk * 4 + j
                        ts("dve", tailx0[:, c, :], bk[:, 125:128], nf[:, 0:1], None, ALU.mult, None, [br, "nf"], ["tailx0"])
                    else:
                        c = (blk - 8) * 4 + j
                        ts("dve", ptail[:, c, :], bk[:, 113:128], nf[:, 0:1], None, ALU.mult, None, [br, "nf"], ["ptail"])

            checkpoint("halo", {"tailx0": tailx0, "xpb": ptail, "uT": uT[:, :, 0:128]})
            def slab(si, pre, src=None, skip_uT=False, hook=None):
                t0 = 2 * si
                bstate["n"] = 8
                if skip_uT:
                    pass
                elif src is None:
                    make_uT(lambda tti: H[:, t0 + tti, :], "H", 2)
                else:
                    make_uT(src, "sz_tok", 2)
                if not pre:
                    for blk in range(2):
                        wsb, wr_ = wblock(w_in_b[:, OFF_P + blk * 512: OFF_P + (blk + 1) * 512], ["w_in_b"])
                        for j in range(4):
                            c = blk * 4 + j
                            g = c // 2
                            w = 2 << g
                            bk, br = bank()
                            mms([(bk[:, 0:TS], wsb[:, dc, j * 128:(j + 1) * 128], uT[:, dc, :], dc == 0, dc == 7) for dc in range(8)],
                                [wr_, "uT"], [br])
                            xw = xpw[c % 2]; xwn = f"xpw{c % 2}"
                            cp("pool", xw[:, 0:15], ptail[:, c, :], ["ptail"], [xwn])
                            cp("act", xw[:, 15:15 + TS], bk[:, 0:TS], [br], [xwn])
                            cp("pool", ptail[:, c, :], xw[:, TS:TS + 15], [xwn], ["ptail"])
                            src = xw[:]
                            lo = 0
                            step = 1
                            k = 0
                            while step < w:
                                dst = pS[k % 2]
                                nlo = lo + step
                                tt("dve", dst[:, nlo:15 + TS], src[:, nlo:15 + TS], src[:, nlo - step:15 + TS - step], ALU.add,
                                   [xwn, f"pS{(k + 1) % 2}"], [f"pS{k % 2}"])
                                src = dst[:]
                                lo = nlo
                                step *= 2
                                k += 1
                            sname = f"pS{(k - 1) % 2}"
                            if si == 0:
                                tt("dve", src[:, 15:31], src[:, 15:31], pcorr[:, g, :], ALU.mult, [sname, "pcorr"], [sname])
                            stt("dve", pooledT[:, c, :], src[:, 15:15 + TS], 1.0 / w, xw[:, 15:15 + TS], ALU.mult, ALU.subtract,
                                [sname, xwn], ["pooledT"])
                            pass
                    for g in range(4):
                        for dch in range(2):
                            bk, br = bank()
                            mms([(bk[:, 0:TS], poolw[:, 2 * g + kc, dch * 128:(dch + 1) * 128], pooledT[:, 2 * g + kc, :], kc == 0, kc == 1)
                                 for kc in range(2)], ["poolw", "pooledT"], [br])
                            ts("dve", mixedT[:, 2 * g + dch, :], bk[:, 0:TS], cols[:, C_PSC + 2 * g + dch:C_PSC + 2 * g + dch + 1], None,
                               ALU.mult, None, [br, "cols"], ["mixedT"])
                    for blk in range(2):
                        wsb, wr_ = wblock(wbp_b[:, blk * 512:(blk + 1) * 512], ["wbp_b"])
                        wsg, wg_ = wblock(w_in_b[:, OFF_GP + blk * 512: OFF_GP + (blk + 1) * 512], ["w_in_b"])
                        for j in range(4):
                            ec = blk * 4 + j
                            bk, br = bank()
                            mms([(bk[:, 0:TS], wsg[:, dc, j * 128:(j + 1) * 128], uT[:, dc, :], dc == 0, dc == 7) for dc in range(8)],
                                [wg_, "uT"], [br])
                            act(gsig[:], bk[:, 0:TS], AF.Sigmoid, [br], ["gsig"])
                            bk2, br2 = bank()
                            mms([(bk2[:, 0:TS], wsb[:, dc, j * 128:(j + 1) * 128], mixedT[:, dc, :], dc == 0, dc == 7) for dc in range(8)],
                                [wr_, "mixedT"], [br2])
                            tt("dve", gp[:, ec, :], bk2[:, 0:TS], gsig[:], ALU.mult, [br2, "gsig"], ["gp"])
                checkpoint(f"ut{int(pre)}", {"uT": uT})
                checkpoint(f"pool{int(pre)}", {"gp": gp})
                nblk = 6 if pre else 8
                pend = []

                def tick():
                    for p_ in pend:
                        p_[0] -= 1
                    while pend and pend[0][0] <= 0:
                        pend.pop(0)[1]()

                for blk in range(nblk):
                    wsb, wr_ = wblock(w_in_b[:, OFF_X + blk * 512: OFF_X + (blk + 1) * 512], ["w_in_b"])
                    for j in range(4):
                        c = blk * 4 + j
                        bk, br = bank()
                        mms([(bk[:, 0:TS], wsb[:, dc, j * 128:(j + 1) * 128], uT[:, dc, :], dc == 0, dc == 7) for dc in range(8)],
                            [wr_, "uT"], [br])
                        xc = xcs[c % NSET]; cacc = caccs[c % NSET]; xcn = f"xc{c % NSET}"; can = f"cacc{c % NSET}"
                        Bf = Bfs[c % 2]; Bfn = f"Bf{c % 2}"
                        cp("pool", xc[:, 0:3], tailx[:, c, :], [f"tailx{c}"], [xcn])
                        cp("act", xc[:, 3:3 + TS], bk[:, 0:TS], [br], [xcn])
                        cp("pool", tailx[:, c, :], xc[:, TS:TS + 3], [xcn], [f"tailx{c}"])
                        cw = lambda k_: cols[:, C_CW + c * 4 + k_: C_CW + c * 4 + k_ + 1]
                        P.op("act", lambda e, cacc=cacc, bk=bk, s_=cw(3): e.activation(out=cacc[:], in_=bk[:, 0:TS], func=AF.Copy, scale=s_),
                             rl([br, "cols"]), rl([can]))
                        for k_ in (0, 1, 2):
                            stt("dve", cacc[:], xc[:, k_:k_ + TS], cw(k_), cacc[:], ALU.mult, ALU.add, [xcn, "cols", can], [can])
                        bcol = cols[:, C_CB + c:C_CB + c + 1]
                        if c < 16:
                            hb = (c // 4) % 2; hbn = "bigA" if hb == 0 else "bigB"
                            act(big[:, hb * 1024 + (c % 4) * TS: hb * 1024 + (c % 4 + 1) * TS], cacc[:], AF.Silu, [can, "cols"], [hbn], bias=bcol)
                            if c % 4 == 3:
                                def st2x(cg=c // 4, hb=hb, hbn=hbn):
                                    for tti in range(2):
                                        bk2, br2 = bank()
                                        trs([(bk2[:, q * 128:(q + 1) * 128], big[:, hb * 1024 + q * TS + tti * 128: hb * 1024 + q * TS + (tti + 1) * 128], identF[:])
                                             for q in range(4)], [hbn, "identF"], [br2])
                                        cp("dve", xs_tok[:, tti, cg * 512:(cg + 1) * 512], bk2[:], [br2], ["xs_tok"])
                                pend.append([3, st2x])
                        elif c < 24:
                            g = c - 16
                            act(Bf[:], cacc[:], AF.Silu, [can, "cols"], [Bfn], bias=bcol)
                            def st2b(g=g, Bf=Bf, Bfn=Bfn):
                                cp("dve", BT[:, g, :], Bf[:], [Bfn], ["BT"])
                                bk2, br2 = bank()
                                trs([(bk2[:, tti * 128:(tti + 1) * 128], Bf[:, tti * 128:(tti + 1) * 128], identF[:]) for tti in range(2)],
                                    [Bfn, "identF"], [br2])
                                cp("dve", B_tok[:, :, g * 128:(g + 1) * 128], bk2[:, 0:256].rearrange("p (t n) -> p t n", n=128), [br2], ["B_tok"])
                            pend.append([2, st2b])
                        else:
                            g = c - 24
                            act(CT[:, g, :], cacc[:], AF.Silu, [can, "cols"], ["CT"], bias=bcol)
                        tick()
                while pend:
                    pend.pop(0)[1]()
                checkpoint(f"conv{int(pre)}", {"xs_tok": xs_tok, "B_tok": B_tok})
                for tti in range(2):
                    bk, br = bank()
                    mms([(bk[:, 0:32], uT[:, dc, tti * 128:(tti + 1) * 128], wdt[:, dc, :], dc == 0, dc == 7) for dc in range(8)],
                        ["uT", "wdt"], [br])
                    tt("dve", dtb[:, tti, :], bk[:, 0:32], rowS[:, 0:32], ALU.add, [br, "rowS"], ["dtb"])
                    act(dtb[:, tti, :], dtb[:, tti, :], AF.Exp, ["dtb"], ["dtb"])
                    act(dtb[:, tti, :], dtb[:, tti, :], AF.Ln, ["dtb"], ["dtb"], bias=1.0)
                    tt("dve", ab[:, tti, :], dtb[:, tti, :], rowS[:, 32:64], ALU.mult, ["dtb", "rowS"], ["ab"])
                    cp("dve", ab16[:, tti, :], ab[:, tti, :], ["ab"], ["ab16"])
                checkpoint(f"dt{int(pre)}", {"dtb": dtb, "ab": ab})
                if not pre:
                    for zc in range(4):
                        wsb, wr_ = wblock(w_in_b[:, OFF_Z + zc * 512: OFF_Z + (zc + 1) * 512], ["w_in_b"])
                        for tti in range(2):
                            bk, br = bank()
                            mms([(bk[:], uT[:, dc, tti * 128:(tti + 1) * 128], wsb[:, dc, :], dc == 0, dc == 7) for dc in range(8)],
                                ["uT", wr_], [br])
                            act(sz_tok[:, tti, zc * 512:(zc + 1) * 512], bk[:], AF.Silu, [br], ["sz_tok"])
                checkpoint(f"z{int(pre)}", {"sz_tok": sz_tok})
                if hook is not None:
                    hook()
                if not pre:
                    bstate["n"] = 4
                    bstate["i"] = 0
                for tti in range(2):
                    tsl = slice(tti * 128, (tti + 1) * 128)
                    bk, br = bank()
                    mms([(bk[:, 0:32], SLf[:], ab[:, tti, :], True, True)], ["SLf", "ab"], [br])
                    bkB, brB = bank()
                    mms([(bkB[:, 0:32], ONf[:], ab[:, tti, :], True, True)], ["ONf", "ab"], [brB])
                    act(sm[:, 0, :], bk[:, 0:32], AF.Exp, [br], ["sm"])
                    act(sm[:, 1, :], bkB[:, 0:32], AF.Exp, [brB], ["sm"])
                    checkpoint(f"ssdA{int(pre)}", {"sm": sm[:, 0:2, :]})
                    checkpoint(f"ssdA2{int(pre)}", {"logD": logD})
                    tt("dve", xdt[:].rearrange("p (h d) -> p h d", h=32), xs_tok[:, tti, :].rearrange("p (h d) -> p h d", h=32),
                       dtb[:, tti, :].unsqueeze(2).to_broadcast([128, 32, 64]), ALU.mult, ["xs_tok", "dtb"], ["xdt"])
                    checkpoint(f"ssdA3{int(pre)}", {"xdt": xdt})
                    tt(PB, xdtd[:].rearrange("p (h d) -> p h d", h=32), xdt[:].rearrange("p (h d) -> p h d", h=32),
                       sm[:, 0, :].unsqueeze(2).to_broadcast([128, 32, 64]), ALU.mult, ["xdt", "sm"], ["xdtd"])
                    checkpoint(f"ssdB{int(pre)}", {"xdt": xdt, "xdtd": xdtd})
                    if not pre:
                        for gh in range(2):
                            bk, br = bank()
                            mms([(bk[:, q * 128:(q + 1) * 128], BT[:, gh * 4 + q, tsl], CT[:, gh * 4 + q, tsl], True, True) for q in range(4)],
                                ["BT", "CT"], [br])
                            tt("dve", CBm[:, gh * 4:(gh + 1) * 4, :], bk[:].rearrange("p (q n) -> p q n", n=128),
                               MKf[:].unsqueeze(1).to_broadcast([128, 4, 128]), ALU.mult, [br, "MKf"], ["CBm"])
                        checkpoint(f"cb{int(pre)}", {"CBm": CBm})
                        ybanks = []
                        for g in range(8):
                            a4 = ab16[:, tti, 4 * g:4 * g + 4].unsqueeze(2).to_broadcast([128, 4, 128])
                            tt("dve", AL[:], SLb[:].unsqueeze(1).to_broadcast([128, 4, 128]), a4, ALU.mult, ["SLb", "ab16"], ["AL"])
                            tt("dve", AO[:], ONb[:].unsqueeze(1).to_broadcast([128, 4, 128]), a4, ALU.mult, ["ONb", "ab16"], ["AO"])
                            bk, br = bank()
                            mms([(bk[:, q * 128:(q + 1) * 128], AL[:, q, :], UTb[:], True, True) for q in range(4)], ["AL", "UTb"], [br])
                            act(LT[:], bk[:].rearrange("p (q n) -> p q n", n=128), AF.Exp, [br], ["LT"])
                            bk2, br2 = bank()
                            mms([(bk2[:, q * 128:(q + 1) * 128], AO[:, q, :], UTb[:], True, True) for q in range(4)], ["AO", "UTb"], [br2])
                            act(Eb[:], bk2[:].rearrange("p (q n) -> p q n", n=128), AF.Exp, [br2], ["Eb"])
                            tt("dve", MT[:], LT[:], CBm[:, g, :].unsqueeze(1).to_broadcast([128, 4, 128]), ALU.mult, ["LT", "CBm"], ["MT"])
                            tt("dve", CE[:], Eb[:], CT[:, g, tsl].unsqueeze(1).to_broadcast([128, 4, 128]), ALU.mult, ["Eb", "CT"], ["CE"])
                            if g % 2 == 0:
                                yb, ybr = ybank(g // 2)
                                ybanks.append((yb, ybr))
                            lst = []
                            for q in range(4):
                                h = 4 * g + q
                                o = yb[:, (g % 2) * 256 + q * 64:(g % 2) * 256 + (q + 1) * 64]
                                lst.append((o, MT[:, q, :], xdt[:, h * 64:(h + 1) * 64], True, False))
                                lst.append((o, CE[:, q, :], state_bf[:, h * 64:(h + 1) * 64], False, True))
                            mms(lst, ["MT", "CE", "xdt", "state_bf"], [ybr])
                        checkpoint(f"grp{int(pre)}", {"MT": MT, "CE": CE})
                        for b4 in range(4):
                            yb, ybr = ybanks[b4]
                            csl = slice(b4 * 512, (b4 + 1) * 512)
                            tt("dve", tmpD[:, 0:512].rearrange("p (h d) -> p h d", h=8), xs_tok[:, tti, csl].rearrange("p (h d) -> p h d", h=8),
                               rowS[:, 64 + b4 * 8:64 + b4 * 8 + 8].unsqueeze(2).to_broadcast([128, 8, 64]), ALU.mult,
                               ["xs_tok", "rowS"], ["tmpD"])
                            tt("dve", yn[:, csl], yb[:], tmpD[:, 0:512], ALU.add, [ybr, "tmpD"], ["yn"])
                            tt("dve", yn[:, csl], yn[:, csl], sz_tok[:, tti, csl], ALU.mult, ["yn", "sz_tok"], ["yn"])
                        for g in range(8):
                            act(junk[:], yn[:, g * 256:(g + 1) * 256], AF.Square, ["yn"], ["junk", "ss8"], accum_out=ss8[:, g:g + 1])
                        act(ss8[:], ss8[:], AF.Sqrt, ["ss8"], ["ss8"], bias=EPS, scale=1.0 / 256)
                        P.op("dve", lambda e: e.reciprocal(out=ss8[:], in_=ss8[:]), rl(["ss8"]), rl(["ss8"]))
                        tt("dve", yn[:, 0:2048].rearrange("p (g d) -> p g d", g=8), yn[:, 0:2048].rearrange("p (g d) -> p g d", g=8),
                           ss8[:].unsqueeze(2).to_broadcast([128, 8, 256]), ALU.mult, ["yn", "ss8"], ["yn"])
                        checkpoint(f"yn{int(pre)}", {"yn": yn[:, 0:2048]})
                        for q4 in range(4):
                            bk, br = bank()
                            trs([(bk[:, q * 128:(q + 1) * 128], yn[:, (q4 * 4 + q) * 128:(q4 * 4 + q + 1) * 128], identF[:]) for q in range(4)],
                                ["yn", "identF"], [br])
                            cp("act", ynT[:, q4 * 4:(q4 + 1) * 4, tsl], bk[:].rearrange("p (q n) -> p q n", n=128), [br], ["ynT"])
                    checkpoint(f"ynT{int(pre)}", {"ynT": ynT[:, :, 0:128]})
                    sbanks = []
                    for b4 in range(4):
                        bk, br = bank()
                        mms([(bk[:, q * 256:(q + 1) * 256], B_tok[:, tti, (2 * b4 + q) * 128:(2 * b4 + q + 1) * 128],
                              xdtd[:, (2 * b4 + q) * 256:(2 * b4 + q + 1) * 256], True, True) for q in range(2)], ["B_tok", "xdtd"], [br])
                        sbanks.append((bk, br))
                    checkpoint(f"ssdC{int(pre)}", {"xdt": xdt})
                    tt(PB, state[:].rearrange("p (h d) -> p h d", h=32), state[:].rearrange("p (h d) -> p h d", h=32),
                       sm[:, 1, :].unsqueeze(2).to_broadcast([128, 32, 64]), ALU.mult, ["state", "sm", "state_bf"], ["state"])
                    for b4 in range(4):
                        bk, br = sbanks[b4]
                        tt("dve", state[:, b4 * 512:(b4 + 1) * 512], bk[:], state[:, b4 * 512:(b4 + 1) * 512], ALU.add, ["state", br], ["state"])
                    if not pre:
                        cp("act", state_bf[:], state[:], ["state"], ["state_bf"])
                if pre:
                    return
                for eh in range(2):
                    pb = [ybank(q_) for q_ in range(4)]
                    for kh in range(2):
                        wsb, wr_ = wblock(wbs_b[kh * 1024:(kh + 1) * 1024, eh * 512:(eh + 1) * 512], ["wbs_b"])
                        for j in range(4):
                            mms([(pb[j][0][:, 0:TS], wsb[:, kc, j * 128:(j + 1) * 128], ynT[:, kh * 8 + kc, :], (kh == 0 and kc == 0),
                                  (kh == 1 and kc == 7)) for kc in range(8)], [wr_, "ynT"], [pb[j][1]])
                    wsg, wg_ = wblock(w_in_b[:, OFF_GS + eh * 512: OFF_GS + (eh + 1) * 512], ["w_in_b"])
                    for j in range(4):
                        ec = eh * 4 + j
                        bk, br = bank()
                        mms([(bk[:, 0:TS], wsg[:, dc, j * 128:(j + 1) * 128], uT[:, dc, :], dc == 0, dc == 7) for dc in range(8)],
                            [wg_, "uT"], [br])
                        act(gsig[:], bk[:, 0:TS], AF.Sigmoid, [br], ["gsig"])
                        tt("dve", gsig[:], pb[j][0][:, 0:TS], gsig[:], ALU.mult, [pb[j][1], "gsig"], ["gsig"])
                        tt("dve", combT[:, ec, :], gsig[:], gp[:, ec, :], ALU.add, ["gsig", "gp"], ["combT"])
                checkpoint(f"comb{int(pre)}", {"combT": combT})
                wo = [wblock(wout_b[:, hh * 512:(hh + 1) * 512], ["wout_b"]) for hh in range(2)]
                for tti in range(2):
                    for hh in range(2):
                        bk, br = bank()
                        mms([(bk[:], combT[:, ec, tti * 128:(tti + 1) * 128], wo[hh][0][:, ec, :], ec == 0, ec == 7) for ec in range(8)],
                            ["combT", wo[hh][1]], [br])
                        tt("dve", H[:, t0 + tti, hh * 512:(hh + 1) * 512], bk[:], H[:, t0 + tti, hh * 512:(hh + 1) * 512], ALU.add,
                           ["H", br], ["H"])

            xst = lambda tti: sz_tok[:, tti, :].bitcast(F32)
            Iacc = ynT[:].rearrange("p a b -> p (a b)").bitcast(F32)
            P.op("pool", lambda e: e.memset(state[:], 0.0), [], rl(["state"]))
            P.op("pool", lambda e: e.memset(Iacc, 0.0), [], rl(["ynT"]))
            P.op("pool", lambda e: e.memset(tailx[:], 0.0), [], rl([f"tailx{c_}" for c_ in range(32)]))
            seq = [(j, si) for j in range(n_pre_slots) for si in range(n_slabs)]

            def prep_uT(idx):
                j, si = seq[idx]
                r0 = j * T + si * TS
                for tti in range(2):
                    dma("sp", xst(tti), x_all[r0 + tti * 128: r0 + (tti + 1) * 128, :], [], ["sz_tok"])
                make_uT(xst, "sz_tok", 2)

            if seq:
                prep_uT(0)
            for idx, (j, si) in enumerate(seq):
                hk = (lambda idx=idx: prep_uT(idx + 1)) if idx + 1 < len(seq) else None
                slab(si, True, src=xst, skip_uT=True, hook=hk)
                if si == n_slabs - 1:
                    stt("dve", Iacc, state[:], rk[:, j:j + 1], Iacc, ALU.mult, ALU.add, ["state", "rk", "ynT"], ["ynT"])
            checkpoint("pre", {"state": state, "xs_tok": xs_tok, "B_tok": B_tok, "dtb": dtb})
            cp("dve", state[:], Iacc, ["ynT"], ["state"])
            cp("act", state_bf[:], state[:], ["state"], ["state_bf"])
            cp("pool", tailx[:], tailx0[:], ["tailx0"], [f"tailx{c_}" for c_ in range(32)])
            checkpoint("xchg", {"state": state})
            for si in range(n_slabs):
                slab(si, False)
            checkpoint("main", {"H": H, "ynT": ynT, "gp": gp, "combT": combT, "sz_tok": sz_tok, "CT": CT, "BT": BT})
            barrier()
            P.flush()

        if DEBUG:
            dbg_out = nc.dram_tensor("dbg_h", [T, D], F32, kind="ExternalOutput").ap()
            dma("sp", dbg_out.rearrange("(t p) d -> p t d", p=128), H[:], ["H"], [])

        with ExitStack() as sB:
            U2 = sb(sB, "U2", [128, 8, T], BF16)
            u2f = sb(sB, "u2f", [128, 8, 128], F32)
            xn2 = sb(sB, "xn2", [128, D], F32)
            st2 = sb(sB, "st2", [128, 4], F32)
            wr = sb(sB, "wr", [128, 8, 64], F32)
            wtab = sb(sB, "wtab", [128, NT, 65], F32)
            r64 = [sb(sB, f"r64_{i}", [128, 64], F32) for i in range(4)]
            r8 = [sb(sB, f"r8_{i}", [128, 8], F32) for i in range(5)]
            wguS = [sb(sB, f"wguS{i}", [128, 8, 512], BF16) for i in range(2)]
            wdF = [sb(sB, f"wdF{i}", [128, 2, D], F32) for i in range(2)]
            wdS = [sb(sB, f"wdS{i}", [128, 2, D], BF16) for i in range(2)]
            hidT = [sb(sB, f"hidT{i}", [128, 2, T], BF16) for i in range(2)]
            sg = [sb(sB, f"sg{i}", [128, 512], BF16) for i in range(2)]
            fng = sb(sB, "fng", [128, D], F32)
            g2bc = sb(sB, "g2bc", [128, D], F32)
            dma("sp", g2bc[:], g2_d, ["g2_d"], ["g2bc"])
            ob = [sb(sB, f"ob{i}", [128, D], F32) for i in range(2)]

            dma("sp", wr[:], w_router.rearrange("(c p) n -> p c n", p=128), [], ["wr"])
            dma("sp", fng[:], rows_d[:, R_FNG:R_FNG + D], [], ["fng"])
            P.op("pool", lambda e: e.memset(wtab[:], 1.0), [], rl(["wtab"]))
            for ti in range(NT):
                xin = H[:, ti, :]
                act(xn2[:], xin, AF.Square, ["H"], ["xn2", "st2"], accum_out=st2[:, 0:1])
                act(st2[:, 1:2], st2[:, 0:1], AF.Sqrt, ["st2"], ["st2"], bias=EPS, scale=1.0 / D)
                P.op("dve", lambda e: e.reciprocal(out=st2[:, 2:3], in_=st2[:, 1:2]), rl(["st2"]), rl(["st2"]))
                ts("dve", xn2[:], xin, st2[:, 2:3], None, ALU.mult, None, ["H", "st2"], ["xn2"])
                for half in range(2):
                    bk, br = bank()
                    trs([(bk[:, j * 128:(j + 1) * 128], xn2[:, (half * 4 + j) * 128:(half * 4 + j + 1) * 128], identF[:]) for j in range(4)],
                        ["xn2", "identF"], [br])
                    for j in range(4):
                        dc = half * 4 + j
                        P.op("act", lambda e, dc=dc, j=j, bk=bk: e.activation(out=u2f[:, dc, :], in_=bk[:, j * 128:(j + 1) * 128], func=AF.Identity,
                                                                         bias=mcol[:, 16 + dc:17 + dc], scale=gsc[:, 8 + dc:9 + dc]),
                             rl([br, "gsc", "mcol"]), rl(["u2f"]))
                cp("pool", U2[:, :, ti * 128:(ti + 1) * 128], u2f[:], ["u2f"], ["U2"])
                bk, br = bank()
                mms([(bk[:, 0:64], u2f[:, dc, :], wr[:, dc, :], dc == 0, dc == 7) for dc in range(8)], ["u2f", "wr"], [br])
                s_, ch, t1, t2 = r64
                m1, m2, gs, g8, gm = r8
                act(s_[:], bk[:, 0:64], AF.Sigmoid, [br], ["r0"])
                tt("dve", ch[:], s_[:], rowS[:, 96:160], ALU.add, ["r0", "rowS"], ["r1"])
                v3 = lambda a: a[:].rearrange("p (g e) -> p g e", g=8)
                P.op("dve", lambda e: e.tensor_reduce(out=m1[:], in_=v3(ch), axis=AX.X, op=ALU.max), rl(["r1"]), rl(["q0"]))
                tt("dve", v3(t1), v3(ch), m1[:].unsqueeze(2).to_broadcast([128, 8, 8]), ALU.is_equal, ["r1", "q0"], ["r2"])
                stt("dve", t1[:], t1[:], -BIG, ch[:], ALU.mult, ALU.add, ["r2", "r1"], ["r2"])
                P.op("dve", lambda e: e.tensor_reduce(out=m2[:], in_=v3(t1), axis=AX.X, op=ALU.max), rl(["r2"]), rl(["q1"]))
                tt("dve", gs[:], m1[:], m2[:], ALU.add, ["q0", "q1"], ["q2"])
                P.op("dve", lambda e: e.max(out=g8[:], in_=gs[:]), rl(["q2"]), rl(["q3"]))
                ts("dve", gm[:], gs[:], g8[:, 3:4], None, ALU.is_ge, None, ["q2", "q3"], ["q4"])
                tt("dve", v3(t1), v3(ch), gm[:].unsqueeze(2).to_broadcast([128, 8, 8]), ALU.mult, ["r1", "q4"], ["r2"])
                ts("dve", gm[:], gm[:], -1.0, BIG, ALU.add, ALU.mult, ["q4"], ["q4"])
                tt("dve", v3(t1), v3(t1), gm[:].unsqueeze(2).to_broadcast([128, 8, 8]), ALU.add, ["r2", "q4"], ["r2"])
                P.op("dve", lambda e: e.max(out=g8[:], in_=t1[:]), rl(["r2"]), rl(["q3"]))
                ts("dve", t2[:], t1[:], g8[:, 7:8], None, ALU.is_ge, None, ["r2", "q3"], ["r3"])
                tt("dve", t2[:], t2[:], s_[:], ALU.mult, ["r3", "r0"], ["r3"])
                P.op("dve", lambda e: e.tensor_reduce(out=m1[:, 0:1], in_=t2[:], axis=AX.X, op=ALU.add), rl(["r3"]), rl(["q0"]))
                P.op("dve", lambda e: e.reciprocal(out=m1[:, 0:1], in_=m1[:, 0:1]), rl(["q0"]), rl(["q0"]))
                ts("dve", wtab[:, ti, 0:64], t2[:], m1[:, 0:1], 2.5, ALU.mult, ALU.mult, ["r3", "q0"], ["wtab"])

            ne = n_experts if do_moe else 0
            for e_ in range(ne):
                b = e_ % 2
                P.dma("pool", lambda e, e_=e_, b=b: e.dma_start(out=wguS[b][:], in_=wgu[e_], max_dma_last_dim=4096), rl([]), rl([f"wguS{b}"]))
                dma("sp", wdF[b][:], wdn[e_], [], [f"wdF{b}"])
                tt("pool", wdS[b][:], wdF[b][:], g2bc[:].unsqueeze(1).to_broadcast([128, 2, D]), ALU.mult, [f"wdF{b}", "g2bc"], [f"wdS{b}"])
                k = 0
                for jc in range(2):
                    for sl in range(4):
                        bg, bgr = bank()
                        bu, bur = bank()
                        rhs = lambda dc: U2[:, dc, sl * 512:(sl + 1) * 512]
                        mms([(bg[:], wguS[b][:, dc, jc * 128:(jc + 1) * 128], rhs(dc), dc == 0, dc == 7) for dc in range(8)],
                            [f"wguS{b}", "U2"], [bgr])
                        mms([(bu[:], wguS[b][:, dc, 256 + jc * 128:256 + (jc + 1) * 128], rhs(dc), dc == 0, dc == 7) for dc in range(8)],
                            [f"wguS{b}", "U2"], [bur])
                        act(sg[k % 2][:], bg[:], AF.Silu, [bgr], [f"sg{k % 2}"])
                        tt("dve", hidT[b][:, jc, sl * 512:(sl + 1) * 512], bu[:], sg[k % 2][:], ALU.mult, [bur, f"sg{k % 2}"], [f"hidT{b}"])
                        k += 1
                for ti in range(NT):
                    for hh in range(2):
                        bk, br = ybank()
                        mms([(bk[:], hidT[b][:, jc, ti * 128:(ti + 1) * 128], wdS[b][:, jc, hh * 512:(hh + 1) * 512], jc == 0, jc == 1)
                             for jc in range(2)], [f"hidT{b}", f"wdS{b}"], [br])
                        stt("dve", H[:, ti, hh * 512:(hh + 1) * 512], bk[:], wtab[:, ti, e_:e_ + 1], H[:, ti, hh * 512:(hh + 1) * 512],
                            ALU.mult, ALU.add, [br, "wtab", f"Ht{ti}"], [f"Ht{ti}"])
            for ti in range(NT):
                b = ti % 2
                act(xn2[:], H[:, ti, :], AF.Square, ["H", f"Ht{ti}"], ["xn2", "st2"], accum_out=st2[:, 0:1])
                act(st2[:, 1:2], st2[:, 0:1], AF.Sqrt, ["st2"], ["st2"], bias=EPS, scale=1.0 / D)
                P.op("dve", lambda e: e.reciprocal(out=st2[:, 2:3], in_=st2[:, 1:2]), rl(["st2"]), rl(["st2"]))
                stt("dve", ob[b][:], H[:, ti, :], st2[:, 2:3], fng[:], ALU.mult, ALU.mult, ["H", f"Ht{ti}", "st2", "fng"], [f"ob{b}"])
                dma("sp", out_d[ti * 128:(ti + 1) * 128, :], ob[b][:], [f"ob{b}"], [])
            P.wait_all("sp", P.all_events())
            P.flush()
    except _Stop:
        return nc
    top.close()
    _top0.close()
    return nc


def _colform(v):
    v = np.asarray(v, np.float32).reshape(-1)
    return np.ascontiguousarray(v.reshape(-1, 128).T)


def _prep_inputs(x, c, w_ada, b_ada, norm_mix_g, w_in, conv_w, conv_b, dt_bias, A_log, D_skip, ssd_norm_g,
                 pool_w, pool_scale, w_br_ssd, w_br_pool, w_out, norm_ffn_g, w_router, router_bias,
                 we_gate, we_up, we_down, ws_gate, ws_up, ws_down, final_norm_g):
    f = lambda a: np.ascontiguousarray(np.asarray(a, np.float32))
    x2 = f(x).reshape(NCORES * T, D)
    cols = np.zeros((128, NCOL), np.float32)
    cols[:, C_NMG:C_NMG + 8] = _colform(norm_mix_g)
    cols[:, C_NFG:C_NFG + 8] = _colform(norm_ffn_g)
    cols[:, C_PSC:C_PSC + 8] = _colform(pool_scale)
    cols[:, C_SNG:C_SNG + 16] = _colform(ssd_norm_g)
    cols[:, C_C:C_C + 8] = _colform(c)
    cols[:, C_CB:C_CB + 32] = _colform(conv_b)
    cw = f(conv_w).reshape(4, 32, 128)
    cols[:, C_CW:C_CW + 128] = cw.transpose(2, 1, 0).reshape(128, 128)
    rows = np.zeros((1, NROW), np.float32)
    rows[0, R_BADA:R_BADA + 6144] = f(b_ada).reshape(-1)
    rows[0, R_DTB:R_DTB + 32] = f(dt_bias).reshape(-1)
    rows[0, R_ALOG:R_ALOG + 32] = f(A_log).reshape(-1)
    rows[0, R_DSK:R_DSK + 32] = f(D_skip).reshape(-1)
    rows[0, R_RB:R_RB + 64] = f(router_bias).reshape(-1)
    rows[0, R_FNG:R_FNG + 1024] = f(final_norm_g).reshape(-1)
    rows = np.ascontiguousarray(np.broadcast_to(rows, (128, NROW)))
    wg = f(we_gate)[0].reshape(64, 8, 128, 256)
    wu = f(we_up)[0].reshape(64, 8, 128, 256)
    wgu = np.empty((65, 128, 8, 512), np.float32)
    wgu[:64, :, :, :256] = wg.transpose(0, 2, 1, 3)
    wgu[:64, :, :, 256:] = wu.transpose(0, 2, 1, 3)
    wgu[64, :, :, :256] = f(ws_gate)[0].reshape(8, 128, 256).transpose(1, 0, 2)
    wgu[64, :, :, 256:] = f(ws_up)[0].reshape(8, 128, 256).transpose(1, 0, 2)
    wdn = np.empty((65, 128, 2, 1024), np.float32)
    wdn[:64] = f(we_down)[0].reshape(64, 2, 128, 1024).transpose(0, 2, 1, 3)
    wdn[64] = f(ws_down)[0].reshape(2, 128, 1024).transpose(1, 0, 2)
    shared = {
        "cols": cols, "rows": rows, "w_ada": f(w_ada)[0], "w_in": f(w_in)[0],
        "pool_w": f(pool_w)[0].reshape(1024, 256), "w_br_ssd": f(w_br_ssd)[0], "w_br_pool": f(w_br_pool)[0],
        "w_out": f(w_out)[0], "w_router": f(w_router)[0], "wgu": wgu, "wdn": wdn,
    }
    in_maps = []
    for k in range(NCORES):
        m = dict(shared)
        m["x_own"] = x2[k * T:(k + 1) * T]
        m["x_halo"] = x2[k * T - 128:k * T] if k > 0 else np.zeros((128, D), np.float32)
        m["rk"] = np.ascontiguousarray(np.broadcast_to((np.arange(8) == k - 1).astype(np.float32)[None, :], (128, 8)))
        m["x_all"] = x2[:(NCORES - 1) * T]
        m["nf"] = np.full((128, 1), 1.0 if k > 0 else 0.0, np.float32)
        m["posrow"] = np.ascontiguousarray(np.broadcast_to((k * T + 1 + np.arange(16)).astype(np.float32)[None, :], (128, 16)))
        in_maps.append(m)
    return in_maps


_NC_CACHE = {}


def kernel(**inputs):
    in_maps = _prep_inputs(**inputs)
    if "nc" not in _NC_CACHE:
        _NC_CACHE["nc"] = build_nc()
    res = run_bass_kernel_spmd(_NC_CACHE["nc"], in_maps, core_ids=list(range(NCORES)))
    out = np.concatenate([np.asarray(r["out"], np.float32) for r in res.results], axis=0)
    return out.reshape(1, NCORES * T, D)
```

```python
import numpy as np
import concourse.bass as bass
import concourse.mybir as mybir

F32 = mybir.dt.float32
BF16 = mybir.dt.bfloat16
AF = mybir.ActivationFunctionType
ALU = mybir.AluOpType
AX = mybir.AxisListType


class Reg:
    __slots__ = ("name", "w", "r")

    ALL = []

    def __init__(self, name):
        self.name = name
        self.w = None
        self.r = []
        Reg.ALL.append(self)


class Prog:
    ENGS = ("pe", "act", "dve", "pool", "sp")

    def __init__(self, nc, ndma_sems=8, same_engine_sync=True):
        self.nc = nc
        self.q = {e: [] for e in self.ENGS}
        self.cnt = {e: 0 for e in self.ENGS}
        self.known = {e: {} for e in self.ENGS}
        self.sems = {}
        self.same = same_engine_sync
        self.ndma = ndma_sems
        self.dma_rr = {e: 0 for e in self.ENGS}
        self.dma_val = {}
        self.stack = None

    def _semkeys(self):
        keys = list(self.ENGS) + ["cc"]
        for e in ("sp", "pool", "act"):
            for i in range(self.ndma):
                keys.append(f"d_{e}_{i}")
        return keys

    def _need(self, eng, events):
        mx = {}
        for ev in events:
            if ev is None:
                continue
            k, v = ev
            if k == eng and not self.same:
                continue
            if k == "cc" and eng != "pool":
                continue
            if v > mx.get(k, 0):
                mx[k] = v
        out = []
        kn = self.known[eng]
        for k, v in mx.items():
            if kn.get(k, 0) >= v:
                continue
            kn[k] = v
            out.append((k, v))
        return out

    def _deps(self, reads, writes):
        evs = []
        for r in reads:
            evs.append(r.w)
        for w in writes:
            evs.append(w.w)
            evs.extend(w.r)
        return evs

    def _commit(self, ev, reads, writes):
        for r in reads:
            r.r.append(ev)
        for w in writes:
            w.w = ev
            w.r = []

    def op(self, eng, fn, reads=(), writes=(), extra=()):
        evs = self._deps(reads, writes) + list(extra)
        waits = self._need(eng, evs)
        self.cnt[eng] += 1
        ev = (eng, self.cnt[eng])
        self.q[eng].append((waits, fn, (eng, 1)))
        self._commit(ev, reads, writes)
        return ev

    def dma(self, eng, fn, reads=(), writes=(), extra=()):
        i = self.dma_rr[eng]
        self.dma_rr[eng] = (i + 1) % self.ndma
        key = f"d_{eng}_{i}"
        prev = self.dma_val.get(key, 0)
        evs = self._deps(reads, writes) + list(extra)
        if prev:
            evs.append((key, prev))
        waits = self._need(eng, evs)
        val = prev + 16
        self.dma_val[key] = val
        ev = (key, val)
        self.q[eng].append((waits, fn, (key, 16)))
        self._commit(ev, reads, writes)
        return ev

    def cc(self, fn, reads=(), writes=(), extra=(), inc=1):
        eng = "pool"
        evs = self._deps(reads, writes) + list(extra)
        waits = self._need(eng, evs)
        val = self.dma_val.get("cc", 0) + inc
        self.dma_val["cc"] = val
        ev = ("cc", val)
        self.q[eng].append((waits, fn, ("cc", inc)))
        self._commit(ev, reads, writes)
        return ev

    def wait_all(self, eng, events):
        waits = self._need(eng, list(events))
        self.q[eng].append((waits, None, None))

    def all_events(self):
        evs = [(e, c) for e, c in self.cnt.items() if c]
        evs += [(k, v) for k, v in self.dma_val.items()]
        return evs

    def begin(self, stack):
        self.gen = 0
        self.semh = {k: stack.enter_context(self.nc.semaphore(k)) for k in self._semkeys()}

    def rebase(self, stack):
        self.gen += 1
        self.semh = {k: stack.enter_context(self.nc.semaphore(f"{k}_g{self.gen}")) for k in self._semkeys()}
        self.cnt = {e: 0 for e in self.ENGS}
        self.known = {e: {} for e in self.ENGS}
        self.dma_rr = {e: 0 for e in self.ENGS}
        self.dma_val = {}
        for r in Reg.ALL:
            r.w = None
            r.r = []

    def flush(self):
        nc = self.nc
        sems = self.semh
        q = self.q

        def run(engh, lst):
            for waits, fn, inc in lst:
                for k, v in waits:
                    engh.wait_ge(sems[k], v)
                if fn is None:
                    continue
                ins = fn(engh)
                ins.then_inc(sems[inc[0]], inc[1])

        with nc.Block() as block:
            @block.tensor
            def _(e):
                run(e, q["pe"])

            @block.scalar
            def _(e):
                run(e, q["act"])

            @block.vector
            def _(e):
                run(e, q["dve"])

            @block.gpsimd
            def _(e):
                run(e, q["pool"])

            @block.sync
            def _(e):
                run(e, q["sp"])
        self.q = {e: [] for e in self.ENGS}


from contextlib import ExitStack
from concourse.bass_utils import run_bass_kernel_spmd

NCORES = 8
T = 2048
NT = 16
TS = 256
NSL = T // TS
D = 1024
EPS = 1e-6
OFF_Z, OFF_X, OFF_B, OFF_C, OFF_DT, OFF_P, OFF_GS, OFF_GP = 0, 2048, 4096, 5120, 6144, 6176, 7200, 8224
DIN = 9248
PB = "dve"
NCOL = 208
C_NMG, C_NFG, C_PSC, C_SNG, C_C, C_CB, C_CW = 0, 8, 16, 24, 40, 48, 80
R_BADA, R_DTB, R_ALOG, R_DSK, R_RB, R_FNG = 0, 6144, 6176, 6208, 6240, 6304
NROW = 7328
BIG = 1.0e4
DEBUG = False
import os
DBGV = int(os.environ.get("DBGV", "0"))


class _Stop(Exception):
    pass


def build_nc(n_experts=65, do_moe=True, stop_after=None, n_slabs=NSL, n_pre_slots=NCORES - 1):
    nc = bass.Bass("TRN2", target_bir_lowering=False)
    di = lambda n, s, dt=F32: nc.dram_tensor(n, s, dt, kind="ExternalInput").ap()
    x_own = di("x_own", [T, D]); x_halo = di("x_halo", [128, D]); x_all = di("x_all", [(NCORES - 1) * T, D])
    rk_d = di("rk", [128, 8]); nf_d = di("nf", [128, 1]); pos_d = di("posrow", [128, 16])
    cols_d = di("cols", [128, NCOL]); rows_d = di("rows", [128, NROW])
    w_ada = di("w_ada", [D, 6 * D]); w_in = di("w_in", [D, DIN])
    pool_w = di("pool_w", [1024, 256]); w_br_ssd = di("w_br_ssd", [2048, D])
    w_br_pool = di("w_br_pool", [D, D]); w_out = di("w_out", [D, D])
    w_router = di("w_router", [D, 64])
    NEW = 65 if stop_after is None else 1
    wgu = di("wgu", [NEW, 128, 8, 512]); wdn = di("wdn", [NEW, 128, 2, 1024])
    out_d = nc.dram_tensor("out", [T, D], F32, kind="ExternalOutput").ap()
    w_in_b = nc.dram_tensor("w_in_b", [D, DIN], BF16).ap()
    wbp_b = nc.dram_tensor("wbp_b", [D, D], BF16).ap()
    wbs_b = nc.dram_tensor("wbs_b", [2048, D], BF16).ap()
    wout_b = nc.dram_tensor("wout_b", [D, D], BF16).ap()
    bounce = nc.dram_tensor("bounce", [128, 2080], F32).ap()
    gathered = nc.dram_tensor("gathered", [1024, 2080], F32).ap()
    dbg = {}

    Reg.ALL.clear()
    P = Prog(nc)
    regs = {}
    _top0 = ExitStack()
    P.begin(_top0)

    ALIAS = {"yn": ["bigA", "bigB"], "big": ["bigA", "bigB"], "junk": ["gsig"], "xn": ["bigB"]}

    def R(name):
        if name not in regs:
            regs[name] = Reg(name)
        return regs[name]

    top = ExitStack()

    def sb(st, name, shape, dt):
        return st.enter_context(nc.sbuf_tensor("s_" + name, shape, dt))

    def rl(x):
        out = []
        for n in x:
            if isinstance(n, str):
                for m in ALIAS.get(n, [n]):
                    out.append(R(m))
            else:
                out.append(n)
        return out

    def tt(eng, out, a, b, op, rd, wr):
        return P.op(eng, lambda e: e.tensor_tensor(out=out, in0=a, in1=b, op=op), rl(rd), rl(wr))

    def ts(eng, out, a, s1, s2, op0, op1, rd, wr):
        if op1 is None:
            return P.op(eng, lambda e: e.tensor_single_scalar(out=out, in_=a, scalar=s1, op=op0), rl(rd), rl(wr))
        return P.op(eng, lambda e: e.tensor_scalar(out=out, in0=a, scalar1=s1, scalar2=s2, op0=op0, op1=op1), rl(rd), rl(wr))

    def stt(eng, out, a, sc, b, op0, op1, rd, wr):
        return P.op(eng, lambda e: e.scalar_tensor_tensor(out=out, in0=a, scalar=sc, in1=b, op0=op0, op1=op1), rl(rd), rl(wr))

    def act(out, in_, func, rd, wr, bias=0.0, scale=1.0, accum_out=None):
        if accum_out is None:
            return P.op("act", lambda e: e.activation(out=out, in_=in_, func=func, bias=bias, scale=scale), rl(rd), rl(wr))
        return P.op("act", lambda e: e.activation(out=out, in_=in_, func=func, bias=bias, scale=scale, accum_out=accum_out), rl(rd), rl(wr))

    def cp(eng, out, in_, rd, wr):
        if eng == "act":
            return P.op("act", lambda e: e.copy(out=out, in_=in_), rl(rd), rl(wr))
        return P.op(eng, lambda e: e.tensor_copy(out=out, in_=in_), rl(rd), rl(wr))

    def mms(lst, rd, wr):
        def f(e):
            for (o, l, r, s0, s1) in lst:
                ins = e.matmul(o, lhsT=l, rhs=r, start=s0, stop=s1)
            return ins
        return P.op("pe", f, rl(rd), rl(wr))

    def trs(lst, rd, wr):
        def f(e):
            for (o, i, idn) in lst:
                ins = e.transpose(out=o, in_=i, identity=idn)
            return ins
        return P.op("pe", f, rl(rd), rl(wr))

    def dma(q, out, in_, rd, wr):
        if q == "pool":
            return P.dma(q, lambda e: e.dma_start(out=out, in_=in_, max_dma_last_dim=4096), rl(rd), rl(wr))
        return P.dma(q, lambda e: e.dma_start(out=out, in_=in_), rl(rd), rl(wr))

    def barrier():
        evs = P.all_events()
        for e in ("pe", "act", "dve", "pool", "sp"):
            P.wait_all(e, evs)

    def checkpoint(name, tensors):
        if stop_after != name:
            return
        for tn, t in tensors.items():
            a = t if hasattr(t, "ap") and not hasattr(t, "__enter__") and type(t).__name__ == "AP" else t[:]
            shp = list(a.shape)
            d = nc.dram_tensor("dbg_" + tn, shp, a.dtype, kind="ExternalOutput").ap()
            dma("sp", d, a, [tn], [])
        P.wait_all("sp", P.all_events())
        P.flush()
        raise _Stop()

    banks = [top.enter_context(nc.psum_tensor(f"bank{i}", [128, 512], F32)) for i in range(8)]
    bstate = {"i": 0, "y": 0}

    def bank():
        n = bstate.get("n", 4)
        i = bstate["i"] % n
        bstate["i"] = (i + 1) % n
        return banks[i], R(f"bank{i}")

    def ybank(i=None):
        if i is None:
            i = bstate["y"]
            bstate["y"] = (i + 1) % 4
        return banks[4 + i], R(f"bank{4 + i}")

    H = sb(top, "H", [128, NT, D], F32)
    state = sb(top, "state", [128, 2048], F32)
    state_bf = sb(top, "state_bf", [128, 2048], BF16)
    identF = sb(top, "identF", [128, 128], F32)
    SLf = sb(top, "SLf", [128, 128], F32)
    ONf = sb(top, "ONf", [128, 128], F32)
    SLb = sb(top, "SLb", [128, 128], BF16)
    ONb = sb(top, "ONb", [128, 128], BF16)
    UTb = sb(top, "UTb", [128, 128], BF16)
    MKf = sb(top, "MKf", [128, 128], F32)
    cols = sb(top, "cols", [128, NCOL], F32)
    rowS = sb(top, "rowS", [128, 160], F32)
    rk = sb(top, "rk_s", [128, 8], F32)
    nf = sb(top, "nf_s", [128, 1], F32)
    mcol = sb(top, "mcol", [128, 32], F32)
    gsc = sb(top, "gsc", [128, 16], F32)
    g2_d = nc.dram_tensor("g2_d", [128, D], F32).ap()

    def mask(tn, tensor, cmp, sgn=1):
        P.op("pool", lambda e: e.memset(tensor[:], 1.0), [], [R(tn)])
        P.op("pool", lambda e: e.affine_select(out=tensor[:], in_=tensor[:], pattern=[[-sgn, 128]], compare_op=cmp,
                                               fill=0.0, base=0, channel_multiplier=sgn), [R(tn)], [R(tn)])
    mask("identF", identF, ALU.is_equal)
    mask("SLf", SLf, ALU.is_gt)
    mask("MKf", MKf, ALU.is_ge, -1)
    P.op("pool", lambda e: e.memset(ONf[:], 1.0), [], [R("ONf")])
    cp("pool", SLb[:], SLf[:], ["SLf"], ["SLb"])
    cp("pool", ONb[:], ONf[:], ["ONf"], ["ONb"])
    cp("pool", UTb[:], MKf[:], ["MKf"], ["UTb"])

    dma("sp", cols[:], cols_d, [], ["cols"])
    dma("sp", rowS[:], rows_d[:, R_DTB:R_DTB + 160], [], ["rowS"])
    dma("sp", rk[:], rk_d, [], ["rk"])
    dma("sp", nf[:], nf_d, [], ["nf"])
    dma("sp", H[:], x_own.rearrange("(t p) d -> p t d", p=128), [], ["H"])
    _wv_o = w_in_b.rearrange("a b -> (a b)").rearrange("(r c) -> r c", c=2048)
    _wv_i = w_in.rearrange("a b -> (a b)").rearrange("(r c) -> r c", c=2048)
    for _q in range(8):
        dma("pool", _wv_o[_q * 578:(_q + 1) * 578, :], _wv_i[_q * 578:(_q + 1) * 578, :], [], ["w_in_b"])
    dma("pool", wbp_b, w_br_pool, [], ["wbp_b"])

    try:
        with ExitStack() as s0:
            bcm = sb(s0, "bcm", [128, 6 * D], F32)
            cact = sb(s0, "cact", [128, 8], F32)
            CBC = sb(s0, "CBC", [128, 8, 128], F32)
            wa = [sb(s0, f"wa{i}", [128, 8, 512], F32) for i in range(2)]
            ba = [sb(s0, f"ba{i}", [128, 512], F32) for i in range(2)]
            stg = [sb(s0, f"stg{i}", [128, 4, D], F32) for i in range(2)]
            stb = [sb(s0, f"stb{i}", [128, 4, D], BF16) for i in range(2)]
            act(cact[:], cols[:, C_C:C_C + 8], AF.Silu, ["cols"], ["cact"])
            cp("dve", CBC[:], cact[:].unsqueeze(2).to_broadcast([128, 8, 128]), ["cact"], ["CBC"])
            for ns in range(12):
                b = ns % 2
                dma("sp", wa[b][:], w_ada[:, ns * 512:(ns + 1) * 512].rearrange("(c p) n -> p c n", p=128), [], [f"wa{b}"])
                dma("sp", ba[b][:], rows_d[:, R_BADA + ns * 512:R_BADA + (ns + 1) * 512], [], [f"ba{b}"])
                bk, br = bank()
                mms([(bk[:], CBC[:, dc, :], wa[b][:, dc, :], dc == 0, dc == 7) for dc in range(8)], ["CBC", f"wa{b}"], [br])
                tt("dve", bcm[:, ns * 512:(ns + 1) * 512], bk[:], ba[b][:], ALU.add, [br, f"ba{b}"], ["bcm"])
            for vi, v in enumerate((0, 1, 3, 4)):
                for q in range(2):
                    bk, br = bank()
                    trs([(bk[:, j * 128:(j + 1) * 128], bcm[:, v * D + (q * 4 + j) * 128: v * D + (q * 4 + j + 1) * 128], identF[:])
                         for j in range(4)], ["bcm", "identF"], [br])
                    cp("dve", mcol[:, vi * 8 + q * 4: vi * 8 + q * 4 + 4],
                       bk[:].rearrange("p (j n) -> p j n", n=128)[:, :, 0], [br], ["mcol"])
            stt("dve", gsc[:, 0:8], mcol[:, 8:16], 1.0, cols[:, C_NMG:C_NMG + 8], ALU.add, ALU.mult, ["mcol", "cols"], ["gsc"])
            stt("dve", gsc[:, 8:16], mcol[:, 24:32], 1.0, cols[:, C_NFG:C_NFG + 8], ALU.add, ALU.mult, ["mcol", "cols"], ["gsc"])
            dma("sp", g2_d, bcm[:, 5 * D:6 * D], ["bcm"], ["g2_d"])
            act(rowS[:, 32:64], rowS[:, 32:64], AF.Exp, ["rowS"], ["rowS"])
            ts("dve", rowS[:, 32:64], rowS[:, 32:64], -1.0, None, ALU.mult, None, ["rowS"], ["rowS"])
            k = 0
            for half in range(2):
                b = k % 2; k += 1
                dma("sp", stg[b][:], w_out[half * 512:(half + 1) * 512, :].rearrange("(c p) n -> p c n", p=128), [], [f"stg{b}"])
                tt("dve", stb[b][:], stg[b][:], bcm[:, 2 * D:3 * D].unsqueeze(1).to_broadcast([128, 4, D]), ALU.mult,
                   [f"stg{b}", "bcm"], [f"stb{b}"])
                dma("sp", wout_b[half * 512:(half + 1) * 512, :].rearrange("(c p) n -> p c n", p=128), stb[b][:], [f"stb{b}"], ["wout_b"])
            for q in range(4):
                b = k % 2; k += 1
                dma("sp", stg[b][:], w_br_ssd[q * 512:(q + 1) * 512, :].rearrange("(c p) n -> p c n", p=128), [], [f"stg{b}"])
                for j in range(4):
                    ts("pool", stb[b][:, j, :], stg[b][:, j, :], cols[:, C_SNG + q * 4 + j:C_SNG + q * 4 + j + 1], None, ALU.mult, None,
                       [f"stg{b}", "cols"], [f"stb{b}"])
                dma("sp", wbs_b[q * 512:(q + 1) * 512, :].rearrange("(c p) n -> p c n", p=128), stb[b][:], [f"stb{b}"], ["wbs_b"])
            checkpoint("p0", {"mcol": mcol, "gsc": gsc, "rowS": rowS, "bcm": bcm})
            barrier()
            P.flush()

        with ExitStack() as sA:
            WS = [sb(sA, f"WS{i}", [128, 8, 512], BF16) for i in range(3)]
            wsi = {"i": 0}
            wdt = sb(sA, "wdt", [128, 8, 32], BF16)
            poolw = sb(sA, "poolw", [128, 8, 256], BF16)
            pcorr = sb(sA, "pcorr", [128, 4, 16], F32)
            posr = sb(sA, "posr", [128, 16], F32)
            tailx = sb(sA, "tailx", [128, 32, 3], F32)
            tailx0 = sb(sA, "tailx0", [128, 32, 3], F32)
            ptail = sb(sA, "ptail", [128, 8, 15], F32)
            xpw = [sb(sA, f"xpw{i}", [128, 15 + TS], F32) for i in range(2)]
            logD = sb(sA, "logD", [128, 32], F32)
            logDp = [logD, sb(sA, "logD1", [128, 32], F32)]
            ldi = {"i": 0}
            uT = sb(sA, "uT", [128, 8, TS], BF16)
            xn_late = True
            st1 = sb(sA, "st1", [128, 4], F32)
            pooledT = sb(sA, "pooledT", [128, 8, TS], BF16)
            mixedT = sb(sA, "mixedT", [128, 8, TS], BF16)
            gp = sb(sA, "gp", [128, 8, TS], BF16)
            pS = [sb(sA, f"pS{i}", [128, 15 + TS], F32) for i in range(2)]
            gsig = sb(sA, "gsig", [128, TS], F32)
            NSET = 3
            xcs = [sb(sA, f"xc{i}", [128, 3 + TS], F32) for i in range(NSET)]
            caccs = [sb(sA, f"cacc{i}", [128, TS], F32) for i in range(NSET)]
            big = sb(sA, "big", [128, 2080], F32)
            xn = big[:, 1024:2048]
            tmpD = sb(sA, "tmpD", [128, 512], F32)
            Bfs = [sb(sA, f"Bf{i}", [128, TS], F32) for i in range(2)]
            xs_tok = sb(sA, "xs_tok", [128, 2, 2048], BF16)
            B_tok = sb(sA, "B_tok", [128, 2, 1024], BF16)
            BT = sb(sA, "BT", [128, 8, TS], BF16)
            CT = sb(sA, "CT", [128, 8, TS], BF16)
            dtb = sb(sA, "dtb", [128, 2, 32], F32)
            ab = sb(sA, "ab", [128, 2, 32], F32)
            ab16 = sb(sA, "ab16", [128, 2, 32], BF16)
            sm = sb(sA, "sm", [128, 4, 32], F32)
            sz_tok = sb(sA, "sz_tok", [128, 2, 2048], BF16)
            xdt = sb(sA, "xdt", [128, 2048], BF16)
            xdtd = sb(sA, "xdtd", [128, 2048], BF16)
            AL = sb(sA, "AL", [128, 4, 128], BF16)
            AO = sb(sA, "AO", [128, 4, 128], BF16)
            LT = sb(sA, "LT", [128, 4, 128], BF16)
            Eb = sb(sA, "Eb", [128, 4, 128], BF16)
            CBm = sb(sA, "CBm", [128, 8, 128], BF16)
            MT = sb(sA, "MT", [128, 4, 128], BF16)
            CE = sb(sA, "CE", [128, 4, 128], BF16)
            yn = big
            junk = gsig
            ss8 = sb(sA, "ss8", [128, 8], F32)
            ynT = sb(sA, "ynT", [128, 16, TS], BF16)
            combT = sb(sA, "combT", [128, 8, TS], BF16)
            stS = big

            def wblock(src, rd):
                i = wsi["i"]; wsi["i"] = (i + 1) % 3
                dma("sp", WS[i][:], src.rearrange("(c p) n -> p c n", p=128), rd, [f"WS{i}"])
                return WS[i], f"WS{i}"

            dma("pool", wdt[:], w_in[:, OFF_DT:OFF_DT + 32].rearrange("(c p) n -> p c n", p=128), [], ["wdt"])
            dma("pool", poolw[:], pool_w.rearrange("(c p) n -> p c n", p=128), [], ["poolw"])
            dma("sp", posr[:], pos_d, [], ["posr"])
            for g, w in enumerate((2, 4, 8, 16)):
                ts("dve", pcorr[:, g, :], posr[:], float(w), None, ALU.min, None, ["posr"], ["pcorr"])
                P.op("dve", lambda e, g=g: e.reciprocal(out=pcorr[:, g, :], in_=pcorr[:, g, :]), rl(["pcorr"]), rl(["pcorr"]))
                ts("dve", pcorr[:, g, :], pcorr[:, g, :], float(w), None, ALU.mult, None, ["pcorr"], ["pcorr"])

            def make_uT(src_ap, rdname, n_tiles, col0=0):
                for tti in range(n_tiles):
                    xin = src_ap(tti)
                    act(junk_big[:], xin, AF.Square, [rdname], ["xn", "st1"], accum_out=st1[:, 0:1])
                    act(st1[:, 1:2], st1[:, 0:1], AF.Sqrt, ["st1"], ["st1"], bias=EPS, scale=1.0 / D)
                    P.op("dve", lambda e: e.reciprocal(out=st1[:, 2:3], in_=st1[:, 1:2]), rl(["st1"]), rl(["st1"]))
                    ts("dve", xn[:], xin, st1[:, 2:3], None, ALU.mult, None, [rdname, "st1"], ["xn"])
                    for half in range(2):
                        bk, br = bank()
                        trs([(bk[:, j * 128:(j + 1) * 128], xn[:, (half * 4 + j) * 128:(half * 4 + j + 1) * 128], identF[:])
                             for j in range(4)], ["xn", "identF"], [br])
                        for j in range(4):
                            dc = half * 4 + j
                            ts("dve" if half else "pool_no", uT[:, dc, col0 + tti * 128: col0 + (tti + 1) * 128], bk[:, j * 128:(j + 1) * 128],
                               gsc[:, dc:dc + 1], mcol[:, dc:dc + 1], ALU.mult, ALU.add, [br, "gsc", "mcol"], ["uT"])

            junk_big = xn

            _ts_orig = ts

            def ts(eng, out, a, s1, s2, op0, op1, rd, wr):
                if eng == "pool_no":
                    return P.op("act", lambda e: e.activation(out=out, in_=a, func=AF.Identity, bias=s2, scale=s1), rl(rd), rl(wr))
                return _ts_orig(eng, out, a, s1, s2, op0, op1, rd, wr)

            xh = xsA = big
            dma("sp", big[:, 0:D], x_halo, [], ["bigA"])
            make_uT(lambda tti: big[:, 0:D], "bigA", 1)
            for blk in range(10):
                c0 = OFF_X + blk * 512 if blk < 8 else OFF_P + (blk - 8) * 512
                wsb, wr_ = wblock(w_in_b[:, c0:c0 + 512], ["w_in_b"])
                for j in range(4):
                    bk, br = bank()
                    mms([(bk[:, 0:128], wsb[:, dc, j * 128:(j + 1) * 128], uT[:, dc, 0:128], dc == 0, dc == 7) for dc in range(8)],
                        [wr_, "uT"], [br])
                    if blk < 8:
                        c = blk * 4 + j
                        ts("dve", tailx0[:, c, :], bk[:, 125:128], nf[:, 0:1], None, ALU.mult, None, [br, "nf"], ["tailx0"])
                    else:
                        c = (blk - 8) * 4 + j
                        ts("dve", ptail[:, c, :], bk[:, 113:128], nf[:, 0:1], None, ALU.mult, None, [br, "nf"], ["ptail"])

            checkpoint("halo", {"tailx0": tailx0, "xpb": ptail, "uT": uT[:, :, 0:128]})
            def slab(si, pre, src=None, skip_uT=False, hook=None):
                t0 = 2 * si
                bstate["n"] = 8
                if skip_uT:
                    pass
                elif src is None:
                    make_uT(lambda tti: H[:, t0 + tti, :], "H", 2)
                else:
                    make_uT(src, "sz_tok", 2)
                if not pre:
                    for blk in range(2):
                        wsb, wr_ = wblock(w_in_b[:, OFF_P + blk * 512: OFF_P + (blk + 1) * 512], ["w_in_b"])
                        for j in range(4):
                            c = blk * 4 + j
                            g = c // 2
                            w = 2 << g
                            bk, br = bank()
                            mms([(bk[:, 0:TS], wsb[:, dc, j * 128:(j + 1) * 128], uT[:, dc, :], dc == 0, dc == 7) for dc in range(8)],
                                [wr_, "uT"], [br])
                            xw = xpw[c % 2]; xwn = f"xpw{c % 2}"
                            cp("pool", xw[:, 0:15], ptail[:, c, :], ["ptail"], [xwn])
                            cp("act", xw[:, 15:15 + TS], bk[:, 0:TS], [br], [xwn])
                            cp("pool", ptail[:, c, :], xw[:, TS:TS + 15], [xwn], ["ptail"])
                            src = xw[:]
                            lo = 0
                            step = 1
                            k = 0
                            while step < w:
                                dst = pS[k % 2]
                                nlo = lo + step
                                tt("dve", dst[:, nlo:15 + TS], src[:, nlo:15 + TS], src[:, nlo - step:15 + TS - step], ALU.add,
                                   [xwn, f"pS{(k + 1) % 2}"], [f"pS{k % 2}"])
                                src = dst[:]
                                lo = nlo
                                step *= 2
                                k += 1
                            sname = f"pS{(k - 1) % 2}"
                            if si == 0:
                                tt("dve", src[:, 15:31], src[:, 15:31], pcorr[:, g, :], ALU.mult, [sname, "pcorr"], [sname])
                            stt("dve", pooledT[:, c, :], src[:, 15:15 + TS], 1.0 / w, xw[:, 15:15 + TS], ALU.mult, ALU.subtract,
                                [sname, xwn], ["pooledT"])
                            pass
                    for g in range(4):
                        for dch in range(2):
                            bk, br = bank()
                            mms([(bk[:, 0:TS], poolw[:, 2 * g + kc, dch * 128:(dch + 1) * 128], pooledT[:, 2 * g + kc, :], kc == 0, kc == 1)
                                 for kc in range(2)], ["poolw", "pooledT"], [br])
                            ts("dve", mixedT[:, 2 * g + dch, :], bk[:, 0:TS], cols[:, C_PSC + 2 * g + dch:C_PSC + 2 * g + dch + 1], None,
                               ALU.mult, None, [br, "cols"], ["mixedT"])
                    for blk in range(2):
                        wsb, wr_ = wblock(wbp_b[:, blk * 512:(blk + 1) * 512], ["wbp_b"])
                        wsg, wg_ = wblock(w_in_b[:, OFF_GP + blk * 512: OFF_GP + (blk + 1) * 512], ["w_in_b"])
                        for j in range(4):
                            ec = blk * 4 + j
                            bk, br = bank()
                            mms([(bk[:, 0:TS], wsg[:, dc, j * 128:(j + 1) * 128], uT[:, dc, :], dc == 0, dc == 7) for dc in range(8)],
                                [wg_, "uT"], [br])
                            act(gsig[:], bk[:, 0:TS], AF.Sigmoid, [br], ["gsig"])
                            bk2, br2 = bank()
                            mms([(bk2[:, 0:TS], wsb[:, dc, j * 128:(j + 1) * 128], mixedT[:, dc, :], dc == 0, dc == 7) for dc in range(8)],
                                [wr_, "mixedT"], [br2])
                            tt("dve", gp[:, ec, :], bk2[:, 0:TS], gsig[:], ALU.mult, [br2, "gsig"], ["gp"])
                checkpoint(f"ut{int(pre)}", {"uT": uT})
                checkpoint(f"pool{int(pre)}", {"gp": gp})
                nblk = 6 if pre else 8
                pend = []

                def tick():
                    for p_ in pend:
                        p_[0] -= 1
                    while pend and pend[0][0] <= 0:
                        pend.pop(0)[1]()

                for blk in range(nblk):
                    wsb, wr_ = wblock(w_in_b[:, OFF_X + blk * 512: OFF_X + (blk + 1) * 512], ["w_in_b"])
                    for j in range(4):
                        c = blk * 4 + j
                        bk, br = bank()
                        mms([(bk[:, 0:TS], wsb[:, dc, j * 128:(j + 1) * 128], uT[:, dc, :], dc == 0, dc == 7) for dc in range(8)],
                            [wr_, "uT"], [br])
                        xc = xcs[c % NSET]; cacc = caccs[c % NSET]; xcn = f"xc{c % NSET}"; can = f"cacc{c % NSET}"
                        Bf = Bfs[c % 2]; Bfn = f"Bf{c % 2}"
                        cp("pool", xc[:, 0:3], tailx[:, c, :], [f"tailx{c}"], [xcn])
                        cp("act", xc[:, 3:3 + TS], bk[:, 0:TS], [br], [xcn])
                        cp("pool", tailx[:, c, :], xc[:, TS:TS + 3], [xcn], [f"tailx{c}"])
                        cw = lambda k_: cols[:, C_CW + c * 4 + k_: C_CW + c * 4 + k_ + 1]
                        P.op("act", lambda e, cacc=cacc, bk=bk, s_=cw(3): e.activation(out=cacc[:], in_=bk[:, 0:TS], func=AF.Copy, scale=s_),
                             rl([br, "cols"]), rl([can]))
                        for k_ in (0, 1, 2):
                            stt("dve", cacc[:], xc[:, k_:k_ + TS], cw(k_), cacc[:], ALU.mult, ALU.add, [xcn, "cols", can], [can])
                        bcol = cols[:, C_CB + c:C_CB + c + 1]
                        if c < 16:
                            hb = (c // 4) % 2; hbn = "bigA" if hb == 0 else "bigB"
                            pend.append([2, (lambda c=c, hb=hb, hbn=hbn, cacc=cacc, can=can, bcol=bcol:
                                             act(big[:, hb * 1024 + (c % 4) * TS: hb * 1024 + (c % 4 + 1) * TS], cacc[:], AF.Silu, [can, "cols"], [hbn], bias=bcol))])
                            if c % 4 == 3:
                                def st2x(cg=c // 4, hb=hb, hbn=hbn):
                                    for tti in range(2):
                                        bk2, br2 = bank()
                                        trs([(bk2[:, q * 128:(q + 1) * 128], big[:, hb * 1024 + q * TS + tti * 128: hb * 1024 + q * TS + (tti + 1) * 128], identF[:])
                                             for q in range(4)], [hbn, "identF"], [br2])
                                        cp("dve", xs_tok[:, tti, cg * 512:(cg + 1) * 512], bk2[:], [br2], ["xs_tok"])
                                pend.append([3, st2x])
                        elif c < 24:
                            g = c - 16
                            pend.append([2, (lambda Bf=Bf, Bfn=Bfn, cacc=cacc, can=can, bcol=bcol:
                                             act(Bf[:], cacc[:], AF.Silu, [can, "cols"], [Bfn], bias=bcol))])
                            def st2b(g=g, Bf=Bf, Bfn=Bfn):
                                cp("dve", BT[:, g, :], Bf[:], [Bfn], ["BT"])
                                bk2, br2 = bank()
                                trs([(bk2[:, tti * 128:(tti + 1) * 128], Bf[:, tti * 128:(tti + 1) * 128], identF[:]) for tti in range(2)],
                                    [Bfn, "identF"], [br2])
                                cp("dve", B_tok[:, :, g * 128:(g + 1) * 128], bk2[:, 0:256].rearrange("p (t n) -> p t n", n=128), [br2], ["B_tok"])
                            pend.append([2, st2b])
                        else:
                            g = c - 24
                            pend.append([2, (lambda g=g, cacc=cacc, can=can, bcol=bcol:
                                             act(CT[:, g, :], cacc[:], AF.Silu, [can, "cols"], ["CT"], bias=bcol))])
                        tick()
                while pend:
                    pend.pop(0)[1]()
                checkpoint(f"conv{int(pre)}", {"xs_tok": xs_tok, "B_tok": B_tok})
                for tti in range(2):
                    bk, br = bank()
                    mms([(bk[:, 0:32], uT[:, dc, tti * 128:(tti + 1) * 128], wdt[:, dc, :], dc == 0, dc == 7) for dc in range(8)],
                        ["uT", "wdt"], [br])
                    tt("dve", dtb[:, tti, :], bk[:, 0:32], rowS[:, 0:32], ALU.add, [br, "rowS"], ["dtb"])
                    act(dtb[:, tti, :], dtb[:, tti, :], AF.Exp, ["dtb"], ["dtb"])
                    act(dtb[:, tti, :], dtb[:, tti, :], AF.Ln, ["dtb"], ["dtb"], bias=1.0)
                    tt("dve", ab[:, tti, :], dtb[:, tti, :], rowS[:, 32:64], ALU.mult, ["dtb", "rowS"], ["ab"])
                    cp("dve", ab16[:, tti, :], ab[:, tti, :], ["ab"], ["ab16"])
                checkpoint(f"dt{int(pre)}", {"dtb": dtb, "ab": ab})
                if not pre:
                    for zc in range(4):
                        wsb, wr_ = wblock(w_in_b[:, OFF_Z + zc * 512: OFF_Z + (zc + 1) * 512], ["w_in_b"])
                        for tti in range(2):
                            bk, br = bank()
                            mms([(bk[:], uT[:, dc, tti * 128:(tti + 1) * 128], wsb[:, dc, :], dc == 0, dc == 7) for dc in range(8)],
                                ["uT", wr_], [br])
                            act(sz_tok[:, tti, zc * 512:(zc + 1) * 512], bk[:], AF.Silu, [br], ["sz_tok"])
                checkpoint(f"z{int(pre)}", {"sz_tok": sz_tok})
                if hook is not None:
                    hook()
                if not pre:
                    bstate["n"] = 4
                    bstate["i"] = 0
                for tti in range(2):
                    tsl = slice(tti * 128, (tti + 1) * 128)
                    bk, br = bank()
                    mms([(bk[:, 0:32], SLf[:], ab[:, tti, :], True, True)], ["SLf", "ab"], [br])
                    bkB, brB = bank()
                    mms([(bkB[:, 0:32], ONf[:], ab[:, tti, :], True, True)], ["ONf", "ab"], [brB])
                    act(sm[:, 0, :], bk[:, 0:32], AF.Exp, [br], ["sm"])
                    act(sm[:, 1, :], bkB[:, 0:32], AF.Exp, [brB], ["sm"])
                    checkpoint(f"ssdA{int(pre)}", {"sm": sm[:, 0:2, :]})
                    checkpoint(f"ssdA2{int(pre)}", {"logD": logD})
                    tt("dve", xdt[:].rearrange("p (h d) -> p h d", h=32), xs_tok[:, tti, :].rearrange("p (h d) -> p h d", h=32),
                       dtb[:, tti, :].unsqueeze(2).to_broadcast([128, 32, 64]), ALU.mult, ["xs_tok", "dtb"], ["xdt"])
                    checkpoint(f"ssdA3{int(pre)}", {"xdt": xdt})
                    tt(PB, xdtd[:].rearrange("p (h d) -> p h d", h=32), xdt[:].rearrange("p (h d) -> p h d", h=32),
                       sm[:, 0, :].unsqueeze(2).to_broadcast([128, 32, 64]), ALU.mult, ["xdt", "sm"], ["xdtd"])
                    checkpoint(f"ssdB{int(pre)}", {"xdt": xdt, "xdtd": xdtd})
                    if not pre:
                        for gh in range(2):
                            bk, br = bank()
                            mms([(bk[:, q * 128:(q + 1) * 128], BT[:, gh * 4 + q, tsl], CT[:, gh * 4 + q, tsl], True, True) for q in range(4)],
                                ["BT", "CT"], [br])
                            tt("dve", CBm[:, gh * 4:(gh + 1) * 4, :], bk[:].rearrange("p (q n) -> p q n", n=128),
                               MKf[:].unsqueeze(1).to_broadcast([128, 4, 128]), ALU.mult, [br, "MKf"], ["CBm"])
                        checkpoint(f"cb{int(pre)}", {"CBm": CBm})
                        ybanks = []
                        for g in range(8):
                            a4 = ab16[:, tti, 4 * g:4 * g + 4].unsqueeze(2).to_broadcast([128, 4, 128])
                            tt("dve", AL[:], SLb[:].unsqueeze(1).to_broadcast([128, 4, 128]), a4, ALU.mult, ["SLb", "ab16"], ["AL"])
                            tt("dve", AO[:], ONb[:].unsqueeze(1).to_broadcast([128, 4, 128]), a4, ALU.mult, ["ONb", "ab16"], ["AO"])
                            bk, br = bank()
                            mms([(bk[:, q * 128:(q + 1) * 128], AL[:, q, :], UTb[:], True, True) for q in range(4)], ["AL", "UTb"], [br])
                            act(LT[:], bk[:].rearrange("p (q n) -> p q n", n=128), AF.Exp, [br], ["LT"])
                            bk2, br2 = bank()
                            mms([(bk2[:, q * 128:(q + 1) * 128], AO[:, q, :], UTb[:], True, True) for q in range(4)], ["AO", "UTb"], [br2])
                            act(Eb[:], bk2[:].rearrange("p (q n) -> p q n", n=128), AF.Exp, [br2], ["Eb"])
                            tt("dve", MT[:], LT[:], CBm[:, g, :].unsqueeze(1).to_broadcast([128, 4, 128]), ALU.mult, ["LT", "CBm"], ["MT"])
                            tt("dve", CE[:], Eb[:], CT[:, g, tsl].unsqueeze(1).to_broadcast([128, 4, 128]), ALU.mult, ["Eb", "CT"], ["CE"])
                            if g % 2 == 0:
                                yb, ybr = ybank(g // 2)
                                ybanks.append((yb, ybr))
                            lst = []
                            for q in range(4):
                                h = 4 * g + q
                                o = yb[:, (g % 2) * 256 + q * 64:(g % 2) * 256 + (q + 1) * 64]
                                lst.append((o, MT[:, q, :], xdt[:, h * 64:(h + 1) * 64], True, False))
                                lst.append((o, CE[:, q, :], state_bf[:, h * 64:(h + 1) * 64], False, True))
                            mms(lst, ["MT", "CE", "xdt", "state_bf"], [ybr])
                        checkpoint(f"grp{int(pre)}", {"MT": MT, "CE": CE})
                        for b4 in range(4):
                            yb, ybr = ybanks[b4]
                            csl = slice(b4 * 512, (b4 + 1) * 512)
                            tt("dve", tmpD[:, 0:512].rearrange("p (h d) -> p h d", h=8), xs_tok[:, tti, csl].rearrange("p (h d) -> p h d", h=8),
                               rowS[:, 64 + b4 * 8:64 + b4 * 8 + 8].unsqueeze(2).to_broadcast([128, 8, 64]), ALU.mult,
                               ["xs_tok", "rowS"], ["tmpD"])
                            tt("dve", yn[:, csl], yb[:], tmpD[:, 0:512], ALU.add, [ybr, "tmpD"], ["yn"])
                            tt("dve", yn[:, csl], yn[:, csl], sz_tok[:, tti, csl], ALU.mult, ["yn", "sz_tok"], ["yn"])
                        for g in range(8):
                            act(junk[:], yn[:, g * 256:(g + 1) * 256], AF.Square, ["yn"], ["junk", "ss8"], accum_out=ss8[:, g:g + 1])
                        act(ss8[:], ss8[:], AF.Sqrt, ["ss8"], ["ss8"], bias=EPS, scale=1.0 / 256)
                        P.op("dve", lambda e: e.reciprocal(out=ss8[:], in_=ss8[:]), rl(["ss8"]), rl(["ss8"]))
                        tt("dve", yn[:, 0:2048].rearrange("p (g d) -> p g d", g=8), yn[:, 0:2048].rearrange("p (g d) -> p g d", g=8),
                           ss8[:].unsqueeze(2).to_broadcast([128, 8, 256]), ALU.mult, ["yn", "ss8"], ["yn"])
                        checkpoint(f"yn{int(pre)}", {"yn": yn[:, 0:2048]})
                        for q4 in range(4):
                            bk, br = bank()
                            trs([(bk[:, q * 128:(q + 1) * 128], yn[:, (q4 * 4 + q) * 128:(q4 * 4 + q + 1) * 128], identF[:]) for q in range(4)],
                                ["yn", "identF"], [br])
                            cp("act", ynT[:, q4 * 4:(q4 + 1) * 4, tsl], bk[:].rearrange("p (q n) -> p q n", n=128), [br], ["ynT"])
                    checkpoint(f"ynT{int(pre)}", {"ynT": ynT[:, :, 0:128]})
                    sbanks = []
                    for b4 in range(4):
                        bk, br = bank()
                        mms([(bk[:, q * 256:(q + 1) * 256], B_tok[:, tti, (2 * b4 + q) * 128:(2 * b4 + q + 1) * 128],
                              xdtd[:, (2 * b4 + q) * 256:(2 * b4 + q + 1) * 256], True, True) for q in range(2)], ["B_tok", "xdtd"], [br])
                        sbanks.append((bk, br))
                    checkpoint(f"ssdC{int(pre)}", {"xdt": xdt})
                    tt(PB, state[:].rearrange("p (h d) -> p h d", h=32), state[:].rearrange("p (h d) -> p h d", h=32),
                       sm[:, 1, :].unsqueeze(2).to_broadcast([128, 32, 64]), ALU.mult, ["state", "sm", "state_bf"], ["state"])
                    for b4 in range(4):
                        bk, br = sbanks[b4]
                        tt("dve", state[:, b4 * 512:(b4 + 1) * 512], bk[:], state[:, b4 * 512:(b4 + 1) * 512], ALU.add, ["state", br], ["state"])
                    if not pre:
                        cp("act", state_bf[:], state[:], ["state"], ["state_bf"])
                if pre:
                    return
                for eh in range(2):
                    pb = [ybank(q_) for q_ in range(4)]
                    for kh in range(2):
                        wsb, wr_ = wblock(wbs_b[kh * 1024:(kh + 1) * 1024, eh * 512:(eh + 1) * 512], ["wbs_b"])
                        for j in range(4):
                            mms([(pb[j][0][:, 0:TS], wsb[:, kc, j * 128:(j + 1) * 128], ynT[:, kh * 8 + kc, :], (kh == 0 and kc == 0),
                                  (kh == 1 and kc == 7)) for kc in range(8)], [wr_, "ynT"], [pb[j][1]])
                    wsg, wg_ = wblock(w_in_b[:, OFF_GS + eh * 512: OFF_GS + (eh + 1) * 512], ["w_in_b"])
                    for j in range(4):
                        ec = eh * 4 + j
                        bk, br = bank()
                        mms([(bk[:, 0:TS], wsg[:, dc, j * 128:(j + 1) * 128], uT[:, dc, :], dc == 0, dc == 7) for dc in range(8)],
                            [wg_, "uT"], [br])
                        act(gsig[:], bk[:, 0:TS], AF.Sigmoid, [br], ["gsig"])
                        tt("dve", gsig[:], pb[j][0][:, 0:TS], gsig[:], ALU.mult, [pb[j][1], "gsig"], ["gsig"])
                        tt("dve", combT[:, ec, :], gsig[:], gp[:, ec, :], ALU.add, ["gsig", "gp"], ["combT"])
                checkpoint(f"comb{int(pre)}", {"combT": combT})
                wo = [wblock(wout_b[:, hh * 512:(hh + 1) * 512], ["wout_b"]) for hh in range(2)]
                for tti in range(2):
                    for hh in range(2):
                        bk, br = bank()
                        mms([(bk[:], combT[:, ec, tti * 128:(tti + 1) * 128], wo[hh][0][:, ec, :], ec == 0, ec == 7) for ec in range(8)],
                            ["combT", wo[hh][1]], [br])
                        tt("dve", H[:, t0 + tti, hh * 512:(hh + 1) * 512], bk[:], H[:, t0 + tti, hh * 512:(hh + 1) * 512], ALU.add,
                           ["H", br], ["H"])

            xst = lambda tti: sz_tok[:, tti, :].bitcast(F32)
            Iacc = ynT[:].rearrange("p a b -> p (a b)").bitcast(F32)
            P.op("pool", lambda e: e.memset(state[:], 0.0), [], rl(["state"]))
            P.op("pool", lambda e: e.memset(Iacc, 0.0), [], rl(["ynT"]))
            P.op("pool", lambda e: e.memset(tailx[:], 0.0), [], rl([f"tailx{c_}" for c_ in range(32)]))
            seq = [(j, si) for j in range(n_pre_slots) for si in range(n_slabs)]

            def prep_uT(idx):
                j, si = seq[idx]
                r0 = j * T + si * TS
                for tti in range(2):
                    dma("sp", xst(tti), x_all[r0 + tti * 128: r0 + (tti + 1) * 128, :], [], ["sz_tok"])
                make_uT(xst, "sz_tok", 2)

            if seq:
                prep_uT(0)
            for idx, (j, si) in enumerate(seq):
                hk = (lambda idx=idx: prep_uT(idx + 1)) if idx + 1 < len(seq) else None
                slab(si, True, src=xst, skip_uT=True, hook=hk)
                if si == n_slabs - 1:
                    stt("dve", Iacc, state[:], rk[:, j:j + 1], Iacc, ALU.mult, ALU.add, ["state", "rk", "ynT"], ["ynT"])
            checkpoint("pre", {"state": state, "xs_tok": xs_tok, "B_tok": B_tok, "dtb": dtb})
            cp("dve", state[:], Iacc, ["ynT"], ["state"])
            cp("act", state_bf[:], state[:], ["state"], ["state_bf"])
            cp("pool", tailx[:], tailx0[:], ["tailx0"], [f"tailx{c_}" for c_ in range(32)])
            checkpoint("xchg", {"state": state})
            for si in range(n_slabs):
                slab(si, False)
            checkpoint("main", {"H": H, "ynT": ynT, "gp": gp, "combT": combT, "sz_tok": sz_tok, "CT": CT, "BT": BT})
            barrier()
            P.flush()

        if DEBUG:
            dbg_out = nc.dram_tensor("dbg_h", [T, D], F32, kind="ExternalOutput").ap()
            dma("sp", dbg_out.rearrange("(t p) d -> p t d", p=128), H[:], ["H"], [])

        with ExitStack() as sB:
            U2 = sb(sB, "U2", [128, 8, T], BF16)
            u2f = sb(sB, "u2f", [128, 8, 128], F32)
            xn2 = sb(sB, "xn2", [128, D], F32)
            st2 = sb(sB, "st2", [128, 4], F32)
            wr = sb(sB, "wr", [128, 8, 64], F32)
            wtab = sb(sB, "wtab", [128, NT, 65], F32)
            r64 = [sb(sB, f"r64_{i}", [128, 64], F32) for i in range(4)]
            r8 = [sb(sB, f"r8_{i}", [128, 8], F32) for i in range(5)]
            wguS = [sb(sB, f"wguS{i}", [128, 8, 512], BF16) for i in range(2)]
            wdF = [sb(sB, f"wdF{i}", [128, 2, D], F32) for i in range(2)]
            wdS = [sb(sB, f"wdS{i}", [128, 2, D], BF16) for i in range(2)]
            hidT = [sb(sB, f"hidT{i}", [128, 2, T], BF16) for i in range(2)]
            sg = [sb(sB, f"sg{i}", [128, 512], BF16) for i in range(2)]
            fng = sb(sB, "fng", [128, D], F32)
            g2bc = sb(sB, "g2bc", [128, D], F32)
            dma("sp", g2bc[:], g2_d, ["g2_d"], ["g2bc"])
            ob = [sb(sB, f"ob{i}", [128, D], F32) for i in range(2)]

            dma("sp", wr[:], w_router.rearrange("(c p) n -> p c n", p=128), [], ["wr"])
            dma("sp", fng[:], rows_d[:, R_FNG:R_FNG + D], [], ["fng"])
            P.op("pool", lambda e: e.memset(wtab[:], 1.0), [], rl(["wtab"]))
            for ti in range(NT):
                xin = H[:, ti, :]
                act(xn2[:], xin, AF.Square, ["H"], ["xn2", "st2"], accum_out=st2[:, 0:1])
                act(st2[:, 1:2], st2[:, 0:1], AF.Sqrt, ["st2"], ["st2"], bias=EPS, scale=1.0 / D)
                P.op("dve", lambda e: e.reciprocal(out=st2[:, 2:3], in_=st2[:, 1:2]), rl(["st2"]), rl(["st2"]))
                ts("dve", xn2[:], xin, st2[:, 2:3], None, ALU.mult, None, ["H", "st2"], ["xn2"])
                for half in range(2):
                    bk, br = bank()
                    trs([(bk[:, j * 128:(j + 1) * 128], xn2[:, (half * 4 + j) * 128:(half * 4 + j + 1) * 128], identF[:]) for j in range(4)],
                        ["xn2", "identF"], [br])
                    for j in range(4):
                        dc = half * 4 + j
                        P.op("act", lambda e, dc=dc, j=j, bk=bk: e.activation(out=u2f[:, dc, :], in_=bk[:, j * 128:(j + 1) * 128], func=AF.Identity,
                                                                         bias=mcol[:, 16 + dc:17 + dc], scale=gsc[:, 8 + dc:9 + dc]),
                             rl([br, "gsc", "mcol"]), rl(["u2f"]))
                cp("pool", U2[:, :, ti * 128:(ti + 1) * 128], u2f[:], ["u2f"], ["U2"])
                bk, br = bank()
                mms([(bk[:, 0:64], u2f[:, dc, :], wr[:, dc, :], dc == 0, dc == 7) for dc in range(8)], ["u2f", "wr"], [br])
                s_, ch, t1, t2 = r64
                m1, m2, gs, g8, gm = r8
                act(s_[:], bk[:, 0:64], AF.Sigmoid, [br], ["r0"])
                tt("dve", ch[:], s_[:], rowS[:, 96:160], ALU.add, ["r0", "rowS"], ["r1"])
                v3 = lambda a: a[:].rearrange("p (g e) -> p g e", g=8)
                P.op("dve", lambda e: e.tensor_reduce(out=m1[:], in_=v3(ch), axis=AX.X, op=ALU.max), rl(["r1"]), rl(["q0"]))
                tt("dve", v3(t1), v3(ch), m1[:].unsqueeze(2).to_broadcast([128, 8, 8]), ALU.is_equal, ["r1", "q0"], ["r2"])
                stt("dve", t1[:], t1[:], -BIG, ch[:], ALU.mult, ALU.add, ["r2", "r1"], ["r2"])
                P.op("dve", lambda e: e.tensor_reduce(out=m2[:], in_=v3(t1), axis=AX.X, op=ALU.max), rl(["r2"]), rl(["q1"]))
                tt("dve", gs[:], m1[:], m2[:], ALU.add, ["q0", "q1"], ["q2"])
                P.op("dve", lambda e: e.max(out=g8[:], in_=gs[:]), rl(["q2"]), rl(["q3"]))
                ts("dve", gm[:], gs[:], g8[:, 3:4], None, ALU.is_ge, None, ["q2", "q3"], ["q4"])
                tt("dve", v3(t1), v3(ch), gm[:].unsqueeze(2).to_broadcast([128, 8, 8]), ALU.mult, ["r1", "q4"], ["r2"])
                ts("dve", gm[:], gm[:], -1.0, BIG, ALU.add, ALU.mult, ["q4"], ["q4"])
                tt("dve", v3(t1), v3(t1), gm[:].unsqueeze(2).to_broadcast([128, 8, 8]), ALU.add, ["r2", "q4"], ["r2"])
                P.op("dve", lambda e: e.max(out=g8[:], in_=t1[:]), rl(["r2"]), rl(["q3"]))
                ts("dve", t2[:], t1[:], g8[:, 7:8], None, ALU.is_ge, None, ["r2", "q3"], ["r3"])
                tt("dve", t2[:], t2[:], s_[:], ALU.mult, ["r3", "r0"], ["r3"])
                P.op("dve", lambda e: e.tensor_reduce(out=m1[:, 0:1], in_=t2[:], axis=AX.X, op=ALU.add), rl(["r3"]), rl(["q0"]))
                P.op("dve", lambda e: e.reciprocal(out=m1[:, 0:1], in_=m1[:, 0:1]), rl(["q0"]), rl(["q0"]))
                ts("dve", wtab[:, ti, 0:64], t2[:], m1[:, 0:1], 2.5, ALU.mult, ALU.mult, ["r3", "q0"], ["wtab"])

            ne = n_experts if do_moe else 0
            for e_ in range(ne):
                b = e_ % 2
                P.dma("pool", lambda e, e_=e_, b=b: e.dma_start(out=wguS[b][:], in_=wgu[e_], max_dma_last_dim=4096), rl([]), rl([f"wguS{b}"]))
                dma("sp", wdF[b][:], wdn[e_], [], [f"wdF{b}"])
                tt("pool", wdS[b][:], wdF[b][:], g2bc[:].unsqueeze(1).to_broadcast([128, 2, D]), ALU.mult, [f"wdF{b}", "g2bc"], [f"wdS{b}"])
                k = 0
                for jc in range(2):
                    for sl in range(4):
                        bg, bgr = bank()
                        bu, bur = bank()
                        rhs = lambda dc: U2[:, dc, sl * 512:(sl + 1) * 512]
                        mms([(bg[:], wguS[b][:, dc, jc * 128:(jc + 1) * 128], rhs(dc), dc == 0, dc == 7) for dc in range(8)],
                            [f"wguS{b}", "U2"], [bgr])
                        mms([(bu[:], wguS[b][:, dc, 256 + jc * 128:256 + (jc + 1) * 128], rhs(dc), dc == 0, dc == 7) for dc in range(8)],
                            [f"wguS{b}", "U2"], [bur])
                        act(sg[k % 2][:], bg[:], AF.Silu, [bgr], [f"sg{k % 2}"])
                        tt("dve", hidT[b][:, jc, sl * 512:(sl + 1) * 512], bu[:], sg[k % 2][:], ALU.mult, [bur, f"sg{k % 2}"], [f"hidT{b}"])
                        k += 1
                for ti in range(NT):
                    for hh in range(2):
                        bk, br = ybank()
                        mms([(bk[:], hidT[b][:, jc, ti * 128:(ti + 1) * 128], wdS[b][:, jc, hh * 512:(hh + 1) * 512], jc == 0, jc == 1)
                             for jc in range(2)], [f"hidT{b}", f"wdS{b}"], [br])
                        stt("dve", H[:, ti, hh * 512:(hh + 1) * 512], bk[:], wtab[:, ti, e_:e_ + 1], H[:, ti, hh * 512:(hh + 1) * 512],
                            ALU.mult, ALU.add, [br, "wtab", f"Ht{ti}"], [f"Ht{ti}"])
            for ti in range(NT):
                b = ti % 2
                act(xn2[:], H[:, ti, :], AF.Square, ["H", f"Ht{ti}"], ["xn2", "st2"], accum_out=st2[:, 0:1])
                act(st2[:, 1:2], st2[:, 0:1], AF.Sqrt, ["st2"], ["st2"], bias=EPS, scale=1.0 / D)
                P.op("dve", lambda e: e.reciprocal(out=st2[:, 2:3], in_=st2[:, 1:2]), rl(["st2"]), rl(["st2"]))
                stt("dve", ob[b][:], H[:, ti, :], st2[:, 2:3], fng[:], ALU.mult, ALU.mult, ["H", f"Ht{ti}", "st2", "fng"], [f"ob{b}"])
                dma("sp", out_d[ti * 128:(ti + 1) * 128, :], ob[b][:], [f"ob{b}"], [])
            P.wait_all("sp", P.all_events())
            P.flush()
    except _Stop:
        return nc
    top.close()
    _top0.close()
    return nc


def _colform(v):
    v = np.asarray(v, np.float32).reshape(-1)
    return np.ascontiguousarray(v.reshape(-1, 128).T)


def _prep_inputs(x, c, w_ada, b_ada, norm_mix_g, w_in, conv_w, conv_b, dt_bias, A_log, D_skip, ssd_norm_g,
                 pool_w, pool_scale, w_br_ssd, w_br_pool, w_out, norm_ffn_g, w_router, router_bias,
                 we_gate, we_up, we_down, ws_gate, ws_up, ws_down, final_norm_g):
    f = lambda a: np.ascontiguousarray(np.asarray(a, np.float32))
    x2 = f(x).reshape(NCORES * T, D)
    cols = np.zeros((128, NCOL), np.float32)
    cols[:, C_NMG:C_NMG + 8] = _colform(norm_mix_g)
    cols[:, C_NFG:C_NFG + 8] = _colform(norm_ffn_g)
    cols[:, C_PSC:C_PSC + 8] = _colform(pool_scale)
    cols[:, C_SNG:C_SNG + 16] = _colform(ssd_norm_g)
    cols[:, C_C:C_C + 8] = _colform(c)
    cols[:, C_CB:C_CB + 32] = _colform(conv_b)
    cw = f(conv_w).reshape(4, 32, 128)
    cols[:, C_CW:C_CW + 128] = cw.transpose(2, 1, 0).reshape(128, 128)
    rows = np.zeros((1, NROW), np.float32)
    rows[0, R_BADA:R_BADA + 6144] = f(b_ada).reshape(-1)
    rows[0, R_DTB:R_DTB + 32] = f(dt_bias).reshape(-1)
    rows[0, R_ALOG:R_ALOG + 32] = f(A_log).reshape(-1)
    rows[0, R_DSK:R_DSK + 32] = f(D_skip).reshape(-1)
    rows[0, R_RB:R_RB + 64] = f(router_bias).reshape(-1)
    rows[0, R_FNG:R_FNG + 1024] = f(final_norm_g).reshape(-1)
    rows = np.ascontiguousarray(np.broadcast_to(rows, (128, NROW)))
    wg = f(we_gate)[0].reshape(64, 8, 128, 256)
    wu = f(we_up)[0].reshape(64, 8, 128, 256)
    wgu = np.empty((65, 128, 8, 512), np.float32)
    wgu[:64, :, :, :256] = wg.transpose(0, 2, 1, 3)
    wgu[:64, :, :, 256:] = wu.transpose(0, 2, 1, 3)
    wgu[64, :, :, :256] = f(ws_gate)[0].reshape(8, 128, 256).transpose(1, 0, 2)
    wgu[64, :, :, 256:] = f(ws_up)[0].reshape(8, 128, 256).transpose(1, 0, 2)
    wdn = np.empty((65, 128, 2, 1024), np.float32)
    wdn[:64] = f(we_down)[0].reshape(64, 2, 128, 1024).transpose(0, 2, 1, 3)
    wdn[64] = f(ws_down)[0].reshape(2, 128, 1024).transpose(1, 0, 2)
    shared = {
        "cols": cols, "rows": rows, "w_ada": f(w_ada)[0], "w_in": f(w_in)[0],
        "pool_w": f(pool_w)[0].reshape(1024, 256), "w_br_ssd": f(w_br_ssd)[0], "w_br_pool": f(w_br_pool)[0],
        "w_out": f(w_out)[0], "w_router": f(w_router)[0], "wgu": wgu, "wdn": wdn,
    }
    in_maps = []
    for k in range(NCORES):
        m = dict(shared)
        m["x_own"] = x2[k * T:(k + 1) * T]
        m["x_halo"] = x2[k * T - 128:k * T] if k > 0 else np.zeros((128, D), np.float32)
        m["rk"] = np.ascontiguousarray(np.broadcast_to((np.arange(8) == k - 1).astype(np.float32)[None, :], (128, 8)))
        m["x_all"] = x2[:(NCORES - 1) * T]
        m["nf"] = np.full((128, 1), 1.0 if k > 0 else 0.0, np.float32)
        m["posrow"] = np.ascontiguousarray(np.broadcast_to((k * T + 1 + np.arange(16)).astype(np.float32)[None, :], (128, 16)))
        in_maps.append(m)
    return in_maps


_NC_CACHE = {}


def kernel(**inputs):
    in_maps = _prep_inputs(**inputs)
    if "nc" not in _NC_CACHE:
        _NC_CACHE["nc"] = build_nc()
    res = run_bass_kernel_spmd(_NC_CACHE["nc"], in_maps, core_ids=list(range(NCORES)))
    out = np.concatenate([np.asarray(r["out"], np.float32) for r in res.results], axis=0)
    return out.reshape(1, NCORES * T, D)
```

```python
import numpy as np
import concourse.bass as bass
import concourse.mybir as mybir

F32 = mybir.dt.float32
BF16 = mybir.dt.bfloat16
AF = mybir.ActivationFunctionType
ALU = mybir.AluOpType
AX = mybir.AxisListType


class Reg:
    __slots__ = ("name", "w", "r")

    ALL = []

    def __init__(self, name):
        self.name = name
        self.w = None
        self.r = []
        Reg.ALL.append(self)


class Prog:
    ENGS = ("pe", "act", "dve", "pool", "sp")

    def __init__(self, nc, ndma_sems=8, same_engine_sync=True):
        self.nc = nc
        self.q = {e: [] for e in self.ENGS}
        self.cnt = {e: 0 for e in self.ENGS}
        self.known = {e: {} for e in self.ENGS}
        self.sems = {}
        self.same = same_engine_sync
        self.ndma = ndma_sems
        self.dma_rr = {e: 0 for e in self.ENGS}
        self.dma_val = {}
        self.stack = None

    def _semkeys(self):
        keys = list(self.ENGS) + ["cc"]
        for e in ("sp", "pool", "act"):
            for i in range(self.ndma):
                keys.append(f"d_{e}_{i}")
        return keys

    def _need(self, eng, events):
        mx = {}
        for ev in events:
            if ev is None:
                continue
            k, v = ev
            if k == eng and not self.same:
                continue
            if k == "cc" and eng != "pool":
                continue
            if v > mx.get(k, 0):
                mx[k] = v
        out = []
        kn = self.known[eng]
        for k, v in mx.items():
            if kn.get(k, 0) >= v:
                continue
            kn[k] = v
            out.append((k, v))
        return out

    def _deps(self, reads, writes):
        evs = []
        for r in reads:
            evs.append(r.w)
        for w in writes:
            evs.append(w.w)
            evs.extend(w.r)
        return evs

    def _commit(self, ev, reads, writes):
        for r in reads:
            r.r.append(ev)
        for w in writes:
            w.w = ev
            w.r = []

    def op(self, eng, fn, reads=(), writes=(), extra=()):
        evs = self._deps(reads, writes) + list(extra)
        waits = self._need(eng, evs)
        self.cnt[eng] += 1
        ev = (eng, self.cnt[eng])
        self.q[eng].append((waits, fn, (eng, 1)))
        self._commit(ev, reads, writes)
        return ev

    def dma(self, eng, fn, reads=(), writes=(), extra=()):
        i = self.dma_rr[eng]
        self.dma_rr[eng] = (i + 1) % self.ndma
        key = f"d_{eng}_{i}"
        prev = self.dma_val.get(key, 0)
        evs = self._deps(reads, writes) + list(extra)
        if prev:
            evs.append((key, prev))
        waits = self._need(eng, evs)
        val = prev + 16
        self.dma_val[key] = val
        ev = (key, val)
        self.q[eng].append((waits, fn, (key, 16)))
        self._commit(ev, reads, writes)
        return ev

    def cc(self, fn, reads=(), writes=(), extra=(), inc=1):
        eng = "pool"
        evs = self._deps(reads, writes) + list(extra)
        waits = self._need(eng, evs)
        val = self.dma_val.get("cc", 0) + inc
        self.dma_val["cc"] = val
        ev = ("cc", val)
        self.q[eng].append((waits, fn, ("cc", inc)))
        self._commit(ev, reads, writes)
        return ev

    def wait_all(self, eng, events):
        waits = self._need(eng, list(events))
        self.q[eng].append((waits, None, None))

    def all_events(self):
        evs = [(e, c) for e, c in self.cnt.items() if c]
        evs += [(k, v) for k, v in self.dma_val.items()]
        return evs

    def begin(self, stack):
        self.gen = 0
        self.semh = {k: stack.enter_context(self.nc.semaphore(k)) for k in self._semkeys()}

    def rebase(self, stack):
        self.gen += 1
        self.semh = {k: stack.enter_context(self.nc.semaphore(f"{k}_g{self.gen}")) for k in self._semkeys()}
        self.cnt = {e: 0 for e in self.ENGS}
        self.known = {e: {} for e in self.ENGS}
        self.dma_rr = {e: 0 for e in self.ENGS}
        self.dma_val = {}
        for r in Reg.ALL:
            r.w = None
            r.r = []

    def flush(self):
        nc = self.nc
        sems = self.semh
        q = self.q

        def run(engh, lst):
            for waits, fn, inc in lst:
                for k, v in waits:
                    engh.wait_ge(sems[k], v)
                if fn is None:
                    continue
                ins = fn(engh)
                ins.then_inc(sems[inc[0]], inc[1])

        with nc.Block() as block:
            @block.tensor
            def _(e):
                run(e, q["pe"])

            @block.scalar
            def _(e):
                run(e, q["act"])

            @block.vector
            def _(e):
                run(e, q["dve"])

            @block.gpsimd
            def _(e):
                run(e, q["pool"])

            @block.sync
            def _(e):
                run(e, q["sp"])
        self.q = {e: [] for e in self.ENGS}


from contextlib import ExitStack
from concourse.bass_utils import run_bass_kernel_spmd

NCORES = 8
T = 2048
NT = 16
TS = 256
NSL = T // TS
D = 1024
EPS = 1e-6
OFF_Z, OFF_X, OFF_B, OFF_C, OFF_DT, OFF_P, OFF_GS, OFF_GP = 0, 2048, 4096, 5120, 6144, 6176, 7200, 8224
DIN = 9248
PB = "dve"
NCOL = 208
C_NMG, C_NFG, C_PSC, C_SNG, C_C, C_CB, C_CW = 0, 8, 16, 24, 40, 48, 80
R_BADA, R_DTB, R_ALOG, R_DSK, R_RB, R_FNG = 0, 6144, 6176, 6208, 6240, 6304
NROW = 7328
BIG = 1.0e4
DEBUG = False
import os
DBGV = int(os.environ.get("DBGV", "0"))


class _Stop(Exception):
    pass


def build_nc(n_experts=65, do_moe=True, stop_after=None, n_slabs=NSL, n_pre_slots=NCORES - 1):
    nc = bass.Bass("TRN2", target_bir_lowering=False)
    di = lambda n, s, dt=F32: nc.dram_tensor(n, s, dt, kind="ExternalInput").ap()
    x_own = di("x_own", [T, D]); x_halo = di("x_halo", [128, D]); x_all = di("x_all", [(NCORES - 1) * T, D])
    rk_d = di("rk", [128, 8]); nf_d = di("nf", [128, 1]); pos_d = di("posrow", [128, 16])
    cols_d = di("cols", [128, NCOL]); rows_d = di("rows", [128, NROW])
    w_ada = di("w_ada", [D, 6 * D]); w_in = di("w_in", [D, DIN])
    pool_w = di("pool_w", [1024, 256]); w_br_ssd = di("w_br_ssd", [2048, D])
    w_br_pool = di("w_br_pool", [D, D]); w_out = di("w_out", [D, D])
    w_router = di("w_router", [D, 64])
    NEW = 65 if stop_after is None else 1
    wgu = di("wgu", [NEW, 128, 8, 512]); wdn = di("wdn", [NEW, 128, 2, 1024])
    out_d = nc.dram_tensor("out", [T, D], F32, kind="ExternalOutput").ap()
    w_in_b = nc.dram_tensor("w_in_b", [D, DIN], BF16).ap()
    wbp_b = nc.dram_tensor("wbp_b", [D, D], BF16).ap()
    wbs_b = nc.dram_tensor("wbs_b", [2048, D], BF16).ap()
    wout_b = nc.dram_tensor("wout_b", [D, D], BF16).ap()
    bounce = nc.dram_tensor("bounce", [128, 2080], F32).ap()
    gathered = nc.dram_tensor("gathered", [1024, 2080], F32).ap()
    dbg = {}

    Reg.ALL.clear()
    P = Prog(nc)
    regs = {}
    _top0 = ExitStack()
    P.begin(_top0)

    ALIAS = {"yn": ["bigA", "bigB"], "big": ["bigA", "bigB"], "junk": ["gsig"], "xn": ["bigB"]}

    def R(name):
        if name not in regs:
            regs[name] = Reg(name)
        return regs[name]

    top = ExitStack()

    def sb(st, name, shape, dt):
        return st.enter_context(nc.sbuf_tensor("s_" + name, shape, dt))

    def rl(x):
        out = []
        for n in x:
            if isinstance(n, str):
                for m in ALIAS.get(n, [n]):
                    out.append(R(m))
            else:
                out.append(n)
        return out

    def tt(eng, out, a, b, op, rd, wr):
        return P.op(eng, lambda e: e.tensor_tensor(out=out, in0=a, in1=b, op=op), rl(rd), rl(wr))

    def ts(eng, out, a, s1, s2, op0, op1, rd, wr):
        if op1 is None:
            return P.op(eng, lambda e: e.tensor_single_scalar(out=out, in_=a, scalar=s1, op=op0), rl(rd), rl(wr))
        return P.op(eng, lambda e: e.tensor_scalar(out=out, in0=a, scalar1=s1, scalar2=s2, op0=op0, op1=op1), rl(rd), rl(wr))

    def stt(eng, out, a, sc, b, op0, op1, rd, wr):
        return P.op(eng, lambda e: e.scalar_tensor_tensor(out=out, in0=a, scalar=sc, in1=b, op0=op0, op1=op1), rl(rd), rl(wr))

    def act(out, in_, func, rd, wr, bias=0.0, scale=1.0, accum_out=None):
        if accum_out is None:
            return P.op("act", lambda e: e.activation(out=out, in_=in_, func=func, bias=bias, scale=scale), rl(rd), rl(wr))
        return P.op("act", lambda e: e.activation(out=out, in_=in_, func=func, bias=bias, scale=scale, accum_out=accum_out), rl(rd), rl(wr))

    def cp(eng, out, in_, rd, wr):
        if eng == "act":
            return P.op("act", lambda e: e.copy(out=out, in_=in_), rl(rd), rl(wr))
        return P.op(eng, lambda e: e.tensor_copy(out=out, in_=in_), rl(rd), rl(wr))

    def mms(lst, rd, wr):
        def f(e):
            for (o, l, r, s0, s1) in lst:
                ins = e.matmul(o, lhsT=l, rhs=r, start=s0, stop=s1)
            return ins
        return P.op("pe", f, rl(rd), rl(wr))

    def trs(lst, rd, wr):
        def f(e):
            for (o, i, idn) in lst:
                ins = e.transpose(out=o, in_=i, identity=idn)
            return ins
        return P.op("pe", f, rl(rd), rl(wr))

    def dma(q, out, in_, rd, wr):
        if q == "pool":
            return P.dma(q, lambda e: e.dma_start(out=out, in_=in_, max_dma_last_dim=4096), rl(rd), rl(wr))
        return P.dma(q, lambda e: e.dma_start(out=out, in_=in_), rl(rd), rl(wr))

    def barrier():
        evs = P.all_events()
        for e in ("pe", "act", "dve", "pool", "sp"):
            P.wait_all(e, evs)

    def checkpoint(name, tensors):
        if stop_after != name:
            return
        for tn, t in tensors.items():
            a = t if hasattr(t, "ap") and not hasattr(t, "__enter__") and type(t).__name__ == "AP" else t[:]
            shp = list(a.shape)
            d = nc.dram_tensor("dbg_" + tn, shp, a.dtype, kind="ExternalOutput").ap()
            dma("sp", d, a, [tn], [])
        P.wait_all("sp", P.all_events())
        P.flush()
        raise _Stop()

    banks = [top.enter_context(nc.psum_tensor(f"bank{i}", [128, 512], F32)) for i in range(8)]
    bstate = {"i": 0, "y": 0}

    def bank():
        n = bstate.get("n", 4)
        i = bstate["i"] % n
        bstate["i"] = (i + 1) % n
        return banks[i], R(f"bank{i}")

    def ybank(i=None):
        if i is None:
            i = bstate["y"]
            bstate["y"] = (i + 1) % 4
        return banks[4 + i], R(f"bank{4 + i}")

    H = sb(top, "H", [128, NT, D], F32)
    state = sb(top, "state", [128, 2048], F32)
    state_bf = sb(top, "state_bf", [128, 2048], BF16)
    identF = sb(top, "identF", [128, 128], F32)
    SLf = sb(top, "SLf", [128, 128], F32)
    ONf = sb(top, "ONf", [128, 128], F32)
    SLb = sb(top, "SLb", [128, 128], BF16)
    ONb = sb(top, "ONb", [128, 128], BF16)
    UTb = sb(top, "UTb", [128, 128], BF16)
    MKf = sb(top, "MKf", [128, 128], F32)
    cols = sb(top, "cols", [128, NCOL], F32)
    rowS = sb(top, "rowS", [128, 160], F32)
    rk = sb(top, "rk_s", [128, 8], F32)
    nf = sb(top, "nf_s", [128, 1], F32)
    mcol = sb(top, "mcol", [128, 32], F32)
    gsc = sb(top, "gsc", [128, 16], F32)
    g2_d = nc.dram_tensor("g2_d", [128, D], F32).ap()

    def mask(tn, tensor, cmp, sgn=1):
        P.op("pool", lambda e: e.memset(tensor[:], 1.0), [], [R(tn)])
        P.op("pool", lambda e: e.affine_select(out=tensor[:], in_=tensor[:], pattern=[[-sgn, 128]], compare_op=cmp,
                                               fill=0.0, base=0, channel_multiplier=sgn), [R(tn)], [R(tn)])
    mask("identF", identF, ALU.is_equal)
    mask("SLf", SLf, ALU.is_gt)
    mask("MKf", MKf, ALU.is_ge, -1)
    P.op("pool", lambda e: e.memset(ONf[:], 1.0), [], [R("ONf")])
    cp("pool", SLb[:], SLf[:], ["SLf"], ["SLb"])
    cp("pool", ONb[:], ONf[:], ["ONf"], ["ONb"])
    cp("pool", UTb[:], MKf[:], ["MKf"], ["UTb"])

    dma("sp", cols[:], cols_d, [], ["cols"])
    dma("sp", rowS[:], rows_d[:, R_DTB:R_DTB + 160], [], ["rowS"])
    dma("sp", rk[:], rk_d, [], ["rk"])
    dma("sp", nf[:], nf_d, [], ["nf"])
    dma("sp", H[:], x_own.rearrange("(t p) d -> p t d", p=128), [], ["H"])
    _wv_o = w_in_b.rearrange("a b -> (a b)").rearrange("(r c) -> r c", c=2048)
    _wv_i = w_in.rearrange("a b -> (a b)").rearrange("(r c) -> r c", c=2048)
    for _q in range(8):
        dma("pool", _wv_o[_q * 578:(_q + 1) * 578, :], _wv_i[_q * 578:(_q + 1) * 578, :], [], ["w_in_b"])
    dma("pool", wbp_b, w_br_pool, [], ["wbp_b"])

    try:
        with ExitStack() as s0:
            bcm = sb(s0, "bcm", [128, 6 * D], F32)
            cact = sb(s0, "cact", [128, 8], F32)
            CBC = sb(s0, "CBC", [128, 8, 128], F32)
            wa = [sb(s0, f"wa{i}", [128, 8, 512], F32) for i in range(2)]
            ba = [sb(s0, f"ba{i}", [128, 512], F32) for i in range(2)]
            stg = [sb(s0, f"stg{i}", [128, 4, D], F32) for i in range(2)]
            stb = [sb(s0, f"stb{i}", [128, 4, D], BF16) for i in range(2)]
            act(cact[:], cols[:, C_C:C_C + 8], AF.Silu, ["cols"], ["cact"])
            cp("dve", CBC[:], cact[:].unsqueeze(2).to_broadcast([128, 8, 128]), ["cact"], ["CBC"])
            for ns in range(12):
                b = ns % 2
                dma("sp", wa[b][:], w_ada[:, ns * 512:(ns + 1) * 512].rearrange("(c p) n -> p c n", p=128), [], [f"wa{b}"])
                dma("sp", ba[b][:], rows_d[:, R_BADA + ns * 512:R_BADA + (ns + 1) * 512], [], [f"ba{b}"])
                bk, br = bank()
                mms([(bk[:], CBC[:, dc, :], wa[b][:, dc, :], dc == 0, dc == 7) for dc in range(8)], ["CBC", f"wa{b}"], [br])
                tt("dve", bcm[:, ns * 512:(ns + 1) * 512], bk[:], ba[b][:], ALU.add, [br, f"ba{b}"], ["bcm"])
            for vi, v in enumerate((0, 1, 3, 4)):
                for q in range(2):
                    bk, br = bank()
                    trs([(bk[:, j * 128:(j + 1) * 128], bcm[:, v * D + (q * 4 + j) * 128: v * D + (q * 4 + j + 1) * 128], identF[:])
                         for j in range(4)], ["bcm", "identF"], [br])
                    cp("dve", mcol[:, vi * 8 + q * 4: vi * 8 + q * 4 + 4],
                       bk[:].rearrange("p (j n) -> p j n", n=128)[:, :, 0], [br], ["mcol"])
            stt("dve", gsc[:, 0:8], mcol[:, 8:16], 1.0, cols[:, C_NMG:C_NMG + 8], ALU.add, ALU.mult, ["mcol", "cols"], ["gsc"])
            stt("dve", gsc[:, 8:16], mcol[:, 24:32], 1.0, cols[:, C_NFG:C_NFG + 8], ALU.add, ALU.mult, ["mcol", "cols"], ["gsc"])
            dma("sp", g2_d, bcm[:, 5 * D:6 * D], ["bcm"], ["g2_d"])
            act(rowS[:, 32:64], rowS[:, 32:64], AF.Exp, ["rowS"], ["rowS"])
            ts("dve", rowS[:, 32:64], rowS[:, 32:64], -1.0, None, ALU.mult, None, ["rowS"], ["rowS"])
            k = 0
            for half in range(2):
                b = k % 2; k += 1
                dma("sp", stg[b][:], w_out[half * 512:(half + 1) * 512, :].rearrange("(c p) n -> p c n", p=128), [], [f"stg{b}"])
                tt("dve", stb[b][:], stg[b][:], bcm[:, 2 * D:3 * D].unsqueeze(1).to_broadcast([128, 4, D]), ALU.mult,
                   [f"stg{b}", "bcm"], [f"stb{b}"])
                dma("sp", wout_b[half * 512:(half + 1) * 512, :].rearrange("(c p) n -> p c n", p=128), stb[b][:], [f"stb{b}"], ["wout_b"])
            for q in range(4):
                b = k % 2; k += 1
                dma("sp", stg[b][:], w_br_ssd[q * 512:(q + 1) * 512, :].rearrange("(c p) n -> p c n", p=128), [], [f"stg{b}"])
                for j in range(4):
                    ts("pool", stb[b][:, j, :], stg[b][:, j, :], cols[:, C_SNG + q * 4 + j:C_SNG + q * 4 + j + 1], None, ALU.mult, None,
                       [f"stg{b}", "cols"], [f"stb{b}"])
                dma("sp", wbs_b[q * 512:(q + 1) * 512, :].rearrange("(c p) n -> p c n", p=128), stb[b][:], [f"stb{b}"], ["wbs_b"])
            checkpoint("p0", {"mcol": mcol, "gsc": gsc, "rowS": rowS, "bcm": bcm})
            barrier()
            P.flush()

        with ExitStack() as sA:
            WS = [sb(sA, f"WS{i}", [128, 8, 512], BF16) for i in range(3)]
            wsi = {"i": 0}
            wdt = sb(sA, "wdt", [128, 8, 32], BF16)
            poolw = sb(sA, "poolw", [128, 8, 256], BF16)
            pcorr = sb(sA, "pcorr", [128, 4, 16], F32)
            posr = sb(sA, "posr", [128, 16], F32)
            tailx = sb(sA, "tailx", [128, 32, 3], F32)
            tailx0 = sb(sA, "tailx0", [128, 32, 3], F32)
            ptail = sb(sA, "ptail", [128, 8, 15], F32)
            xpw = [sb(sA, f"xpw{i}", [128, 15 + TS], F32) for i in range(2)]
            logD = sb(sA, "logD", [128, 32], F32)
            logDp = [logD, sb(sA, "logD1", [128, 32], F32)]
            ldi = {"i": 0}
            uT = sb(sA, "uT", [128, 8, TS], BF16)
            xn_late = True
            st1 = sb(sA, "st1", [128, 4], F32)
            pooledT = sb(sA, "pooledT", [128, 8, TS], BF16)
            mixedT = sb(sA, "mixedT", [128, 8, TS], BF16)
            gp = sb(sA, "gp", [128, 8, TS], BF16)
            pS = [sb(sA, f"pS{i}", [128, 15 + TS], F32) for i in range(2)]
            gsig = sb(sA, "gsig", [128, TS], F32)
            NSET = 3
            xcs = [sb(sA, f"xc{i}", [128, 3 + TS], F32) for i in range(NSET)]
            caccs = [sb(sA, f"cacc{i}", [128, TS], F32) for i in range(NSET)]
            big = sb(sA, "big", [128, 2080], F32)
            xn = big[:, 1024:2048]
            tmpD = sb(sA, "tmpD", [128, 512], F32)
            Bfs = [sb(sA, f"Bf{i}", [128, TS], F32) for i in range(2)]
            xs_tok = sb(sA, "xs_tok", [128, 2, 2048], BF16)
            B_tok = sb(sA, "B_tok", [128, 2, 1024], BF16)
            BT = sb(sA, "BT", [128, 8, TS], BF16)
            CT = sb(sA, "CT", [128, 8, TS], BF16)
            dtb = sb(sA, "dtb", [128, 2, 32], F32)
            ab = sb(sA, "ab", [128, 2, 32], F32)
            ab16 = sb(sA, "ab16", [128, 2, 32], BF16)
            sm = sb(sA, "sm", [128, 4, 32], F32)
            sz_tok = sb(sA, "sz_tok", [128, 2, 2048], BF16)
            xdt = sb(sA, "xdt", [128, 2048], BF16)
            xdtd = sb(sA, "xdtd", [128, 2048], BF16)
            AL = sb(sA, "AL", [128, 4, 128], BF16)
            AO = sb(sA, "AO", [128, 4, 128], BF16)
            LT = sb(sA, "LT", [128, 4, 128], BF16)
            Eb = sb(sA, "Eb", [128, 4, 128], BF16)
            CBm = sb(sA, "CBm", [128, 8, 128], BF16)
            MT = sb(sA, "MT", [128, 4, 128], BF16)
            CE = sb(sA, "CE", [128, 4, 128], BF16)
            yn = big
            junk = gsig
            ss8 = sb(sA, "ss8", [128, 8], F32)
            ynT = sb(sA, "ynT", [128, 16, TS], BF16)
            combT = sb(sA, "combT", [128, 8, TS], BF16)
            stS = big

            def wblock(src, rd):
                i = wsi["i"]; wsi["i"] = (i + 1) % 3
                dma("sp", WS[i][:], src.rearrange("(c p) n -> p c n", p=128), rd, [f"WS{i}"])
                return WS[i], f"WS{i}"

            dma("pool", wdt[:], w_in[:, OFF_DT:OFF_DT + 32].rearrange("(c p) n -> p c n", p=128), [], ["wdt"])
            dma("pool", poolw[:], pool_w.rearrange("(c p) n -> p c n", p=128), [], ["poolw"])
            dma("sp", posr[:], pos_d, [], ["posr"])
            for g, w in enumerate((2, 4, 8, 16)):
                ts("dve", pcorr[:, g, :], posr[:], float(w), None, ALU.min, None, ["posr"], ["pcorr"])
                P.op("dve", lambda e, g=g: e.reciprocal(out=pcorr[:, g, :], in_=pcorr[:, g, :]), rl(["pcorr"]), rl(["pcorr"]))
                ts("dve", pcorr[:, g, :], pcorr[:, g, :], float(w), None, ALU.mult, None, ["pcorr"], ["pcorr"])

            def make_uT(src_ap, rdname, n_tiles, col0=0):
                for tti in range(n_tiles):
                    xin = src_ap(tti)
                    act(junk_big[:], xin, AF.Square, [rdname], ["xn", "st1"], accum_out=st1[:, 0:1])
                    act(st1[:, 1:2], st1[:, 0:1], AF.Sqrt, ["st1"], ["st1"], bias=EPS, scale=1.0 / D)
                    P.op("dve", lambda e: e.reciprocal(out=st1[:, 2:3], in_=st1[:, 1:2]), rl(["st1"]), rl(["st1"]))
                    ts("dve", xn[:], xin, st1[:, 2:3], None, ALU.mult, None, [rdname, "st1"], ["xn"])
                    for half in range(2):
                        bk, br = bank()
                        trs([(bk[:, j * 128:(j + 1) * 128], xn[:, (half * 4 + j) * 128:(half * 4 + j + 1) * 128], identF[:])
                             for j in range(4)], ["xn", "identF"], [br])
                        for j in range(4):
                            dc = half * 4 + j
                            ts("dve" if half else "pool_no", uT[:, dc, col0 + tti * 128: col0 + (tti + 1) * 128], bk[:, j * 128:(j + 1) * 128],
                               gsc[:, dc:dc + 1], mcol[:, dc:dc + 1], ALU.mult, ALU.add, [br, "gsc", "mcol"], ["uT"])

            junk_big = xn

            _ts_orig = ts

            def ts(eng, out, a, s1, s2, op0, op1, rd, wr):
                if eng == "pool_no":
                    return P.op("act", lambda e: e.activation(out=out, in_=a, func=AF.Identity, bias=s2, scale=s1), rl(rd), rl(wr))
                return _ts_orig(eng, out, a, s1, s2, op0, op1, rd, wr)

            xh = xsA = big
            dma("sp", big[:, 0:D], x_halo, [], ["bigA"])
            make_uT(lambda tti: big[:, 0:D], "bigA", 1)
            for blk in range(10):
                c0 = OFF_X + blk * 512 if blk < 8 else OFF_P + (blk - 8) * 512
                wsb, wr_ = wblock(w_in_b[:, c0:c0 + 512], ["w_in_b"])
                for j in range(4):
                    bk, br = bank()
                    mms([(bk[:, 0:128], wsb[:, dc, j * 128:(j + 1) * 128], uT[:, dc, 0:128], dc == 0, dc == 7) for dc in range(8)],
                        [wr_, "uT"], [br])
                    if blk < 8:
                        c = blk * 4 + j
                        ts("dve", tailx0[:, c, :], bk[:, 125:128], nf[:, 0:1], None, ALU.mult, None, [br, "nf"], ["tailx0"])
                    else:
                        c = (blk - 8) * 4 + j
                        ts("dve", ptail[:, c, :], bk[:, 113:128], nf[:, 0:1], None, ALU.mult, None, [br, "nf"], ["ptail"])

            checkpoint("halo", {"tailx0": tailx0, "xpb": ptail, "uT": uT[:, :, 0:128]})
            def slab(si, pre, src=None, skip_uT=False, hook=None):
                t0 = 2 * si
                bstate["n"] = 8
                if skip_uT:
                    pass
                elif src is None:
                    make_uT(lambda tti: H[:, t0 + tti, :], "H", 2)
                else:
                    make_uT(src, "sz_tok", 2)
                if not pre:
                    for blk in range(2):
                        wsb, wr_ = wblock(w_in_b[:, OFF_P + blk * 512: OFF_P + (blk + 1) * 512], ["w_in_b"])
                        for j in range(4):
                            c = blk * 4 + j
                            g = c // 2
                            w = 2 << g
                            bk, br = bank()
                            mms([(bk[:, 0:TS], wsb[:, dc, j * 128:(j + 1) * 128], uT[:, dc, :], dc == 0, dc == 7) for dc in range(8)],
                                [wr_, "uT"], [br])
                            xw = xpw[c % 2]; xwn = f"xpw{c % 2}"
                            cp("pool", xw[:, 0:15], ptail[:, c, :], ["ptail"], [xwn])
                            cp("act", xw[:, 15:15 + TS], bk[:, 0:TS], [br], [xwn])
                            cp("pool", ptail[:, c, :], xw[:, TS:TS + 15], [xwn], ["ptail"])
                            src = xw[:]
                            lo = 0
                            step = 1
                            k = 0
                            while step < w:
                                dst = pS[k % 2]
                                nlo = lo + step
                                tt("dve", dst[:, nlo:15 + TS], src[:, nlo:15 + TS], src[:, nlo - step:15 + TS - step], ALU.add,
                                   [xwn, f"pS{(k + 1) % 2}"], [f"pS{k % 2}"])
                                src = dst[:]
                                lo = nlo
                                step *= 2
                                k += 1
                            sname = f"pS{(k - 1) % 2}"
                            if si == 0:
                                tt("dve", src[:, 15:31], src[:, 15:31], pcorr[:, g, :], ALU.mult, [sname, "pcorr"], [sname])
                            stt("dve", pooledT[:, c, :], src[:, 15:15 + TS], 1.0 / w, xw[:, 15:15 + TS], ALU.mult, ALU.subtract,
                                [sname, xwn], ["pooledT"])
                            pass
                    for g in range(4):
                        for dch in range(2):
                            bk, br = bank()
                            mms([(bk[:, 0:TS], poolw[:, 2 * g + kc, dch * 128:(dch + 1) * 128], pooledT[:, 2 * g + kc, :], kc == 0, kc == 1)
                                 for kc in range(2)], ["poolw", "pooledT"], [br])
                            ts("dve", mixedT[:, 2 * g + dch, :], bk[:, 0:TS], cols[:, C_PSC + 2 * g + dch:C_PSC + 2 * g + dch + 1], None,
                               ALU.mult, None, [br, "cols"], ["mixedT"])
                    for blk in range(2):
                        wsb, wr_ = wblock(wbp_b[:, blk * 512:(blk + 1) * 512], ["wbp_b"])
                        wsg, wg_ = wblock(w_in_b[:, OFF_GP + blk * 512: OFF_GP + (blk + 1) * 512], ["w_in_b"])
                        for j in range(4):
                            ec = blk * 4 + j
                            bk, br = bank()
                            mms([(bk[:, 0:TS], wsg[:, dc, j * 128:(j + 1) * 128], uT[:, dc, :], dc == 0, dc == 7) for dc in range(8)],
                                [wg_, "uT"], [br])
                            act(gsig[:], bk[:, 0:TS], AF.Sigmoid, [br], ["gsig"])
                            bk2, br2 = bank()
                            mms([(bk2[:, 0:TS], wsb[:, dc, j * 128:(j + 1) * 128], mixedT[:, dc, :], dc == 0, dc == 7) for dc in range(8)],
                                [wr_, "mixedT"], [br2])
                            tt("dve", gp[:, ec, :], bk2[:, 0:TS], gsig[:], ALU.mult, [br2, "gsig"], ["gp"])
                checkpoint(f"ut{int(pre)}", {"uT": uT})
                checkpoint(f"pool{int(pre)}", {"gp": gp})
                nblk = 6 if pre else 8
                pend = []

                def tick():
                    for p_ in pend:
                        p_[0] -= 1
                    while pend and pend[0][0] <= 0:
                        pend.pop(0)[1]()

                for blk in range(nblk):
                    wsb, wr_ = wblock(w_in_b[:, OFF_X + blk * 512: OFF_X + (blk + 1) * 512], ["w_in_b"])
                    for j in range(4):
                        c = blk * 4 + j
                        bk, br = bank()
                        mms([(bk[:, 0:TS], wsb[:, dc, j * 128:(j + 1) * 128], uT[:, dc, :], dc == 0, dc == 7) for dc in range(8)],
                            [wr_, "uT"], [br])
                        xc = xcs[c % NSET]; cacc = caccs[c % NSET]; xcn = f"xc{c % NSET}"; can = f"cacc{c % NSET}"
                        Bf = Bfs[c % 2]; Bfn = f"Bf{c % 2}"
                        cp("pool", xc[:, 0:3], tailx[:, c, :], [f"tailx{c}"], [xcn])
                        cp("act", xc[:, 3:3 + TS], bk[:, 0:TS], [br], [xcn])
                        cp("pool", tailx[:, c, :], xc[:, TS:TS + 3], [xcn], [f"tailx{c}"])
                        cw = lambda k_: cols[:, C_CW + c * 4 + k_: C_CW + c * 4 + k_ + 1]
                        P.op("act", lambda e, cacc=cacc, bk=bk, s_=cw(3): e.activation(out=cacc[:], in_=bk[:, 0:TS], func=AF.Copy, scale=s_),
                             rl([br, "cols"]), rl([can]))
                        for k_ in (0, 1, 2):
                            stt("dve", cacc[:], xc[:, k_:k_ + TS], cw(k_), cacc[:], ALU.mult, ALU.add, [xcn, "cols", can], [can])
                        bcol = cols[:, C_CB + c:C_CB + c + 1]
                        if c < 16:
                            hb = (c // 4) % 2; hbn = "bigA" if hb == 0 else "bigB"
                            pend.append([2, (lambda c=c, hb=hb, hbn=hbn, cacc=cacc, can=can, bcol=bcol:
                                             act(big[:, hb * 1024 + (c % 4) * TS: hb * 1024 + (c % 4 + 1) * TS], cacc[:], AF.Silu, [can, "cols"], [hbn], bias=bcol))])
                            if c % 4 == 3:
                                def st2x(cg=c // 4, hb=hb, hbn=hbn):
                                    for tti in range(2):
                                        bk2, br2 = bank()
                                        trs([(bk2[:, q * 128:(q + 1) * 128], big[:, hb * 1024 + q * TS + tti * 128: hb * 1024 + q * TS + (tti + 1) * 128], identF[:])
                                             for q in range(4)], [hbn, "identF"], [br2])
                                        cp("act", xs_tok[:, tti, cg * 512:(cg + 1) * 512], bk2[:], [br2], ["xs_tok"])
                                pend.append([3, st2x])
                        elif c < 24:
                            g = c - 16
                            pend.append([2, (lambda Bf=Bf, Bfn=Bfn, cacc=cacc, can=can, bcol=bcol:
                                             act(Bf[:], cacc[:], AF.Silu, [can, "cols"], [Bfn], bias=bcol))])
                            def st2b(g=g, Bf=Bf, Bfn=Bfn):
                                cp("pool", BT[:, g, :], Bf[:], [Bfn], ["BT"])
                                bk2, br2 = bank()
                                trs([(bk2[:, tti * 128:(tti + 1) * 128], Bf[:, tti * 128:(tti + 1) * 128], identF[:]) for tti in range(2)],
                                    [Bfn, "identF"], [br2])
                                cp("act", B_tok[:, :, g * 128:(g + 1) * 128], bk2[:, 0:256].rearrange("p (t n) -> p t n", n=128), [br2], ["B_tok"])
                            pend.append([2, st2b])
                        else:
                            g = c - 24
                            pend.append([2, (lambda g=g, cacc=cacc, can=can, bcol=bcol:
                                             act(CT[:, g, :], cacc[:], AF.Silu, [can, "cols"], ["CT"], bias=bcol))])
                        tick()
                while pend:
                    pend.pop(0)[1]()
                checkpoint(f"conv{int(pre)}", {"xs_tok": xs_tok, "B_tok": B_tok})
                for tti in range(2):
                    bk, br = bank()
                    mms([(bk[:, 0:32], uT[:, dc, tti * 128:(tti + 1) * 128], wdt[:, dc, :], dc == 0, dc == 7) for dc in range(8)],
                        ["uT", "wdt"], [br])
                    tt("dve", dtb[:, tti, :], bk[:, 0:32], rowS[:, 0:32], ALU.add, [br, "rowS"], ["dtb"])
                    act(dtb[:, tti, :], dtb[:, tti, :], AF.Exp, ["dtb"], ["dtb"])
                    act(dtb[:, tti, :], dtb[:, tti, :], AF.Ln, ["dtb"], ["dtb"], bias=1.0)
                    tt("dve", ab[:, tti, :], dtb[:, tti, :], rowS[:, 32:64], ALU.mult, ["dtb", "rowS"], ["ab"])
                    cp("dve", ab16[:, tti, :], ab[:, tti, :], ["ab"], ["ab16"])
                checkpoint(f"dt{int(pre)}", {"dtb": dtb, "ab": ab})
                if not pre:
                    for zc in range(4):
                        wsb, wr_ = wblock(w_in_b[:, OFF_Z + zc * 512: OFF_Z + (zc + 1) * 512], ["w_in_b"])
                        for tti in range(2):
                            bk, br = bank()
                            mms([(bk[:], uT[:, dc, tti * 128:(tti + 1) * 128], wsb[:, dc, :], dc == 0, dc == 7) for dc in range(8)],
                                ["uT", wr_], [br])
                            act(sz_tok[:, tti, zc * 512:(zc + 1) * 512], bk[:], AF.Silu, [br], ["sz_tok"])
                checkpoint(f"z{int(pre)}", {"sz_tok": sz_tok})
                if hook is not None:
                    hook()
                if not pre:
                    bstate["n"] = 4
                    bstate["i"] = 0
                for tti in range(2):
                    tsl = slice(tti * 128, (tti + 1) * 128)
                    bk, br = bank()
                    mms([(bk[:, 0:32], SLf[:], ab[:, tti, :], True, True)], ["SLf", "ab"], [br])
                    bkB, brB = bank()
                    mms([(bkB[:, 0:32], ONf[:], ab[:, tti, :], True, True)], ["ONf", "ab"], [brB])
                    act(sm[:, 0, :], bk[:, 0:32], AF.Exp, [br], ["sm"])
                    act(sm[:, 1, :], bkB[:, 0:32], AF.Exp, [brB], ["sm"])
                    checkpoint(f"ssdA{int(pre)}", {"sm": sm[:, 0:2, :]})
                    checkpoint(f"ssdA2{int(pre)}", {"logD": logD})
                    tt("dve", xdt[:].rearrange("p (h d) -> p h d", h=32), xs_tok[:, tti, :].rearrange("p (h d) -> p h d", h=32),
                       dtb[:, tti, :].unsqueeze(2).to_broadcast([128, 32, 64]), ALU.mult, ["xs_tok", "dtb"], ["xdt"])
                    checkpoint(f"ssdA3{int(pre)}", {"xdt": xdt})
                    tt(PB, xdtd[:].rearrange("p (h d) -> p h d", h=32), xdt[:].rearrange("p (h d) -> p h d", h=32),
                       sm[:, 0, :].unsqueeze(2).to_broadcast([128, 32, 64]), ALU.mult, ["xdt", "sm"], ["xdtd"])
                    checkpoint(f"ssdB{int(pre)}", {"xdt": xdt, "xdtd": xdtd})
                    if not pre:
                        for gh in range(2):
                            bk, br = bank()
                            mms([(bk[:, q * 128:(q + 1) * 128], BT[:, gh * 4 + q, tsl], CT[:, gh * 4 + q, tsl], True, True) for q in range(4)],
                                ["BT", "CT"], [br])
                            tt("dve", CBm[:, gh * 4:(gh + 1) * 4, :], bk[:].rearrange("p (q n) -> p q n", n=128),
                               MKf[:].unsqueeze(1).to_broadcast([128, 4, 128]), ALU.mult, [br, "MKf"], ["CBm"])
                        checkpoint(f"cb{int(pre)}", {"CBm": CBm})
                        ybanks = []
                        gb = {}
                        for g in range(9):
                            if g < 8:
                                a4 = ab16[:, tti, 4 * g:4 * g + 4].unsqueeze(2).to_broadcast([128, 4, 128])
                                tt("dve", AL[:], SLb[:].unsqueeze(1).to_broadcast([128, 4, 128]), a4, ALU.mult, ["SLb", "ab16"], ["AL"])
                                tt("dve", AO[:], ONb[:].unsqueeze(1).to_broadcast([128, 4, 128]), a4, ALU.mult, ["ONb", "ab16"], ["AO"])
                                bk, br = bank()
                                mms([(bk[:, q * 128:(q + 1) * 128], AL[:, q, :], UTb[:], True, True) for q in range(4)], ["AL", "UTb"], [br])
                                bk2, br2 = bank()
                                mms([(bk2[:, q * 128:(q + 1) * 128], AO[:, q, :], UTb[:], True, True) for q in range(4)], ["AO", "UTb"], [br2])
                                gb[g] = (bk, br, bk2, br2)
                            if g >= 1:
                                gp_ = g - 1
                                tt("dve", MT[:], LT[:], CBm[:, gp_, :].unsqueeze(1).to_broadcast([128, 4, 128]), ALU.mult, ["LT", "CBm"], ["MT"])
                                tt("dve", CE[:], Eb[:], CT[:, gp_, tsl].unsqueeze(1).to_broadcast([128, 4, 128]), ALU.mult, ["Eb", "CT"], ["CE"])
                            if g < 8:
                                bk, br, bk2, br2 = gb[g]
                                act(LT[:], bk[:].rearrange("p (q n) -> p q n", n=128), AF.Exp, [br], ["LT"])
                                act(Eb[:], bk2[:].rearrange("p (q n) -> p q n", n=128), AF.Exp, [br2], ["Eb"])
                            if g >= 1:
                                gp_ = g - 1
                                if gp_ % 2 == 0:
                                    yb, ybr = ybank(gp_ // 2)
                                    ybanks.append((yb, ybr))
                                lst = []
                                for q in range(4):
                                    h = 4 * gp_ + q
                                    o = yb[:, (gp_ % 2) * 256 + q * 64:(gp_ % 2) * 256 + (q + 1) * 64]
                                    lst.append((o, MT[:, q, :], xdt[:, h * 64:(h + 1) * 64], True, False))
                                    lst.append((o, CE[:, q, :], state_bf[:, h * 64:(h + 1) * 64], False, True))
                                mms(lst, ["MT", "CE", "xdt", "state_bf"], [ybr])
                        checkpoint(f"grp{int(pre)}", {"MT": MT, "CE": CE})
                        for b4 in range(4):
                            yb, ybr = ybanks[b4]
                            csl = slice(b4 * 512, (b4 + 1) * 512)
                            tt("dve", tmpD[:, 0:512].rearrange("p (h d) -> p h d", h=8), xs_tok[:, tti, csl].rearrange("p (h d) -> p h d", h=8),
                               rowS[:, 64 + b4 * 8:64 + b4 * 8 + 8].unsqueeze(2).to_broadcast([128, 8, 64]), ALU.mult,
                               ["xs_tok", "rowS"], ["tmpD"])
                            tt("dve", yn[:, csl], yb[:], tmpD[:, 0:512], ALU.add, [ybr, "tmpD"], ["yn"])
                            tt("dve", yn[:, csl], yn[:, csl], sz_tok[:, tti, csl], ALU.mult, ["yn", "sz_tok"], ["yn"])
                        for g in range(8):
                            act(junk[:], yn[:, g * 256:(g + 1) * 256], AF.Square, ["yn"], ["junk", "ss8"], accum_out=ss8[:, g:g + 1])
                        act(ss8[:], ss8[:], AF.Sqrt, ["ss8"], ["ss8"], bias=EPS, scale=1.0 / 256)
                        P.op("dve", lambda e: e.reciprocal(out=ss8[:], in_=ss8[:]), rl(["ss8"]), rl(["ss8"]))
                        tt("dve", yn[:, 0:2048].rearrange("p (g d) -> p g d", g=8), yn[:, 0:2048].rearrange("p (g d) -> p g d", g=8),
                           ss8[:].unsqueeze(2).to_broadcast([128, 8, 256]), ALU.mult, ["yn", "ss8"], ["yn"])
                        checkpoint(f"yn{int(pre)}", {"yn": yn[:, 0:2048]})
                        for q4 in range(4):
                            bk, br = bank()
                            trs([(bk[:, q * 128:(q + 1) * 128], yn[:, (q4 * 4 + q) * 128:(q4 * 4 + q + 1) * 128], identF[:]) for q in range(4)],
                                ["yn", "identF"], [br])
                            cp("act", ynT[:, q4 * 4:(q4 + 1) * 4, tsl], bk[:].rearrange("p (q n) -> p q n", n=128), [br], ["ynT"])
                    checkpoint(f"ynT{int(pre)}", {"ynT": ynT[:, :, 0:128]})
                    sbanks = []
                    for b4 in range(4):
                        bk, br = bank()
                        mms([(bk[:, q * 256:(q + 1) * 256], B_tok[:, tti, (2 * b4 + q) * 128:(2 * b4 + q + 1) * 128],
                              xdtd[:, (2 * b4 + q) * 256:(2 * b4 + q + 1) * 256], True, True) for q in range(2)], ["B_tok", "xdtd"], [br])
                        sbanks.append((bk, br))
                    checkpoint(f"ssdC{int(pre)}", {"xdt": xdt})
                    tt(PB, state[:].rearrange("p (h d) -> p h d", h=32), state[:].rearrange("p (h d) -> p h d", h=32),
                       sm[:, 1, :].unsqueeze(2).to_broadcast([128, 32, 64]), ALU.mult, ["state", "sm", "state_bf"], ["state"])
                    for b4 in range(4):
                        bk, br = sbanks[b4]
                        tt("dve", state[:, b4 * 512:(b4 + 1) * 512], bk[:], state[:, b4 * 512:(b4 + 1) * 512], ALU.add, ["state", br], ["state"])
                    if not pre:
                        cp("act", state_bf[:], state[:], ["state"], ["state_bf"])
                if pre:
                    return
                for eh in range(2):
                    pb = [ybank(q_) for q_ in range(4)]
                    for kh in range(2):
                        wsb, wr_ = wblock(wbs_b[kh * 1024:(kh + 1) * 1024, eh * 512:(eh + 1) * 512], ["wbs_b"])
                        for j in range(4):
                            mms([(pb[j][0][:, 0:TS], wsb[:, kc, j * 128:(j + 1) * 128], ynT[:, kh * 8 + kc, :], (kh == 0 and kc == 0),
                                  (kh == 1 and kc == 7)) for kc in range(8)], [wr_, "ynT"], [pb[j][1]])
                    wsg, wg_ = wblock(w_in_b[:, OFF_GS + eh * 512: OFF_GS + (eh + 1) * 512], ["w_in_b"])
                    for j in range(4):
                        ec = eh * 4 + j
                        bk, br = bank()
                        mms([(bk[:, 0:TS], wsg[:, dc, j * 128:(j + 1) * 128], uT[:, dc, :], dc == 0, dc == 7) for dc in range(8)],
                            [wg_, "uT"], [br])
                        act(gsig[:], bk[:, 0:TS], AF.Sigmoid, [br], ["gsig"])
                        tt("dve", gsig[:], pb[j][0][:, 0:TS], gsig[:], ALU.mult, [pb[j][1], "gsig"], ["gsig"])
                        tt("dve", combT[:, ec, :], gsig[:], gp[:, ec, :], ALU.add, ["gsig", "gp"], ["combT"])
                checkpoint(f"comb{int(pre)}", {"combT": combT})
                wo = [wblock(wout_b[:, hh * 512:(hh + 1) * 512], ["wout_b"]) for hh in range(2)]
                for tti in range(2):
                    for hh in range(2):
                        bk, br = bank()
                        mms([(bk[:], combT[:, ec, tti * 128:(tti + 1) * 128], wo[hh][0][:, ec, :], ec == 0, ec == 7) for ec in range(8)],
                            ["combT", wo[hh][1]], [br])
                        tt("dve", H[:, t0 + tti, hh * 512:(hh + 1) * 512], bk[:], H[:, t0 + tti, hh * 512:(hh + 1) * 512], ALU.add,
                           ["H", br], ["H"])

            xst = lambda tti: sz_tok[:, tti, :].bitcast(F32)
            Iacc = ynT[:].rearrange("p a b -> p (a b)").bitcast(F32)
            P.op("pool", lambda e: e.memset(state[:], 0.0), [], rl(["state"]))
            P.op("pool", lambda e: e.memset(Iacc, 0.0), [], rl(["ynT"]))
            P.op("pool", lambda e: e.memset(tailx[:], 0.0), [], rl([f"tailx{c_}" for c_ in range(32)]))
            seq = [(j, si) for j in range(n_pre_slots) for si in range(n_slabs)]

            def prep_uT(idx):
                j, si = seq[idx]
                r0 = j * T + si * TS
                for tti in range(2):
                    dma("sp", xst(tti), x_all[r0 + tti * 128: r0 + (tti + 1) * 128, :], [], ["sz_tok"])
                make_uT(xst, "sz_tok", 2)

            if seq:
                prep_uT(0)
            for idx, (j, si) in enumerate(seq):
                hk = (lambda idx=idx: prep_uT(idx + 1)) if idx + 1 < len(seq) else None
                slab(si, True, src=xst, skip_uT=True, hook=hk)
                if si == n_slabs - 1:
                    stt("dve", Iacc, state[:], rk[:, j:j + 1], Iacc, ALU.mult, ALU.add, ["state", "rk", "ynT"], ["ynT"])
            checkpoint("pre", {"state": state, "xs_tok": xs_tok, "B_tok": B_tok, "dtb": dtb})
            cp("dve", state[:], Iacc, ["ynT"], ["state"])
            cp("act", state_bf[:], state[:], ["state"], ["state_bf"])
            cp("pool", tailx[:], tailx0[:], ["tailx0"], [f"tailx{c_}" for c_ in range(32)])
            checkpoint("xchg", {"state": state})
            for si in range(n_slabs):
                slab(si, False)
            checkpoint("main", {"H": H, "ynT": ynT, "gp": gp, "combT": combT, "sz_tok": sz_tok, "CT": CT, "BT": BT})
            barrier()
            P.flush()

        if DEBUG:
            dbg_out = nc.dram_tensor("dbg_h", [T, D], F32, kind="ExternalOutput").ap()
            dma("sp", dbg_out.rearrange("(t p) d -> p t d", p=128), H[:], ["H"], [])

        with ExitStack() as sB:
            U2 = sb(sB, "U2", [128, 8, T], BF16)
            u2f = sb(sB, "u2f", [128, 8, 128], F32)
            xn2 = sb(sB, "xn2", [128, D], F32)
            st2 = sb(sB, "st2", [128, 4], F32)
            wr = sb(sB, "wr", [128, 8, 64], F32)
            wtab = sb(sB, "wtab", [128, NT, 65], F32)
            r64 = [sb(sB, f"r64_{i}", [128, 64], F32) for i in range(4)]
            r8 = [sb(sB, f"r8_{i}", [128, 8], F32) for i in range(5)]
            wguS = [sb(sB, f"wguS{i}", [128, 8, 512], BF16) for i in range(2)]
            wdF = [sb(sB, f"wdF{i}", [128, 2, D], F32) for i in range(2)]
            wdS = [sb(sB, f"wdS{i}", [128, 2, D], BF16) for i in range(2)]
            hidT = [sb(sB, f"hidT{i}", [128, 2, T], BF16) for i in range(2)]
            sg = [sb(sB, f"sg{i}", [128, 512], BF16) for i in range(2)]
            fng = sb(sB, "fng", [128, D], F32)
            g2bc = sb(sB, "g2bc", [128, D], F32)
            dma("sp", g2bc[:], g2_d, ["g2_d"], ["g2bc"])
            ob = [sb(sB, f"ob{i}", [128, D], F32) for i in range(2)]

            dma("sp", wr[:], w_router.rearrange("(c p) n -> p c n", p=128), [], ["wr"])
            dma("sp", fng[:], rows_d[:, R_FNG:R_FNG + D], [], ["fng"])
            P.op("pool", lambda e: e.memset(wtab[:], 1.0), [], rl(["wtab"]))
            for ti in range(NT):
                xin = H[:, ti, :]
                act(xn2[:], xin, AF.Square, ["H"], ["xn2", "st2"], accum_out=st2[:, 0:1])
                act(st2[:, 1:2], st2[:, 0:1], AF.Sqrt, ["st2"], ["st2"], bias=EPS, scale=1.0 / D)
                P.op("dve", lambda e: e.reciprocal(out=st2[:, 2:3], in_=st2[:, 1:2]), rl(["st2"]), rl(["st2"]))
                ts("dve", xn2[:], xin, st2[:, 2:3], None, ALU.mult, None, ["H", "st2"], ["xn2"])
                for half in range(2):
                    bk, br = bank()
                    trs([(bk[:, j * 128:(j + 1) * 128], xn2[:, (half * 4 + j) * 128:(half * 4 + j + 1) * 128], identF[:]) for j in range(4)],
                        ["xn2", "identF"], [br])
                    for j in range(4):
                        dc = half * 4 + j
                        P.op("act", lambda e, dc=dc, j=j, bk=bk: e.activation(out=u2f[:, dc, :], in_=bk[:, j * 128:(j + 1) * 128], func=AF.Identity,
                                                                         bias=mcol[:, 16 + dc:17 + dc], scale=gsc[:, 8 + dc:9 + dc]),
                             rl([br, "gsc", "mcol"]), rl(["u2f"]))
                cp("pool", U2[:, :, ti * 128:(ti + 1) * 128], u2f[:], ["u2f"], ["U2"])
                bk, br = bank()
                mms([(bk[:, 0:64], u2f[:, dc, :], wr[:, dc, :], dc == 0, dc == 7) for dc in range(8)], ["u2f", "wr"], [br])
                s_, ch, t1, t2 = r64
                m1, m2, gs, g8, gm = r8
                act(s_[:], bk[:, 0:64], AF.Sigmoid, [br], ["r0"])
                tt("dve", ch[:], s_[:], rowS[:, 96:160], ALU.add, ["r0", "rowS"], ["r1"])
                v3 = lambda a: a[:].rearrange("p (g e) -> p g e", g=8)
                P.op("dve", lambda e: e.tensor_reduce(out=m1[:], in_=v3(ch), axis=AX.X, op=ALU.max), rl(["r1"]), rl(["q0"]))
                tt("dve", v3(t1), v3(ch), m1[:].unsqueeze(2).to_broadcast([128, 8, 8]), ALU.is_equal, ["r1", "q0"], ["r2"])
                stt("dve", t1[:], t1[:], -BIG, ch[:], ALU.mult, ALU.add, ["r2", "r1"], ["r2"])
                P.op("dve", lambda e: e.tensor_reduce(out=m2[:], in_=v3(t1), axis=AX.X, op=ALU.max), rl(["r2"]), rl(["q1"]))
                tt("dve", gs[:], m1[:], m2[:], ALU.add, ["q0", "q1"], ["q2"])
                P.op("dve", lambda e: e.max(out=g8[:], in_=gs[:]), rl(["q2"]), rl(["q3"]))
                ts("dve", gm[:], gs[:], g8[:, 3:4], None, ALU.is_ge, None, ["q2", "q3"], ["q4"])
                tt("dve", v3(t1), v3(ch), gm[:].unsqueeze(2).to_broadcast([128, 8, 8]), ALU.mult, ["r1", "q4"], ["r2"])
                ts("dve", gm[:], gm[:], -1.0, BIG, ALU.add, ALU.mult, ["q4"], ["q4"])
                tt("dve", v3(t1), v3(t1), gm[:].unsqueeze(2).to_broadcast([128, 8, 8]), ALU.add, ["r2", "q4"], ["r2"])
                P.op("dve", lambda e: e.max(out=g8[:], in_=t1[:]), rl(["r2"]), rl(["q3"]))
                ts("dve", t2[:], t1[:], g8[:, 7:8], None, ALU.is_ge, None, ["r2", "q3"], ["r3"])
                tt("dve", t2[:], t2[:], s_[:], ALU.mult, ["r3", "r0"], ["r3"])
                P.op("dve", lambda e: e.tensor_reduce(out=m1[:, 0:1], in_=t2[:], axis=AX.X, op=ALU.add), rl(["r3"]), rl(["q0"]))
                P.op("dve", lambda e: e.reciprocal(out=m1[:, 0:1], in_=m1[:, 0:1]), rl(["q0"]), rl(["q0"]))
                ts("dve", wtab[:, ti, 0:64], t2[:], m1[:, 0:1], 2.5, ALU.mult, ALU.mult, ["r3", "q0"], ["wtab"])

            ne = n_experts if do_moe else 0
            for e_ in range(ne):
                b = e_ % 2
                P.dma("pool", lambda e, e_=e_, b=b: e.dma_start(out=wguS[b][:], in_=wgu[e_], max_dma_last_dim=4096), rl([]), rl([f"wguS{b}"]))
                dma("sp", wdF[b][:], wdn[e_], [], [f"wdF{b}"])
                tt("pool", wdS[b][:], wdF[b][:], g2bc[:].unsqueeze(1).to_broadcast([128, 2, D]), ALU.mult, [f"wdF{b}", "g2bc"], [f"wdS{b}"])
                k = 0
                for jc in range(2):
                    for sl in range(4):
                        bg, bgr = bank()
                        bu, bur = bank()
                        rhs = lambda dc: U2[:, dc, sl * 512:(sl + 1) * 512]
                        mms([(bg[:], wguS[b][:, dc, jc * 128:(jc + 1) * 128], rhs(dc), dc == 0, dc == 7) for dc in range(8)],
                            [f"wguS{b}", "U2"], [bgr])
                        mms([(bu[:], wguS[b][:, dc, 256 + jc * 128:256 + (jc + 1) * 128], rhs(dc), dc == 0, dc == 7) for dc in range(8)],
                            [f"wguS{b}", "U2"], [bur])
                        act(sg[k % 2][:], bg[:], AF.Silu, [bgr], [f"sg{k % 2}"])
                        tt("dve", hidT[b][:, jc, sl * 512:(sl + 1) * 512], bu[:], sg[k % 2][:], ALU.mult, [bur, f"sg{k % 2}"], [f"hidT{b}"])
                        k += 1
                for ti in range(NT):
                    for hh in range(2):
                        bk, br = ybank()
                        mms([(bk[:], hidT[b][:, jc, ti * 128:(ti + 1) * 128], wdS[b][:, jc, hh * 512:(hh + 1) * 512], jc == 0, jc == 1)
                             for jc in range(2)], [f"hidT{b}", f"wdS{b}"], [br])
                        stt("dve", H[:, ti, hh * 512:(hh + 1) * 512], bk[:], wtab[:, ti, e_:e_ + 1], H[:, ti, hh * 512:(hh + 1) * 512],
                            ALU.mult, ALU.add, [br, "wtab", f"Ht{ti}"], [f"Ht{ti}"])
            for ti in range(NT):
                b = ti % 2
                act(xn2[:], H[:, ti, :], AF.Square, ["H", f"Ht{ti}"], ["xn2", "st2"], accum_out=st2[:, 0:1])
                act(st2[:, 1:2], st2[:, 0:1], AF.Sqrt, ["st2"], ["st2"], bias=EPS, scale=1.0 / D)
                P.op("dve", lambda e: e.reciprocal(out=st2[:, 2:3], in_=st2[:, 1:2]), rl(["st2"]), rl(["st2"]))
                stt("dve", ob[b][:], H[:, ti, :], st2[:, 2:3], fng[:], ALU.mult, ALU.mult, ["H", f"Ht{ti}", "st2", "fng"], [f"ob{b}"])
                dma("sp", out_d[ti * 128:(ti + 1) * 128, :], ob[b][:], [f"ob{b}"], [])
            P.wait_all("sp", P.all_events())
            P.flush()
    except _Stop:
        return nc
    top.close()
    _top0.close()
    return nc


def _colform(v):
    v = np.asarray(v, np.float32).reshape(-1)
    return np.ascontiguousarray(v.reshape(-1, 128).T)


def _prep_inputs(x, c, w_ada, b_ada, norm_mix_g, w_in, conv_w, conv_b, dt_bias, A_log, D_skip, ssd_norm_g,
                 pool_w, pool_scale, w_br_ssd, w_br_pool, w_out, norm_ffn_g, w_router, router_bias,
                 we_gate, we_up, we_down, ws_gate, ws_up, ws_down, final_norm_g):
    f = lambda a: np.ascontiguousarray(np.asarray(a, np.float32))
    x2 = f(x).reshape(NCORES * T, D)
    cols = np.zeros((128, NCOL), np.float32)
    cols[:, C_NMG:C_NMG + 8] = _colform(norm_mix_g)
    cols[:, C_NFG:C_NFG + 8] = _colform(norm_ffn_g)
    cols[:, C_PSC:C_PSC + 8] = _colform(pool_scale)
    cols[:, C_SNG:C_SNG + 16] = _colform(ssd_norm_g)
    cols[:, C_C:C_C + 8] = _colform(c)
    cols[:, C_CB:C_CB + 32] = _colform(conv_b)
    cw = f(conv_w).reshape(4, 32, 128)
    cols[:, C_CW:C_CW + 128] = cw.transpose(2, 1, 0).reshape(128, 128)
    rows = np.zeros((1, NROW), np.float32)
    rows[0, R_BADA:R_BADA + 6144] = f(b_ada).reshape(-1)
    rows[0, R_DTB:R_DTB + 32] = f(dt_bias).reshape(-1)
    rows[0, R_ALOG:R_ALOG + 32] = f(A_log).reshape(-1)
    rows[0, R_DSK:R_DSK + 32] = f(D_skip).reshape(-1)
    rows[0, R_RB:R_RB + 64] = f(router_bias).reshape(-1)
    rows[0, R_FNG:R_FNG + 1024] = f(final_norm_g).reshape(-1)
    rows = np.ascontiguousarray(np.broadcast_to(rows, (128, NROW)))
    wg = f(we_gate)[0].reshape(64, 8, 128, 256)
    wu = f(we_up)[0].reshape(64, 8, 128, 256)
    wgu = np.empty((65, 128, 8, 512), np.float32)
    wgu[:64, :, :, :256] = wg.transpose(0, 2, 1, 3)
    wgu[:64, :, :, 256:] = wu.transpose(0, 2, 1, 3)
    wgu[64, :, :, :256] = f(ws_gate)[0].reshape(8, 128, 256).transpose(1, 0, 2)
    wgu[64, :, :, 256:] = f(ws_up)[0].reshape(8, 128, 256).transpose(1, 0, 2)
    wdn = np.empty((65, 128, 2, 1024), np.float32)
    wdn[:64] = f(we_down)[0].reshape(64, 2, 128, 1024).transpose(0, 2, 1, 3)
    wdn[64] = f(ws_down)[0].reshape(2, 128, 1024).transpose(1, 0, 2)
    shared = {
        "cols": cols, "rows": rows, "w_ada": f(w_ada)[0], "w_in": f(w_in)[0],
        "pool_w": f(pool_w)[0].reshape(1024, 256), "w_br_ssd": f(w_br_ssd)[0], "w_br_pool": f(w_br_pool)[0],
        "w_out": f(w_out)[0], "w_router": f(w_router)[0], "wgu": wgu, "wdn": wdn,
    }
    in_maps = []
    for k in range(NCORES):
        m = dict(shared)
        m["x_own"] = x2[k * T:(k + 1) * T]
        m["x_halo"] = x2[k * T - 128:k * T] if k > 0 else np.zeros((128, D), np.float32)
        m["rk"] = np.ascontiguousarray(np.broadcast_to((np.arange(8) == k - 1).astype(np.float32)[None, :], (128, 8)))
        m["x_all"] = x2[:(NCORES - 1) * T]
        m["nf"] = np.full((128, 1), 1.0 if k > 0 else 0.0, np.float32)
        m["posrow"] = np.ascontiguousarray(np.broadcast_to((k * T + 1 + np.arange(16)).astype(np.float32)[None, :], (128, 16)))
        in_maps.append(m)
    return in_maps


_NC_CACHE = {}


def kernel(**inputs):
    in_maps = _prep_inputs(**inputs)
    if "nc" not in _NC_CACHE:
        _NC_CACHE["nc"] = build_nc()
    res = run_bass_kernel_spmd(_NC_CACHE["nc"], in_maps, core_ids=list(range(NCORES)))
    out = np.concatenate([np.asarray(r["out"], np.float32) for r in res.results], axis=0)
    return out.reshape(1, NCORES * T, D)
```

```python
import numpy as np
import concourse.bass as bass
import concourse.mybir as mybir

F32 = mybir.dt.float32
BF16 = mybir.dt.bfloat16
AF = mybir.ActivationFunctionType
ALU = mybir.AluOpType
AX = mybir.AxisListType


class Reg:
    __slots__ = ("name", "w", "r")

    ALL = []

    def __init__(self, name):
        self.name = name
        self.w = None
        self.r = []
        Reg.ALL.append(self)


class Prog:
    ENGS = ("pe", "act", "dve", "pool", "sp")

    def __init__(self, nc, ndma_sems=8, same_engine_sync=True):
        self.nc = nc
        self.q = {e: [] for e in self.ENGS}
        self.cnt = {e: 0 for e in self.ENGS}
        self.known = {e: {} for e in self.ENGS}
        self.sems = {}
        self.same = same_engine_sync
        self.ndma = ndma_sems
        self.dma_rr = {e: 0 for e in self.ENGS}
        self.dma_val = {}
        self.stack = None

    def _semkeys(self):
        keys = list(self.ENGS) + ["cc"]
        for e in ("sp", "pool", "act"):
            for i in range(self.ndma):
                keys.append(f"d_{e}_{i}")
        return keys

    def _need(self, eng, events):
        mx = {}
        for ev in events:
            if ev is None:
                continue
            k, v = ev
            if k == eng and (not self.same or eng == "pe"):
                continue
            if k == "cc" and eng != "pool":
                continue
            if v > mx.get(k, 0):
                mx[k] = v
        out = []
        kn = self.known[eng]
        for k, v in mx.items():
            if kn.get(k, 0) >= v:
                continue
            kn[k] = v
            out.append((k, v))
        return out

    def _deps(self, reads, writes):
        evs = []
        for r in reads:
            evs.append(r.w)
        for w in writes:
            evs.append(w.w)
            evs.extend(w.r)
        return evs

    def _commit(self, ev, reads, writes):
        for r in reads:
            r.r.append(ev)
        for w in writes:
            w.w = ev
            w.r = []

    def op(self, eng, fn, reads=(), writes=(), extra=()):
        evs = self._deps(reads, writes) + list(extra)
        waits = self._need(eng, evs)
        self.cnt[eng] += 1
        ev = (eng, self.cnt[eng])
        self.q[eng].append((waits, fn, (eng, 1)))
        self._commit(ev, reads, writes)
        return ev

    def dma(self, eng, fn, reads=(), writes=(), extra=()):
        i = self.dma_rr[eng]
        self.dma_rr[eng] = (i + 1) % self.ndma
        key = f"d_{eng}_{i}"
        prev = self.dma_val.get(key, 0)
        evs = self._deps(reads, writes) + list(extra)
        if prev:
            evs.append((key, prev))
        waits = self._need(eng, evs)
        val = prev + 16
        self.dma_val[key] = val
        ev = (key, val)
        self.q[eng].append((waits, fn, (key, 16)))
        self._commit(ev, reads, writes)
        return ev

    def cc(self, fn, reads=(), writes=(), extra=(), inc=1):
        eng = "pool"
        evs = self._deps(reads, writes) + list(extra)
        waits = self._need(eng, evs)
        val = self.dma_val.get("cc", 0) + inc
        self.dma_val["cc"] = val
        ev = ("cc", val)
        self.q[eng].append((waits, fn, ("cc", inc)))
        self._commit(ev, reads, writes)
        return ev

    def wait_all(self, eng, events):
        waits = self._need(eng, list(events))
        self.q[eng].append((waits, None, None))

    def all_events(self):
        evs = [(e, c) for e, c in self.cnt.items() if c]
        evs += [(k, v) for k, v in self.dma_val.items()]
        return evs

    def begin(self, stack):
        self.gen = 0
        self.semh = {k: stack.enter_context(self.nc.semaphore(k)) for k in self._semkeys()}

    def rebase(self, stack):
        self.gen += 1
        self.semh = {k: stack.enter_context(self.nc.semaphore(f"{k}_g{self.gen}")) for k in self._semkeys()}
        self.cnt = {e: 0 for e in self.ENGS}
        self.known = {e: {} for e in self.ENGS}
        self.dma_rr = {e: 0 for e in self.ENGS}
        self.dma_val = {}
        for r in Reg.ALL:
            r.w = None
            r.r = []

    def flush(self):
        nc = self.nc
        sems = self.semh
        q = self.q

        def run(engh, lst):
            for waits, fn, inc in lst:
                for k, v in waits:
                    engh.wait_ge(sems[k], v)
                if fn is None:
                    continue
                ins = fn(engh)
                ins.then_inc(sems[inc[0]], inc[1])

        with nc.Block() as block:
            @block.tensor
            def _(e):
                run(e, q["pe"])

            @block.scalar
            def _(e):
                run(e, q["act"])

            @block.vector
            def _(e):
                run(e, q["dve"])

            @block.gpsimd
            def _(e):
                run(e, q["pool"])

            @block.sync
            def _(e):
                run(e, q["sp"])
        self.q = {e: [] for e in self.ENGS}


from contextlib import ExitStack
from concourse.bass_utils import run_bass_kernel_spmd

NCORES = 8
T = 2048
NT = 16
TS = 256
NSL = T // TS
D = 1024
EPS = 1e-6
OFF_Z, OFF_X, OFF_B, OFF_C, OFF_DT, OFF_P, OFF_GS, OFF_GP = 0, 2048, 4096, 5120, 6144, 6176, 7200, 8224
DIN = 9248
PB = "dve"
NCOL = 208
C_NMG, C_NFG, C_PSC, C_SNG, C_C, C_CB, C_CW = 0, 8, 16, 24, 40, 48, 80
R_BADA, R_DTB, R_ALOG, R_DSK, R_RB, R_FNG = 0, 6144, 6176, 6208, 6240, 6304
NROW = 7328
BIG = 1.0e4
DEBUG = False
import os
DBGV = int(os.environ.get("DBGV", "0"))


class _Stop(Exception):
    pass


def build_nc(n_experts=65, do_moe=True, stop_after=None, n_slabs=NSL, n_pre_slots=NCORES - 1):
    nc = bass.Bass("TRN2", target_bir_lowering=False)
    di = lambda n, s, dt=F32: nc.dram_tensor(n, s, dt, kind="ExternalInput").ap()
    x_own = di("x_own", [T, D]); x_halo = di("x_halo", [128, D]); x_all = di("x_all", [(NCORES - 1) * T, D])
    rk_d = di("rk", [128, 8]); nf_d = di("nf", [128, 1]); pos_d = di("posrow", [128, 16])
    cols_d = di("cols", [128, NCOL]); rows_d = di("rows", [128, NROW])
    w_ada = di("w_ada", [D, 6 * D]); w_in = di("w_in", [D, DIN])
    pool_w = di("pool_w", [1024, 256]); w_br_ssd = di("w_br_ssd", [2048, D])
    w_br_pool = di("w_br_pool", [D, D]); w_out = di("w_out", [D, D])
    w_router = di("w_router", [D, 64])
    NEW = 65 if stop_after is None else 1
    wgu = di("wgu", [NEW, 128, 8, 512]); wdn = di("wdn", [NEW, 128, 2, 1024])
    out_d = nc.dram_tensor("out", [T, D], F32, kind="ExternalOutput").ap()
    w_in_b = nc.dram_tensor("w_in_b", [D, DIN], BF16).ap()
    wbp_b = nc.dram_tensor("wbp_b", [D, D], BF16).ap()
    wbs_b = nc.dram_tensor("wbs_b", [2048, D], BF16).ap()
    wout_b = nc.dram_tensor("wout_b", [D, D], BF16).ap()
    bounce = nc.dram_tensor("bounce", [128, 2080], F32).ap()
    gathered = nc.dram_tensor("gathered", [1024, 2080], F32).ap()
    dbg = {}

    Reg.ALL.clear()
    P = Prog(nc)
    regs = {}
    _top0 = ExitStack()
    P.begin(_top0)

    ALIAS = {"yn": ["bigA", "bigB"], "big": ["bigA", "bigB"], "junk": ["gsig"], "xn": ["bigB"]}

    def R(name):
        if name not in regs:
            regs[name] = Reg(name)
        return regs[name]

    top = ExitStack()

    def sb(st, name, shape, dt):
        return st.enter_context(nc.sbuf_tensor("s_" + name, shape, dt))

    def rl(x):
        out = []
        for n in x:
            if isinstance(n, str):
                for m in ALIAS.get(n, [n]):
                    out.append(R(m))
            else:
                out.append(n)
        return out

    def tt(eng, out, a, b, op, rd, wr):
        return P.op(eng, lambda e: e.tensor_tensor(out=out, in0=a, in1=b, op=op), rl(rd), rl(wr))

    def ts(eng, out, a, s1, s2, op0, op1, rd, wr):
        if op1 is None:
            return P.op(eng, lambda e: e.tensor_single_scalar(out=out, in_=a, scalar=s1, op=op0), rl(rd), rl(wr))
        return P.op(eng, lambda e: e.tensor_scalar(out=out, in0=a, scalar1=s1, scalar2=s2, op0=op0, op1=op1), rl(rd), rl(wr))

    def stt(eng, out, a, sc, b, op0, op1, rd, wr):
        return P.op(eng, lambda e: e.scalar_tensor_tensor(out=out, in0=a, scalar=sc, in1=b, op0=op0, op1=op1), rl(rd), rl(wr))

    def act(out, in_, func, rd, wr, bias=0.0, scale=1.0, accum_out=None):
        if accum_out is None:
            return P.op("act", lambda e: e.activation(out=out, in_=in_, func=func, bias=bias, scale=scale), rl(rd), rl(wr))
        return P.op("act", lambda e: e.activation(out=out, in_=in_, func=func, bias=bias, scale=scale, accum_out=accum_out), rl(rd), rl(wr))

    def cp(eng, out, in_, rd, wr):
        if eng == "act":
            return P.op("act", lambda e: e.copy(out=out, in_=in_), rl(rd), rl(wr))
        return P.op(eng, lambda e: e.tensor_copy(out=out, in_=in_), rl(rd), rl(wr))

    def mms(lst, rd, wr):
        def f(e):
            for (o, l, r, s0, s1) in lst:
                ins = e.matmul(o, lhsT=l, rhs=r, start=s0, stop=s1)
            return ins
        return P.op("pe", f, rl(rd), rl(wr))

    def trs(lst, rd, wr):
        def f(e):
            for (o, i, idn) in lst:
                ins = e.transpose(out=o, in_=i, identity=idn)
            return ins
        return P.op("pe", f, rl(rd), rl(wr))

    def dma(q, out, in_, rd, wr):
        if q == "pool":
            return P.dma(q, lambda e: e.dma_start(out=out, in_=in_, max_dma_last_dim=4096), rl(rd), rl(wr))
        return P.dma(q, lambda e: e.dma_start(out=out, in_=in_), rl(rd), rl(wr))

    def barrier():
        evs = P.all_events()
        for e in ("pe", "act", "dve", "pool", "sp"):
            P.wait_all(e, evs)

    def checkpoint(name, tensors):
        if stop_after != name:
            return
        for tn, t in tensors.items():
            a = t if hasattr(t, "ap") and not hasattr(t, "__enter__") and type(t).__name__ == "AP" else t[:]
            shp = list(a.shape)
            d = nc.dram_tensor("dbg_" + tn, shp, a.dtype, kind="ExternalOutput").ap()
            dma("sp", d, a, [tn], [])
        P.wait_all("sp", P.all_events())
        P.flush()
        raise _Stop()

    banks = [top.enter_context(nc.psum_tensor(f"bank{i}", [128, 512], F32)) for i in range(8)]
    bstate = {"i": 0, "y": 0}

    def bank():
        n = bstate.get("n", 4)
        i = bstate["i"] % n
        bstate["i"] = (i + 1) % n
        return banks[i], R(f"bank{i}")

    def ybank(i=None):
        if i is None:
            i = bstate["y"]
            bstate["y"] = (i + 1) % 4
        return banks[4 + i], R(f"bank{4 + i}")

    H = sb(top, "H", [128, NT, D], F32)
    state = sb(top, "state", [128, 2048], F32)
    state_bf = sb(top, "state_bf", [128, 2048], BF16)
    identF = sb(top, "identF", [128, 128], F32)
    SLf = sb(top, "SLf", [128, 128], F32)
    ONf = sb(top, "ONf", [128, 128], F32)
    SLb = sb(top, "SLb", [128, 128], BF16)
    ONb = sb(top, "ONb", [128, 128], BF16)
    UTb = sb(top, "UTb", [128, 128], BF16)
    MKf = sb(top, "MKf", [128, 128], F32)
    cols = sb(top, "cols", [128, NCOL], F32)
    rowS = sb(top, "rowS", [128, 160], F32)
    rk = sb(top, "rk_s", [128, 8], F32)
    nf = sb(top, "nf_s", [128, 1], F32)
    mcol = sb(top, "mcol", [128, 32], F32)
    gsc = sb(top, "gsc", [128, 16], F32)
    g2_d = nc.dram_tensor("g2_d", [128, D], F32).ap()

    def mask(tn, tensor, cmp, sgn=1):
        P.op("pool", lambda e: e.memset(tensor[:], 1.0), [], [R(tn)])
        P.op("pool", lambda e: e.affine_select(out=tensor[:], in_=tensor[:], pattern=[[-sgn, 128]], compare_op=cmp,
                                               fill=0.0, base=0, channel_multiplier=sgn), [R(tn)], [R(tn)])
    mask("identF", identF, ALU.is_equal)
    mask("SLf", SLf, ALU.is_gt)
    mask("MKf", MKf, ALU.is_ge, -1)
    P.op("pool", lambda e: e.memset(ONf[:], 1.0), [], [R("ONf")])
    cp("pool", SLb[:], SLf[:], ["SLf"], ["SLb"])
    cp("pool", ONb[:], ONf[:], ["ONf"], ["ONb"])
    cp("pool", UTb[:], MKf[:], ["MKf"], ["UTb"])

    dma("sp", cols[:], cols_d, [], ["cols"])
    dma("sp", rowS[:], rows_d[:, R_DTB:R_DTB + 160], [], ["rowS"])
    dma("sp", rk[:], rk_d, [], ["rk"])
    dma("sp", nf[:], nf_d, [], ["nf"])
    dma("sp", H[:], x_own.rearrange("(t p) d -> p t d", p=128), [], ["H"])
    _wv_o = w_in_b.rearrange("a b -> (a b)").rearrange("(r c) -> r c", c=2048)
    _wv_i = w_in.rearrange("a b -> (a b)").rearrange("(r c) -> r c", c=2048)
    for _q in range(8):
        dma("pool", _wv_o[_q * 578:(_q + 1) * 578, :], _wv_i[_q * 578:(_q + 1) * 578, :], [], ["w_in_b"])
    dma("pool", wbp_b, w_br_pool, [], ["wbp_b"])

    try:
        with ExitStack() as s0:
            bcm = sb(s0, "bcm", [128, 6 * D], F32)
            cact = sb(s0, "cact", [128, 8], F32)
            CBC = sb(s0, "CBC", [128, 8, 128], F32)
            wa = [sb(s0, f"wa{i}", [128, 8, 512], F32) for i in range(2)]
            ba = [sb(s0, f"ba{i}", [128, 512], F32) for i in range(2)]
            stg = [sb(s0, f"stg{i}", [128, 4, D], F32) for i in range(2)]
            stb = [sb(s0, f"stb{i}", [128, 4, D], BF16) for i in range(2)]
            act(cact[:], cols[:, C_C:C_C + 8], AF.Silu, ["cols"], ["cact"])
            cp("dve", CBC[:], cact[:].unsqueeze(2).to_broadcast([128, 8, 128]), ["cact"], ["CBC"])
            for ns in range(12):
                b = ns % 2
                dma("sp", wa[b][:], w_ada[:, ns * 512:(ns + 1) * 512].rearrange("(c p) n -> p c n", p=128), [], [f"wa{b}"])
                dma("sp", ba[b][:], rows_d[:, R_BADA + ns * 512:R_BADA + (ns + 1) * 512], [], [f"ba{b}"])
                bk, br = bank()
                mms([(bk[:], CBC[:, dc, :], wa[b][:, dc, :], dc == 0, dc == 7) for dc in range(8)], ["CBC", f"wa{b}"], [br])
                tt("dve", bcm[:, ns * 512:(ns + 1) * 512], bk[:], ba[b][:], ALU.add, [br, f"ba{b}"], ["bcm"])
            for vi, v in enumerate((0, 1, 3, 4)):
                for q in range(2):
                    bk, br = bank()
                    trs([(bk[:, j * 128:(j + 1) * 128], bcm[:, v * D + (q * 4 + j) * 128: v * D + (q * 4 + j + 1) * 128], identF[:])
                         for j in range(4)], ["bcm", "identF"], [br])
                    cp("dve", mcol[:, vi * 8 + q * 4: vi * 8 + q * 4 + 4],
                       bk[:].rearrange("p (j n) -> p j n", n=128)[:, :, 0], [br], ["mcol"])
            stt("dve", gsc[:, 0:8], mcol[:, 8:16], 1.0, cols[:, C_NMG:C_NMG + 8], ALU.add, ALU.mult, ["mcol", "cols"], ["gsc"])
            stt("dve", gsc[:, 8:16], mcol[:, 24:32], 1.0, cols[:, C_NFG:C_NFG + 8], ALU.add, ALU.mult, ["mcol", "cols"], ["gsc"])
            dma("sp", g2_d, bcm[:, 5 * D:6 * D], ["bcm"], ["g2_d"])
            act(rowS[:, 32:64], rowS[:, 32:64], AF.Exp, ["rowS"], ["rowS"])
            ts("dve", rowS[:, 32:64], rowS[:, 32:64], -1.0, None, ALU.mult, None, ["rowS"], ["rowS"])
            k = 0
            for half in range(2):
                b = k % 2; k += 1
                dma("sp", stg[b][:], w_out[half * 512:(half + 1) * 512, :].rearrange("(c p) n -> p c n", p=128), [], [f"stg{b}"])
                tt("dve", stb[b][:], stg[b][:], bcm[:, 2 * D:3 * D].unsqueeze(1).to_broadcast([128, 4, D]), ALU.mult,
                   [f"stg{b}", "bcm"], [f"stb{b}"])
                dma("sp", wout_b[half * 512:(half + 1) * 512, :].rearrange("(c p) n -> p c n", p=128), stb[b][:], [f"stb{b}"], ["wout_b"])
            for q in range(4):
                b = k % 2; k += 1
                dma("sp", stg[b][:], w_br_ssd[q * 512:(q + 1) * 512, :].rearrange("(c p) n -> p c n", p=128), [], [f"stg{b}"])
                for j in range(4):
                    ts("pool", stb[b][:, j, :], stg[b][:, j, :], cols[:, C_SNG + q * 4 + j:C_SNG + q * 4 + j + 1], None, ALU.mult, None,
                       [f"stg{b}", "cols"], [f"stb{b}"])
                dma("sp", wbs_b[q * 512:(q + 1) * 512, :].rearrange("(c p) n -> p c n", p=128), stb[b][:], [f"stb{b}"], ["wbs_b"])
            checkpoint("p0", {"mcol": mcol, "gsc": gsc, "rowS": rowS, "bcm": bcm})
            barrier()
            P.flush()

        with ExitStack() as sA:
            WS = [sb(sA, f"WS{i}", [128, 8, 512], BF16) for i in range(3)]
            wsi = {"i": 0}
            wdt = sb(sA, "wdt", [128, 8, 32], BF16)
            poolw = sb(sA, "poolw", [128, 8, 256], BF16)
            pcorr = sb(sA, "pcorr", [128, 4, 16], F32)
            posr = sb(sA, "posr", [128, 16], F32)
            tailx = sb(sA, "tailx", [128, 32, 3], F32)
            tailx0 = sb(sA, "tailx0", [128, 32, 3], F32)
            ptail = sb(sA, "ptail", [128, 8, 15], F32)
            xpw = [sb(sA, f"xpw{i}", [128, 15 + TS], F32) for i in range(2)]
            logD = sb(sA, "logD", [128, 32], F32)
            logDp = [logD, sb(sA, "logD1", [128, 32], F32)]
            ldi = {"i": 0}
            uT = sb(sA, "uT", [128, 8, TS], BF16)
            xn_late = True
            st1 = sb(sA, "st1", [128, 4], F32)
            pooledT = sb(sA, "pooledT", [128, 8, TS], BF16)
            mixedT = sb(sA, "mixedT", [128, 8, TS], BF16)
            gp = sb(sA, "gp", [128, 8, TS], BF16)
            pS = [sb(sA, f"pS{i}", [128, 15 + TS], F32) for i in range(2)]
            gsig = sb(sA, "gsig", [128, TS], F32)
            NSET = 3
            xcs = [sb(sA, f"xc{i}", [128, 3 + TS], F32) for i in range(NSET)]
            caccs = [sb(sA, f"cacc{i}", [128, TS], F32) for i in range(NSET)]
            big = sb(sA, "big", [128, 2080], F32)
            xn = big[:, 1024:2048]
            tmpD = sb(sA, "tmpD", [128, 512], F32)
            Bfs = [sb(sA, f"Bf{i}", [128, TS], F32) for i in range(2)]
            xs_tok = sb(sA, "xs_tok", [128, 2, 2048], BF16)
            B_tok = sb(sA, "B_tok", [128, 2, 1024], BF16)
            BT = sb(sA, "BT", [128, 8, TS], BF16)
            CT = sb(sA, "CT", [128, 8, TS], BF16)
            dtb = sb(sA, "dtb", [128, 2, 32], F32)
            ab = sb(sA, "ab", [128, 2, 32], F32)
            ab16 = sb(sA, "ab16", [128, 2, 32], BF16)
            sm = sb(sA, "sm", [128, 4, 32], F32)
            sz_tok = sb(sA, "sz_tok", [128, 2, 2048], BF16)
            xdt = sb(sA, "xdt", [128, 2048], BF16)
            xdtd = sb(sA, "xdtd", [128, 2048], BF16)
            AL = sb(sA, "AL", [128, 4, 128], BF16)
            AO = sb(sA, "AO", [128, 4, 128], BF16)
            LT = sb(sA, "LT", [128, 4, 128], BF16)
            Eb = sb(sA, "Eb", [128, 4, 128], BF16)
            CBm = sb(sA, "CBm", [128, 8, 128], BF16)
            MT = sb(sA, "MT", [128, 4, 128], BF16)
            CE = sb(sA, "CE", [128, 4, 128], BF16)
            yn = big
            junk = gsig
            ss8 = sb(sA, "ss8", [128, 8], F32)
            ynT = sb(sA, "ynT", [128, 16, TS], BF16)
            combT = sb(sA, "combT", [128, 8, TS], BF16)
            stS = big

            def wblock(src, rd):
                i = wsi["i"]; wsi["i"] = (i + 1) % 3
                dma("sp", WS[i][:], src.rearrange("(c p) n -> p c n", p=128), rd, [f"WS{i}"])
                return WS[i], f"WS{i}"

            dma("pool", wdt[:], w_in[:, OFF_DT:OFF_DT + 32].rearrange("(c p) n -> p c n", p=128), [], ["wdt"])
            dma("pool", poolw[:], pool_w.rearrange("(c p) n -> p c n", p=128), [], ["poolw"])
            dma("sp", posr[:], pos_d, [], ["posr"])
            for g, w in enumerate((2, 4, 8, 16)):
                ts("dve", pcorr[:, g, :], posr[:], float(w), None, ALU.min, None, ["posr"], ["pcorr"])
                P.op("dve", lambda e, g=g: e.reciprocal(out=pcorr[:, g, :], in_=pcorr[:, g, :]), rl(["pcorr"]), rl(["pcorr"]))
                ts("dve", pcorr[:, g, :], pcorr[:, g, :], float(w), None, ALU.mult, None, ["pcorr"], ["pcorr"])

            def make_uT(src_ap, rdname, n_tiles, col0=0):
                for tti in range(n_tiles):
                    xin = src_ap(tti)
                    act(junk_big[:], xin, AF.Square, [rdname], ["xn", "st1"], accum_out=st1[:, 0:1])
                    act(st1[:, 1:2], st1[:, 0:1], AF.Sqrt, ["st1"], ["st1"], bias=EPS, scale=1.0 / D)
                    P.op("dve", lambda e: e.reciprocal(out=st1[:, 2:3], in_=st1[:, 1:2]), rl(["st1"]), rl(["st1"]))
                    ts("dve", xn[:], xin, st1[:, 2:3], None, ALU.mult, None, [rdname, "st1"], ["xn"])
                    for half in range(2):
                        bk, br = bank()
                        trs([(bk[:, j * 128:(j + 1) * 128], xn[:, (half * 4 + j) * 128:(half * 4 + j + 1) * 128], identF[:])
                             for j in range(4)], ["xn", "identF"], [br])
                        for j in range(4):
                            dc = half * 4 + j
                            ts("dve" if half else "pool_no", uT[:, dc, col0 + tti * 128: col0 + (tti + 1) * 128], bk[:, j * 128:(j + 1) * 128],
                               gsc[:, dc:dc + 1], mcol[:, dc:dc + 1], ALU.mult, ALU.add, [br, "gsc", "mcol"], ["uT"])

            junk_big = xn

            _ts_orig = ts

            def ts(eng, out, a, s1, s2, op0, op1, rd, wr):
                if eng == "pool_no":
                    return P.op("act", lambda e: e.activation(out=out, in_=a, func=AF.Identity, bias=s2, scale=s1), rl(rd), rl(wr))
                return _ts_orig(eng, out, a, s1, s2, op0, op1, rd, wr)

            xh = xsA = big
            dma("sp", big[:, 0:D], x_halo, [], ["bigA"])
            make_uT(lambda tti: big[:, 0:D], "bigA", 1)
            for blk in range(10):
                c0 = OFF_X + blk * 512 if blk < 8 else OFF_P + (blk - 8) * 512
                wsb, wr_ = wblock(w_in_b[:, c0:c0 + 512], ["w_in_b"])
                for j in range(4):
                    bk, br = bank()
                    mms([(bk[:, 0:128], wsb[:, dc, j * 128:(j + 1) * 128], uT[:, dc, 0:128], dc == 0, dc == 7) for dc in range(8)],
                        [wr_, "uT"], [br])
                    if blk < 8:
                        c = blk * 4 + j
                        ts("dve", tailx0[:, c, :], bk[:, 125:128], nf[:, 0:1], None, ALU.mult, None, [br, "nf"], ["tailx0"])
                    else:
                        c = (blk - 8) * 4 + j
                        ts("dve", ptail[:, c, :], bk[:, 113:128], nf[:, 0:1], None, ALU.mult, None, [br, "nf"], ["ptail"])

            checkpoint("halo", {"tailx0": tailx0, "xpb": ptail, "uT": uT[:, :, 0:128]})
            def slab(si, pre, src=None, skip_uT=False, hook=None):
                t0 = 2 * si
                bstate["n"] = 8
                if skip_uT:
                    pass
                elif src is None:
                    make_uT(lambda tti: H[:, t0 + tti, :], "H", 2)
                else:
                    make_uT(src, "sz_tok", 2)
                if not pre:
                    for blk in range(2):
                        wsb, wr_ = wblock(w_in_b[:, OFF_P + blk * 512: OFF_P + (blk + 1) * 512], ["w_in_b"])
                        for j in range(4):
                            c = blk * 4 + j
                            g = c // 2
                            w = 2 << g
                            bk, br = bank()
                            mms([(bk[:, 0:TS], wsb[:, dc, j * 128:(j + 1) * 128], uT[:, dc, :], dc == 0, dc == 7) for dc in range(8)],
                                [wr_, "uT"], [br])
                            xw = xpw[c % 2]; xwn = f"xpw{c % 2}"
                            cp("pool", xw[:, 0:15], ptail[:, c, :], ["ptail"], [xwn])
                            cp("act", xw[:, 15:15 + TS], bk[:, 0:TS], [br], [xwn])
                            cp("pool", ptail[:, c, :], xw[:, TS:TS + 15], [xwn], ["ptail"])
                            src = xw[:]
                            lo = 0
                            step = 1
                            k = 0
                            while step < w:
                                dst = pS[k % 2]
                                nlo = lo + step
                                tt("dve", dst[:, nlo:15 + TS], src[:, nlo:15 + TS], src[:, nlo - step:15 + TS - step], ALU.add,
                                   [xwn, f"pS{(k + 1) % 2}"], [f"pS{k % 2}"])
                                src = dst[:]
                                lo = nlo
                                step *= 2
                                k += 1
                            sname = f"pS{(k - 1) % 2}"
                            if si == 0:
                                tt("dve", src[:, 15:31], src[:, 15:31], pcorr[:, g, :], ALU.mult, [sname, "pcorr"], [sname])
                            stt("dve", pooledT[:, c, :], src[:, 15:15 + TS], 1.0 / w, xw[:, 15:15 + TS], ALU.mult, ALU.subtract,
                                [sname, xwn], ["pooledT"])
                            pass
                    for g in range(4):
                        for dch in range(2):
                            bk, br = bank()
                            mms([(bk[:, 0:TS], poolw[:, 2 * g + kc, dch * 128:(dch + 1) * 128], pooledT[:, 2 * g + kc, :], kc == 0, kc == 1)
                                 for kc in range(2)], ["poolw", "pooledT"], [br])
                            ts("dve", mixedT[:, 2 * g + dch, :], bk[:, 0:TS], cols[:, C_PSC + 2 * g + dch:C_PSC + 2 * g + dch + 1], None,
                               ALU.mult, None, [br, "cols"], ["mixedT"])
                    for blk in range(2):
                        wsb, wr_ = wblock(wbp_b[:, blk * 512:(blk + 1) * 512], ["wbp_b"])
                        wsg, wg_ = wblock(w_in_b[:, OFF_GP + blk * 512: OFF_GP + (blk + 1) * 512], ["w_in_b"])
                        for j in range(4):
                            ec = blk * 4 + j
                            bk, br = bank()
                            mms([(bk[:, 0:TS], wsg[:, dc, j * 128:(j + 1) * 128], uT[:, dc, :], dc == 0, dc == 7) for dc in range(8)],
                                [wg_, "uT"], [br])
                            act(gsig[:], bk[:, 0:TS], AF.Sigmoid, [br], ["gsig"])
                            bk2, br2 = bank()
                            mms([(bk2[:, 0:TS], wsb[:, dc, j * 128:(j + 1) * 128], mixedT[:, dc, :], dc == 0, dc == 7) for dc in range(8)],
                                [wr_, "mixedT"], [br2])
                            tt("dve", gp[:, ec, :], bk2[:, 0:TS], gsig[:], ALU.mult, [br2, "gsig"], ["gp"])
                checkpoint(f"ut{int(pre)}", {"uT": uT})
                checkpoint(f"pool{int(pre)}", {"gp": gp})
                nblk = 6 if pre else 8
                pend = []

                def tick():
                    for p_ in pend:
                        p_[0] -= 1
                    while pend and pend[0][0] <= 0:
                        pend.pop(0)[1]()

                for blk in range(nblk):
                    wsb, wr_ = wblock(w_in_b[:, OFF_X + blk * 512: OFF_X + (blk + 1) * 512], ["w_in_b"])
                    for j in range(4):
                        c = blk * 4 + j
                        bk, br = bank()
                        mms([(bk[:, 0:TS], wsb[:, dc, j * 128:(j + 1) * 128], uT[:, dc, :], dc == 0, dc == 7) for dc in range(8)],
                            [wr_, "uT"], [br])
                        xc = xcs[c % NSET]; cacc = caccs[c % NSET]; xcn = f"xc{c % NSET}"; can = f"cacc{c % NSET}"
                        Bf = Bfs[c % 2]; Bfn = f"Bf{c % 2}"
                        cp("pool", xc[:, 0:3], tailx[:, c, :], [f"tailx{c}"], [xcn])
                        cp("act", xc[:, 3:3 + TS], bk[:, 0:TS], [br], [xcn])
                        cp("pool", tailx[:, c, :], xc[:, TS:TS + 3], [xcn], [f"tailx{c}"])
                        cw = lambda k_: cols[:, C_CW + c * 4 + k_: C_CW + c * 4 + k_ + 1]
                        P.op("act", lambda e, cacc=cacc, bk=bk, s_=cw(3): e.activation(out=cacc[:], in_=bk[:, 0:TS], func=AF.Copy, scale=s_),
                             rl([br, "cols"]), rl([can]))
                        for k_ in (0, 1, 2):
                            stt("dve", cacc[:], xc[:, k_:k_ + TS], cw(k_), cacc[:], ALU.mult, ALU.add, [xcn, "cols", can], [can])
                        bcol = cols[:, C_CB + c:C_CB + c + 1]
                        if c < 16:
                            hb = (c // 4) % 2; hbn = "bigA" if hb == 0 else "bigB"
                            pend.append([2, (lambda c=c, hb=hb, hbn=hbn, cacc=cacc, can=can, bcol=bcol:
                                             act(big[:, hb * 1024 + (c % 4) * TS: hb * 1024 + (c % 4 + 1) * TS], cacc[:], AF.Silu, [can, "cols"], [hbn], bias=bcol))])
                            if c % 4 == 3:
                                def st2x(cg=c // 4, hb=hb, hbn=hbn):
                                    for tti in range(2):
                                        bk2, br2 = bank()
                                        trs([(bk2[:, q * 128:(q + 1) * 128], big[:, hb * 1024 + q * TS + tti * 128: hb * 1024 + q * TS + (tti + 1) * 128], identF[:])
                                             for q in range(4)], [hbn, "identF"], [br2])
                                        cp("dve", xs_tok[:, tti, cg * 512:(cg + 1) * 512], bk2[:], [br2], ["xs_tok"])
                                pend.append([3, st2x])
                        elif c < 24:
                            g = c - 16
                            pend.append([2, (lambda Bf=Bf, Bfn=Bfn, cacc=cacc, can=can, bcol=bcol:
                                             act(Bf[:], cacc[:], AF.Silu, [can, "cols"], [Bfn], bias=bcol))])
                            def st2b(g=g, Bf=Bf, Bfn=Bfn):
                                cp("dve", BT[:, g, :], Bf[:], [Bfn], ["BT"])
                                bk2, br2 = bank()
                                trs([(bk2[:, tti * 128:(tti + 1) * 128], Bf[:, tti * 128:(tti + 1) * 128], identF[:]) for tti in range(2)],
                                    [Bfn, "identF"], [br2])
                                cp("dve", B_tok[:, :, g * 128:(g + 1) * 128], bk2[:, 0:256].rearrange("p (t n) -> p t n", n=128), [br2], ["B_tok"])
                            pend.append([2, st2b])
                        else:
                            g = c - 24
                            pend.append([2, (lambda g=g, cacc=cacc, can=can, bcol=bcol:
                                             act(CT[:, g, :], cacc[:], AF.Silu, [can, "cols"], ["CT"], bias=bcol))])
                        tick()
                while pend:
                    pend.pop(0)[1]()
                checkpoint(f"conv{int(pre)}", {"xs_tok": xs_tok, "B_tok": B_tok})
                for tti in range(2):
                    bk, br = bank()
                    mms([(bk[:, 0:32], uT[:, dc, tti * 128:(tti + 1) * 128], wdt[:, dc, :], dc == 0, dc == 7) for dc in range(8)],
                        ["uT", "wdt"], [br])
                    tt("dve", dtb[:, tti, :], bk[:, 0:32], rowS[:, 0:32], ALU.add, [br, "rowS"], ["dtb"])
                    act(dtb[:, tti, :], dtb[:, tti, :], AF.Exp, ["dtb"], ["dtb"])
                    act(dtb[:, tti, :], dtb[:, tti, :], AF.Ln, ["dtb"], ["dtb"], bias=1.0)
                    tt("dve", ab[:, tti, :], dtb[:, tti, :], rowS[:, 32:64], ALU.mult, ["dtb", "rowS"], ["ab"])
                    cp("dve", ab16[:, tti, :], ab[:, tti, :], ["ab"], ["ab16"])
                checkpoint(f"dt{int(pre)}", {"dtb": dtb, "ab": ab})
                if not pre:
                    for zc in range(4):
                        wsb, wr_ = wblock(w_in_b[:, OFF_Z + zc * 512: OFF_Z + (zc + 1) * 512], ["w_in_b"])
                        for tti in range(2):
                            bk, br = bank()
                            mms([(bk[:], uT[:, dc, tti * 128:(tti + 1) * 128], wsb[:, dc, :], dc == 0, dc == 7) for dc in range(8)],
                                ["uT", wr_], [br])
                            act(sz_tok[:, tti, zc * 512:(zc + 1) * 512], bk[:], AF.Silu, [br], ["sz_tok"])
                checkpoint(f"z{int(pre)}", {"sz_tok": sz_tok})
                if hook is not None:
                    hook()
                if not pre:
                    bstate["n"] = 4
                    bstate["i"] = 0
                for tti in range(2):
                    tsl = slice(tti * 128, (tti + 1) * 128)
                    bk, br = bank()
                    mms([(bk[:, 0:32], SLf[:], ab[:, tti, :], True, True)], ["SLf", "ab"], [br])
                    bkB, brB = bank()
                    mms([(bkB[:, 0:32], ONf[:], ab[:, tti, :], True, True)], ["ONf", "ab"], [brB])
                    act(sm[:, 0, :], bk[:, 0:32], AF.Exp, [br], ["sm"])
                    act(sm[:, 1, :], bkB[:, 0:32], AF.Exp, [brB], ["sm"])
                    checkpoint(f"ssdA{int(pre)}", {"sm": sm[:, 0:2, :]})
                    checkpoint(f"ssdA2{int(pre)}", {"logD": logD})
                    tt("dve", xdt[:].rearrange("p (h d) -> p h d", h=32), xs_tok[:, tti, :].rearrange("p (h d) -> p h d", h=32),
                       dtb[:, tti, :].unsqueeze(2).to_broadcast([128, 32, 64]), ALU.mult, ["xs_tok", "dtb"], ["xdt"])
                    checkpoint(f"ssdA3{int(pre)}", {"xdt": xdt})
                    tt(PB, xdtd[:].rearrange("p (h d) -> p h d", h=32), xdt[:].rearrange("p (h d) -> p h d", h=32),
                       sm[:, 0, :].unsqueeze(2).to_broadcast([128, 32, 64]), ALU.mult, ["xdt", "sm"], ["xdtd"])
                    checkpoint(f"ssdB{int(pre)}", {"xdt": xdt, "xdtd": xdtd})
                    if not pre:
                        for gh in range(2):
                            bk, br = bank()
                            mms([(bk[:, q * 128:(q + 1) * 128], BT[:, gh * 4 + q, tsl], CT[:, gh * 4 + q, tsl], True, True) for q in range(4)],
                                ["BT", "CT"], [br])
                            tt("dve", CBm[:, gh * 4:(gh + 1) * 4, :], bk[:].rearrange("p (q n) -> p q n", n=128),
                               MKf[:].unsqueeze(1).to_broadcast([128, 4, 128]), ALU.mult, [br, "MKf"], ["CBm"])
                        checkpoint(f"cb{int(pre)}", {"CBm": CBm})
                        ybanks = []
                        gb = {}
                        for g in range(9):
                            if g < 8:
                                a4 = ab16[:, tti, 4 * g:4 * g + 4].unsqueeze(2).to_broadcast([128, 4, 128])
                                tt("dve", AL[:], SLb[:].unsqueeze(1).to_broadcast([128, 4, 128]), a4, ALU.mult, ["SLb", "ab16"], ["AL"])
                                tt("dve", AO[:], ONb[:].unsqueeze(1).to_broadcast([128, 4, 128]), a4, ALU.mult, ["ONb", "ab16"], ["AO"])
                                bk, br = bank()
                                mms([(bk[:, q * 128:(q + 1) * 128], AL[:, q, :], UTb[:], True, True) for q in range(4)], ["AL", "UTb"], [br])
                                bk2, br2 = bank()
                                mms([(bk2[:, q * 128:(q + 1) * 128], AO[:, q, :], UTb[:], True, True) for q in range(4)], ["AO", "UTb"], [br2])
                                gb[g] = (bk, br, bk2, br2)
                            if g >= 1:
                                gp_ = g - 1
                                tt("dve", MT[:], LT[:], CBm[:, gp_, :].unsqueeze(1).to_broadcast([128, 4, 128]), ALU.mult, ["LT", "CBm"], ["MT"])
                                tt("dve", CE[:], Eb[:], CT[:, gp_, tsl].unsqueeze(1).to_broadcast([128, 4, 128]), ALU.mult, ["Eb", "CT"], ["CE"])
                            if g < 8:
                                bk, br, bk2, br2 = gb[g]
                                act(LT[:], bk[:].rearrange("p (q n) -> p q n", n=128), AF.Exp, [br], ["LT"])
                                act(Eb[:], bk2[:].rearrange("p (q n) -> p q n", n=128), AF.Exp, [br2], ["Eb"])
                            if g >= 1:
                                gp_ = g - 1
                                if gp_ % 2 == 0:
                                    yb, ybr = ybank(gp_ // 2)
                                    ybanks.append((yb, ybr))
                                lst = []
                                for q in range(4):
                                    h = 4 * gp_ + q
                                    o = yb[:, (gp_ % 2) * 256 + q * 64:(gp_ % 2) * 256 + (q + 1) * 64]
                                    lst.append((o, MT[:, q, :], xdt[:, h * 64:(h + 1) * 64], True, False))
                                    lst.append((o, CE[:, q, :], state_bf[:, h * 64:(h + 1) * 64], False, True))
                                mms(lst, ["MT", "CE", "xdt", "state_bf"], [ybr])
                        checkpoint(f"grp{int(pre)}", {"MT": MT, "CE": CE})
                        for b4 in range(4):
                            yb, ybr = ybanks[b4]
                            csl = slice(b4 * 512, (b4 + 1) * 512)
                            tt("dve", tmpD[:, 0:512].rearrange("p (h d) -> p h d", h=8), xs_tok[:, tti, csl].rearrange("p (h d) -> p h d", h=8),
                               rowS[:, 64 + b4 * 8:64 + b4 * 8 + 8].unsqueeze(2).to_broadcast([128, 8, 64]), ALU.mult,
                               ["xs_tok", "rowS"], ["tmpD"])
                            tt("dve", yn[:, csl], yb[:], tmpD[:, 0:512], ALU.add, [ybr, "tmpD"], ["yn"])
                            tt("dve", yn[:, csl], yn[:, csl], sz_tok[:, tti, csl], ALU.mult, ["yn", "sz_tok"], ["yn"])
                        for g in range(8):
                            act(junk[:], yn[:, g * 256:(g + 1) * 256], AF.Square, ["yn"], ["junk", "ss8"], accum_out=ss8[:, g:g + 1])
                        act(ss8[:], ss8[:], AF.Sqrt, ["ss8"], ["ss8"], bias=EPS, scale=1.0 / 256)
                        P.op("dve", lambda e: e.reciprocal(out=ss8[:], in_=ss8[:]), rl(["ss8"]), rl(["ss8"]))
                        tt("dve", yn[:, 0:2048].rearrange("p (g d) -> p g d", g=8), yn[:, 0:2048].rearrange("p (g d) -> p g d", g=8),
                           ss8[:].unsqueeze(2).to_broadcast([128, 8, 256]), ALU.mult, ["yn", "ss8"], ["yn"])
                        checkpoint(f"yn{int(pre)}", {"yn": yn[:, 0:2048]})
                        for q4 in range(4):
                            bk, br = bank()
                            trs([(bk[:, q * 128:(q + 1) * 128], yn[:, (q4 * 4 + q) * 128:(q4 * 4 + q + 1) * 128], identF[:]) for q in range(4)],
                                ["yn", "identF"], [br])
                            cp("act", ynT[:, q4 * 4:(q4 + 1) * 4, tsl], bk[:].rearrange("p (q n) -> p q n", n=128), [br], ["ynT"])
                    checkpoint(f"ynT{int(pre)}", {"ynT": ynT[:, :, 0:128]})
                    sbanks = []
                    for b4 in range(4):
                        bk, br = bank()
                        mms([(bk[:, q * 256:(q + 1) * 256], B_tok[:, tti, (2 * b4 + q) * 128:(2 * b4 + q + 1) * 128],
                              xdtd[:, (2 * b4 + q) * 256:(2 * b4 + q + 1) * 256], True, True) for q in range(2)], ["B_tok", "xdtd"], [br])
                        sbanks.append((bk, br))
                    checkpoint(f"ssdC{int(pre)}", {"xdt": xdt})
                    tt(PB, state[:].rearrange("p (h d) -> p h d", h=32), state[:].rearrange("p (h d) -> p h d", h=32),
                       sm[:, 1, :].unsqueeze(2).to_broadcast([128, 32, 64]), ALU.mult, ["state", "sm", "state_bf"], ["state"])
                    for b4 in range(4):
                        bk, br = sbanks[b4]
                        tt("dve", state[:, b4 * 512:(b4 + 1) * 512], bk[:], state[:, b4 * 512:(b4 + 1) * 512], ALU.add, ["state", br], ["state"])
                    if not pre:
                        cp("act", state_bf[:], state[:], ["state"], ["state_bf"])
                if pre:
                    return
                for eh in range(2):
                    pb = [ybank(q_) for q_ in range(4)]
                    for kh in range(2):
                        wsb, wr_ = wblock(wbs_b[kh * 1024:(kh + 1) * 1024, eh * 512:(eh + 1) * 512], ["wbs_b"])
                        for j in range(4):
                            mms([(pb[j][0][:, 0:TS], wsb[:, kc, j * 128:(j + 1) * 128], ynT[:, kh * 8 + kc, :], (kh == 0 and kc == 0),
                                  (kh == 1 and kc == 7)) for kc in range(8)], [wr_, "ynT"], [pb[j][1]])
                    wsg, wg_ = wblock(w_in_b[:, OFF_GS + eh * 512: OFF_GS + (eh + 1) * 512], ["w_in_b"])
                    for j in range(4):
                        ec = eh * 4 + j
                        bk, br = bank()
                        mms([(bk[:, 0:TS], wsg[:, dc, j * 128:(j + 1) * 128], uT[:, dc, :], dc == 0, dc == 7) for dc in range(8)],
                            [wg_, "uT"], [br])
                        act(gsig[:], bk[:, 0:TS], AF.Sigmoid, [br], ["gsig"])
                        tt("dve", gsig[:], pb[j][0][:, 0:TS], gsig[:], ALU.mult, [pb[j][1], "gsig"], ["gsig"])
                        tt("dve", combT[:, ec, :], gsig[:], gp[:, ec, :], ALU.add, ["gsig", "gp"], ["combT"])
                checkpoint(f"comb{int(pre)}", {"combT": combT})
                wo = [wblock(wout_b[:, hh * 512:(hh + 1) * 512], ["wout_b"]) for hh in range(2)]
                for tti in range(2):
                    for hh in range(2):
                        bk, br = bank()
                        mms([(bk[:], combT[:, ec, tti * 128:(tti + 1) * 128], wo[hh][0][:, ec, :], ec == 0, ec == 7) for ec in range(8)],
                            ["combT", wo[hh][1]], [br])
                        tt("dve", H[:, t0 + tti, hh * 512:(hh + 1) * 512], bk[:], H[:, t0 + tti, hh * 512:(hh + 1) * 512], ALU.add,
                           ["H", br], ["H"])

            xst = lambda tti: sz_tok[:, tti, :].bitcast(F32)
            Iacc = ynT[:].rearrange("p a b -> p (a b)").bitcast(F32)
            P.op("pool", lambda e: e.memset(state[:], 0.0), [], rl(["state"]))
            P.op("pool", lambda e: e.memset(Iacc, 0.0), [], rl(["ynT"]))
            P.op("pool", lambda e: e.memset(tailx[:], 0.0), [], rl([f"tailx{c_}" for c_ in range(32)]))
            seq = [(j, si) for j in range(n_pre_slots) for si in range(n_slabs)]

            def prep_uT(idx):
                j, si = seq[idx]
                r0 = j * T + si * TS
                for tti in range(2):
                    dma("sp", xst(tti), x_all[r0 + tti * 128: r0 + (tti + 1) * 128, :], [], ["sz_tok"])
                make_uT(xst, "sz_tok", 2)

            if seq:
                prep_uT(0)
            for idx, (j, si) in enumerate(seq):
                hk = (lambda idx=idx: prep_uT(idx + 1)) if idx + 1 < len(seq) else None
                slab(si, True, src=xst, skip_uT=True, hook=hk)
                if si == n_slabs - 1:
                    stt("dve", Iacc, state[:], rk[:, j:j + 1], Iacc, ALU.mult, ALU.add, ["state", "rk", "ynT"], ["ynT"])
            checkpoint("pre", {"state": state, "xs_tok": xs_tok, "B_tok": B_tok, "dtb": dtb})
            cp("dve", state[:], Iacc, ["ynT"], ["state"])
            cp("act", state_bf[:], state[:], ["state"], ["state_bf"])
            cp("pool", tailx[:], tailx0[:], ["tailx0"], [f"tailx{c_}" for c_ in range(32)])
            checkpoint("xchg", {"state": state})
            for si in range(n_slabs):
                slab(si, False)
            checkpoint("main", {"H": H, "ynT": ynT, "gp": gp, "combT": combT, "sz_tok": sz_tok, "CT": CT, "BT": BT})
            barrier()
            P.flush()

        if DEBUG:
            dbg_out = nc.dram_tensor("dbg_h", [T, D], F32, kind="ExternalOutput").ap()
            dma("sp", dbg_out.rearrange("(t p) d -> p t d", p=128), H[:], ["H"], [])

        with ExitStack() as sB:
            U2 = sb(sB, "U2", [128, 8, T], BF16)
            u2f = sb(sB, "u2f", [128, 8, 128], F32)
            xn2 = sb(sB, "xn2", [128, D], F32)
            st2 = sb(sB, "st2", [128, 4], F32)
            wr = sb(sB, "wr", [128, 8, 64], F32)
            wtab = sb(sB, "wtab", [128, NT, 65], F32)
            r64 = [sb(sB, f"r64_{i}", [128, 64], F32) for i in range(4)]
            r8 = [sb(sB, f"r8_{i}", [128, 8], F32) for i in range(5)]
            wguS = [sb(sB, f"wguS{i}", [128, 8, 512], BF16) for i in range(2)]
            wdF = [sb(sB, f"wdF{i}", [128, 2, D], F32) for i in range(2)]
            wdS = [sb(sB, f"wdS{i}", [128, 2, D], BF16) for i in range(2)]
            hidT = [sb(sB, f"hidT{i}", [128, 2, T], BF16) for i in range(2)]
            sg = [sb(sB, f"sg{i}", [128, 512], BF16) for i in range(2)]
            fng = sb(sB, "fng", [128, D], F32)
            g2bc = sb(sB, "g2bc", [128, D], F32)
            dma("sp", g2bc[:], g2_d, ["g2_d"], ["g2bc"])
            ob = [sb(sB, f"ob{i}", [128, D], F32) for i in range(2)]

            dma("sp", wr[:], w_router.rearrange("(c p) n -> p c n", p=128), [], ["wr"])
            dma("sp", fng[:], rows_d[:, R_FNG:R_FNG + D], [], ["fng"])
            P.op("pool", lambda e: e.memset(wtab[:], 1.0), [], rl(["wtab"]))
            for ti in range(NT):
                xin = H[:, ti, :]
                act(xn2[:], xin, AF.Square, ["H"], ["xn2", "st2"], accum_out=st2[:, 0:1])
                act(st2[:, 1:2], st2[:, 0:1], AF.Sqrt, ["st2"], ["st2"], bias=EPS, scale=1.0 / D)
                P.op("dve", lambda e: e.reciprocal(out=st2[:, 2:3], in_=st2[:, 1:2]), rl(["st2"]), rl(["st2"]))
                ts("dve", xn2[:], xin, st2[:, 2:3], None, ALU.mult, None, ["H", "st2"], ["xn2"])
                for half in range(2):
                    bk, br = bank()
                    trs([(bk[:, j * 128:(j + 1) * 128], xn2[:, (half * 4 + j) * 128:(half * 4 + j + 1) * 128], identF[:]) for j in range(4)],
                        ["xn2", "identF"], [br])
                    for j in range(4):
                        dc = half * 4 + j
                        P.op("act", lambda e, dc=dc, j=j, bk=bk: e.activation(out=u2f[:, dc, :], in_=bk[:, j * 128:(j + 1) * 128], func=AF.Identity,
                                                                         bias=mcol[:, 16 + dc:17 + dc], scale=gsc[:, 8 + dc:9 + dc]),
                             rl([br, "gsc", "mcol"]), rl(["u2f"]))
                cp("pool", U2[:, :, ti * 128:(ti + 1) * 128], u2f[:], ["u2f"], ["U2"])
                bk, br = bank()
                mms([(bk[:, 0:64], u2f[:, dc, :], wr[:, dc, :], dc == 0, dc == 7) for dc in range(8)], ["u2f", "wr"], [br])
                s_, ch, t1, t2 = r64
                m1, m2, gs, g8, gm = r8
                act(s_[:], bk[:, 0:64], AF.Sigmoid, [br], ["r0"])
                tt("dve", ch[:], s_[:], rowS[:, 96:160], ALU.add, ["r0", "rowS"], ["r1"])
                v3 = lambda a: a[:].rearrange("p (g e) -> p g e", g=8)
                P.op("dve", lambda e: e.tensor_reduce(out=m1[:], in_=v3(ch), axis=AX.X, op=ALU.max), rl(["r1"]), rl(["q0"]))
                tt("dve", v3(t1), v3(ch), m1[:].unsqueeze(2).to_broadcast([128, 8, 8]), ALU.is_equal, ["r1", "q0"], ["r2"])
                stt("dve", t1[:], t1[:], -BIG, ch[:], ALU.mult, ALU.add, ["r2", "r1"], ["r2"])
                P.op("dve", lambda e: e.tensor_reduce(out=m2[:], in_=v3(t1), axis=AX.X, op=ALU.max), rl(["r2"]), rl(["q1"]))
                tt("dve", gs[:], m1[:], m2[:], ALU.add, ["q0", "q1"], ["q2"])
                P.op("dve", lambda e: e.max(out=g8[:], in_=gs[:]), rl(["q2"]), rl(["q3"]))
                ts("dve", gm[:], gs[:], g8[:, 3:4], None, ALU.is_ge, None, ["q2", "q3"], ["q4"])
                tt("dve", v3(t1), v3(ch), gm[:].unsqueeze(2).to_broadcast([128, 8, 8]), ALU.mult, ["r1", "q4"], ["r2"])
                ts("dve", gm[:], gm[:], -1.0, BIG, ALU.add, ALU.mult, ["q4"], ["q4"])
                tt("dve", v3(t1), v3(t1), gm[:].unsqueeze(2).to_broadcast([128, 8, 8]), ALU.add, ["r2", "q4"], ["r2"])
                P.op("dve", lambda e: e.max(out=g8[:], in_=t1[:]), rl(["r2"]), rl(["q3"]))
                ts("dve", t2[:], t1[:], g8[:, 7:8], None, ALU.is_ge, None, ["r2", "q3"], ["r3"])
                tt("dve", t2[:], t2[:], s_[:], ALU.mult, ["r3", "r0"], ["r3"])
                P.op("dve", lambda e: e.tensor_reduce(out=m1[:, 0:1], in_=t2[:], axis=AX.X, op=ALU.add), rl(["r3"]), rl(["q0"]))
                P.op("dve", lambda e: e.reciprocal(out=m1[:, 0:1], in_=m1[:, 0:1]), rl(["q0"]), rl(["q0"]))
                ts("dve", wtab[:, ti, 0:64], t2[:], m1[:, 0:1], 2.5, ALU.mult, ALU.mult, ["r3", "q0"], ["wtab"])

            ne = n_experts if do_moe else 0
            for e_ in range(ne):
                b = e_ % 2
                P.dma("pool", lambda e, e_=e_, b=b: e.dma_start(out=wguS[b][:], in_=wgu[e_], max_dma_last_dim=4096), rl([]), rl([f"wguS{b}"]))
                dma("sp", wdF[b][:], wdn[e_], [], [f"wdF{b}"])
                tt("pool", wdS[b][:], wdF[b][:], g2bc[:].unsqueeze(1).to_broadcast([128, 2, D]), ALU.mult, [f"wdF{b}", "g2bc"], [f"wdS{b}"])
                k = 0
                for jc in range(2):
                    for sl in range(4):
                        bg, bgr = bank()
                        bu, bur = bank()
                        rhs = lambda dc: U2[:, dc, sl * 512:(sl + 1) * 512]
                        mms([(bg[:], wguS[b][:, dc, jc * 128:(jc + 1) * 128], rhs(dc), dc == 0, dc == 7) for dc in range(8)],
                            [f"wguS{b}", "U2"], [bgr])
                        mms([(bu[:], wguS[b][:, dc, 256 + jc * 128:256 + (jc + 1) * 128], rhs(dc), dc == 0, dc == 7) for dc in range(8)],
                            [f"wguS{b}", "U2"], [bur])
                        act(sg[k % 2][:], bg[:], AF.Silu, [bgr], [f"sg{k % 2}"])
                        tt("dve", hidT[b][:, jc, sl * 512:(sl + 1) * 512], bu[:], sg[k % 2][:], ALU.mult, [bur, f"sg{k % 2}"], [f"hidT{b}"])
                        k += 1
                for ti in range(NT):
                    for hh in range(2):
                        bk, br = ybank()
                        mms([(bk[:], hidT[b][:, jc, ti * 128:(ti + 1) * 128], wdS[b][:, jc, hh * 512:(hh + 1) * 512], jc == 0, jc == 1)
                             for jc in range(2)], [f"hidT{b}", f"wdS{b}"], [br])
                        stt("dve", H[:, ti, hh * 512:(hh + 1) * 512], bk[:], wtab[:, ti, e_:e_ + 1], H[:, ti, hh * 512:(hh + 1) * 512],
                            ALU.mult, ALU.add, [br, "wtab", f"Ht{ti}"], [f"Ht{ti}"])
            for ti in range(NT):
                b = ti % 2
                act(xn2[:], H[:, ti, :], AF.Square, ["H", f"Ht{ti}"], ["xn2", "st2"], accum_out=st2[:, 0:1])
                act(st2[:, 1:2], st2[:, 0:1], AF.Sqrt, ["st2"], ["st2"], bias=EPS, scale=1.0 / D)
                P.op("dve", lambda e: e.reciprocal(out=st2[:, 2:3], in_=st2[:, 1:2]), rl(["st2"]), rl(["st2"]))
                stt("dve", ob[b][:], H[:, ti, :], st2[:, 2:3], fng[:], ALU.mult, ALU.mult, ["H", f"Ht{ti}", "st2", "fng"], [f"ob{b}"])
                dma("sp", out_d[ti * 128:(ti + 1) * 128, :], ob[b][:], [f"ob{b}"], [])
            P.wait_all("sp", P.all_events())
            P.flush()
    except _Stop:
        return nc
    top.close()
    _top0.close()
    return nc


def _colform(v):
    v = np.asarray(v, np.float32).reshape(-1)
    return np.ascontiguousarray(v.reshape(-1, 128).T)


def _prep_inputs(x, c, w_ada, b_ada, norm_mix_g, w_in, conv_w, conv_b, dt_bias, A_log, D_skip, ssd_norm_g,
                 pool_w, pool_scale, w_br_ssd, w_br_pool, w_out, norm_ffn_g, w_router, router_bias,
                 we_gate, we_up, we_down, ws_gate, ws_up, ws_down, final_norm_g):
    f = lambda a: np.ascontiguousarray(np.asarray(a, np.float32))
    x2 = f(x).reshape(NCORES * T, D)
    cols = np.zeros((128, NCOL), np.float32)
    cols[:, C_NMG:C_NMG + 8] = _colform(norm_mix_g)
    cols[:, C_NFG:C_NFG + 8] = _colform(norm_ffn_g)
    cols[:, C_PSC:C_PSC + 8] = _colform(pool_scale)
    cols[:, C_SNG:C_SNG + 16] = _colform(ssd_norm_g)
    cols[:, C_C:C_C + 8] = _colform(c)
    cols[:, C_CB:C_CB + 32] = _colform(conv_b)
    cw = f(conv_w).reshape(4, 32, 128)
    cols[:, C_CW:C_CW + 128] = cw.transpose(2, 1, 0).reshape(128, 128)
    rows = np.zeros((1, NROW), np.float32)
    rows[0, R_BADA:R_BADA + 6144] = f(b_ada).reshape(-1)
    rows[0, R_DTB:R_DTB + 32] = f(dt_bias).reshape(-1)
    rows[0, R_ALOG:R_ALOG + 32] = f(A_log).reshape(-1)
    rows[0, R_DSK:R_DSK + 32] = f(D_skip).reshape(-1)
    rows[0, R_RB:R_RB + 64] = f(router_bias).reshape(-1)
    rows[0, R_FNG:R_FNG + 1024] = f(final_norm_g).reshape(-1)
    rows = np.ascontiguousarray(np.broadcast_to(rows, (128, NROW)))
    wg = f(we_gate)[0].reshape(64, 8, 128, 256)
    wu = f(we_up)[0].reshape(64, 8, 128, 256)
    wgu = np.empty((65, 128, 8, 512), np.float32)
    wgu[:64, :, :, :256] = wg.transpose(0, 2, 1, 3)
    wgu[:64, :, :, 256:] = wu.transpose(0, 2, 1, 3)
    wgu[64, :, :, :256] = f(ws_gate)[0].reshape(8, 128, 256).transpose(1, 0, 2)
    wgu[64, :, :, 256:] = f(ws_up)[0].reshape(8, 128, 256).transpose(1, 0, 2)
    wdn = np.empty((65, 128, 2, 1024), np.float32)
    wdn[:64] = f(we_down)[0].reshape(64, 2, 128, 1024).transpose(0, 2, 1, 3)
    wdn[64] = f(ws_down)[0].reshape(2, 128, 1024).transpose(1, 0, 2)
    shared = {
        "cols": cols, "rows": rows, "w_ada": f(w_ada)[0], "w_in": f(w_in)[0],
        "pool_w": f(pool_w)[0].reshape(1024, 256), "w_br_ssd": f(w_br_ssd)[0], "w_br_pool": f(w_br_pool)[0],
        "w_out": f(w_out)[0], "w_router": f(w_router)[0], "wgu": wgu, "wdn": wdn,
    }
    in_maps = []
    for k in range(NCORES):
        m = dict(shared)
        m["x_own"] = x2[k * T:(k + 1) * T]
        m["x_halo"] = x2[k * T - 128:k * T] if k > 0 else np.zeros((128, D), np.float32)
        m["rk"] = np.ascontiguousarray(np.broadcast_to((np.arange(8) == k - 1).astype(np.float32)[None, :], (128, 8)))
        m["x_all"] = x2[:(NCORES - 1) * T]
        m["nf"] = np.full((128, 1), 1.0 if k > 0 else 0.0, np.float32)
        m["posrow"] = np.ascontiguousarray(np.broadcast_to((k * T + 1 + np.arange(16)).astype(np.float32)[None, :], (128, 16)))
        in_maps.append(m)
    return in_maps


_NC_CACHE = {}


def kernel(**inputs):
    in_maps = _prep_inputs(**inputs)
    if "nc" not in _NC_CACHE:
        _NC_CACHE["nc"] = build_nc()
    res = run_bass_kernel_spmd(_NC_CACHE["nc"], in_maps, core_ids=list(range(NCORES)))
    out = np.concatenate([np.asarray(r["out"], np.float32) for r in res.results], axis=0)
    return out.reshape(1, NCORES * T, D)
```

```python
import numpy as np
import concourse.bass as bass
import concourse.mybir as mybir

F32 = mybir.dt.float32
BF16 = mybir.dt.bfloat16
AF = mybir.ActivationFunctionType
ALU = mybir.AluOpType
AX = mybir.AxisListType


class Reg:
    __slots__ = ("name", "w", "r")

    ALL = []

    def __init__(self, name):
        self.name = name
        self.w = None
        self.r = []
        Reg.ALL.append(self)


class Prog:
    ENGS = ("pe", "act", "dve", "pool", "sp")

    def __init__(self, nc, ndma_sems=8, same_engine_sync=True):
        self.nc = nc
        self.q = {e: [] for e in self.ENGS}
        self.cnt = {e: 0 for e in self.ENGS}
        self.known = {e: {} for e in self.ENGS}
        self.sems = {}
        self.same = same_engine_sync
        self.ndma = ndma_sems
        self.dma_rr = {e: 0 for e in self.ENGS}
        self.dma_val = {}
        self.stack = None

    def _semkeys(self):
        keys = list(self.ENGS) + ["cc"]
        for e in ("sp", "pool", "act"):
            for i in range(self.ndma):
                keys.append(f"d_{e}_{i}")
        return keys

    def _need(self, eng, events):
        mx = {}
        for ev in events:
            if ev is None:
                continue
            k, v = ev
            if k == eng and not self.same:
                continue
            if k == "cc" and eng != "pool":
                continue
            if v > mx.get(k, 0):
                mx[k] = v
        out = []
        kn = self.known[eng]
        for k, v in mx.items():
            if kn.get(k, 0) >= v:
                continue
            kn[k] = v
            out.append((k, v))
        return out

    def _deps(self, reads, writes):
        evs = []
        for r in reads:
            evs.append(r.w)
        for w in writes:
            evs.append(w.w)
            evs.extend(w.r)
        return evs

    def _commit(self, ev, reads, writes):
        for r in reads:
            r.r.append(ev)
        for w in writes:
            w.w = ev
            w.r = []

    def op(self, eng, fn, reads=(), writes=(), extra=()):
        evs = self._deps(reads, writes) + list(extra)
        waits = self._need(eng, evs)
        self.cnt[eng] += 1
        ev = (eng, self.cnt[eng])
        self.q[eng].append((waits, fn, (eng, 1)))
        self._commit(ev, reads, writes)
        return ev

    def dma(self, eng, fn, reads=(), writes=(), extra=()):
        i = self.dma_rr[eng]
        self.dma_rr[eng] = (i + 1) % self.ndma
        key = f"d_{eng}_{i}"
        prev = self.dma_val.get(key, 0)
        evs = self._deps(reads, writes) + list(extra)
        if prev:
            evs.append((key, prev))
        waits = self._need(eng, evs)
        val = prev + 16
        self.dma_val[key] = val
        ev = (key, val)
        self.q[eng].append((waits, fn, (key, 16)))
        self._commit(ev, reads, writes)
        return ev

    def cc(self, fn, reads=(), writes=(), extra=(), inc=1):
        eng = "pool"
        evs = self._deps(reads, writes) + list(extra)
        waits = self._need(eng, evs)
        val = self.dma_val.get("cc", 0) + inc
        self.dma_val["cc"] = val
        ev = ("cc", val)
        self.q[eng].append((waits, fn, ("cc", inc)))
        self._commit(ev, reads, writes)
        return ev

    def wait_all(self, eng, events):
        waits = self._need(eng, list(events))
        self.q[eng].append((waits, None, None))

    def all_events(self):
        evs = [(e, c) for e, c in self.cnt.items() if c]
        evs += [(k, v) for k, v in self.dma_val.items()]
        return evs

    def begin(self, stack):
        self.gen = 0
        self.semh = {k: stack.enter_context(self.nc.semaphore(k)) for k in self._semkeys()}

    def rebase(self, stack):
        self.gen += 1
        self.semh = {k: stack.enter_context(self.nc.semaphore(f"{k}_g{self.gen}")) for k in self._semkeys()}
        self.cnt = {e: 0 for e in self.ENGS}
        self.known = {e: {} for e in self.ENGS}
        self.dma_rr = {e: 0 for e in self.ENGS}
        self.dma_val = {}
        for r in Reg.ALL:
            r.w = None
            r.r = []

    def flush(self):
        nc = self.nc
        sems = self.semh
        q = self.q

        def run(engh, lst):
            for waits, fn, inc in lst:
                for k, v in waits:
                    engh.wait_ge(sems[k], v)
                if fn is None:
                    continue
                ins = fn(engh)
                ins.then_inc(sems[inc[0]], inc[1])

        with nc.Block() as block:
            @block.tensor
            def _(e):
                run(e, q["pe"])

            @block.scalar
            def _(e):
                run(e, q["act"])

            @block.vector
            def _(e):
                run(e, q["dve"])

            @block.gpsimd
            def _(e):
                run(e, q["pool"])

            @block.sync
            def _(e):
                run(e, q["sp"])
        self.q = {e: [] for e in self.ENGS}


from contextlib import ExitStack
from concourse.bass_utils import run_bass_kernel_spmd

NCORES = 8
T = 2048
NT = 16
TS = 256
NSL = T // TS
D = 1024
EPS = 1e-6
OFF_Z, OFF_X, OFF_B, OFF_C, OFF_DT, OFF_P, OFF_GS, OFF_GP = 0, 2048, 4096, 5120, 6144, 6176, 7200, 8224
DIN = 9248
PB = "dve"
NCOL = 208
C_NMG, C_NFG, C_PSC, C_SNG, C_C, C_CB, C_CW = 0, 8, 16, 24, 40, 48, 80
R_BADA, R_DTB, R_ALOG, R_DSK, R_RB, R_FNG = 0, 6144, 6176, 6208, 6240, 6304
NROW = 7328
BIG = 1.0e4
DEBUG = False
import os
DBGV = int(os.environ.get("DBGV", "0"))


class _Stop(Exception):
    pass


def build_nc(n_experts=65, do_moe=True, stop_after=None, n_slabs=NSL, n_pre_slots=NCORES - 1):
    nc = bass.Bass("TRN2", target_bir_lowering=False)
    di = lambda n, s, dt=F32: nc.dram_tensor(n, s, dt, kind="ExternalInput").ap()
    x_own = di("x_own", [T, D]); x_halo = di("x_halo", [128, D]); x_all = di("x_all", [(NCORES - 1) * T, D])
    rk_d = di("rk", [128, 8]); nf_d = di("nf", [128, 1]); pos_d = di("posrow", [128, 16])
    cols_d = di("cols", [128, NCOL]); rows_d = di("rows", [128, NROW])
    w_ada = di("w_ada", [D, 6 * D]); w_in = di("w_in", [D, DIN])
    pool_w = di("pool_w", [1024, 256]); w_br_ssd = di("w_br_ssd", [2048, D])
    w_br_pool = di("w_br_pool", [D, D]); w_out = di("w_out", [D, D])
    w_router = di("w_router", [D, 64])
    NEW = 65 if stop_after is None else 1
    wgu = di("wgu", [NEW, 128, 8, 512]); wdn = di("wdn", [NEW, 128, 2, 1024])
    out_d = nc.dram_tensor("out", [T, D], F32, kind="ExternalOutput").ap()
    w_in_b = nc.dram_tensor("w_in_b", [D, DIN], BF16).ap()
    wbp_b = nc.dram_tensor("wbp_b", [D, D], BF16).ap()
    wbs_b = nc.dram_tensor("wbs_b", [2048, D], BF16).ap()
    wout_b = nc.dram_tensor("wout_b", [D, D], BF16).ap()
    bounce = nc.dram_tensor("bounce", [128, 2080], F32).ap()
    gathered = nc.dram_tensor("gathered", [1024, 2080], F32).ap()
    dbg = {}

    Reg.ALL.clear()
    P = Prog(nc)
    regs = {}
    _top0 = ExitStack()
    P.begin(_top0)

    ALIAS = {"yn": ["bigA", "bigB"], "big": ["bigA", "bigB"], "junk": ["gsig"], "xn": ["bigB"]}

    def R(name):
        if name not in regs:
            regs[name] = Reg(name)
        return regs[name]

    top = ExitStack()

    def sb(st, name, shape, dt):
        return st.enter_context(nc.sbuf_tensor("s_" + name, shape, dt))

    def rl(x):
        out = []
        for n in x:
            if isinstance(n, str):
                for m in ALIAS.get(n, [n]):
                    out.append(R(m))
            else:
                out.append(n)
        return out

    def tt(eng, out, a, b, op, rd, wr):
        return P.op(eng, lambda e: e.tensor_tensor(out=out, in0=a, in1=b, op=op), rl(rd), rl(wr))

    def ts(eng, out, a, s1, s2, op0, op1, rd, wr):
        if op1 is None:
            return P.op(eng, lambda e: e.tensor_single_scalar(out=out, in_=a, scalar=s1, op=op0), rl(rd), rl(wr))
        return P.op(eng, lambda e: e.tensor_scalar(out=out, in0=a, scalar1=s1, scalar2=s2, op0=op0, op1=op1), rl(rd), rl(wr))

    def stt(eng, out, a, sc, b, op0, op1, rd, wr):
        return P.op(eng, lambda e: e.scalar_tensor_tensor(out=out, in0=a, scalar=sc, in1=b, op0=op0, op1=op1), rl(rd), rl(wr))

    def act(out, in_, func, rd, wr, bias=0.0, scale=1.0, accum_out=None):
        if accum_out is None:
            return P.op("act", lambda e: e.activation(out=out, in_=in_, func=func, bias=bias, scale=scale), rl(rd), rl(wr))
        return P.op("act", lambda e: e.activation(out=out, in_=in_, func=func, bias=bias, scale=scale, accum_out=accum_out), rl(rd), rl(wr))

    def cp(eng, out, in_, rd, wr):
        if eng == "act":
            return P.op("act", lambda e: e.copy(out=out, in_=in_), rl(rd), rl(wr))
        return P.op(eng, lambda e: e.tensor_copy(out=out, in_=in_), rl(rd), rl(wr))

    def mms(lst, rd, wr):
        def f(e):
            for (o, l, r, s0, s1) in lst:
                ins = e.matmul(o, lhsT=l, rhs=r, start=s0, stop=s1)
            return ins
        return P.op("pe", f, rl(rd), rl(wr))

    def trs(lst, rd, wr):
        def f(e):
            for (o, i, idn) in lst:
                ins = e.transpose(out=o, in_=i, identity=idn)
            return ins
        return P.op("pe", f, rl(rd), rl(wr))

    def dma(q, out, in_, rd, wr):
        if q == "pool":
            return P.dma(q, lambda e: e.dma_start(out=out, in_=in_, max_dma_last_dim=4096), rl(rd), rl(wr))
        return P.dma(q, lambda e: e.dma_start(out=out, in_=in_), rl(rd), rl(wr))

    def barrier():
        evs = P.all_events()
        for e in ("pe", "act", "dve", "pool", "sp"):
            P.wait_all(e, evs)

    def checkpoint(name, tensors):
        if stop_after != name:
            return
        for tn, t in tensors.items():
            a = t if hasattr(t, "ap") and not hasattr(t, "__enter__") and type(t).__name__ == "AP" else t[:]
            shp = list(a.shape)
            d = nc.dram_tensor("dbg_" + tn, shp, a.dtype, kind="ExternalOutput").ap()
            dma("sp", d, a, [tn], [])
        P.wait_all("sp", P.all_events())
        P.flush()
        raise _Stop()

    banks = [top.enter_context(nc.psum_tensor(f"bank{i}", [128, 512], F32)) for i in range(8)]
    bstate = {"i": 0, "y": 0}

    def bank():
        n = bstate.get("n", 4)
        i = bstate["i"] % n
        bstate["i"] = (i + 1) % n
        return banks[i], R(f"bank{i}")

    def ybank(i=None):
        if i is None:
            i = bstate["y"]
            bstate["y"] = (i + 1) % 4
        return banks[4 + i], R(f"bank{4 + i}")

    H = sb(top, "H", [128, NT, D], F32)
    state = sb(top, "state", [128, 2048], F32)
    state_bf = sb(top, "state_bf", [128, 2048], BF16)
    identF = sb(top, "identF", [128, 128], F32)
    SLf = sb(top, "SLf", [128, 128], F32)
    ONf = sb(top, "ONf", [128, 128], F32)
    SLb = sb(top, "SLb", [128, 128], BF16)
    ONb = sb(top, "ONb", [128, 128], BF16)
    UTb = sb(top, "UTb", [128, 128], BF16)
    MKf = sb(top, "MKf", [128, 128], F32)
    cols = sb(top, "cols", [128, NCOL], F32)
    rowS = sb(top, "rowS", [128, 160], F32)
    rk = sb(top, "rk_s", [128, 8], F32)
    nf = sb(top, "nf_s", [128, 1], F32)
    mcol = sb(top, "mcol", [128, 32], F32)
    gsc = sb(top, "gsc", [128, 16], F32)
    g2_d = nc.dram_tensor("g2_d", [128, D], F32).ap()

    def mask(tn, tensor, cmp, sgn=1):
        P.op("pool", lambda e: e.memset(tensor[:], 1.0), [], [R(tn)])
        P.op("pool", lambda e: e.affine_select(out=tensor[:], in_=tensor[:], pattern=[[-sgn, 128]], compare_op=cmp,
                                               fill=0.0, base=0, channel_multiplier=sgn), [R(tn)], [R(tn)])
    mask("identF", identF, ALU.is_equal)
    mask("SLf", SLf, ALU.is_gt)
    mask("MKf", MKf, ALU.is_ge, -1)
    P.op("pool", lambda e: e.memset(ONf[:], 1.0), [], [R("ONf")])
    cp("pool", SLb[:], SLf[:], ["SLf"], ["SLb"])
    cp("pool", ONb[:], ONf[:], ["ONf"], ["ONb"])
    cp("pool", UTb[:], MKf[:], ["MKf"], ["UTb"])

    dma("sp", cols[:], cols_d, [], ["cols"])
    dma("sp", rowS[:], rows_d[:, R_DTB:R_DTB + 160], [], ["rowS"])
    dma("sp", rk[:], rk_d, [], ["rk"])
    dma("sp", nf[:], nf_d, [], ["nf"])
    dma("sp", H[:], x_own.rearrange("(t p) d -> p t d", p=128), [], ["H"])
    _wv_o = w_in_b.rearrange("a b -> (a b)").rearrange("(r c) -> r c", c=2048)
    _wv_i = w_in.rearrange("a b -> (a b)").rearrange("(r c) -> r c", c=2048)
    for _q in range(8):
        dma("pool", _wv_o[_q * 578:(_q + 1) * 578, :], _wv_i[_q * 578:(_q + 1) * 578, :], [], ["w_in_b"])
    dma("pool", wbp_b, w_br_pool, [], ["wbp_b"])

    try:
        with ExitStack() as s0:
            bcm = sb(s0, "bcm", [128, 6 * D], F32)
            cact = sb(s0, "cact", [128, 8], F32)
            CBC = sb(s0, "CBC", [128, 8, 128], F32)
            wa = [sb(s0, f"wa{i}", [128, 8, 512], F32) for i in range(2)]
            ba = [sb(s0, f"ba{i}", [128, 512], F32) for i in range(2)]
            stg = [sb(s0, f"stg{i}", [128, 4, D], F32) for i in range(2)]
            stb = [sb(s0, f"stb{i}", [128, 4, D], BF16) for i in range(2)]
            act(cact[:], cols[:, C_C:C_C + 8], AF.Silu, ["cols"], ["cact"])
            cp("dve", CBC[:], cact[:].unsqueeze(2).to_broadcast([128, 8, 128]), ["cact"], ["CBC"])
            for ns in range(12):
                b = ns % 2
                dma("sp", wa[b][:], w_ada[:, ns * 512:(ns + 1) * 512].rearrange("(c p) n -> p c n", p=128), [], [f"wa{b}"])
                dma("sp", ba[b][:], rows_d[:, R_BADA + ns * 512:R_BADA + (ns + 1) * 512], [], [f"ba{b}"])
                bk, br = bank()
                mms([(bk[:], CBC[:, dc, :], wa[b][:, dc, :], dc == 0, dc == 7) for dc in range(8)], ["CBC", f"wa{b}"], [br])
                tt("dve", bcm[:, ns * 512:(ns + 1) * 512], bk[:], ba[b][:], ALU.add, [br, f"ba{b}"], ["bcm"])
            for vi, v in enumerate((0, 1, 3, 4)):
                for q in range(2):
                    bk, br = bank()
                    trs([(bk[:, j * 128:(j + 1) * 128], bcm[:, v * D + (q * 4 + j) * 128: v * D + (q * 4 + j + 1) * 128], identF[:])
                         for j in range(4)], ["bcm", "identF"], [br])
                    cp("dve", mcol[:, vi * 8 + q * 4: vi * 8 + q * 4 + 4],
                       bk[:].rearrange("p (j n) -> p j n", n=128)[:, :, 0], [br], ["mcol"])
            stt("dve", gsc[:, 0:8], mcol[:, 8:16], 1.0, cols[:, C_NMG:C_NMG + 8], ALU.add, ALU.mult, ["mcol", "cols"], ["gsc"])
            stt("dve", gsc[:, 8:16], mcol[:, 24:32], 1.0, cols[:, C_NFG:C_NFG + 8], ALU.add, ALU.mult, ["mcol", "cols"], ["gsc"])
            dma("sp", g2_d, bcm[:, 5 * D:6 * D], ["bcm"], ["g2_d"])
            act(rowS[:, 32:64], rowS[:, 32:64], AF.Exp, ["rowS"], ["rowS"])
            ts("dve", rowS[:, 32:64], rowS[:, 32:64], -1.0, None, ALU.mult, None, ["rowS"], ["rowS"])
            k = 0
            for half in range(2):
                b = k % 2; k += 1
                dma("sp", stg[b][:], w_out[half * 512:(half + 1) * 512, :].rearrange("(c p) n -> p c n", p=128), [], [f"stg{b}"])
                tt("dve", stb[b][:], stg[b][:], bcm[:, 2 * D:3 * D].unsqueeze(1).to_broadcast([128, 4, D]), ALU.mult,
                   [f"stg{b}", "bcm"], [f"stb{b}"])
                dma("sp", wout_b[half * 512:(half + 1) * 512, :].rearrange("(c p) n -> p c n", p=128), stb[b][:], [f"stb{b}"], ["wout_b"])
            for q in range(4):
                b = k % 2; k += 1
                dma("sp", stg[b][:], w_br_ssd[q * 512:(q + 1) * 512, :].rearrange("(c p) n -> p c n", p=128), [], [f"stg{b}"])
                for j in range(4):
                    ts("pool", stb[b][:, j, :], stg[b][:, j, :], cols[:, C_SNG + q * 4 + j:C_SNG + q * 4 + j + 1], None, ALU.mult, None,
                       [f"stg{b}", "cols"], [f"stb{b}"])
                dma("sp", wbs_b[q * 512:(q + 1) * 512, :].rearrange("(c p) n -> p c n", p=128), stb[b][:], [f"stb{b}"], ["wbs_b"])
            checkpoint("p0", {"mcol": mcol, "gsc": gsc, "rowS": rowS, "bcm": bcm})
            barrier()
            P.flush()

        with ExitStack() as sA:
            WS = [sb(sA, f"WS{i}", [128, 8, 512], BF16) for i in range(3)]
            wsi = {"i": 0}
            wdt = sb(sA, "wdt", [128, 8, 32], BF16)
            poolw = sb(sA, "poolw", [128, 8, 256], BF16)
            pcorr = sb(sA, "pcorr", [128, 4, 16], F32)
            posr = sb(sA, "posr", [128, 16], F32)
            tailx = sb(sA, "tailx", [128, 32, 3], F32)
            tailx0 = sb(sA, "tailx0", [128, 32, 3], F32)
            ptail = sb(sA, "ptail", [128, 8, 15], F32)
            xpw = [sb(sA, f"xpw{i}", [128, 15 + TS], F32) for i in range(2)]
            logD = sb(sA, "logD", [128, 32], F32)
            logDp = [logD, sb(sA, "logD1", [128, 32], F32)]
            ldi = {"i": 0}
            uT = sb(sA, "uT", [128, 8, TS], BF16)
            xn_late = True
            st1 = sb(sA, "st1", [128, 4], F32)
            pooledT = sb(sA, "pooledT", [128, 8, TS], BF16)
            mixedT = sb(sA, "mixedT", [128, 8, TS], BF16)
            gp = sb(sA, "gp", [128, 8, TS], BF16)
            pS = [sb(sA, f"pS{i}", [128, 15 + TS], F32) for i in range(2)]
            gsig = sb(sA, "gsig", [128, TS], F32)
            NSET = 3
            xcs = [sb(sA, f"xc{i}", [128, 3 + TS], F32) for i in range(NSET)]
            caccs = [sb(sA, f"cacc{i}", [128, TS], F32) for i in range(NSET)]
            big = sb(sA, "big", [128, 2080], F32)
            xn = big[:, 1024:2048]
            tmpD = sb(sA, "tmpD", [128, 512], F32)
            Bfs = [sb(sA, f"Bf{i}", [128, TS], F32) for i in range(2)]
            xs_tok = sb(sA, "xs_tok", [128, 2, 2048], BF16)
            B_tok = sb(sA, "B_tok", [128, 2, 1024], BF16)
            BT = sb(sA, "BT", [128, 8, TS], BF16)
            CT = sb(sA, "CT", [128, 8, TS], BF16)
            dtb = sb(sA, "dtb", [128, 2, 32], F32)
            ab = sb(sA, "ab", [128, 2, 32], F32)
            ab16 = sb(sA, "ab16", [128, 2, 32], BF16)
            sm = sb(sA, "sm", [128, 4, 32], F32)
            sz_tok = sb(sA, "sz_tok", [128, 2, 2048], BF16)
            xdt = sb(sA, "xdt", [128, 2048], BF16)
            xdtd = sb(sA, "xdtd", [128, 2048], BF16)
            AL = sb(sA, "AL", [128, 4, 128], BF16)
            AO = sb(sA, "AO", [128, 4, 128], BF16)
            LT = sb(sA, "LT", [128, 4, 128], BF16)
            Eb = sb(sA, "Eb", [128, 4, 128], BF16)
            CBm = sb(sA, "CBm", [128, 8, 128], BF16)
            MT = sb(sA, "MT", [128, 4, 128], BF16)
            CE = sb(sA, "CE", [128, 4, 128], BF16)
            yn = big
            junk = gsig
            ss8 = sb(sA, "ss8", [128, 8], F32)
            ynT = sb(sA, "ynT", [128, 16, TS], BF16)
            combT = sb(sA, "combT", [128, 8, TS], BF16)
            stS = big

            def wblock(src, rd):
                i = wsi["i"]; wsi["i"] = (i + 1) % 3
                dma("sp", WS[i][:], src.rearrange("(c p) n -> p c n", p=128), rd, [f"WS{i}"])
                return WS[i], f"WS{i}"

            dma("pool", wdt[:], w_in[:, OFF_DT:OFF_DT + 32].rearrange("(c p) n -> p c n", p=128), [], ["wdt"])
            dma("pool", poolw[:], pool_w.rearrange("(c p) n -> p c n", p=128), [], ["poolw"])
            dma("sp", posr[:], pos_d, [], ["posr"])
            for g, w in enumerate((2, 4, 8, 16)):
                ts("dve", pcorr[:, g, :], posr[:], float(w), None, ALU.min, None, ["posr"], ["pcorr"])
                P.op("dve", lambda e, g=g: e.reciprocal(out=pcorr[:, g, :], in_=pcorr[:, g, :]), rl(["pcorr"]), rl(["pcorr"]))
                ts("dve", pcorr[:, g, :], pcorr[:, g, :], float(w), None, ALU.mult, None, ["pcorr"], ["pcorr"])

            def make_uT(src_ap, rdname, n_tiles, col0=0):
                for tti in range(n_tiles):
                    xin = src_ap(tti)
                    act(junk_big[:], xin, AF.Square, [rdname], ["xn", "st1"], accum_out=st1[:, 0:1])
                    act(st1[:, 1:2], st1[:, 0:1], AF.Sqrt, ["st1"], ["st1"], bias=EPS, scale=1.0 / D)
                    P.op("dve", lambda e: e.reciprocal(out=st1[:, 2:3], in_=st1[:, 1:2]), rl(["st1"]), rl(["st1"]))
                    ts("dve", xn[:], xin, st1[:, 2:3], None, ALU.mult, None, [rdname, "st1"], ["xn"])
                    for half in range(2):
                        bk, br = bank()
                        trs([(bk[:, j * 128:(j + 1) * 128], xn[:, (half * 4 + j) * 128:(half * 4 + j + 1) * 128], identF[:])
                             for j in range(4)], ["xn", "identF"], [br])
                        for j in range(4):
                            dc = half * 4 + j
                            ts("dve" if half else "pool_no", uT[:, dc, col0 + tti * 128: col0 + (tti + 1) * 128], bk[:, j * 128:(j + 1) * 128],
                               gsc[:, dc:dc + 1], mcol[:, dc:dc + 1], ALU.mult, ALU.add, [br, "gsc", "mcol"], ["uT"])

            junk_big = xn

            _ts_orig = ts

            def ts(eng, out, a, s1, s2, op0, op1, rd, wr):
                if eng == "pool_no":
                    return P.op("act", lambda e: e.activation(out=out, in_=a, func=AF.Identity, bias=s2, scale=s1), rl(rd), rl(wr))
                return _ts_orig(eng, out, a, s1, s2, op0, op1, rd, wr)

            xh = xsA = big
            dma("sp", big[:, 0:D], x_halo, [], ["bigA"])
            make_uT(lambda tti: big[:, 0:D], "bigA", 1)
            for blk in range(10):
                c0 = OFF_X + blk * 512 if blk < 8 else OFF_P + (blk - 8) * 512
                wsb, wr_ = wblock(w_in_b[:, c0:c0 + 512], ["w_in_b"])
                for j in range(4):
                    bk, br = bank()
                    mms([(bk[:, 0:128], wsb[:, dc, j * 128:(j + 1) * 128], uT[:, dc, 0:128], dc == 0, dc == 7) for dc in range(8)],
                        [wr_, "uT"], [br])
                    if blk < 8:
                        c = blk * 4 + j
                        ts("dve", tailx0[:, c, :], bk[:, 125:128], nf[:, 0:1], None, ALU.mult, None, [br, "nf"], ["tailx0"])
                    else:
                        c = (blk - 8) * 4 + j
                        ts("dve", ptail[:, c, :], bk[:, 113:128], nf[:, 0:1], None, ALU.mult, None, [br, "nf"], ["ptail"])

            checkpoint("halo", {"tailx0": tailx0, "xpb": ptail, "uT": uT[:, :, 0:128]})
            def slab(si, pre, src=None, skip_uT=False, hook=None):
                t0 = 2 * si
                bstate["n"] = 8
                if skip_uT:
                    pass
                elif src is None:
                    make_uT(lambda tti: H[:, t0 + tti, :], "H", 2)
                else:
                    make_uT(src, "sz_tok", 2)
                if not pre:
                    for blk in range(2):
                        wsb, wr_ = wblock(w_in_b[:, OFF_P + blk * 512: OFF_P + (blk + 1) * 512], ["w_in_b"])
                        for j in range(4):
                            c = blk * 4 + j
                            g = c // 2
                            w = 2 << g
                            bk, br = bank()
                            mms([(bk[:, 0:TS], wsb[:, dc, j * 128:(j + 1) * 128], uT[:, dc, :], dc == 0, dc == 7) for dc in range(8)],
                                [wr_, "uT"], [br])
                            xw = xpw[c % 2]; xwn = f"xpw{c % 2}"
                            cp("pool", xw[:, 0:15], ptail[:, c, :], ["ptail"], [xwn])
                            cp("act", xw[:, 15:15 + TS], bk[:, 0:TS], [br], [xwn])
                            cp("pool", ptail[:, c, :], xw[:, TS:TS + 15], [xwn], ["ptail"])
                            src = xw[:]
                            lo = 0
                            step = 1
                            k = 0
                            while step < w:
                                dst = pS[k % 2]
                                nlo = lo + step
                                tt("dve", dst[:, nlo:15 + TS], src[:, nlo:15 + TS], src[:, nlo - step:15 + TS - step], ALU.add,
                                   [xwn, f"pS{(k + 1) % 2}"], [f"pS{k % 2}"])
                                src = dst[:]
                                lo = nlo
                                step *= 2
                                k += 1
                            sname = f"pS{(k - 1) % 2}"
                            if si == 0:
                                tt("dve", src[:, 15:31], src[:, 15:31], pcorr[:, g, :], ALU.mult, [sname, "pcorr"], [sname])
                            stt("dve", pooledT[:, c, :], src[:, 15:15 + TS], 1.0 / w, xw[:, 15:15 + TS], ALU.mult, ALU.subtract,
                                [sname, xwn], ["pooledT"])
                            pass
                    for g in range(4):
                        for dch in range(2):
                            bk, br = bank()
                            mms([(bk[:, 0:TS], poolw[:, 2 * g + kc, dch * 128:(dch + 1) * 128], pooledT[:, 2 * g + kc, :], kc == 0, kc == 1)
                                 for kc in range(2)], ["poolw", "pooledT"], [br])
                            ts("dve", mixedT[:, 2 * g + dch, :], bk[:, 0:TS], cols[:, C_PSC + 2 * g + dch:C_PSC + 2 * g + dch + 1], None,
                               ALU.mult, None, [br, "cols"], ["mixedT"])
                    for blk in range(2):
                        wsb, wr_ = wblock(wbp_b[:, blk * 512:(blk + 1) * 512], ["wbp_b"])
                        wsg, wg_ = wblock(w_in_b[:, OFF_GP + blk * 512: OFF_GP + (blk + 1) * 512], ["w_in_b"])
                        for j in range(4):
                            ec = blk * 4 + j
                            bk, br = bank()
                            mms([(bk[:, 0:TS], wsg[:, dc, j * 128:(j + 1) * 128], uT[:, dc, :], dc == 0, dc == 7) for dc in range(8)],
                                [wg_, "uT"], [br])
                            act(gsig[:], bk[:, 0:TS], AF.Sigmoid, [br], ["gsig"])
                            bk2, br2 = bank()
                            mms([(bk2[:, 0:TS], wsb[:, dc, j * 128:(j + 1) * 128], mixedT[:, dc, :], dc == 0, dc == 7) for dc in range(8)],
                                [wr_, "mixedT"], [br2])
                            tt("dve", gp[:, ec, :], bk2[:, 0:TS], gsig[:], ALU.mult, [br2, "gsig"], ["gp"])
                checkpoint(f"ut{int(pre)}", {"uT": uT})
                checkpoint(f"pool{int(pre)}", {"gp": gp})
                nblk = 6 if pre else 8
                pend = []

                def tick():
                    for p_ in pend:
                        p_[0] -= 1
                    while pend and pend[0][0] <= 0:
                        pend.pop(0)[1]()

                for blk in range(nblk):
                    wsb, wr_ = wblock(w_in_b[:, OFF_X + blk * 512: OFF_X + (blk + 1) * 512], ["w_in_b"])
                    for j in range(4):
                        c = blk * 4 + j
                        bk, br = bank()
                        mms([(bk[:, 0:TS], wsb[:, dc, j * 128:(j + 1) * 128], uT[:, dc, :], dc == 0, dc == 7) for dc in range(8)],
                            [wr_, "uT"], [br])
                        xc = xcs[c % NSET]; cacc = caccs[c % NSET]; xcn = f"xc{c % NSET}"; can = f"cacc{c % NSET}"
                        Bf = Bfs[c % 2]; Bfn = f"Bf{c % 2}"
                        cp("pool", xc[:, 0:3], tailx[:, c, :], [f"tailx{c}"], [xcn])
                        cp("act", xc[:, 3:3 + TS], bk[:, 0:TS], [br], [xcn])
                        cp("pool", tailx[:, c, :], xc[:, TS:TS + 3], [xcn], [f"tailx{c}"])
                        cw = lambda k_: cols[:, C_CW + c * 4 + k_: C_CW + c * 4 + k_ + 1]
                        P.op("act", lambda e, cacc=cacc, bk=bk, s_=cw(3): e.activation(out=cacc[:], in_=bk[:, 0:TS], func=AF.Copy, scale=s_),
                             rl([br, "cols"]), rl([can]))
                        for k_ in (0, 1, 2):
                            stt("dve", cacc[:], xc[:, k_:k_ + TS], cw(k_), cacc[:], ALU.mult, ALU.add, [xcn, "cols", can], [can])
                        bcol = cols[:, C_CB + c:C_CB + c + 1]
                        if c < 16:
                            hb = (c // 4) % 2; hbn = "bigA" if hb == 0 else "bigB"
                            pend.append([2, (lambda c=c, hb=hb, hbn=hbn, cacc=cacc, can=can, bcol=bcol:
                                             act(big[:, hb * 1024 + (c % 4) * TS: hb * 1024 + (c % 4 + 1) * TS], cacc[:], AF.Silu, [can, "cols"], [hbn], bias=bcol))])
                            if c % 4 == 3:
                                def st2x(cg=c // 4, hb=hb, hbn=hbn):
                                    for tti in range(2):
                                        bk2, br2 = bank()
                                        trs([(bk2[:, q * 128:(q + 1) * 128], big[:, hb * 1024 + q * TS + tti * 128: hb * 1024 + q * TS + (tti + 1) * 128], identF[:])
                                             for q in range(4)], [hbn, "identF"], [br2])
                                        cp("dve", xs_tok[:, tti, cg * 512:(cg + 1) * 512], bk2[:], [br2], ["xs_tok"])
                                pend.append([3, st2x])
                        elif c < 24:
                            g = c - 16
                            pend.append([2, (lambda Bf=Bf, Bfn=Bfn, cacc=cacc, can=can, bcol=bcol:
                                             act(Bf[:], cacc[:], AF.Silu, [can, "cols"], [Bfn], bias=bcol))])
                            def st2b(g=g, Bf=Bf, Bfn=Bfn):
                                cp("dve", BT[:, g, :], Bf[:], [Bfn], ["BT"])
                                bk2, br2 = bank()
                                trs([(bk2[:, tti * 128:(tti + 1) * 128], Bf[:, tti * 128:(tti + 1) * 128], identF[:]) for tti in range(2)],
                                    [Bfn, "identF"], [br2])
                                cp("dve", B_tok[:, :, g * 128:(g + 1) * 128], bk2[:, 0:256].rearrange("p (t n) -> p t n", n=128), [br2], ["B_tok"])
                            pend.append([2, st2b])
                        else:
                            g = c - 24
                            pend.append([2, (lambda g=g, cacc=cacc, can=can, bcol=bcol:
                                             act(CT[:, g, :], cacc[:], AF.Silu, [can, "cols"], ["CT"], bias=bcol))])
                        tick()
                while pend:
                    pend.pop(0)[1]()
                checkpoint(f"conv{int(pre)}", {"xs_tok": xs_tok, "B_tok": B_tok})
                for tti in range(2):
                    bk, br = bank()
                    mms([(bk[:, 0:32], uT[:, dc, tti * 128:(tti + 1) * 128], wdt[:, dc, :], dc == 0, dc == 7) for dc in range(8)],
                        ["uT", "wdt"], [br])
                    tt("dve", dtb[:, tti, :], bk[:, 0:32], rowS[:, 0:32], ALU.add, [br, "rowS"], ["dtb"])
                    act(dtb[:, tti, :], dtb[:, tti, :], AF.Exp, ["dtb"], ["dtb"])
                    act(dtb[:, tti, :], dtb[:, tti, :], AF.Ln, ["dtb"], ["dtb"], bias=1.0)
                    tt("dve", ab[:, tti, :], dtb[:, tti, :], rowS[:, 32:64], ALU.mult, ["dtb", "rowS"], ["ab"])
                    cp("dve", ab16[:, tti, :], ab[:, tti, :], ["ab"], ["ab16"])
                checkpoint(f"dt{int(pre)}", {"dtb": dtb, "ab": ab})
                if not pre:
                    for zc in range(4):
                        wsb, wr_ = wblock(w_in_b[:, OFF_Z + zc * 512: OFF_Z + (zc + 1) * 512], ["w_in_b"])
                        for tti in range(2):
                            bk, br = bank()
                            mms([(bk[:], uT[:, dc, tti * 128:(tti + 1) * 128], wsb[:, dc, :], dc == 0, dc == 7) for dc in range(8)],
                                ["uT", wr_], [br])
                            act(sz_tok[:, tti, zc * 512:(zc + 1) * 512], bk[:], AF.Silu, [br], ["sz_tok"])
                checkpoint(f"z{int(pre)}", {"sz_tok": sz_tok})
                if hook is not None:
                    hook()
                if not pre:
                    bstate["n"] = 4
                    bstate["i"] = 0
                for tti in range(2):
                    tsl = slice(tti * 128, (tti + 1) * 128)
                    bk, br = bank()
                    mms([(bk[:, 0:32], SLf[:], ab[:, tti, :], True, True)], ["SLf", "ab"], [br])
                    bkB, brB = bank()
                    mms([(bkB[:, 0:32], ONf[:], ab[:, tti, :], True, True)], ["ONf", "ab"], [brB])
                    act(sm[:, 0, :], bk[:, 0:32], AF.Exp, [br], ["sm"])
                    act(sm[:, 1, :], bkB[:, 0:32], AF.Exp, [brB], ["sm"])
                    checkpoint(f"ssdA{int(pre)}", {"sm": sm[:, 0:2, :]})
                    checkpoint(f"ssdA2{int(pre)}", {"logD": logD})
                    if pre:
                        tt("dve", sm[:, 3, :], dtb[:, tti, :], sm[:, 0, :], ALU.mult, ["dtb", "sm"], ["sm3"])
                        tt("dve", xdtd[:].rearrange("p (h d) -> p h d", h=32), xs_tok[:, tti, :].rearrange("p (h d) -> p h d", h=32),
                           sm[:, 3, :].unsqueeze(2).to_broadcast([128, 32, 64]), ALU.mult, ["xs_tok", "sm3"], ["xdtd"])
                    else:
                        tt("dve", xdt[:].rearrange("p (h d) -> p h d", h=32), xs_tok[:, tti, :].rearrange("p (h d) -> p h d", h=32),
                           dtb[:, tti, :].unsqueeze(2).to_broadcast([128, 32, 64]), ALU.mult, ["xs_tok", "dtb"], ["xdt"])
                        checkpoint(f"ssdA3{int(pre)}", {"xdt": xdt})
                        tt(PB, xdtd[:].rearrange("p (h d) -> p h d", h=32), xdt[:].rearrange("p (h d) -> p h d", h=32),
                           sm[:, 0, :].unsqueeze(2).to_broadcast([128, 32, 64]), ALU.mult, ["xdt", "sm"], ["xdtd"])
                    checkpoint(f"ssdB{int(pre)}", {"xdtd": xdtd})
                    if not pre:
                        for gh in range(2):
                            bk, br = bank()
                            mms([(bk[:, q * 128:(q + 1) * 128], BT[:, gh * 4 + q, tsl], CT[:, gh * 4 + q, tsl], True, True) for q in range(4)],
                                ["BT", "CT"], [br])
                            tt("dve", CBm[:, gh * 4:(gh + 1) * 4, :], bk[:].rearrange("p (q n) -> p q n", n=128),
                               MKf[:].unsqueeze(1).to_broadcast([128, 4, 128]), ALU.mult, [br, "MKf"], ["CBm"])
                        checkpoint(f"cb{int(pre)}", {"CBm": CBm})
                        ybanks = []
                        gb = {}
                        for g in range(9):
                            if g < 8:
                                a4 = ab16[:, tti, 4 * g:4 * g + 4].unsqueeze(2).to_broadcast([128, 4, 128])
                                tt("dve", AL[:], SLb[:].unsqueeze(1).to_broadcast([128, 4, 128]), a4, ALU.mult, ["SLb", "ab16"], ["AL"])
                                tt("dve", AO[:], ONb[:].unsqueeze(1).to_broadcast([128, 4, 128]), a4, ALU.mult, ["ONb", "ab16"], ["AO"])
                                bk, br = bank()
                                mms([(bk[:, q * 128:(q + 1) * 128], AL[:, q, :], UTb[:], True, True) for q in range(4)], ["AL", "UTb"], [br])
                                bk2, br2 = bank()
                                mms([(bk2[:, q * 128:(q + 1) * 128], AO[:, q, :], UTb[:], True, True) for q in range(4)], ["AO", "UTb"], [br2])
                                gb[g] = (bk, br, bk2, br2)
                            if g >= 1:
                                gp_ = g - 1
                                tt("dve", MT[:], LT[:], CBm[:, gp_, :].unsqueeze(1).to_broadcast([128, 4, 128]), ALU.mult, ["LT", "CBm"], ["MT"])
                                tt("dve", CE[:], Eb[:], CT[:, gp_, tsl].unsqueeze(1).to_broadcast([128, 4, 128]), ALU.mult, ["Eb", "CT"], ["CE"])
                            if g < 8:
                                bk, br, bk2, br2 = gb[g]
                                act(LT[:], bk[:].rearrange("p (q n) -> p q n", n=128), AF.Exp, [br], ["LT"])
                                act(Eb[:], bk2[:].rearrange("p (q n) -> p q n", n=128), AF.Exp, [br2], ["Eb"])
                            if g >= 1:
                                gp_ = g - 1
                                if gp_ % 2 == 0:
                                    yb, ybr = ybank(gp_ // 2)
                                    ybanks.append((yb, ybr))
                                lst = []
                                for q in range(4):
                                    h = 4 * gp_ + q
                                    o = yb[:, (gp_ % 2) * 256 + q * 64:(gp_ % 2) * 256 + (q + 1) * 64]
                                    lst.append((o, MT[:, q, :], xdt[:, h * 64:(h + 1) * 64], True, False))
                                    lst.append((o, CE[:, q, :], state_bf[:, h * 64:(h + 1) * 64], False, True))
                                mms(lst, ["MT", "CE", "xdt", "state_bf"], [ybr])
                        checkpoint(f"grp{int(pre)}", {"MT": MT, "CE": CE})
                        for b4 in range(4):
                            yb, ybr = ybanks[b4]
                            csl = slice(b4 * 512, (b4 + 1) * 512)
                            tt("dve", tmpD[:, 0:512].rearrange("p (h d) -> p h d", h=8), xs_tok[:, tti, csl].rearrange("p (h d) -> p h d", h=8),
                               rowS[:, 64 + b4 * 8:64 + b4 * 8 + 8].unsqueeze(2).to_broadcast([128, 8, 64]), ALU.mult,
                               ["xs_tok", "rowS"], ["tmpD"])
                            tt("dve", yn[:, csl], yb[:], tmpD[:, 0:512], ALU.add, [ybr, "tmpD"], ["yn"])
                            tt("dve", yn[:, csl], yn[:, csl], sz_tok[:, tti, csl], ALU.mult, ["yn", "sz_tok"], ["yn"])
                        for g in range(8):
                            act(junk[:], yn[:, g * 256:(g + 1) * 256], AF.Square, ["yn"], ["junk", "ss8"], accum_out=ss8[:, g:g + 1])
                        act(ss8[:], ss8[:], AF.Sqrt, ["ss8"], ["ss8"], bias=EPS, scale=1.0 / 256)
                        P.op("dve", lambda e: e.reciprocal(out=ss8[:], in_=ss8[:]), rl(["ss8"]), rl(["ss8"]))
                        tt("dve", yn[:, 0:2048].rearrange("p (g d) -> p g d", g=8), yn[:, 0:2048].rearrange("p (g d) -> p g d", g=8),
                           ss8[:].unsqueeze(2).to_broadcast([128, 8, 256]), ALU.mult, ["yn", "ss8"], ["yn"])
                        checkpoint(f"yn{int(pre)}", {"yn": yn[:, 0:2048]})
                        for q4 in range(4):
                            bk, br = bank()
                            trs([(bk[:, q * 128:(q + 1) * 128], yn[:, (q4 * 4 + q) * 128:(q4 * 4 + q + 1) * 128], identF[:]) for q in range(4)],
                                ["yn", "identF"], [br])
                            cp("act", ynT[:, q4 * 4:(q4 + 1) * 4, tsl], bk[:].rearrange("p (q n) -> p q n", n=128), [br], ["ynT"])
                    checkpoint(f"ynT{int(pre)}", {"ynT": ynT[:, :, 0:128]})
                    sbanks = []
                    for b4 in range(4):
                        bk, br = bank()
                        mms([(bk[:, q * 256:(q + 1) * 256], B_tok[:, tti, (2 * b4 + q) * 128:(2 * b4 + q + 1) * 128],
                              xdtd[:, (2 * b4 + q) * 256:(2 * b4 + q + 1) * 256], True, True) for q in range(2)], ["B_tok", "xdtd"], [br])
                        sbanks.append((bk, br))
                    checkpoint(f"ssdC{int(pre)}", {"xdtd": xdtd})
                    tt(PB, state[:].rearrange("p (h d) -> p h d", h=32), state[:].rearrange("p (h d) -> p h d", h=32),
                       sm[:, 1, :].unsqueeze(2).to_broadcast([128, 32, 64]), ALU.mult, ["state", "sm", "state_bf"], ["state"])
                    for b4 in range(4):
                        bk, br = sbanks[b4]
                        tt("dve", state[:, b4 * 512:(b4 + 1) * 512], bk[:], state[:, b4 * 512:(b4 + 1) * 512], ALU.add, ["state", br], ["state"])
                    if not pre:
                        cp("act", state_bf[:], state[:], ["state"], ["state_bf"])
                if pre:
                    return
                for eh in range(2):
                    pb = [ybank(q_) for q_ in range(4)]
                    for kh in range(2):
                        wsb, wr_ = wblock(wbs_b[kh * 1024:(kh + 1) * 1024, eh * 512:(eh + 1) * 512], ["wbs_b"])
                        for j in range(4):
                            mms([(pb[j][0][:, 0:TS], wsb[:, kc, j * 128:(j + 1) * 128], ynT[:, kh * 8 + kc, :], (kh == 0 and kc == 0),
                                  (kh == 1 and kc == 7)) for kc in range(8)], [wr_, "ynT"], [pb[j][1]])
                    wsg, wg_ = wblock(w_in_b[:, OFF_GS + eh * 512: OFF_GS + (eh + 1) * 512], ["w_in_b"])
                    for j in range(4):
                        ec = eh * 4 + j
                        bk, br = bank()
                        mms([(bk[:, 0:TS], wsg[:, dc, j * 128:(j + 1) * 128], uT[:, dc, :], dc == 0, dc == 7) for dc in range(8)],
                            [wg_, "uT"], [br])
                        act(gsig[:], bk[:, 0:TS], AF.Sigmoid, [br], ["gsig"])
                        tt("dve", gsig[:], pb[j][0][:, 0:TS], gsig[:], ALU.mult, [pb[j][1], "gsig"], ["gsig"])
                        tt("dve", combT[:, ec, :], gsig[:], gp[:, ec, :], ALU.add, ["gsig", "gp"], ["combT"])
                checkpoint(f"comb{int(pre)}", {"combT": combT})
                wo = [wblock(wout_b[:, hh * 512:(hh + 1) * 512], ["wout_b"]) for hh in range(2)]
                for tti in range(2):
                    for hh in range(2):
                        bk, br = bank()
                        mms([(bk[:], combT[:, ec, tti * 128:(tti + 1) * 128], wo[hh][0][:, ec, :], ec == 0, ec == 7) for ec in range(8)],
                            ["combT", wo[hh][1]], [br])
                        tt("dve", H[:, t0 + tti, hh * 512:(hh + 1) * 512], bk[:], H[:, t0 + tti, hh * 512:(hh + 1) * 512], ALU.add,
                           ["H", br], ["H"])

            xst = lambda tti: sz_tok[:, tti, :].bitcast(F32)
            Iacc = ynT[:].rearrange("p a b -> p (a b)").bitcast(F32)
            P.op("pool", lambda e: e.memset(state[:], 0.0), [], rl(["state"]))
            P.op("pool", lambda e: e.memset(Iacc, 0.0), [], rl(["ynT"]))
            P.op("pool", lambda e: e.memset(tailx[:], 0.0), [], rl([f"tailx{c_}" for c_ in range(32)]))
            seq = [(j, si) for j in range(n_pre_slots) for si in range(n_slabs)]

            def prep_uT(idx):
                j, si = seq[idx]
                r0 = j * T + si * TS
                for tti in range(2):
                    dma("sp", xst(tti), x_all[r0 + tti * 128: r0 + (tti + 1) * 128, :], [], ["sz_tok"])
                make_uT(xst, "sz_tok", 2)

            if seq:
                prep_uT(0)
            for idx, (j, si) in enumerate(seq):
                hk = (lambda idx=idx: prep_uT(idx + 1)) if idx + 1 < len(seq) else None
                slab(si, True, src=xst, skip_uT=True, hook=hk)
                if si == n_slabs - 1:
                    stt("dve", Iacc, state[:], rk[:, j:j + 1], Iacc, ALU.mult, ALU.add, ["state", "rk", "ynT"], ["ynT"])
            checkpoint("pre", {"state": state, "xs_tok": xs_tok, "B_tok": B_tok, "dtb": dtb})
            cp("dve", state[:], Iacc, ["ynT"], ["state"])
            cp("act", state_bf[:], state[:], ["state"], ["state_bf"])
            cp("pool", tailx[:], tailx0[:], ["tailx0"], [f"tailx{c_}" for c_ in range(32)])
            checkpoint("xchg", {"state": state})
            for si in range(n_slabs):
                slab(si, False)
            checkpoint("main", {"H": H, "ynT": ynT, "gp": gp, "combT": combT, "sz_tok": sz_tok, "CT": CT, "BT": BT})
            barrier()
            P.flush()

        if DEBUG:
            dbg_out = nc.dram_tensor("dbg_h", [T, D], F32, kind="ExternalOutput").ap()
            dma("sp", dbg_out.rearrange("(t p) d -> p t d", p=128), H[:], ["H"], [])

        with ExitStack() as sB:
            U2 = sb(sB, "U2", [128, 8, T], BF16)
            u2f = sb(sB, "u2f", [128, 8, 128], F32)
            xn2 = sb(sB, "xn2", [128, D], F32)
            st2 = sb(sB, "st2", [128, 4], F32)
            wr = sb(sB, "wr", [128, 8, 64], F32)
            wtab = sb(sB, "wtab", [128, NT, 65], F32)
            r64 = [sb(sB, f"r64_{i}", [128, 64], F32) for i in range(4)]
            r8 = [sb(sB, f"r8_{i}", [128, 8], F32) for i in range(5)]
            wguS = [sb(sB, f"wguS{i}", [128, 8, 512], BF16) for i in range(2)]
            wdF = [sb(sB, f"wdF{i}", [128, 2, D], F32) for i in range(2)]
            wdS = [sb(sB, f"wdS{i}", [128, 2, D], BF16) for i in range(2)]
            hidT = [sb(sB, f"hidT{i}", [128, 2, T], BF16) for i in range(2)]
            sg = [sb(sB, f"sg{i}", [128, 512], BF16) for i in range(2)]
            fng = sb(sB, "fng", [128, D], F32)
            g2bc = sb(sB, "g2bc", [128, D], F32)
            dma("sp", g2bc[:], g2_d, ["g2_d"], ["g2bc"])
            ob = [sb(sB, f"ob{i}", [128, D], F32) for i in range(2)]

            dma("sp", wr[:], w_router.rearrange("(c p) n -> p c n", p=128), [], ["wr"])
            dma("sp", fng[:], rows_d[:, R_FNG:R_FNG + D], [], ["fng"])
            P.op("pool", lambda e: e.memset(wtab[:], 1.0), [], rl(["wtab"]))
            for ti in range(NT):
                xin = H[:, ti, :]
                act(xn2[:], xin, AF.Square, ["H"], ["xn2", "st2"], accum_out=st2[:, 0:1])
                act(st2[:, 1:2], st2[:, 0:1], AF.Sqrt, ["st2"], ["st2"], bias=EPS, scale=1.0 / D)
                P.op("dve", lambda e: e.reciprocal(out=st2[:, 2:3], in_=st2[:, 1:2]), rl(["st2"]), rl(["st2"]))
                ts("dve", xn2[:], xin, st2[:, 2:3], None, ALU.mult, None, ["H", "st2"], ["xn2"])
                for half in range(2):
                    bk, br = bank()
                    trs([(bk[:, j * 128:(j + 1) * 128], xn2[:, (half * 4 + j) * 128:(half * 4 + j + 1) * 128], identF[:]) for j in range(4)],
                        ["xn2", "identF"], [br])
                    for j in range(4):
                        dc = half * 4 + j
                        P.op("act", lambda e, dc=dc, j=j, bk=bk: e.activation(out=u2f[:, dc, :], in_=bk[:, j * 128:(j + 1) * 128], func=AF.Identity,
                                                                         bias=mcol[:, 16 + dc:17 + dc], scale=gsc[:, 8 + dc:9 + dc]),
                             rl([br, "gsc", "mcol"]), rl(["u2f"]))
                cp("pool", U2[:, :, ti * 128:(ti + 1) * 128], u2f[:], ["u2f"], ["U2"])
                bk, br = bank()
                mms([(bk[:, 0:64], u2f[:, dc, :], wr[:, dc, :], dc == 0, dc == 7) for dc in range(8)], ["u2f", "wr"], [br])
                s_, ch, t1, t2 = r64
                m1, m2, gs, g8, gm = r8
                act(s_[:], bk[:, 0:64], AF.Sigmoid, [br], ["r0"])
                tt("dve", ch[:], s_[:], rowS[:, 96:160], ALU.add, ["r0", "rowS"], ["r1"])
                v3 = lambda a: a[:].rearrange("p (g e) -> p g e", g=8)
                P.op("dve", lambda e: e.tensor_reduce(out=m1[:], in_=v3(ch), axis=AX.X, op=ALU.max), rl(["r1"]), rl(["q0"]))
                tt("dve", v3(t1), v3(ch), m1[:].unsqueeze(2).to_broadcast([128, 8, 8]), ALU.is_equal, ["r1", "q0"], ["r2"])
                stt("dve", t1[:], t1[:], -BIG, ch[:], ALU.mult, ALU.add, ["r2", "r1"], ["r2"])
                P.op("dve", lambda e: e.tensor_reduce(out=m2[:], in_=v3(t1), axis=AX.X, op=ALU.max), rl(["r2"]), rl(["q1"]))
                tt("dve", gs[:], m1[:], m2[:], ALU.add, ["q0", "q1"], ["q2"])
                P.op("dve", lambda e: e.max(out=g8[:], in_=gs[:]), rl(["q2"]), rl(["q3"]))
                ts("dve", gm[:], gs[:], g8[:, 3:4], None, ALU.is_ge, None, ["q2", "q3"], ["q4"])
                tt("dve", v3(t1), v3(ch), gm[:].unsqueeze(2).to_broadcast([128, 8, 8]), ALU.mult, ["r1", "q4"], ["r2"])
                ts("dve", gm[:], gm[:], -1.0, BIG, ALU.add, ALU.mult, ["q4"], ["q4"])
                tt("dve", v3(t1), v3(t1), gm[:].unsqueeze(2).to_broadcast([128, 8, 8]), ALU.add, ["r2", "q4"], ["r2"])
                P.op("dve", lambda e: e.max(out=g8[:], in_=t1[:]), rl(["r2"]), rl(["q3"]))
                ts("dve", t2[:], t1[:], g8[:, 7:8], None, ALU.is_ge, None, ["r2", "q3"], ["r3"])
                tt("dve", t2[:], t2[:], s_[:], ALU.mult, ["r3", "r0"], ["r3"])
                P.op("dve", lambda e: e.tensor_reduce(out=m1[:, 0:1], in_=t2[:], axis=AX.X, op=ALU.add), rl(["r3"]), rl(["q0"]))
                P.op("dve", lambda e: e.reciprocal(out=m1[:, 0:1], in_=m1[:, 0:1]), rl(["q0"]), rl(["q0"]))
                ts("dve", wtab[:, ti, 0:64], t2[:], m1[:, 0:1], 2.5, ALU.mult, ALU.mult, ["r3", "q0"], ["wtab"])

            ne = n_experts if do_moe else 0
            for e_ in range(ne):
                b = e_ % 2
                P.dma("pool", lambda e, e_=e_, b=b: e.dma_start(out=wguS[b][:], in_=wgu[e_], max_dma_last_dim=4096), rl([]), rl([f"wguS{b}"]))
                dma("sp", wdF[b][:], wdn[e_], [], [f"wdF{b}"])
                tt("pool", wdS[b][:], wdF[b][:], g2bc[:].unsqueeze(1).to_broadcast([128, 2, D]), ALU.mult, [f"wdF{b}", "g2bc"], [f"wdS{b}"])
                k = 0
                for jc in range(2):
                    for sl in range(4):
                        bg, bgr = bank()
                        bu, bur = bank()
                        rhs = lambda dc: U2[:, dc, sl * 512:(sl + 1) * 512]
                        mms([(bg[:], wguS[b][:, dc, jc * 128:(jc + 1) * 128], rhs(dc), dc == 0, dc == 7) for dc in range(8)],
                            [f"wguS{b}", "U2"], [bgr])
                        mms([(bu[:], wguS[b][:, dc, 256 + jc * 128:256 + (jc + 1) * 128], rhs(dc), dc == 0, dc == 7) for dc in range(8)],
                            [f"wguS{b}", "U2"], [bur])
                        act(sg[k % 2][:], bg[:], AF.Silu, [bgr], [f"sg{k % 2}"])
                        tt("dve", hidT[b][:, jc, sl * 512:(sl + 1) * 512], bu[:], sg[k % 2][:], ALU.mult, [bur, f"sg{k % 2}"], [f"hidT{b}"])
                        k += 1
                for ti in range(NT):
                    for hh in range(2):
                        bk, br = ybank()
                        mms([(bk[:], hidT[b][:, jc, ti * 128:(ti + 1) * 128], wdS[b][:, jc, hh * 512:(hh + 1) * 512], jc == 0, jc == 1)
                             for jc in range(2)], [f"hidT{b}", f"wdS{b}"], [br])
                        stt("dve", H[:, ti, hh * 512:(hh + 1) * 512], bk[:], wtab[:, ti, e_:e_ + 1], H[:, ti, hh * 512:(hh + 1) * 512],
                            ALU.mult, ALU.add, [br, "wtab", f"Ht{ti}"], [f"Ht{ti}"])
            for ti in range(NT):
                b = ti % 2
                act(xn2[:], H[:, ti, :], AF.Square, ["H", f"Ht{ti}"], ["xn2", "st2"], accum_out=st2[:, 0:1])
                act(st2[:, 1:2], st2[:, 0:1], AF.Sqrt, ["st2"], ["st2"], bias=EPS, scale=1.0 / D)
                P.op("dve", lambda e: e.reciprocal(out=st2[:, 2:3], in_=st2[:, 1:2]), rl(["st2"]), rl(["st2"]))
                stt("dve", ob[b][:], H[:, ti, :], st2[:, 2:3], fng[:], ALU.mult, ALU.mult, ["H", f"Ht{ti}", "st2", "fng"], [f"ob{b}"])
                dma("sp", out_d[ti * 128:(ti + 1) * 128, :], ob[b][:], [f"ob{b}"], [])
            P.wait_all("sp", P.all_events())
            P.flush()
    except _Stop:
        return nc
    top.close()
    _top0.close()
    return nc


def _colform(v):
    v = np.asarray(v, np.float32).reshape(-1)
    return np.ascontiguousarray(v.reshape(-1, 128).T)


def _prep_inputs(x, c, w_ada, b_ada, norm_mix_g, w_in, conv_w, conv_b, dt_bias, A_log, D_skip, ssd_norm_g,
                 pool_w, pool_scale, w_br_ssd, w_br_pool, w_out, norm_ffn_g, w_router, router_bias,
                 we_gate, we_up, we_down, ws_gate, ws_up, ws_down, final_norm_g):
    f = lambda a: np.ascontiguousarray(np.asarray(a, np.float32))
    x2 = f(x).reshape(NCORES * T, D)
    cols = np.zeros((128, NCOL), np.float32)
    cols[:, C_NMG:C_NMG + 8] = _colform(norm_mix_g)
    cols[:, C_NFG:C_NFG + 8] = _colform(norm_ffn_g)
    cols[:, C_PSC:C_PSC + 8] = _colform(pool_scale)
    cols[:, C_SNG:C_SNG + 16] = _colform(ssd_norm_g)
    cols[:, C_C:C_C + 8] = _colform(c)
    cols[:, C_CB:C_CB + 32] = _colform(conv_b)
    cw = f(conv_w).reshape(4, 32, 128)
    cols[:, C_CW:C_CW + 128] = cw.transpose(2, 1, 0).reshape(128, 128)
    rows = np.zeros((1, NROW), np.float32)
    rows[0, R_BADA:R_BADA + 6144] = f(b_ada).reshape(-1)
    rows[0, R_DTB:R_DTB + 32] = f(dt_bias).reshape(-1)
    rows[0, R_ALOG:R_ALOG + 32] = f(A_log).reshape(-1)
    rows[0, R_DSK:R_DSK + 32] = f(D_skip).reshape(-1)
    rows[0, R_RB:R_RB + 64] = f(router_bias).reshape(-1)
    rows[0, R_FNG:R_FNG + 1024] = f(final_norm_g).reshape(-1)
    rows = np.ascontiguousarray(np.broadcast_to(rows, (128, NROW)))
    wg = f(we_gate)[0].reshape(64, 8, 128, 256)
    wu = f(we_up)[0].reshape(64, 8, 128, 256)
    wgu = np.empty((65, 128, 8, 512), np.float32)
    wgu[:64, :, :, :256] = wg.transpose(0, 2, 1, 3)
    wgu[:64, :, :, 256:] = wu.transpose(0, 2, 1, 3)
    wgu[64, :, :, :256] = f(ws_gate)[0].reshape(8, 128, 256).transpose(1, 0, 2)
    wgu[64, :, :, 256:] = f(ws_up)[0].reshape(8, 128, 256).transpose(1, 0, 2)
    wdn = np.empty((65, 128, 2, 1024), np.float32)
    wdn[:64] = f(we_down)[0].reshape(64, 2, 128, 1024).transpose(0, 2, 1, 3)
    wdn[64] = f(ws_down)[0].reshape(2, 128, 1024).transpose(1, 0, 2)
    shared = {
        "cols": cols, "rows": rows, "w_ada": f(w_ada)[0], "w_in": f(w_in)[0],
        "pool_w": f(pool_w)[0].reshape(1024, 256), "w_br_ssd": f(w_br_ssd)[0], "w_br_pool": f(w_br_pool)[0],
        "w_out": f(w_out)[0], "w_router": f(w_router)[0], "wgu": wgu, "wdn": wdn,
    }
    in_maps = []
    for k in range(NCORES):
        m = dict(shared)
        m["x_own"] = x2[k * T:(k + 1) * T]
        m["x_halo"] = x2[k * T - 128:k * T] if k > 0 else np.zeros((128, D), np.float32)
        m["rk"] = np.ascontiguousarray(np.broadcast_to((np.arange(8) == k - 1).astype(np.float32)[None, :], (128, 8)))
        m["x_all"] = x2[:(NCORES - 1) * T]
        m["nf"] = np.full((128, 1), 1.0 if k > 0 else 0.0, np.float32)
        m["posrow"] = np.ascontiguousarray(np.broadcast_to((k * T + 1 + np.arange(16)).astype(np.float32)[None, :], (128, 16)))
        in_maps.append(m)
    return in_maps


_NC_CACHE = {}


def kernel(**inputs):
    in_maps = _prep_inputs(**inputs)
    if "nc" not in _NC_CACHE:
        _NC_CACHE["nc"] = build_nc()
    res = run_bass_kernel_spmd(_NC_CACHE["nc"], in_maps, core_ids=list(range(NCORES)))
    out = np.concatenate([np.asarray(r["out"], np.float32) for r in res.results], axis=0)
    return out.reshape(1, NCORES * T, D)
```
